# Optimizing a Trainium2 kernel written in Bass

```python
import jax
import jax.numpy as jnp
from jax import lax
import numpy as np

D_MODEL = 1024
BATCH = 8
SEQ = 4096
DEPTH = 2

F32 = jnp.float32

MIX_W = D_MODEL
N_GROUPS = 4
GROUP_W = MIX_W // N_GROUPS
HEAD_DIM = 64
N_HEADS_G = GROUP_W // HEAD_DIM
NORM_EPS = 1e-5
Q_BLOCK = 128

RET_CHUNK = 128
RET_THETA = 10000.0
RWKV_W_LORA = 32
RWKV_A_LORA = 32
RWKV_G_LORA = 64
RWKV_LN_EPS = 64e-5
DSA_Q_LORA = 128
DSA_KV_DIM = HEAD_DIM
IDX_HEADS = 8
IDX_DIM = 32
DSA_TOPK_MAX = 256
ROPE_THETA = 500000.0
ROPE_FRAC = 4
N_EXPERTS = 32
TOP_K = 4
D_FF = D_MODEL
SWIGLU_ALPHA = 1.702
SWIGLU_LIMIT = 7.0
MOE_BLOCK = 512

RET_COLS = 4 * GROUP_W
RWKV_COLS = 3 * GROUP_W + RWKV_W_LORA + RWKV_A_LORA + RWKV_G_LORA
DSA_COLS = DSA_Q_LORA + 2 * DSA_KV_DIM + IDX_DIM + IDX_HEADS
SB_COLS = 3 * GROUP_W
IN_COLS = RET_COLS + RWKV_COLS + DSA_COLS + SB_COLS
GROUP_SPLITS = (RET_COLS, RET_COLS + RWKV_COLS, RET_COLS + RWKV_COLS + DSA_COLS)
RWKV_SPLITS = (GROUP_W, 2 * GROUP_W, 3 * GROUP_W, 3 * GROUP_W + RWKV_W_LORA,
               3 * GROUP_W + RWKV_W_LORA + RWKV_A_LORA)
DSA_SPLITS = (DSA_Q_LORA, DSA_Q_LORA + DSA_KV_DIM, DSA_Q_LORA + 2 * DSA_KV_DIM,
              DSA_Q_LORA + 2 * DSA_KV_DIM + IDX_DIM)

kernel_name = 'hybrid_parallel_heads_moe_decoder'


def rms_norm(x, g, eps=NORM_EPS):
    xf = x.astype(F32)
    y = xf * lax.rsqrt(jnp.mean(xf * xf, axis=-1, keepdims=True) + eps)
    return (y * g.astype(F32)).astype(x.dtype)


def head_norm(x, g, eps):
    xf = x.astype(F32)
    xf = xf - jnp.mean(xf, axis=-1, keepdims=True)
    y = xf * lax.rsqrt(jnp.mean(xf * xf, axis=-1, keepdims=True) + eps)
    return (y.reshape(x.shape[:-2] + (-1,)) * g.astype(F32)).astype(x.dtype)


def rope(x, pos, rot_dim, theta):
    half = rot_dim // 2
    inv = theta ** (-jnp.arange(half, dtype=F32) / half)
    ang = pos.astype(F32)[..., None] * inv
    ang = ang.reshape(ang.shape[:2] + (1,) * (x.ndim - 3) + (half,))
    cos, sin = jnp.cos(ang), jnp.sin(ang)
    xr = x[..., :rot_dim].astype(F32)
    x1, x2 = xr[..., :half], xr[..., half:]
    rot = jnp.concatenate([x1 * cos - x2 * sin, x1 * sin + x2 * cos], axis=-1).astype(x.dtype)
    return jnp.concatenate([rot, x[..., rot_dim:]], axis=-1)


def retention(q, k, v, gate, pos, gn):
    B, S, H, d = q.shape
    q = rope(q, pos, d, RET_THETA)
    k = rope(k, pos, d, RET_THETA) * d ** -0.5
    lg = jnp.log(1.0 - 2.0 ** (-5.0 - jnp.arange(H, dtype=F32)))
    C = RET_CHUNK
    nc = S // C
    idx = jnp.arange(C, dtype=F32)
    diff = idx[:, None] - idx[None, :]
    decay_in = jnp.where(diff >= 0, jnp.exp(lg[:, None, None] * jnp.maximum(diff, 0.0)), 0.0)
    q_dec = jnp.exp(lg[:, None] * (idx + 1.0))
    k_dec = jnp.exp(lg[:, None] * (C - 1.0 - idx))
    chunk_dec = jnp.exp(lg * C)

    def to_chunks(t):
        return t.astype(F32).reshape(B, nc, C, H, d).transpose(1, 0, 3, 2, 4)

    def step(state, qkv):
        qc, kc, vc = qkv
        inner = jnp.einsum('bhnm,bhmv->bhnv', jnp.einsum('bhnd,bhmd->bhnm', qc, kc) * decay_in, vc)
        cross = jnp.einsum('bhnd,bhdv->bhnv', qc * q_dec[..., None], state)
        state = state * chunk_dec[:, None, None] + jnp.einsum('bhmd,bhmv->bhdv', kc * k_dec[..., None], vc)
        return state, inner + cross

    state0 = jnp.zeros((B, H, d, d), F32)
    _, out = lax.scan(step, state0, (to_chunks(q), to_chunks(k), to_chunks(v)))
    out = out.transpose(1, 0, 3, 2, 4).reshape(B, S, H, d).astype(v.dtype)
    return jax.nn.silu(gate) * head_norm(out, gn, NORM_EPS)


def token_shift(z, mu):
    prev = jnp.pad(z, ((0, 0), (1, 0), (0, 0)))[:, :-1]
    return z + (prev - z) * mu


def rwkv7(feats, w0, w2, a0, a2, g2, k_k, k_a, r_k, ln_g):
    B, S, _ = feats.shape
    H, d = N_HEADS_G, HEAD_DIM
    r, k, v, wd, ad, gd = jnp.split(feats, RWKV_SPLITS, axis=-1)
    w_log = -jax.nn.softplus(-(w0 + jnp.tanh(wd) @ w2)) - 0.5
    decay = jnp.exp(-jnp.exp(w_log.astype(F32)))
    a = jax.nn.sigmoid(a0 + ad @ a2)
    gate = jax.nn.sigmoid(gd) @ g2

    def heads(t):
        return t.reshape(B, S, H, d)

    kk = heads((k * k_k).astype(F32))
    kk = kk / jnp.maximum(jnp.sqrt(jnp.sum(kk * kk, axis=-1, keepdims=True)), 1e-12)
    k = k * (1.0 + (a - 1.0) * k_a)
    r_h, k_h, v_h, a_h = heads(r), heads(k), heads(v), heads(a)
    b_h = kk * a_h.astype(F32)

    def step(state, inp):
        rt, wt, kt, vt, kkt, bt = inp
        sa = jnp.einsum('bhvk,bhk->bhv', state, -kkt)
        state = (state * wt[:, :, None, :] + sa[..., None] * bt[:, :, None, :]
                 + vt[..., None] * kt[:, :, None, :])
        return state, jnp.einsum('bhvk,bhk->bhv', state, rt)

    def seq_first(t):
        return t.astype(F32).transpose(1, 0, 2, 3)

    state0 = jnp.zeros((B, H, d, d), F32)
    _, y = lax.scan(step, state0, (seq_first(r_h), seq_first(heads(decay)), seq_first(k_h),
                                   seq_first(v_h), seq_first(kk), seq_first(b_h)))
    y = y.transpose(1, 0, 2, 3).astype(feats.dtype)
    y = head_norm(y, ln_g, RWKV_LN_EPS)
    bonus = (jnp.sum(r_h * k_h * r_k, axis=-1, keepdims=True) * v_h).reshape(B, S, H * d)
    return (y + bonus) * gate


def dsa_attention(cq, k, v, k_idx, w_idx, pos, q_norm, wq_up, wqi_up, o_norm):
    B, S, _ = cq.shape
    H, d = N_HEADS_G, HEAD_DIM
    cq = rms_norm(cq, q_norm)
    q = rope((cq @ wq_up).reshape(B, S, H, d), pos, d // ROPE_FRAC, ROPE_THETA)
    qi = rope((cq @ wqi_up).reshape(B, S, IDX_HEADS, IDX_DIM), pos, IDX_DIM // ROPE_FRAC, ROPE_THETA)
    k = rope(k, pos, d // ROPE_FRAC, ROPE_THETA)
    k_idx = rope(k_idx, pos, IDX_DIM // ROPE_FRAC, ROPE_THETA)
    w_idx = w_idx * (IDX_HEADS ** -0.5 * IDX_DIM ** -0.5)
    topk = min(DSA_TOPK_MAX, S // 4)
    nb = S // Q_BLOCK
    key_pos = jnp.arange(S)

    def blocks(t):
        return t.reshape((B, nb, Q_BLOCK) + t.shape[2:]).swapaxes(0, 1)

    def gather_rows(table, sel):
        return jax.vmap(lambda tb, ib: tb[ib])(table, sel)

    def one_block(args):
        qb, qib, wb, start = args
        q_pos = start + jnp.arange(Q_BLOCK)
        rel = jax.nn.relu(jnp.einsum('bthe,bse->bths', qib, k_idx))
        score = jnp.einsum('bth,bths->bts', wb, rel).astype(F32)
        causal = key_pos[None, :] <= q_pos[:, None]
        score = jnp.where(causal[None], score, -jnp.inf)
        vals, sel = lax.top_k(score, topk)
        valid = jnp.isfinite(vals)
        kg = gather_rows(k, sel)
        vg = gather_rows(v, sel)
        logits = jnp.einsum('bthd,btkd->bthk', qb, kg).astype(F32) * d ** -0.5
        logits = jnp.where(valid[:, :, None, :], logits, -jnp.inf)
        p = jax.nn.softmax(logits, axis=-1).astype(vg.dtype)
        return jnp.einsum('bthk,btkd->bthd', p, vg)

    starts = jnp.arange(nb) * Q_BLOCK
    out = lax.map(one_block, (blocks(q), blocks(qi), blocks(w_idx), starts))
    out = out.swapaxes(0, 1).reshape(B, S, H * d)
    return rms_norm(out, o_norm)


def stick_breaking(q, k, v, o_norm):
    B, S, H, d = q.shape
    nb = S // Q_BLOCK
    key_pos = jnp.arange(S)

    def one_block(args):
        qb, start = args
        q_pos = start + jnp.arange(Q_BLOCK)
        z = jnp.einsum('bthd,bshd->bhts', qb, k).astype(F32) * d ** -0.5
        mask = (key_pos[None, :] < q_pos[:, None])[None, None]
        log_1m = jnp.where(mask, jax.nn.log_sigmoid(-z), 0.0)
        after = lax.cumsum(log_1m, axis=3, reverse=True) - log_1m
        A = jnp.where(mask, jnp.exp(jax.nn.log_sigmoid(z) + after), 0.0)
        return jnp.einsum('bhts,bshd->bthd', A.astype(v.dtype), v)

    q_blocks = q.reshape(B, nb, Q_BLOCK, H, d).swapaxes(0, 1)
    out = lax.map(one_block, (q_blocks, jnp.arange(nb) * Q_BLOCK))
    out = out.swapaxes(0, 1).reshape(B, S, H * d)
    return rms_norm(out, o_norm)


def clamped_swiglu(h):
    glu, lin = jnp.split(h, 2, axis=-1)
    glu = jnp.minimum(glu, SWIGLU_LIMIT)
    lin = jnp.clip(lin, -SWIGLU_LIMIT, SWIGLU_LIMIT)
    return glu * jax.nn.sigmoid(SWIGLU_ALPHA * glu) * (lin + 1.0)


def moe(h, router_w, router_b, w1, b1, w2, b2):
    B, S, D = h.shape
    n = B * S
    xt = h.reshape(n, D)
    logits = (xt @ router_w + router_b).astype(F32)
    top_val, top_idx = lax.top_k(logits, TOP_K)
    gate = jax.nn.softmax(top_val, axis=-1)
    n_assign = n * TOP_K
    expert = top_idx.reshape(-1).astype(jnp.int32)
    token = jnp.arange(n_assign, dtype=jnp.int32) // TOP_K
    weight = gate.reshape(-1)
    order = jnp.argsort(expert)
    expert_s, token_s, weight_s = expert[order], token[order], weight[order]
    counts = jnp.zeros((N_EXPERTS,), jnp.int32).at[expert].add(1)
    starts = jnp.cumsum(counts) - counts
    padded = (counts + MOE_BLOCK - 1) // MOE_BLOCK * MOE_BLOCK
    pad_end = jnp.cumsum(padded)
    pad_start = pad_end - padded
    dest = pad_start[expert_s] + jnp.arange(n_assign, dtype=jnp.int32) - starts[expert_s]
    n_blocks = -(-n_assign // MOE_BLOCK) + N_EXPERTS
    rows = n_blocks * MOE_BLOCK
    row_token = jnp.full((rows,), n, jnp.int32).at[dest].set(token_s)
    row_weight = jnp.zeros((rows,), F32).at[dest].set(weight_s)
    block_start = jnp.arange(n_blocks, dtype=jnp.int32) * MOE_BLOCK
    block_expert = jnp.minimum(jnp.sum(pad_end[None, :] <= block_start[:, None], axis=1), N_EXPERTS - 1)
    x_pad = jnp.concatenate([xt, jnp.zeros((1, D), xt.dtype)], axis=0)

    def one_block(args):
        tok, e = args
        hb = clamped_swiglu(x_pad[tok] @ w1[e] + b1[e])
        return hb @ w2[e] + b2[e]

    y = lax.map(one_block, (row_token.reshape(n_blocks, MOE_BLOCK), block_expert))
    y = y.reshape(rows, D) * row_weight[:, None].astype(y.dtype)
    out = jax.ops.segment_sum(y, row_token, num_segments=n + 1)[:n]
    return out.reshape(B, S, D)


def setup_inputs(seed: int = 0) -> dict:
    key = jax.random.key(seed)
    ks = list(jax.random.split(key, 40))
    kit = iter(ks)

    def nrm(shape, scale):
        return jax.random.normal(next(kit), shape, F32) * scale

    def gain(shape):
        return 1.0 + nrm(shape, 0.02)

    L = DEPTH
    x = nrm((BATCH, SEQ, D_MODEL), 1.0)
    c = nrm((BATCH, D_MODEL), 1.0)
    offsets = jax.random.randint(next(kit), (BATCH, 1), 0, 2048, dtype=jnp.int32)
    positions = offsets + jnp.arange(SEQ, dtype=jnp.int32)[None, :]
    return {
        'x': x,
        'c': c,
        'positions': positions,
        'ada_w': nrm((L, D_MODEL, 6 * D_MODEL), 0.5 * D_MODEL ** -0.5),
        'ada_b': nrm((L, 6 * D_MODEL), 0.02),
        'norm_mix': gain((L, D_MODEL)),
        'norm_ffn': gain((L, D_MODEL)),
        'w_in': nrm((L, D_MODEL, IN_COLS), D_MODEL ** -0.5),
        'ret_gn': gain((L, GROUP_W)),
        'rwkv_mu': jax.random.uniform(next(kit), (L, RWKV_COLS), F32, 0.0, 1.0),
        'rwkv_w0': jax.random.uniform(next(kit), (L, GROUP_W), F32, -6.5, -1.5),
        'rwkv_w2': nrm((L, RWKV_W_LORA, GROUP_W), 0.1),
        'rwkv_a0': nrm((L, GROUP_W), 0.5),
        'rwkv_a2': nrm((L, RWKV_A_LORA, GROUP_W), 0.1),
        'rwkv_g2': nrm((L, RWKV_G_LORA, GROUP_W), RWKV_G_LORA ** -0.5),
        'rwkv_kk': 0.85 + nrm((L, GROUP_W), 0.05),
        'rwkv_ka': 1.0 + nrm((L, GROUP_W), 0.05),
        'rwkv_rk': nrm((L, N_HEADS_G, HEAD_DIM), 0.1),
        'rwkv_ln': gain((L, GROUP_W)),
        'dsa_qnorm': gain((L, DSA_Q_LORA)),
        'dsa_wq_up': nrm((L, DSA_Q_LORA, GROUP_W), DSA_Q_LORA ** -0.5),
        'dsa_wqi_up': nrm((L, DSA_Q_LORA, IDX_HEADS * IDX_DIM), DSA_Q_LORA ** -0.5),
        'dsa_onorm': gain((L, GROUP_W)),
        'sb_onorm': gain((L, GROUP_W)),
        'w_out': nrm((L, MIX_W, D_MODEL), MIX_W ** -0.5),
        'router_w': nrm((L, D_MODEL, N_EXPERTS), D_MODEL ** -0.5),
        'router_b': nrm((L, N_EXPERTS), 0.01),
        'moe_w1': nrm((L, N_EXPERTS, D_MODEL, 2 * D_FF), D_MODEL ** -0.5),
        'moe_b1': nrm((L, N_EXPERTS, 2 * D_FF), 0.02),
        'moe_w2': nrm((L, N_EXPERTS, D_FF, D_MODEL), D_FF ** -0.5),
        'moe_b2': nrm((L, N_EXPERTS, D_MODEL), 0.02),
        'norm_final': gain((D_MODEL,)),
    }


def reference(x, c, positions, ada_w, ada_b, norm_mix, norm_ffn, w_in, ret_gn,
              rwkv_mu, rwkv_w0, rwkv_w2, rwkv_a0, rwkv_a2, rwkv_g2, rwkv_kk, rwkv_ka, rwkv_rk, rwkv_ln,
              dsa_qnorm, dsa_wq_up, dsa_wqi_up, dsa_onorm, sb_onorm, w_out,
              router_w, router_b, moe_w1, moe_b1, moe_w2, moe_b2, norm_final):
    B, S, D = x.shape
    H, d = N_HEADS_G, HEAD_DIM

    def heads(t):
        return t.reshape(B, S, H, d)

    cond = jax.nn.silu(c)
    for l in range(DEPTH):
        mod = cond @ ada_w[l] + ada_b[l]
        sh1, sc1, g1, sh2, sc2, g2 = jnp.split(mod[:, None, :], 6, axis=-1)
        h = rms_norm(x, norm_mix[l]) * (1.0 + sc1) + sh1
        proj = h @ w_in[l]
        ret_f, rwkv_f, dsa_f, sb_f = jnp.split(proj, GROUP_SPLITS, axis=-1)
        rq, rk, rv, rg = jnp.split(ret_f, 4, axis=-1)
        y_ret = retention(heads(rq), heads(rk), heads(rv), rg, positions, ret_gn[l])
        y_rwkv = rwkv7(token_shift(rwkv_f, rwkv_mu[l]), rwkv_w0[l], rwkv_w2[l], rwkv_a0[l], rwkv_a2[l],
                       rwkv_g2[l], rwkv_kk[l], rwkv_ka[l], rwkv_rk[l], rwkv_ln[l])
        cq, dk, dv, dki, dwi = jnp.split(dsa_f, DSA_SPLITS, axis=-1)
        y_dsa = dsa_attention(cq, dk, dv, dki, dwi, positions, dsa_qnorm[l], dsa_wq_up[l],
                              dsa_wqi_up[l], dsa_onorm[l])
        sq, sk, sv = jnp.split(sb_f, 3, axis=-1)
        y_sb = stick_breaking(heads(sq), heads(sk), heads(sv), sb_onorm[l])
        mixed = jnp.concatenate([y_ret, y_rwkv, y_dsa, y_sb], axis=-1) @ w_out[l]
        x = x + g1 * mixed
        h = rms_norm(x, norm_ffn[l]) * (1.0 + sc2) + sh2
        x = x + g2 * moe(h, router_w[l], router_b[l], moe_w1[l], moe_b1[l], moe_w2[l], moe_b2[l])
    return rms_norm(x, norm_final)
```

```python
import numpy as np
from contextlib import ExitStack
import concourse.bass as bass
import concourse.mybir as mybir
from concourse.bass_utils import run_bass_kernel_spmd

F32 = mybir.dt.float32; BF16 = mybir.dt.bfloat16; I32 = mybir.dt.int32; U32 = mybir.dt.uint32
AF = mybir.ActivationFunctionType; ALU = mybir.AluOpType; AX = mybir.AxisListType

D = 1024; SEQ = 4096; NB = 8; L = 2; NT = SEQ // 128
MB = 512; NBLK = 64; NSLOT = NBLK * MB
IN_COLS = 2984
ENGS = ['pe', 'act', 'dve', 'pool', 'sp']
LIMIT = 30000


class TB:
    __slots__ = ('w', 'wd', 'r', 'excl')

    def __init__(self):
        self.w = None; self.wd = {}; self.r = {}; self.excl = False


class Buf:
    def __init__(self, t):
        self.t = t; self.tb = TB()

    def __getitem__(self, k):
        return self.t[k]


def _tb(x):
    return getattr(x, 'tb', x)


class Sched:
    def __init__(self, nc, stack, ndsem=32):
        self.nc = nc; self.stack = stack
        self.ops = {e: [] for e in ENGS}
        self.epoch = {e: 0 for e in ENGS}
        self.cnt = {e: 0 for e in ENGS}
        self.sems = {}
        for e in ENGS:
            self.sems[(e, 0)] = stack.enter_context(nc.semaphore(f"s_{e}_0"))
        self.dsems = [stack.enter_context(nc.semaphore(f"sd_{i}")) for i in range(ndsem)]
        self.dcnt = [0] * ndsem; self.dnext = {'hw': 0, 'sw': 0}
        self.nhw = ndsem // 2
        self.seen = {e: {} for e in ENGS}
        self.nops = 0

    def semof(self, key):
        if key[0] == 'd':
            return self.dsems[key[1]]
        return self.sems[key]

    def op(self, eng, fn, R=(), W=(), dma=False):
        need = {}

        def nd(k, v):
            if need.get(k, 0) < v:
                need[k] = v
        R = [_tb(x) for x in R]; W = [_tb(x) for x in W]
        W = W + [t for t in R if t.excl and t not in W]
        R = [t for t in R if not t.excl]
        for t in R:
            if t.w:
                nd(*t.w)
            for k, v in t.wd.items():
                nd(k, v)
        for t in W:
            if t.w:
                nd(*t.w)
            if not dma:
                for k, v in t.wd.items():
                    nd(k, v)
            for k, v in t.r.items():
                nd(k, v)
        waits = []
        for k, v in need.items():
            if k[0] == 'd':
                v = self.dcnt[k[1]]
            elif k[0] == 'pe' and eng == 'pe':
                continue
            if self.seen[eng].get(k, 0) < v:
                waits.append((k, v)); self.seen[eng][k] = v
        if dma:
            kind = 'sw' if eng == 'pool' else 'hw'
            n = self.nhw if kind == 'hw' else len(self.dsems) - self.nhw
            i = self.dnext[kind] + (0 if kind == 'hw' else self.nhw)
            self.dnext[kind] = (self.dnext[kind] + 1) % n
            self.dcnt[i] += 16; key = ('d', i); val = self.dcnt[i]; inc = 16
        else:
            if self.cnt[eng] >= LIMIT:
                self.epoch[eng] += 1; self.cnt[eng] = 0
                self.sems[(eng, self.epoch[eng])] = self.stack.enter_context(
                    self.nc.semaphore(f"s_{eng}_{self.epoch[eng]}"))
            self.cnt[eng] += 1; key = (eng, self.epoch[eng]); val = self.cnt[eng]; inc = 1
        self.ops[eng].append((waits, fn, key, inc))
        for t in R:
            t.r[key] = val
        for t in W:
            if dma:
                t.wd[key] = val
            else:
                t.w = (key, val); t.wd = {}
                t.r = {}
        self.nops += 1

    def barrier(self):
        for e in ENGS:
            waits = []
            for f in ENGS:
                if f == e:
                    continue
                k = (f, self.epoch[f]); v = self.cnt[f]
                if v > 0 and self.seen[e].get(k, 0) < v:
                    waits.append((k, v)); self.seen[e][k] = v
            for i in range(len(self.dsems)):
                k = ('d', i); v = self.dcnt[i]
                if v > 0 and self.seen[e].get(k, 0) < v:
                    waits.append((k, v)); self.seen[e][k] = v
            self.ops[e].append((waits, None, None, 0))

    def emit(self):
        nc = self.nc
        names = {'pe': 'tensor', 'act': 'scalar', 'dve': 'vector', 'pool': 'gpsimd', 'sp': 'sync'}
        with nc.Block() as block:
            for e in ENGS:
                lst = self.ops[e]

                def body(engine, lst=lst):
                    for waits, fn, key, inc in lst:
                        for k, v in waits:
                            engine.wait_ge(self.semof(k), v)
                        if fn is not None:
                            ins = fn(engine)
                            ins.then_inc(self.semof(key), inc)
                getattr(block, names[e])(body)
        self.ops = {e: [] for e in ENGS}


RET0, RWKV0, DSA0, SB0 = 0, 1024, 1920, 2216


def _swap_cols(base, nheads, hd, half):
    cols = []
    for h in range(nheads):
        for f in range(hd):
            if f < half:
                g = f + half
            elif f < 2 * half:
                g = f - half
            else:
                g = f
            cols.append(base + h * hd + g)
    return cols


def build_wext_cols():
    blocks = {}
    r = lambda a, n: list(range(a, a + n))
    qs = _swap_cols(RET0, 4, 64, 32); ks = _swap_cols(RET0 + 256, 4, 64, 32)
    for hp in range(2):
        blocks[f'ret_q{hp}'] = r(RET0 + hp * 128, 128)
        blocks[f'ret_qs{hp}'] = qs[hp * 128:(hp + 1) * 128]
        blocks[f'ret_k{hp}'] = r(RET0 + 256 + hp * 128, 128)
        blocks[f'ret_ks{hp}'] = ks[hp * 128:(hp + 1) * 128]
        blocks[f'ret_vg{hp}'] = r(RET0 + 512 + hp * 128, 128) + r(RET0 + 768 + hp * 128, 128)
    for hp in range(2):
        blocks[f'sb_q{hp}'] = r(SB0 + hp * 128, 128)
        blocks[f'sb_k{hp}'] = r(SB0 + 256 + hp * 128, 128)
    blocks['sb_v'] = r(SB0 + 512, 256)
    blocks['rw_rkv'] = r(RWKV0, 768)
    blocks['rw_lora'] = r(RWKV0 + 768, 128)
    blocks['ds_cq'] = r(DSA0, 128)
    kcols = r(DSA0 + 128, 64); kscols = _swap_cols(DSA0 + 128, 1, 64, 8)
    blocks['ds_k'] = kcols + kcols
    blocks['ds_ks'] = kscols + kscols
    icols = r(DSA0 + 256, 32); iscols = _swap_cols(DSA0 + 256, 1, 32, 4)
    blocks['ds_ki'] = icols * 4
    blocks['ds_kis'] = iscols * 4
    blocks['ds_vw'] = r(DSA0 + 192, 64) + r(DSA0 + 288, 8)
    off = {}; cols = []
    for k, v in blocks.items():
        off[k] = (len(cols), len(v)); cols += v
    return off, np.array(cols, dtype=np.int64)


WOFF, WCOLS = build_wext_cols()
NEXT = len(WCOLS)


def build_consts():
    c = {}
    p = np.arange(128)
    c['ident'] = np.eye(128, dtype=np.float32)
    sbm = np.zeros((4, 128, 512), np.float32)
    for rr in range(4):
        for qb in range(4):
            if qb > rr:
                sbm[rr, :, qb * 128:(qb + 1) * 128] = 1.0
            elif qb == rr:
                sbm[rr, :, qb * 128:(qb + 1) * 128] = (p[:, None] < p[None, :]).astype(np.float32)
    c['sbmask'] = sbm.transpose(1, 0, 2).reshape(128, 4 * 512)
    c['tri_ge'] = (p[:, None] >= p[None, :]).astype(np.float32)
    c['tri_le'] = (p[:, None] <= p[None, :]).astype(np.float32)
    c['tri_lt'] = (p[:, None] < p[None, :]).astype(np.float32)
    c['tri_gt'] = (p[:, None] > p[None, :]).astype(np.float32)
    c['ntri_ge'] = -c['tri_ge']
    c['nones'] = -np.ones((128, 128), np.float32)
    c['hm4'] = (p[:, None] // 32 == np.arange(4)[None, :]).astype(np.float32)
    c['hm2'] = (p[:, None] // 64 == np.arange(2)[None, :]).astype(np.float32)
    c['iota_blk'] = np.tile(np.arange(64, dtype=np.float32)[None, :], (128, 1))
    c['kp'] = (np.arange(8)[None, :] * 128 + p[:, None]).astype(np.float32)
    c['pcol'] = p.astype(np.float32)[:, None]
    c['last'] = (p == 127).astype(np.float32)[:, None]
    c['negmask'] = np.where(p[None, :] > p[:, None], -1e30, 0.0).astype(np.float32)
    lg = np.log(1.0 - 2.0 ** (-5.0 - np.arange(4, dtype=np.float64)))
    idx = np.arange(128, dtype=np.float64)
    for hp in range(2):
        hs = [2 * hp, 2 * hp + 1]
        hp_of_p = np.array([hs[q // 64] for q in range(128)])
        c[f'ret_qdec{hp}'] = np.exp(lg[hp_of_p][:, None] * (idx[None, :] + 1.0)).astype(np.float32)
        kd = np.zeros((128, 128)); dm = np.zeros((128, 2, 128))
        for j, h in enumerate(hs):
            kd[:, j * 64:(j + 1) * 64] = (np.exp(lg[h] * (127.0 - idx)) / 8.0)[:, None]
            diff = idx[None, :] - idx[:, None]
            dm[:, j, :] = np.where(diff >= 0, np.exp(lg[h] * np.maximum(diff, 0.0)), 0.0) / 8.0
        c[f'ret_kdec{hp}'] = kd.astype(np.float32)
        c[f'ret_dmask{hp}'] = dm.reshape(128, 256).astype(np.float32)
        c[f'ret_cd{hp}'] = np.exp(lg[hp_of_p] * 128.0).astype(np.float32)[:, None]
    f64 = p % 64
    c['rope_ret'] = np.stack([10000.0 ** (-(f64 % 32) / 32.0) / (2 * np.pi), np.where(f64 < 32, -1.0, 1.0)], 1).astype(np.float32)
    inv = np.where(f64 < 16, 500000.0 ** (-(f64 % 8) / 8.0), 0.0) / (2 * np.pi)
    sg = np.where(f64 < 8, -1.0, np.where(f64 < 16, 1.0, 0.0))
    c['rope_dq'] = np.stack([inv, sg], 1).astype(np.float32)
    f32 = p % 32
    inv = np.where(f32 < 8, 500000.0 ** (-(f32 % 4) / 4.0), 0.0) / (2 * np.pi)
    sg = np.where(f32 < 4, -1.0, np.where(f32 < 8, 1.0, 0.0))
    c['rope_di'] = np.stack([inv, sg], 1).astype(np.float32)
    off = {}; n = 0; arrs = []
    for k, v in c.items():
        off[k] = (n, v.shape[1]); n += v.shape[1]; arrs.append(v.astype(np.float32))
    return off, np.ascontiguousarray(np.concatenate(arrs, 1))


COFF, CST = build_consts()


class Prog:
    def __init__(self, debug=None, nlayers=L, flags=None):
        self.debug = debug or {}
        self.flags = flags or {}
        self.nlayers = nlayers
        nc = self.nc = bass.Bass("TRN2", target_bir_lowering=False)
        dt = lambda name, shape, dtype, kind="ExternalInput": nc.dram_tensor(name, shape, dtype, kind=kind).ap()
        self.x = dt("x", [SEQ, D], F32)
        self.c = dt("c", [1, D], F32)
        self.pos = dt("pos", [1, SEQ], I32)
        self.cst = dt("cst", [128, CST.shape[1]], F32)
        self.ada_w = dt("ada_w", [L, D, 6 * D], F32)
        self.ada_b = dt("ada_b", [L, 6 * D], F32)
        self.norm_mix = dt("norm_mix", [L, D], F32)
        self.norm_ffn = dt("norm_ffn", [L, D], F32)
        self.w_ext = dt("w_ext", [L, D, NEXT], F32)
        self.ret_gn = dt("ret_gn", [L, 256], F32)
        self.rwkv_mu = dt("rwkv_mu", [L, 896], F32)
        self.rwkv_w0 = dt("rwkv_w0", [L, 256], F32)
        self.rwkv_lw = dt("rwkv_lw", [L, 128, 256], F32)
        self.rwkv_a0 = dt("rwkv_a0", [L, 256], F32)
        self.rwkv_kk = dt("rwkv_kk", [L, 256], F32)
        self.rwkv_ka = dt("rwkv_ka", [L, 256], F32)
        self.rwkv_rk = dt("rwkv_rk", [L, 256], F32)
        self.rwkv_ln = dt("rwkv_ln", [L, 256], F32)
        self.dsa_qnorm = dt("dsa_qnorm", [L, 128], F32)
        self.dsa_wq_up = dt("dsa_wq_up", [L, 128, 256], F32)
        self.dsa_wqs_up = dt("dsa_wqs_up", [L, 128, 256], F32)
        self.dsa_wqi_up = dt("dsa_wqi_up", [L, 128, 256], F32)
        self.dsa_wqis_up = dt("dsa_wqis_up", [L, 128, 256], F32)
        self.dsa_onorm = dt("dsa_onorm", [L, 256], F32)
        self.sb_onorm = dt("sb_onorm", [L, 256], F32)
        self.w_out = dt("w_out", [L, D, D], F32)
        self.router_w = dt("router_w", [L, D, 32], F32)
        self.router_b = dt("router_b", [L, 32], F32)
        if not self.flags.get('nomoe'):
            self.moe_w1 = dt("moe_w1", [L, 32, D, 2 * D], F32)
            self.moe_b1 = dt("moe_b1", [L, 32, 2 * D], F32)
            self.moe_w2 = dt("moe_w2", [L, 32, D, D], F32)
            self.moe_b2 = dt("moe_b2", [L, 32, D], F32)
        self.norm_final = dt("norm_final", [1, D], F32)
        self.out = dt("out", [SEQ, D], F32, kind="ExternalOutput")
        self.xres = dt("xres", [SEQ, D], F32, kind="Internal")
        self.yT_d = dt("yT_d", [8, 128, SEQ], BF16, kind="Internal")
        self.maskT_d = dt("maskT_d", [NT, 128, NT, 128], BF16, kind="Internal")
        self.hrow_d = dt("hrow_d", [SEQ, D], BF16, kind="Internal")
        if not self.flags.get('nomoe'):
            self.w1b_d = dt("w1b_d", [L * 32 * D, 2 * D], BF16, kind="Internal")
            self.w2b_d = dt("w2b_d", [L * 32 * D, D], BF16, kind="Internal")
        self.conv_tb = {}
        self.tokidx_d = dt("tokidx_d", [NSLOT, 2], I32, kind="Internal")
        self.yslot_d = dt("yslot_d", [NSLOT, D], F32, kind="Internal")
        self.dbg = {}
        for name, (shape, dtype) in self.debug.items():
            self.dbg[name] = dt("dbg_" + name, shape, dtype, kind="ExternalOutput")
        self.final_tbs = []

    def sb(self, st, name, shape, dtype):
        self._uid = getattr(self, '_uid', 0) + 1
        return Buf(st.enter_context(self.nc.sbuf_tensor(f"{name}_{self._uid}", shape, dtype)))

    def ps(self, st, name, shape, dtype):
        self._uid = getattr(self, '_uid', 0) + 1
        b = Buf(st.enter_context(self.nc.psum_tensor(f"{name}_{self._uid}", shape, dtype)))
        b.tb.excl = True
        return b

    def dma(self, eng, out, in_, R=(), W=(), **kw):
        self.S.op(eng, lambda e: e.dma_start(out=out, in_=in_, **kw), R=R, W=W, dma=True)

    def load_const(self, st, name, dtype=F32, eng='sp'):
        o, n = COFF[name]
        b = self.sb(st, "c_" + name, [128, n], dtype)
        kw = dict(allow_slow_non_contiguous=True) if n < 8 else {}
        self.dma('pool' if dtype != F32 else eng, b[:], self.cst[:, o:o + n], W=[b], **kw)
        return b

    def bcast_row(self, st, name, row_ap, n, eng='sp'):
        b = self.sb(st, name, [128, n], F32)
        self.dma(eng, b[:], row_ap.partition_broadcast(128), W=[b])
        return b

    def dbg_out(self, name, src_ap, R, dst=None):
        if name in self.dbg:
            t = TB()
            d = self.dbg[name] if dst is None else dst
            self.dma('sp', d, src_ap, R=R, W=[t])
            self.final_tbs.append(t)

    def build(self):
        nc = self.nc
        with ExitStack() as top:
            S = self.S = Sched(nc, top)
            self.ident = self.load_const(top, 'ident', BF16)
            self.identf = self.load_const(top, 'ident', F32)
            self.modT = self.sb(top, "modT", [128, 48], F32)
            self.gb = [self.sb(top, f"gb{i}", [128, D], F32) for i in range(2)]
            self.ab2 = self.sb(top, "ab2", [128, D], F32); self.shb2 = self.sb(top, "shb2", [128, D], F32)
            self.reg_bc = top.enter_context(nc.gpsimd.register("reg_bc"))

            self.ones_row = self.sb(top, "ones_row", [1, 512], F32)
            self.condT = self.sb(top, "condT", [128, 8], F32)
            self.PF = [self.ps(top, f"pf{i}", [128, 512], F32) for i in range(6)]
            self.PB = [self.ps(top, f"pb{i}", [128, 1024], BF16) for i in range(2)]
            S.op('dve', lambda e: e.memset(self.ones_row[:], 1.0), W=[self.ones_row])
            with ExitStack() as st:
                self.cond_phase(st)
                S.barrier(); S.emit()
            for l in range(self.nlayers):
                self.layer(l)
            if 'stop' not in self.flags:
                self.final_norm()
            need = {}
            for t in self.final_tbs:
                for k in t.wd:
                    need[k] = max(need.get(k, 0), S.dcnt[k[1]])
            S.ops['sp'].append((list(need.items()), None, None, 0))
            S.barrier(); S.emit()
        return nc

    def cond_phase(self, st):
        S = self.S
        crow = self.sb(st, "crow", [1, D], F32)
        self.dma('sp', crow[:], self.c[0:1, :], W=[crow])
        pf = self.PF[0]
        for j in range(8):
            S.op('pe', lambda e, j=j: e.matmul(pf[:, j:j + 1], lhsT=crow[0:1, j * 128:(j + 1) * 128], rhs=self.ones_row[0:1, 0:1],
                                              start=True, stop=True), R=[crow, self.ones_row], W=[pf])
        S.op('act', lambda e: e.activation(out=self.condT[:], in_=pf[:, 0:8], func=AF.Silu), R=[pf], W=[self.condT])

    def row_to_cols(self, row_buf, row_ap_fn, ncols, out_ap, extra_R=()):
        S = self.S; pf = self.PF[0]
        for j in range(ncols):
            S.op('pe', lambda e, j=j: e.matmul(pf[:, j:j + 1], lhsT=row_ap_fn(j), rhs=self.ones_row[0:1, 0:1], start=True, stop=True),
                 R=[row_buf, self.ones_row], W=[pf])
        return pf

    def layer(self, l):
        S = self.S
        self.mod_phase(l)
        if self.flags.get('upto') == 'mod':
            return
        with ExitStack() as hs:
            self.hT = self.sb(hs, "hT", [128, 8, SEQ + 1], BF16)
            S.op('dve', lambda e: e.memset(self.hT[:, :, 0:1], 0.0), W=[self.hT])
            self.norm_phase(l, 0)
            if self.flags.get('upto') == 'norm':
                return
            only = self.flags.get('only')
            if only in (None, 'ret'):
                self.retention_phase(l)
            if only in (None, 'rwkv'):
                self.rwkv_phase(l)
            if only in (None, 'dsa'):
                self.dsa_phase(l)
            if only in (None, 'sb'):
                self.sb_phase(l)
            S.barrier(); S.emit()
        if 'yT' in self.dbg:
            for j in range(8):
                self.dbg_out('yT', self.yT_d[j], [], dst=self.dbg['yT'][j])
            S.barrier(); S.emit()
        if self.flags.get('upto') == 'mix':
            return
        self.wout_phase(l)
        if f'xmix{l}' in self.dbg:
            self.dbg_out(f'xmix{l}', self.xres[:, :], [])
            S.barrier(); S.emit()
        if self.flags.get('upto') == 'wout':
            return
        self.moe_phase(l)
        if f'x{l}' in self.dbg:
            self.dbg_out(f'x{l}', self.xres[:, :], [])
            S.barrier(); S.emit()

    def mod_phase(self, l):
        S = self.S
        with ExitStack() as st:
            self.modrow = self.sb(st, "modrow", [1, 6 * D], F32)
            wt = [self.sb(st, f"adaw{i}", [128, 8, 512], F32) for i in range(2)]
            brow = self.sb(st, "adab", [1, 6 * D], F32)
            self.dma('sp', brow[:], self.ada_b[l:l + 1, :], W=[brow])
            for cg in range(12):
                w = wt[cg % 2]
                self.dma('sp' if cg % 2 == 0 else 'act', w[:], self.ada_w[l, :, cg * 512:(cg + 1) * 512].rearrange("(k p) c -> p k c", p=128), W=[w])
                pf = self.PF[1 + cg % 2]
                for k in range(8):
                    S.op('pe', lambda e, k=k, w=w, pf=pf: e.matmul(pf[0:1, :], lhsT=self.condT[:, k:k + 1], rhs=w[:, k, :], start=(k == 0), stop=(k == 7)),
                         R=[self.condT, w], W=[pf])
                S.op('dve', lambda e, cg=cg, pf=pf: e.tensor_tensor(out=self.modrow[0:1, cg * 512:(cg + 1) * 512], in0=pf[0:1, :],
                                                                  in1=brow[0:1, cg * 512:(cg + 1) * 512], op=ALU.add), R=[pf, brow], W=[self.modrow])
            pf = self.row_to_cols(self.modrow, lambda j: self.modrow[0:1, j * 128:(j + 1) * 128], 48, None)
            S.op('dve', lambda e: e.tensor_copy(out=self.modT[:], in_=pf[:, 0:48]), R=[pf], W=[self.modT])
            onescol = self.sb(st, "ones1", [1, 128], F32)
            S.op('dve', lambda e: e.memset(onescol[:], 1.0), W=[onescol])
            for gi, base in enumerate((2 * D, 5 * D)):
                for hf in range(2):
                    pf2 = self.PF[3 + hf]
                    S.op('pe', lambda e, pf2=pf2, base=base, hf=hf: e.matmul(pf2[:], lhsT=onescol[0:1, :], rhs=self.modrow[0:1, base + hf * 512: base + (hf + 1) * 512],
                                                                            start=True, stop=True), R=[onescol, self.modrow], W=[pf2])
                    S.op('act', lambda e, pf2=pf2, gi=gi, hf=hf: e.activation(out=self.gb[gi][:, hf * 512:(hf + 1) * 512], in_=pf2[:], func=AF.Copy), R=[pf2], W=[self.gb[gi]])
            grow2 = self.sb(st, "grow2", [1, D], F32); a2row = self.sb(st, "a2row", [1, D], F32)
            self.dma('sp', grow2[:], self.norm_ffn[l:l + 1, :], W=[grow2])
            S.op('dve', lambda e: e.scalar_tensor_tensor(out=a2row[:], in0=self.modrow[0:1, 4 * D:5 * D], scalar=1.0, in1=grow2[:], op0=ALU.add, op1=ALU.mult),
                 R=[self.modrow, grow2], W=[a2row])
            for dstb, rowfn, rb in ((self.ab2, lambda hf: a2row[0:1, hf * 512:(hf + 1) * 512], a2row), (self.shb2, lambda hf: self.modrow[0:1, 3 * D + hf * 512: 3 * D + (hf + 1) * 512], self.modrow)):
                for hf in range(2):
                    pf2 = self.PF[3 + hf]
                    S.op('pe', lambda e, pf2=pf2, rowfn=rowfn, hf=hf: e.matmul(pf2[:], lhsT=onescol[0:1, :], rhs=rowfn(hf), start=True, stop=True), R=[onescol, rb], W=[pf2])
                    S.op('act', lambda e, pf2=pf2, dstb=dstb, hf=hf: e.activation(out=dstb[:, hf * 512:(hf + 1) * 512], in_=pf2[:], func=AF.Copy), R=[pf2], W=[dstb])
            self.dbg_out(f'mod{l}', self.modrow[0:1, :], [self.modrow])
            S.barrier(); S.emit()

    def norm_phase(self, l, which):
        S = self.S
        src = self.x if (l == 0 and which == 0) else self.xres
        gsrc = self.norm_mix if which == 0 else self.norm_ffn
        shc, scc = (0, 8) if which == 0 else (24, 32)
        with ExitStack() as st:
            grow = self.sb(st, "grow", [1, D], F32)
            self.dma('sp', grow[:], gsrc[l:l + 1, :], W=[grow])
            pf = self.row_to_cols(grow, lambda j: grow[0:1, j * 128:(j + 1) * 128], 8, None)
            acol = self.sb(st, "acol", [128, 8], F32)
            S.op('dve', lambda e: e.scalar_tensor_tensor(out=acol[:], in0=self.modT[:, scc:scc + 8], scalar=1.0, in1=pf[:, 0:8], op0=ALU.add, op1=ALU.mult),
                 R=[self.modT, pf], W=[acol])
            xt = [self.sb(st, f"xt{i}", [128, D], F32) for i in range(2)]
            xn = [self.sb(st, f"xn{i}", [128, D], BF16) for i in range(2)]
            junk = self.sb(st, "junk", [128, D], BF16)
            hrow = [self.sb(st, f"hrow{i}", [128, D], BF16) for i in range(2)] if which == 1 else None
            ssq = [self.sb(st, f"ssq{i}", [128, 1], F32) for i in range(2)]
            for i in range(NT):
                x_, xn_, ss = xt[i % 2], xn[i % 2], ssq[i % 2]
                self.dma('sp' if i % 2 == 0 else 'act', x_[:], src[i * 128:(i + 1) * 128, :], W=[x_])
                S.op('act', lambda e, x_=x_, ss=ss: e.activation(out=junk[:], in_=x_[:], func=AF.Square, accum_out=ss[:]), R=[x_], W=[junk, ss])
                S.op('dve', lambda e, ss=ss: e.tensor_scalar(out=ss[:], in0=ss[:], scalar1=1.0 / D, scalar2=1e-5, op0=ALU.mult, op1=ALU.add), R=[ss], W=[ss])
                S.op('act', lambda e, ss=ss: e.activation(out=ss[:], in_=ss[:], func=AF.Sqrt), R=[ss], W=[ss])
                S.op('dve', lambda e, ss=ss: e.reciprocal(out=ss[:], in_=ss[:]), R=[ss], W=[ss])
                S.op('dve', lambda e, x_=x_, xn_=xn_, ss=ss: e.tensor_scalar(out=xn_[:], in0=x_[:], scalar1=ss[:, 0:1], scalar2=None, op0=ALU.mult), R=[x_, ss], W=[xn_])
                pb = self.PB[i % 2]
                for j in range(8):
                    S.op('pe', lambda e, j=j, xn_=xn_, pb=pb: e.transpose(out=pb[:, j * 128:(j + 1) * 128], in_=xn_[:, j * 128:(j + 1) * 128], identity=self.ident[:]),
                         R=[xn_, self.ident], W=[pb])
                if which == 1:
                    hr = hrow[i % 2]
                    S.op('pool', lambda e, xn_=xn_, hr=hr: e.tensor_tensor(out=hr[:], in0=xn_[:], in1=self.ab2[:], op=ALU.mult), R=[xn_, self.ab2], W=[hr])
                    S.op('pool', lambda e, hr=hr: e.tensor_tensor(out=hr[:], in0=hr[:], in1=self.shb2[:], op=ALU.add), R=[hr, self.shb2], W=[hr])
                    self.dma('sp', self.hrow_d[i * 128:(i + 1) * 128, :], hr[:], R=[hr], W=[TB()])
                for j in range(8):
                    eng = 'dve' if j % 2 == 0 else 'pool'
                    if eng == 'pool':
                        eng = 'act'
                        S.op('act', lambda e, j=j, pb=pb, i=i: e.activation(out=self.hT[:, j, 1 + i * 128: 1 + (i + 1) * 128], in_=pb[:, j * 128:(j + 1) * 128], func=AF.Identity,
                                                                           scale=acol[:, j:j + 1], bias=self.modT[:, shc + j: shc + j + 1]), R=[pb, acol, self.modT], W=[self.hT])
                    else:
                        S.op('dve', lambda e, j=j, pb=pb, i=i: e.tensor_scalar(out=self.hT[:, j, 1 + i * 128: 1 + (i + 1) * 128], in0=pb[:, j * 128:(j + 1) * 128],
                                                                              scalar1=acol[:, j:j + 1], scalar2=self.modT[:, shc + j: shc + j + 1], op0=ALU.mult, op1=ALU.add),
                             R=[pb, acol, self.modT], W=[self.hT])
            if f'hT{l}' in self.dbg and which == 0:
                for j in range(8):
                    self.dbg_out(f'hT{l}', self.hT[:, j, 1:], [self.hT], dst=self.dbg[f'hT{l}'][j])
            S.barrier(); S.emit()

    def load_w(self, st, name, l, blocks):
        n = sum(WOFF[b][1] for b in blocks)
        wm = self.sb(st, name, [128, 8, n], BF16)
        o = 0; offs = {}
        for b in blocks:
            c0, cn = WOFF[b]
            for k in range(8):
                self.dma('pool', wm[:, k, o:o + cn], self.w_ext[l, k * 128:(k + 1) * 128, c0:c0 + cn], W=[wm])
            offs[b] = o; o += cn
        return wm, offs

    def proj_T(self, wm, c0, pf, tg, shift=0, start=True, stop=True, M=128):
        S = self.S
        for k in range(8):
            S.op('pe', lambda e, k=k: e.matmul(pf[0:M, :], lhsT=wm[:, k, c0:c0 + M], rhs=self.hT[:, k, 1 - shift + tg * 512: 1 - shift + (tg + 1) * 512],
                                              start=(start and k == 0), stop=(stop and k == 7)), R=[wm, self.hT], W=[pf])

    def proj_tok(self, wm, c0, n, pf_ap, pf, i, shift=0, start=True, stop=True):
        S = self.S
        for k in range(8):
            S.op('pe', lambda e, k=k: e.matmul(pf_ap, lhsT=self.hT[:, k, 1 - shift + i * 128: 1 - shift + (i + 1) * 128], rhs=wm[:, k, c0:c0 + n],
                                              start=(start and k == 0), stop=(stop and k == 7)), R=[wm, self.hT], W=[pf])

    def rope_tables(self, st, cname, name):
        S = self.S
        rc = self.load_const(st, cname, F32)
        C = self.sb(st, name + "C", [128, SEQ], BF16); Sg = self.sb(st, name + "S", [128, SEQ], BF16)
        CH = 512
        with ExitStack() as s2:
            posi = self.sb(s2, name + "pi", [128, CH], I32); posf = self.sb(s2, name + "pf", [128, CH], F32)
            u = self.sb(s2, name + "u", [128, CH], F32); ui = self.sb(s2, name + "ui", [128, CH], I32)
            uf = self.sb(s2, name + "uf", [128, CH], F32)
            for ch in range(SEQ // CH):
                cs = slice(ch * CH, (ch + 1) * CH)
                self.dma('sp', posi[:], self.pos[0:1, cs].partition_broadcast(128), W=[posi])
                S.op('dve', lambda e: e.tensor_copy(out=posf[:], in_=posi[:]), R=[posi], W=[posf])
                for phase, dst in ((0.0, Sg), (0.25, C)):
                    S.op('dve', lambda e, phase=phase: e.tensor_scalar(out=u[:], in0=posf[:], scalar1=rc[:, 0:1], scalar2=phase, op0=ALU.mult, op1=ALU.add),
                         R=[posf, rc], W=[u])
                    S.op('dve', lambda e: e.tensor_copy(out=ui[:], in_=u[:]), R=[u], W=[ui])
                    S.op('dve', lambda e: e.tensor_copy(out=uf[:], in_=ui[:]), R=[ui], W=[uf])
                    S.op('dve', lambda e: e.tensor_tensor(out=u[:], in0=u[:], in1=uf[:], op=ALU.subtract), R=[u, uf], W=[u])
                    S.op('dve', lambda e: e.tensor_scalar(out=u[:], in0=u[:], scalar1=0.5, scalar2=-0.5, op0=ALU.min, op1=ALU.max), R=[u], W=[u])
                    if dst is Sg:
                        S.op('act', lambda e: e.activation(out=uf[:], in_=u[:], func=AF.Sin, scale=2 * np.pi), R=[u], W=[uf])
                        S.op('dve', lambda e, cs=cs: e.tensor_scalar(out=Sg[:, cs], in0=uf[:], scalar1=rc[:, 1:2], scalar2=None, op0=ALU.mult), R=[uf, rc], W=[Sg])
                    else:
                        S.op('act', lambda e, cs=cs: e.activation(out=C[:, cs], in_=u[:], func=AF.Sin, scale=2 * np.pi), R=[u], W=[C])
            self.S.barrier(); self.S.emit()
        return C, Sg

    def y_store(self, ytile_idx, i, ytok, ystage, R):
        S = self.S
        pb = self.PB[i % 2]
        for f in range(2):
            S.op('pe', lambda e, f=f: e.transpose(out=pb[:, f * 128:(f + 1) * 128], in_=ytok[:, f * 128:(f + 1) * 128], identity=self.ident[:]),
                 R=[ytok, self.ident] + list(R), W=[pb])
        S.op('act', lambda e: e.activation(out=ystage[:, :, (i % 4) * 128:(i % 4 + 1) * 128], in_=pb[:, 0:256].rearrange("p (f t) -> p f t", f=2), func=AF.Copy),
             R=[pb], W=[ystage])
        if i % 4 == 3:
            g = i // 4
            for f in range(2):
                t = TB()
                self.dma('sp', self.yT_d[ytile_idx + f, :, g * 512:(g + 1) * 512], ystage[:, f, :], R=[ystage], W=[t])

    def retention_phase(self, l):
        S = self.S
        with ExitStack() as st:
            C, Sg = self.rope_tables(st, 'rope_ret', 'rr')
            gnb = self.bcast_row(st, "ret_gnb", self.ret_gn[l, :], 256)
            ytok_all = self.sb(st, "ret_ytok", [128, NT, 256], BF16)
            for hp in range(2):
                with ExitStack() as s2:
                    wm, wo = self.load_w(s2, "wm_ret", l, [f'ret_q{hp}', f'ret_qs{hp}', f'ret_k{hp}', f'ret_ks{hp}', f'ret_vg{hp}'])
                    QT = self.sb(s2, "QT", [128, SEQ], BF16); QS = self.sb(s2, "QS", [128, SEQ], BF16)
                    KT = self.sb(s2, "KT", [128, SEQ], BF16); KS = self.sb(s2, "KS", [128, SEQ], BF16)
                    qdec = self.load_const(s2, f'ret_qdec{hp}', BF16); kdec = self.load_const(s2, f'ret_kdec{hp}', F32)
                    dmask = self.load_const(s2, f'ret_dmask{hp}', F32); cd = self.load_const(s2, f'ret_cd{hp}', F32)
                    for name, dst in ((f'ret_q{hp}', QT), (f'ret_qs{hp}', QS), (f'ret_k{hp}', KT), (f'ret_ks{hp}', KS)):
                        for tg in range(8):
                            pf = self.PF[tg % 4]
                            self.proj_T(wm, wo[name], pf, tg)
                            eng = 'act' if tg % 2 == 0 else 'dve'
                            if eng == 'act':
                                S.op('act', lambda e, pf=pf, dst=dst, tg=tg: e.activation(out=dst[:, tg * 512:(tg + 1) * 512], in_=pf[:], func=AF.Copy), R=[pf], W=[dst])
                            else:
                                S.op('dve', lambda e, pf=pf, dst=dst, tg=tg: e.tensor_copy(out=dst[:, tg * 512:(tg + 1) * 512], in_=pf[:]), R=[pf], W=[dst])
                    for A, B_ in ((QT, QS), (KT, KS)):
                        S.op('dve', lambda e, A=A: e.tensor_tensor(out=A[:], in0=A[:], in1=C[:], op=ALU.mult), R=[A, C], W=[A])
                        S.op('pool', lambda e, B_=B_: e.tensor_tensor(out=B_[:], in0=B_[:], in1=Sg[:], op=ALU.mult), R=[B_, Sg], W=[B_])
                        S.op('dve', lambda e, A=A, B_=B_: e.tensor_tensor(out=A[:], in0=A[:], in1=B_[:], op=ALU.add), R=[A, B_], W=[A])
                    QD = QS
                    S.op('dve', lambda e: e.tensor_tensor(out=QD[:].rearrange("p (c n) -> p c n", n=128), in0=QT[:].rearrange("p (c n) -> p c n", n=128),
                                                         in1=qdec[:].unsqueeze(1).broadcast_to([128, NT, 128]), op=ALU.mult), R=[QT, qdec], W=[QD])
                    state = self.sb(s2, "rstate", [128, 128], F32); state_bf = self.sb(s2, "rstate_bf", [128, 128], BF16)
                    qbd = [self.sb(s2, f"qbd{i}", [128, 256], BF16) for i in range(2)]
                    for i in range(2):
                        S.op('pool', lambda e, i=i: e.memset(qbd[i][:], 0.0), W=[qbd[i]])
                    S.op('dve', lambda e: e.memset(state[:], 0.0), W=[state])
                    S.op('dve', lambda e: e.memset(state_bf[:], 0.0), W=[state_bf])
                    vg = [self.sb(s2, f"rvg{i}", [128, 128], BF16) for i in range(2)]
                    sg = [self.sb(s2, f"rsg{i}", [128, 128], F32) for i in range(2)]
                    kd = [self.sb(s2, f"rkd{i}", [128, 128], BF16) for i in range(2)]
                    pT = [self.sb(s2, f"rpT{i}", [128, 256], BF16) for i in range(2)]
                    o_sb = self.sb(s2, "ro", [128, 128], F32); cen = self.sb(s2, "rcen", [128, 128], F32); sq = self.sb(s2, "rsq", [128, 128], F32)
                    st4 = self.sb(s2, "rst4", [128, 4], F32)
                    for c in range(NT):
                        i2 = c % 2
                        tok = slice(c * 128, (c + 1) * 128)
                        pfv = self.PF[0]
                        self.proj_tok(wm, wo[f'ret_vg{hp}'], 256, pfv[:, 0:256], pfv, c)
                        S.op('dve', lambda e, i2=i2: e.tensor_copy(out=vg[i2][:], in_=pfv[:, 0:128]), R=[pfv], W=[vg[i2]])
                        S.op('act', lambda e, i2=i2: e.activation(out=sg[i2][:], in_=pfv[:, 128:256], func=AF.Silu), R=[pfv], W=[sg[i2]])
                        pb = self.PB[0]
                        S.op('pe', lambda e, tok=tok: e.transpose(out=pb[:, 0:128], in_=KT[:, tok], identity=self.ident[:]), R=[KT, self.ident], W=[pb])
                        S.op('dve', lambda e, i2=i2: e.tensor_tensor(out=kd[i2][:], in0=pb[:, 0:128], in1=kdec[:], op=ALU.mult), R=[pb, kdec], W=[kd[i2]])
                        pfs = self.PF[1]
                        for hh in range(2):
                            pr = slice(hh * 64, (hh + 1) * 64)
                            S.op('pool', lambda e, hh=hh, pr=pr, tok=tok, i2=i2: e.tensor_copy(out=qbd[i2][pr, hh * 128:(hh + 1) * 128], in_=QT[pr, tok]), R=[QT, qbd[i2]], W=[qbd[i2]])
                        S.op('pe', lambda e, tok=tok, i2=i2: e.matmul(pfs[:, 0:256], lhsT=KT[:, tok], rhs=qbd[i2][:], start=True, stop=True), R=[KT, qbd[i2]], W=[pfs])
                        S.op('dve', lambda e, i2=i2: e.tensor_tensor(out=pT[i2][:], in0=pfs[:, 0:256], in1=dmask[:], op=ALU.mult), R=[pfs, dmask], W=[pT[i2]])
                        pfo = self.PF[2]
                        S.op('pe', lambda e, tok=tok: e.matmul(pfo[:, 0:128], lhsT=QD[:, tok], rhs=state_bf[:, :], start=True, stop=False), R=[QD, state_bf], W=[pfo])
                        for hh in range(2):
                            S.op('pe', lambda e, hh=hh, i2=i2: e.matmul(pfo[:, hh * 64:(hh + 1) * 64], lhsT=pT[i2][:, hh * 128:(hh + 1) * 128], rhs=vg[i2][:, hh * 64:(hh + 1) * 64],
                                                                      start=False, stop=(hh == 1)), R=[pT[i2], vg[i2]], W=[pfo])
                        pfu = self.PF[3]
                        S.op('pe', lambda e, i2=i2: e.matmul(pfu[:, 0:128], lhsT=kd[i2][:], rhs=vg[i2][:], start=True, stop=True), R=[kd[i2], vg[i2]], W=[pfu])
                        for hh in range(2):
                            pr = slice(hh * 64, (hh + 1) * 64)
                            cs = slice(hh * 64, (hh + 1) * 64)
                            S.op('dve', lambda e, hh=hh, pr=pr, cs=cs: e.scalar_tensor_tensor(out=state[pr, cs], in0=state[pr, cs], scalar=cd[pr, 0:1], in1=pfu[pr, cs],
                                                                                            op0=ALU.mult, op1=ALU.add), R=[state, cd, pfu], W=[state])
                        S.op('act', lambda e: e.activation(out=state_bf[:], in_=state[:], func=AF.Copy), R=[state], W=[state_bf])
                        S.op('act', lambda e: e.activation(out=o_sb[:], in_=pfo[:, 0:128], func=AF.Copy), R=[pfo], W=[o_sb])
                        self.head_norm(o_sb, cen, sq, st4, 2, 1e-5)
                        S.op('dve', lambda e, hp=hp: e.tensor_tensor(out=cen[:], in0=cen[:], in1=gnb[:, hp * 128:(hp + 1) * 128], op=ALU.mult), R=[cen, gnb], W=[cen])
                        S.op('dve', lambda e, i2=i2, c=c, hp=hp: e.tensor_tensor(out=ytok_all[:, c, hp * 128:(hp + 1) * 128], in0=cen[:], in1=sg[i2][:], op=ALU.mult),
                             R=[cen, sg[i2]], W=[ytok_all])
                    S.barrier(); S.emit()
            ystage = self.sb(st, "ystage", [128, 2, 512], BF16)
            for i in range(NT):
                self.y_store(0, i, _View(ytok_all, i), ystage, [])
            S.barrier(); S.emit()


    def rms_finalize(self, st, o_all, gain_b, ytile_idx, name):
        S = self.S
        ystage = self.sb(st, name + "ystage", [128, 2, 512], BF16)
        junk = self.sb(st, name + "junk", [128, 256], F32)
        ss = [self.sb(st, f"{name}ss{i}", [128, 1], F32) for i in range(2)]
        yt = [self.sb(st, f"{name}yt{i}", [128, 256], BF16) for i in range(2)]
        for i in range(NT):
            s_, y_ = ss[i % 2], yt[i % 2]
            S.op('act', lambda e, i=i, s_=s_: e.activation(out=junk[:], in_=o_all[:, i, :], func=AF.Square, accum_out=s_[:]), R=[o_all], W=[junk, s_])
            S.op('dve', lambda e, s_=s_: e.tensor_scalar(out=s_[:], in0=s_[:], scalar1=1.0 / 256, scalar2=1e-5, op0=ALU.mult, op1=ALU.add), R=[s_], W=[s_])
            S.op('act', lambda e, s_=s_: e.activation(out=s_[:], in_=s_[:], func=AF.Sqrt), R=[s_], W=[s_])
            S.op('dve', lambda e, s_=s_: e.reciprocal(out=s_[:], in_=s_[:]), R=[s_], W=[s_])
            S.op('dve', lambda e, i=i, s_=s_, y_=y_: e.scalar_tensor_tensor(out=y_[:], in0=o_all[:, i, :], scalar=s_[:, 0:1], in1=gain_b[:], op0=ALU.mult, op1=ALU.mult),
                 R=[o_all, s_, gain_b], W=[y_])
            self.y_store(ytile_idx, i, y_, ystage, [])

    def sb_phase(self, l):
        S = self.S
        with ExitStack() as st:
            ntri = self.load_const(st, 'ntri_ge', BF16); nones = self.load_const(st, 'nones', BF16)
            sbmask = self.load_const(st, 'sbmask', BF16)
            zer = self.sb(st, "sbzero", [128, 256], BF16)
            S.op('pool', lambda e: e.memset(zer[:], 0.0), W=[zer])
            onb = self.bcast_row(st, "sb_onb", self.sb_onorm[l, :], 256)
            o_all = self.sb(st, "sb_oall", [128, NT, 256], BF16)
            for hp in range(2):
                with ExitStack() as s2:
                    wm, wo = self.load_w(s2, "wm_sb", l, [f'sb_q{hp}', f'sb_k{hp}', 'sb_v'])
                    QT = self.sb(s2, "sbQT", [128, SEQ], BF16)
                    KM = [self.sb(s2, f"sbKM{i}", [128, SEQ], BF16) for i in range(2)]
                    V = self.sb(s2, "sbV", [128, NT, 128], BF16)
                    for i in range(2):
                        S.op('pool', lambda e, i=i: e.memset(KM[i][:], 0.0), W=[KM[i]])
                    for tg in range(8):
                        pf = self.PF[tg % 2]
                        self.proj_T(wm, wo[f'sb_q{hp}'], pf, tg)
                        S.op('act', lambda e, pf=pf, tg=tg: e.activation(out=QT[:, tg * 512:(tg + 1) * 512], in_=pf[:], func=AF.Copy, scale=0.125), R=[pf], W=[QT])
                        pf2 = self.PF[2 + tg % 2]
                        self.proj_T(wm, wo[f'sb_k{hp}'], pf2, tg)
                        S.op('dve', lambda e, pf2=pf2, tg=tg: e.tensor_copy(out=KM[0][0:64, tg * 512:(tg + 1) * 512], in_=pf2[0:64, :]), R=[pf2], W=[KM[0]])
                        S.op('act', lambda e, pf2=pf2, tg=tg: e.activation(out=KM[1][64:128, tg * 512:(tg + 1) * 512], in_=pf2[64:128, :], func=AF.Copy), R=[pf2], W=[KM[1]])
                    for i in range(NT):
                        pf = self.PF[i % 2]
                        self.proj_tok(wm, wo['sb_v'] + hp * 128, 128, pf[:, 0:128], pf, i)
                        S.op('dve', lambda e, pf=pf, i=i: e.tensor_copy(out=V[:, i, :], in_=pf[:, 0:128]), R=[pf], W=[V])
                    ebuf = [[self.sb(s2, f"sbe{h}{i}", [128, 512], F32) for i in range(2)] for h in range(2)]
                    spm = [[self.sb(s2, f"sbsp{h}{i}", [128, 512], BF16) for i in range(2)] for h in range(2)]
                    tbuf = [[self.sb(s2, f"sbt{h}{i}", [128, 512], F32) for i in range(2)] for h in range(2)]
                    abuf = [[self.sb(s2, f"sba{h}{i}", [128, 512], BF16) for i in range(2)] for h in range(2)]
                    racc = [self.sb(s2, f"sbracc{h}", [128, 512], F32) for h in range(2)]
                    cnt = 0
                    for g in range(8):
                        qs = slice(g * 512, (g + 1) * 512)
                        for hh in range(2):
                            po = self.PF[4 + hh]
                            S.op('pool', lambda e, hh=hh: e.memset(racc[hh][:], 0.0), W=[racc[hh]])
                            S.op('pe', lambda e, po=po: e.matmul(po[:, 0:256], lhsT=zer[:, 0:128], rhs=zer[:, 0:256], start=True, stop=False), R=[zer], W=[po])
                        nkb = 4 * g + 4
                        for kb in reversed(range(nkb)):
                            b2 = cnt % 2; cnt += 1
                            r = kb - 4 * g
                            ks = slice(kb * 128, (kb + 1) * 128)
                            HH = (0, 1)
                            E_ = [ebuf[h][b2] for h in HH]; SP_ = [spm[h][b2] for h in HH]; T_ = [tbuf[h][b2] for h in HH]; A_ = [abuf[h][b2] for h in HH]
                            PZ = [self.PF[0], self.PF[1]]; PC = [self.PF[2], self.PF[3]]; PO = [self.PF[4], self.PF[5]]
                            for hh in HH:
                                S.op('pe', lambda e, hh=hh, ks=ks, qs=qs: e.matmul(PZ[hh][:], lhsT=KM[hh][:, ks], rhs=QT[:, qs], start=True, stop=True), R=[KM[hh], QT], W=[PZ[hh]])
                            for hh in HH:
                                S.op('act', lambda e, hh=hh, E_=E_: e.activation(out=E_[hh][:], in_=PZ[hh][:], func=AF.Exp), R=[PZ[hh]], W=[E_[hh]])
                            for hh in HH:
                                S.op('act', lambda e, hh=hh, E_=E_, SP_=SP_: e.activation(out=SP_[hh][:], in_=E_[hh][:], func=AF.Ln, bias=1.0), R=[E_[hh]], W=[SP_[hh]])
                            if r >= 0:
                                for hh in HH:
                                    S.op('pool', lambda e, hh=hh, SP_=SP_, r=r: e.tensor_tensor(out=SP_[hh][:], in0=SP_[hh][:], in1=sbmask[:, r * 512:(r + 1) * 512], op=ALU.mult), R=[SP_[hh], sbmask], W=[SP_[hh]])
                            for hh in HH:
                                S.op('pe', lambda e, hh=hh, ks=ks, qs=qs: e.matmul(PC[hh][:], lhsT=KM[hh][:, ks], rhs=QT[:, qs], start=True, stop=False), R=[KM[hh], QT], W=[PC[hh]])
                                S.op('pe', lambda e, hh=hh, SP_=SP_: e.matmul(PC[hh][:], lhsT=ntri[:], rhs=SP_[hh][:], start=False, stop=True), R=[ntri, SP_[hh]], W=[PC[hh]])
                            for hh in HH:
                                S.op('dve', lambda e, hh=hh, T_=T_: e.tensor_tensor(out=T_[hh][:], in0=PC[hh][:], in1=racc[hh][:], op=ALU.add), R=[PC[hh], racc[hh]], W=[T_[hh]])
                            for hh in HH:
                                S.op('act', lambda e, hh=hh, T_=T_, A_=A_: e.activation(out=A_[hh][:], in_=T_[hh][:], func=AF.Exp), R=[T_[hh]], W=[A_[hh]])
                            if r >= 0:
                                for hh in HH:
                                    S.op('pool', lambda e, hh=hh, A_=A_, r=r: e.tensor_tensor(out=A_[hh][:], in0=A_[hh][:], in1=sbmask[:, r * 512:(r + 1) * 512], op=ALU.mult), R=[A_[hh], sbmask], W=[A_[hh]])
                            if kb > 0:
                                for hh in HH:
                                    S.op('pe', lambda e, hh=hh, SP_=SP_: e.matmul(PZ[hh][:], lhsT=nones[:], rhs=SP_[hh][:], start=True, stop=True), R=[nones, SP_[hh]], W=[PZ[hh]])
                                for hh in HH:
                                    S.op('dve', lambda e, hh=hh: e.tensor_tensor(out=racc[hh][:], in0=PZ[hh][:], in1=racc[hh][:], op=ALU.add), R=[PZ[hh], racc[hh]], W=[racc[hh]])
                            for hh in HH:
                                for qb in range(4):
                                    if r >= 0 and qb < r:
                                        continue
                                    S.op('pe', lambda e, qb=qb, hh=hh, A_=A_, kb=kb: e.matmul(PO[hh][:, qb * 64:(qb + 1) * 64], lhsT=A_[hh][:, qb * 128:(qb + 1) * 128], rhs=V[:, kb, hh * 64:(hh + 1) * 64],
                                                                                         start=False, stop=(kb == 0 and qb == 3)), R=[A_[hh], V], W=[PO[hh]])
                        for hh in range(2):
                            hcol = (hp * 2 + hh) * 64
                            po = self.PF[4 + hh]
                            S.op('act', lambda e, g=g, hcol=hcol, po=po: e.activation(out=o_all[:, 4 * g:4 * g + 4, hcol:hcol + 64], in_=po[:, 0:256].rearrange("p (q d) -> p q d", d=64), func=AF.Copy),
                                 R=[po], W=[o_all])
                    S.barrier(); S.emit()
            self.rms_finalize(st, o_all, onb, 6, "sbf")
            S.barrier(); S.emit()


    def issue_weight_cast(self, l):
        if l in self.conv_tb or self.flags.get('nomoe'):
            return
        tb = self.conv_tb[l] = TB()
        for e_ in range(32):
            r0 = (l * 32 + e_) * D
            self.dma('pool', self.w1b_d[r0:r0 + D, :], self.moe_w1[l, e_], W=[tb])
            self.dma('pool', self.w2b_d[r0:r0 + D, :], self.moe_w2[l, e_], W=[tb])

    def _chk(self, n):
        if self.flags.get('rw_stop') == n:
            raise _Stop()

    def rwkv_phase(self, l):
        try:
            self.rwkv_phase_(l)
        except _Stop:
            pass

    def rwkv_phase_(self, l):
        S = self.S
        V3 = lambda ap, d=64: ap.rearrange("p (h d) -> p h d", d=d)
        with ExitStack() as st:
          try:
              wm, wo = self.load_w(st, "wm_rw", l, ['rw_rkv', 'rw_lora'])
              wmu = self.sb(st, "rw_wmu", [128, 8, 896], BF16)
              mub = self.bcast_row(st, "rw_mub", self.rwkv_mu[l, :], 896)
              S.op('dve', lambda e: e.tensor_tensor(out=wmu[:], in0=wm[:], in1=mub[:].unsqueeze(1).broadcast_to([128, 8, 896]), op=ALU.mult), R=[wm, mub], W=[wmu])
              S.op('pool', lambda e: e.tensor_tensor(out=wm[:], in0=wm[:], in1=wmu[:], op=ALU.subtract), R=[wm, wmu], W=[wm])
              lwbd = self.sb(st, "rw_lwbd", [128, 768], BF16)
              S.op('pool', lambda e: e.memset(lwbd[:], 0.0), W=[lwbd])
              for (r0, r1, c0) in ((0, 32, 0), (32, 64, 256), (64, 128, 512)):
                  self.dma('pool', lwbd[r0:r1, c0:c0 + 256], self.rwkv_lw[l, r0:r1, :], R=[lwbd], W=[lwbd])
              w0b = self.bcast_row(st, "rw_w0b", self.rwkv_w0[l, :], 256); a0b = self.bcast_row(st, "rw_a0b", self.rwkv_a0[l, :], 256)
              kkb = self.bcast_row(st, "rw_kkb", self.rwkv_kk[l, :], 256); kab = self.bcast_row(st, "rw_kab", self.rwkv_ka[l, :], 256)
              rkb = self.bcast_row(st, "rw_rkb", self.rwkv_rk[l, :], 256); lnb = self.bcast_row(st, "rw_lnb", self.rwkv_ln[l, :], 256)
              tri_le = self.load_const(st, 'tri_le', F32); lastc = self.load_const(st, 'last', F32)
              m_lt = self.load_const(st, 'tri_lt', F32); m_le = self.load_const(st, 'tri_le', F32); m_gt = self.load_const(st, 'tri_gt', F32)
              LT = self.sb(st, "rw_LT", [128, SEQ], BF16)
              for tg in range(8):
                  pf = self.PF[tg % 2]
                  self.proj_T(wm, wo['rw_lora'], pf, tg, shift=0, start=True, stop=False)
                  self.proj_T(wmu, wo['rw_lora'], pf, tg, shift=1, start=False, stop=True)
                  sl = slice(tg * 512, (tg + 1) * 512)
                  S.op('act', lambda e, pf=pf, sl=sl: e.activation(out=LT[0:32, sl], in_=pf[0:32, :], func=AF.Tanh), R=[pf], W=[LT])
                  S.op('act', lambda e, pf=pf, sl=sl: e.activation(out=LT[32:64, sl], in_=pf[32:64, :], func=AF.Copy), R=[pf], W=[LT])
                  S.op('act', lambda e, pf=pf, sl=sl: e.activation(out=LT[64:128, sl], in_=pf[64:128, :], func=AF.Sigmoid), R=[pf], W=[LT])
              self._chk(1)
              f32t = lambda n: self.sb(st, "rw_" + n, [128, 256], F32)
              bft = lambda n: self.sb(st, "rw_" + n, [128, 256], BF16)
              r_sb, k_sb, v_sb, a_sb, kk_sb, k2_sb, lw_sb, cum_sb, t1, t2, t3 = [f32t(n) for n in ('r', 'k', 'v', 'a', 'kk', 'k2', 'lw', 'cum', 't1', 't2', 't3')]
              gate_sb = f32t('gate')
              rt_b, kt_b, bt_b, at_b, v_bf, G_bf, U_bf = [bft(n) for n in ('rt', 'kt', 'bt', 'at', 'vbf', 'G', 'U')]
              st4 = self.sb(st, "rw_st4", [128, 4], F32)
              fm = self.sb(st, "rw_fm", [128, 8, 128], BF16)
              artbd = [self.sb(st, f"rw_artbd{p}", [128, 2, 256], BF16) for p in range(2)]
              btbd = [self.sb(st, f"rw_btbd{p}", [128, 2, 128], BF16) for p in range(2)]
              import os
              SKIP = os.environ.get('RW_SKIP', '').split(',')
              for p in range(2):
                  if 'b' in SKIP: break
                  S.op('pool', lambda e, p=p: e.memset(artbd[p][:], 0.0), W=[artbd[p]])
                  S.op('pool', lambda e, p=p: e.memset(btbd[p][:], 0.0), W=[btbd[p]])
              NU = [self.sb(st, f"rw_NU{i}", [128, 4, 128], F32) for i in range(2)]
              LL = [self.sb(st, f"rw_LL{i}", [128, 4, 128], F32) for i in range(2)]
              XX = [self.sb(st, f"rw_X{i}", [128, 4, 128], F32) for i in range(2)]
              G_f = self.sb(st, "rw_Gf", [128, 256], F32)
              RBm = self.sb(st, "rw_RB", [128, 4, 128], BF16); RKm = self.sb(st, "rw_RK", [128, 4, 128], BF16); MKm = self.sb(st, "rw_MK", [128, 4, 128], BF16)
              ST = [self.sb(st, f"rw_ST{p}", [128, 128], F32) for p in range(2)]
              STb = [self.sb(st, f"rw_STb{p}", [128, 128], BF16) for p in range(2)]
              ecl = self.sb(st, "rw_ecl", [128, 2], F32)
              for p in range(2):
                  if 'c' in SKIP: break
                  S.op('dve', lambda e, p=p: e.memset(ST[p][:], 0.0), W=[ST[p]])
                  S.op('dve', lambda e, p=p: e.memset(STb[p][:], 0.0), W=[STb[p]])
              o_sb = f32t('o'); cen = f32t('cen'); sq = f32t('sq')
              ystage = self.sb(st, "rw_ystage", [128, 2, 512], BF16)
              ytok = [self.sb(st, f"rw_ytok{i}", [128, 256], BF16) for i in range(2)]
              PF = self.PF
              for c in range(NT):
                  tok = slice(1 + c * 128, 1 + (c + 1) * 128); tokp = slice(c * 128, (c + 1) * 128)
                  for (pf, c0, n) in ((PF[0], 0, 512), (PF[1], 512, 256)):
                      if 'd' in SKIP: break
                      for k in range(8):
                          S.op('pe', lambda e, k=k, pf=pf, c0=c0, n=n, tok=tok: e.matmul(pf[:, 0:n], lhsT=self.hT[:, k, tok], rhs=wm[:, k, c0:c0 + n], start=(k == 0), stop=False), R=[wm, self.hT], W=[pf])
                      for k in range(8):
                          S.op('pe', lambda e, k=k, pf=pf, c0=c0, n=n, tokp=tokp: e.matmul(pf[:, 0:n], lhsT=self.hT[:, k, tokp], rhs=wmu[:, k, c0:c0 + n], start=False, stop=(k == 7)), R=[wmu, self.hT], W=[pf])
                  if 'e' in SKIP: self._chk(2)
                  S.op('act', lambda e: e.activation(out=r_sb[:], in_=PF[0][:, 0:256], func=AF.Copy), R=[PF[0]], W=[r_sb])
                  S.op('dve', lambda e: e.tensor_copy(out=k_sb[:], in_=PF[0][:, 256:512]), R=[PF[0]], W=[k_sb])
                  S.op('act', lambda e: e.activation(out=v_sb[:], in_=PF[1][:, 0:256], func=AF.Copy), R=[PF[1]], W=[v_sb])
                  if 'f' not in SKIP:
                      S.op('pool', lambda e: e.tensor_copy(out=v_bf[:], in_=v_sb[:]), R=[v_sb], W=[v_bf])
                  else:
                      S.op('dve', lambda e: e.tensor_copy(out=v_bf[:], in_=v_sb[:]), R=[v_sb], W=[v_bf])
                  self._chk(2)
                  S.op('pe', lambda e, c=c: e.matmul(PF[2][:, 0:512], lhsT=LT[:, c * 128:(c + 1) * 128], rhs=lwbd[:, 0:512], start=True, stop=True), R=[LT, lwbd], W=[PF[2]])
                  S.op('pe', lambda e, c=c: e.matmul(PF[3][:, 0:256], lhsT=LT[:, c * 128:(c + 1) * 128], rhs=lwbd[:, 512:768], start=True, stop=True), R=[LT, lwbd], W=[PF[3]])
                  S.op('act', lambda e: e.activation(out=gate_sb[:], in_=PF[3][:, 0:256], func=AF.Copy), R=[PF[3]], W=[gate_sb])
                  self._chk(3)
                  S.op('dve', lambda e: e.tensor_tensor(out=t1[:], in0=PF[2][:, 0:256], in1=w0b[:], op=ALU.add), R=[PF[2], w0b], W=[t1])
                  S.op('act', lambda e: e.activation(out=t1[:], in_=t1[:], func=AF.Sigmoid), R=[t1], W=[t1])
                  S.op('dve', lambda e: e.tensor_scalar(out=lw_sb[:], in0=t1[:], scalar1=-0.6065306597126334, scalar2=None, op0=ALU.mult), R=[t1], W=[lw_sb])
                  S.op('dve', lambda e: e.tensor_tensor(out=t2[:], in0=PF[2][:, 256:512], in1=a0b[:], op=ALU.add), R=[PF[2], a0b], W=[t2])
                  S.op('act', lambda e: e.activation(out=a_sb[:], in_=t2[:], func=AF.Sigmoid), R=[t2], W=[a_sb])
                  S.op('dve', lambda e: e.tensor_tensor(out=kk_sb[:], in0=k_sb[:], in1=kkb[:], op=ALU.mult), R=[k_sb, kkb], W=[kk_sb])
                  S.op('pool', lambda e: e.tensor_tensor(out=t3[:], in0=kk_sb[:], in1=kk_sb[:], op=ALU.mult), R=[kk_sb], W=[t3])
                  S.op('dve', lambda e: e.tensor_reduce(out=st4[:], in_=V3(t3[:]), axis=AX.X, op=ALU.add), R=[t3], W=[st4])
                  S.op('act', lambda e: e.activation(out=st4[:], in_=st4[:], func=AF.Sqrt), R=[st4], W=[st4])
                  S.op('dve', lambda e: e.tensor_scalar(out=st4[:], in0=st4[:], scalar1=1e-12, scalar2=None, op0=ALU.max), R=[st4], W=[st4])
                  S.op('dve', lambda e: e.reciprocal(out=st4[:], in_=st4[:]), R=[st4], W=[st4])
                  S.op('dve', lambda e: e.tensor_tensor(out=V3(kk_sb[:]), in0=V3(kk_sb[:]), in1=st4[:].unsqueeze(2).broadcast_to([128, 4, 64]), op=ALU.mult), R=[kk_sb, st4], W=[kk_sb])
                  S.op('dve', lambda e: e.scalar_tensor_tensor(out=t2[:], in0=a_sb[:], scalar=-1.0, in1=kab[:], op0=ALU.add, op1=ALU.mult), R=[a_sb, kab], W=[t2])
                  S.op('dve', lambda e: e.scalar_tensor_tensor(out=k2_sb[:], in0=t2[:], scalar=1.0, in1=k_sb[:], op0=ALU.add, op1=ALU.mult), R=[t2, k_sb], W=[k2_sb])
                  self._chk(4)
                  S.op('pe', lambda e: e.matmul(PF[4][:, 0:256], lhsT=tri_le[:], rhs=lw_sb[:], start=True, stop=True), R=[tri_le, lw_sb], W=[PF[4]])
                  S.op('act', lambda e: e.activation(out=cum_sb[:], in_=PF[4][:, 0:256], func=AF.Copy), R=[PF[4]], W=[cum_sb])
                  S.op('act', lambda e: e.activation(out=t1[:], in_=PF[4][:, 0:256], func=AF.Exp), R=[PF[4]], W=[t1])
                  S.op('act', lambda e: e.activation(out=t2[:], in_=PF[4][:, 0:256], func=AF.Exp, scale=-1.0), R=[PF[4]], W=[t2])
                  S.op('dve', lambda e: e.tensor_tensor(out=t3[:], in0=cum_sb[:], in1=lw_sb[:], op=ALU.subtract), R=[cum_sb, lw_sb], W=[t3])
                  S.op('act', lambda e: e.activation(out=t3[:], in_=t3[:], func=AF.Exp), R=[t3], W=[t3])
                  S.op('dve', lambda e: e.tensor_tensor(out=rt_b[:], in0=r_sb[:], in1=t1[:], op=ALU.mult), R=[r_sb, t1], W=[rt_b])
                  S.op('pool', lambda e: e.tensor_tensor(out=kt_b[:], in0=k2_sb[:], in1=t2[:], op=ALU.mult), R=[k2_sb, t2], W=[kt_b])
                  S.op('dve', lambda e: e.scalar_tensor_tensor(out=at_b[:], in0=kk_sb[:], scalar=-1.0, in1=t3[:], op0=ALU.mult, op1=ALU.mult), R=[kk_sb, t3], W=[at_b])
                  S.op('dve', lambda e: e.tensor_tensor(out=t3[:], in0=kk_sb[:], in1=a_sb[:], op=ALU.mult), R=[kk_sb, a_sb], W=[t3])
                  S.op('dve', lambda e: e.tensor_tensor(out=bt_b[:], in0=t3[:], in1=t2[:], op=ALU.mult), R=[t3, t2], W=[bt_b])
                  self._chk(5)
                  for p in range(2):
                      S.op('pe', lambda e, p=p: e.matmul(PF[5][:, p:p + 1], lhsT=cum_sb[:, p * 128:(p + 1) * 128], rhs=lastc[:, 0:1], start=True, stop=True), R=[cum_sb, lastc], W=[PF[5]])
                  S.op('act', lambda e: e.activation(out=ecl[:], in_=PF[5][:, 0:2], func=AF.Exp), R=[PF[5]], W=[ecl])
                  self._chk(6)
                  pb = self.PB[0]
                  for xi, src_ in enumerate((at_b, rt_b, bt_b, kt_b)):
                      for p in range(2):
                          j = xi * 2 + p
                          S.op('pe', lambda e, j=j, src_=src_, p=p: e.transpose(out=pb[:, j * 128:(j + 1) * 128], in_=src_[:, p * 128:(p + 1) * 128], identity=self.ident[:]), R=[src_, self.ident], W=[pb])
                  S.op('act', lambda e: e.activation(out=fm[:].rearrange("p j t -> p (j t)"), in_=pb[:], func=AF.Copy), R=[pb], W=[fm])
                  for p in range(2):
                      for hh in range(2):
                          pr = slice(hh * 64, (hh + 1) * 64)
                          S.op('pool', lambda e, p=p, hh=hh, pr=pr: e.tensor_copy(out=artbd[p][pr, hh, 0:128], in_=fm[pr, 0 + p, :]), R=[fm, artbd[p]], W=[artbd[p]])
                          S.op('pool', lambda e, p=p, hh=hh, pr=pr: e.tensor_copy(out=artbd[p][pr, hh, 128:256], in_=fm[pr, 2 + p, :]), R=[fm, artbd[p]], W=[artbd[p]])
                          S.op('pool', lambda e, p=p, hh=hh, pr=pr: e.tensor_copy(out=btbd[p][pr, hh, :], in_=fm[pr, 4 + p, :]), R=[fm, btbd[p]], W=[btbd[p]])
                  self._chk(7)
                  for p in range(2):
                      P1, P2, P3 = PF[0], PF[1], PF[2]
                      S.op('pe', lambda e, p=p: e.matmul(P1[:, 0:512], lhsT=fm[:, 4 + p, :], rhs=artbd[p][:].rearrange("p h c -> p (h c)"), start=True, stop=True), R=[fm, artbd[p]], W=[P1])
                      S.op('pe', lambda e, p=p: e.matmul(P2[:, 0:512], lhsT=fm[:, 6 + p, :], rhs=artbd[p][:].rearrange("p h c -> p (h c)"), start=True, stop=True), R=[fm, artbd[p]], W=[P2])
                      S.op('pe', lambda e, p=p: e.matmul(P3[:, 0:256], lhsT=fm[:, 0 + p, :], rhs=btbd[p][:].rearrange("p h c -> p (h c)"), start=True, stop=True), R=[fm, btbd[p]], W=[P3])
                      hs = slice(2 * p, 2 * p + 2)
                      v4 = lambda pf_: pf_[:, 0:512].rearrange("p (h w t) -> p h w t", h=2, w=2)
                      bc = lambda m: m[:].unsqueeze(1).broadcast_to([128, 2, 128])
                      S.op('dve', lambda e, hs=hs: e.tensor_tensor(out=NU[0][:, hs, :], in0=v4(P1)[:, :, 0, :], in1=bc(m_lt), op=ALU.mult), R=[P1, m_lt], W=[NU[0]])
                      S.op('dve', lambda e, hs=hs: e.tensor_tensor(out=RBm[:, hs, :], in0=v4(P1)[:, :, 1, :], in1=bc(m_le), op=ALU.mult), R=[P1, m_le], W=[RBm])
                      S.op('dve', lambda e, hs=hs: e.tensor_tensor(out=MKm[:, hs, :], in0=v4(P2)[:, :, 0, :], in1=bc(m_lt), op=ALU.mult), R=[P2, m_lt], W=[MKm])
                      S.op('dve', lambda e, hs=hs: e.tensor_tensor(out=RKm[:, hs, :], in0=v4(P2)[:, :, 1, :], in1=bc(m_le), op=ALU.mult), R=[P2, m_le], W=[RKm])
                      S.op('dve', lambda e, hs=hs: e.tensor_tensor(out=LL[0][:, hs, :], in0=P3[:, 0:256].rearrange("p (h t) -> p h t", h=2), in1=bc(m_gt), op=ALU.mult), R=[P3, m_gt], W=[LL[0]])
                  self._chk(8)
                  S.op('dve', lambda e: e.tensor_tensor(out=XX[0][:], in0=NU[0][:], in1=self.identf[:].unsqueeze(1).broadcast_to([128, 4, 128]), op=ALU.add), R=[NU[0], self.identf], W=[XX[0]])
                  cur = 0
                  for it in range(6):
                      nxt = 1 - cur
                      PL, PN, PX = PF[3], PF[4], PF[5]
                      for h in range(4):
                          S.op('pe', lambda e, h=h, cur=cur: e.matmul(PL[:, h * 128:(h + 1) * 128], lhsT=NU[cur][:, h, :], rhs=LL[cur][:, h, :], start=True, stop=True), R=[NU[cur], LL[cur]], W=[PL])
                      if it < 5:
                          for h in range(4):
                              S.op('pe', lambda e, h=h, cur=cur: e.matmul(PN[:, h * 128:(h + 1) * 128], lhsT=LL[cur][:, h, :], rhs=NU[cur][:, h, :], start=True, stop=True), R=[NU[cur], LL[cur]], W=[PN])
                      S.op('act', lambda e, nxt=nxt: e.activation(out=LL[nxt][:].rearrange("p h t -> p (h t)"), in_=PL[:], func=AF.Copy), R=[PL], W=[LL[nxt]])
                      if it < 5:
                          S.op('dve', lambda e, nxt=nxt: e.tensor_copy(out=NU[nxt][:].rearrange("p h t -> p (h t)"), in_=PN[:]), R=[PN], W=[NU[nxt]])
                      for h in range(4):
                          S.op('pe', lambda e, h=h, cur=cur, nxt=nxt: e.matmul(PX[:, h * 128:(h + 1) * 128], lhsT=LL[nxt][:, h, :], rhs=XX[cur][:, h, :], start=True, stop=True), R=[LL[nxt], XX[cur]], W=[PX])
                      S.op('dve', lambda e, cur=cur, nxt=nxt: e.tensor_tensor(out=XX[nxt][:].rearrange("p h t -> p (h t)"), in0=PX[:], in1=XX[cur][:].rearrange("p h t -> p (h t)"), op=ALU.add),
                           R=[PX, XX[cur]], W=[XX[nxt]])
                      cur = nxt
                  X = XX[cur]
                  self._chk(9)
                  PG, PU, PY, PS_ = PF[0], PF[1], PF[2], PF[3]
                  for p in range(2):
                      S.op('pe', lambda e, p=p: e.matmul(PG[:, p * 128:(p + 1) * 128], lhsT=fm[:, 0 + p, :], rhs=STb[p][:], start=True, stop=False), R=[fm, STb[p]], W=[PG])
                      for hh in range(2):
                          h = 2 * p + hh
                          S.op('pe', lambda e, h=h, hh=hh: e.matmul(PG[:, h * 64:(h + 1) * 64], lhsT=MKm[:, h, :], rhs=v_bf[:, h * 64:(h + 1) * 64], start=False, stop=(hh == 1)), R=[MKm, v_bf], W=[PG])
                  S.op('act', lambda e: e.activation(out=G_f[:], in_=PG[:, 0:256], func=AF.Copy), R=[PG], W=[G_f])
                  for h in range(4):
                      S.op('pe', lambda e, h=h, X=X: e.matmul(PU[:, h * 64:(h + 1) * 64], lhsT=X[:, h, :], rhs=G_f[:, h * 64:(h + 1) * 64], start=True, stop=True), R=[X, G_f], W=[PU])
                  S.op('dve', lambda e: e.tensor_copy(out=U_bf[:], in_=PU[:, 0:256]), R=[PU], W=[U_bf])
                  for p in range(2):
                      S.op('pe', lambda e, p=p: e.matmul(PY[:, p * 128:(p + 1) * 128], lhsT=fm[:, 2 + p, :], rhs=STb[p][:], start=True, stop=False), R=[fm, STb[p]], W=[PY])
                      for hh in range(2):
                          h = 2 * p + hh
                          S.op('pe', lambda e, h=h: e.matmul(PY[:, h * 64:(h + 1) * 64], lhsT=RBm[:, h, :], rhs=U_bf[:, h * 64:(h + 1) * 64], start=False, stop=False), R=[RBm, U_bf], W=[PY])
                          S.op('pe', lambda e, h=h, hh=hh: e.matmul(PY[:, h * 64:(h + 1) * 64], lhsT=RKm[:, h, :], rhs=v_bf[:, h * 64:(h + 1) * 64], start=False, stop=(hh == 1)), R=[RKm, v_bf], W=[PY])
                  S.op('act', lambda e: e.activation(out=o_sb[:], in_=PY[:, 0:256], func=AF.Copy), R=[PY], W=[o_sb])
                  for p in range(2):
                      cs_ = slice(p * 128, (p + 1) * 128)
                      S.op('pe', lambda e, cs_=cs_: e.matmul(PS_[:, cs_], lhsT=bt_b[:, cs_], rhs=U_bf[:, cs_], start=True, stop=False), R=[bt_b, U_bf], W=[PS_])
                      S.op('pe', lambda e, cs_=cs_: e.matmul(PS_[:, cs_], lhsT=kt_b[:, cs_], rhs=v_bf[:, cs_], start=False, stop=True), R=[kt_b, v_bf], W=[PS_])
                      for hh in range(2):
                          pr = slice(hh * 64, (hh + 1) * 64); cc = slice(hh * 64, (hh + 1) * 64); pc_ = slice(p * 128 + hh * 64, p * 128 + (hh + 1) * 64)
                          S.op('dve', lambda e, p=p, pr=pr, cc=cc, pc_=pc_: e.scalar_tensor_tensor(out=ST[p][pr, cc], in0=ST[p][pr, cc], scalar=1.0, in1=PS_[pr, pc_], op0=ALU.mult, op1=ALU.add),
                               R=[ST[p], PS_], W=[ST[p]])
                          S.op('dve', lambda e, p=p, pr=pr, cc=cc: e.tensor_scalar(out=ST[p][pr, cc], in0=ST[p][pr, cc], scalar1=ecl[pr, p:p + 1], scalar2=None, op0=ALU.mult), R=[ST[p], ecl], W=[ST[p]])
                      S.op('act', lambda e, p=p: e.activation(out=STb[p][:], in_=ST[p][:], func=AF.Copy), R=[ST[p]], W=[STb[p]])
                  self._chk(10)
                  self.head_norm(o_sb, cen, sq, st4, 4, 64e-5)
                  S.op('dve', lambda e: e.tensor_tensor(out=cen[:], in0=cen[:], in1=lnb[:], op=ALU.mult), R=[cen, lnb], W=[cen])
                  S.op('dve', lambda e: e.tensor_tensor(out=t1[:], in0=r_sb[:], in1=k2_sb[:], op=ALU.mult), R=[r_sb, k2_sb], W=[t1])
                  S.op('dve', lambda e: e.tensor_tensor(out=t1[:], in0=t1[:], in1=rkb[:], op=ALU.mult), R=[t1, rkb], W=[t1])
                  S.op('dve', lambda e: e.tensor_reduce(out=st4[:], in_=V3(t1[:]), axis=AX.X, op=ALU.add), R=[t1], W=[st4])
                  S.op('dve', lambda e: e.tensor_tensor(out=V3(t1[:]), in0=V3(v_sb[:]), in1=st4[:].unsqueeze(2).broadcast_to([128, 4, 64]), op=ALU.mult), R=[v_sb, st4], W=[t1])
                  S.op('dve', lambda e: e.tensor_tensor(out=cen[:], in0=cen[:], in1=t1[:], op=ALU.add), R=[cen, t1], W=[cen])
                  yt_ = ytok[c % 2]
                  S.op('dve', lambda e, yt_=yt_: e.tensor_tensor(out=yt_[:], in0=cen[:], in1=gate_sb[:], op=ALU.mult), R=[cen, gate_sb], W=[yt_])
                  self.y_store(2, c, yt_, ystage, [])
              S.barrier(); S.emit()
          except _Stop:
            S.barrier(); S.emit()


    def rope_apply(self, A, B_, C, Sg):
        S = self.S
        S.op('dve', lambda e: e.tensor_tensor(out=A[:], in0=A[:], in1=C[:], op=ALU.mult), R=[A, C], W=[A])
        S.op('pool', lambda e: e.tensor_tensor(out=B_[:], in0=B_[:], in1=Sg[:], op=ALU.mult), R=[B_, Sg], W=[B_])
        S.op('dve', lambda e: e.tensor_tensor(out=A[:], in0=A[:], in1=B_[:], op=ALU.add), R=[A, B_], W=[A])

    def dsa_phase(self, l):
        S = self.S
        PF = self.PF
        NBIS = 14
        with ExitStack() as st:
            QT = [self.sb(st, f"dsQT{i}", [128, SEQ], BF16) for i in range(2)]
            kT = self.sb(st, "dskT", [128, SEQ], BF16)
            hm2 = self.load_const(st, 'hm2', F32); hm4 = self.load_const(st, 'hm4', F32)
            vext = self.sb(st, "ds_vext", [128, NT, 65], BF16)
            wsc = self.sb(st, "ds_wsc", [128, NT, 8], F32)
            with ExitStack() as s1:
                qiT = [self.sb(s1, f"dsqiT{i}", [128, SEQ], BF16) for i in range(2)]
                kiT = self.sb(s1, "dskiT", [128, SEQ], BF16)
                with ExitStack() as s2:
                    wm, wo = self.load_w(s2, "wm_ds", l, ['ds_cq', 'ds_k', 'ds_ks', 'ds_ki', 'ds_kis', 'ds_vw'])
                    wup = self.sb(s2, "ds_wup", [128, 4, 256], BF16)
                    for j, src_ in enumerate((self.dsa_wq_up, self.dsa_wqs_up, self.dsa_wqi_up, self.dsa_wqis_up)):
                        self.dma('pool', wup[:, j, :], src_[l], W=[wup])
                    qn = self.sb(s2, "ds_qn", [128, 1], F32)
                    self.dma('sp', qn[:], self.dsa_qnorm[l, :].rearrange("(p o) -> p o", o=1), W=[qn], allow_slow_non_contiguous=True)
                    onesf = self.sb(s2, "ds_ones", [128, 128], F32)
                    S.op('dve', lambda e: e.memset(onesf[:], 1.0), W=[onesf])
                    cqn = self.sb(s2, "ds_cqn", [128, SEQ], BF16)
                    cqf = self.sb(s2, "ds_cqf", [128, 512], F32); cq2 = self.sb(s2, "ds_cq2", [128, 512], F32); rs = self.sb(s2, "ds_rs", [128, 512], F32)
                    for tg in range(8):
                        pf = PF[tg % 2]; pf2 = PF[2 + tg % 2]
                        self.proj_T(wm, wo['ds_cq'], pf, tg)
                        S.op('act', lambda e, pf=pf: e.activation(out=cqf[:], in_=pf[:], func=AF.Copy), R=[pf], W=[cqf])
                        S.op('dve', lambda e: e.tensor_tensor(out=cq2[:], in0=cqf[:], in1=cqf[:], op=ALU.mult), R=[cqf], W=[cq2])
                        S.op('pe', lambda e, pf2=pf2: e.matmul(pf2[:], lhsT=onesf[:], rhs=cq2[:], start=True, stop=True), R=[onesf, cq2], W=[pf2])
                        S.op('dve', lambda e, pf2=pf2: e.tensor_scalar(out=rs[:], in0=pf2[:], scalar1=1.0 / 128, scalar2=1e-5, op0=ALU.mult, op1=ALU.add), R=[pf2], W=[rs])
                        S.op('act', lambda e: e.activation(out=rs[:], in_=rs[:], func=AF.Sqrt), R=[rs], W=[rs])
                        S.op('dve', lambda e: e.reciprocal(out=rs[:], in_=rs[:]), R=[rs], W=[rs])
                        S.op('dve', lambda e, tg=tg: e.scalar_tensor_tensor(out=cqn[:, tg * 512:(tg + 1) * 512], in0=cqf[:], scalar=qn[:, 0:1], in1=rs[:], op0=ALU.mult, op1=ALU.mult),
                             R=[cqf, qn, rs], W=[cqn])
                    S.op('pool', lambda e: e.memset(vext[:, :, 64:65], 1.0), W=[vext])
                    for i in range(NT):
                        pf = PF[i % 2]
                        self.proj_tok(wm, wo['ds_vw'], 72, pf[:, 0:72], pf, i)
                        S.op('act', lambda e, pf=pf, i=i: e.activation(out=vext[:, i, 0:64], in_=pf[:, 0:64], func=AF.Copy), R=[pf], W=[vext])
                        S.op('dve', lambda e, pf=pf, i=i: e.tensor_scalar(out=wsc[:, i, :], in0=pf[:, 64:72], scalar1=1.0 / 16, scalar2=None, op0=ALU.mult), R=[pf], W=[wsc])
                    tmpA = self.sb(s2, "ds_tmpA", [128, SEQ], BF16)

                    def up_proj(j, cols, dst):
                        for tg in range(8):
                            pf = PF[tg % 2]
                            S.op('pe', lambda e, pf=pf, tg=tg: e.matmul(pf[:], lhsT=wup[:, j, cols], rhs=cqn[:, tg * 512:(tg + 1) * 512], start=True, stop=True), R=[wup, cqn], W=[pf])
                            S.op('act', lambda e, pf=pf, tg=tg: e.activation(out=dst[:, tg * 512:(tg + 1) * 512], in_=pf[:], func=AF.Copy), R=[pf], W=[dst])

                    def in_proj(name, dst):
                        for tg in range(8):
                            pf = PF[2 + tg % 2]
                            self.proj_T(wm, wo[name], pf, tg)
                            S.op('dve', lambda e, pf=pf, tg=tg: e.tensor_copy(out=dst[:, tg * 512:(tg + 1) * 512], in_=pf[:]), R=[pf], W=[dst])
                    with ExitStack() as s3:
                        C, Sg = self.rope_tables(s3, 'rope_dq', 'rdq')
                        for pr_ in range(2):
                            cols = slice(pr_ * 128, (pr_ + 1) * 128)
                            up_proj(0, cols, QT[pr_]); up_proj(1, cols, tmpA)
                            self.rope_apply(QT[pr_], tmpA, C, Sg)
                        in_proj('ds_k', kT); in_proj('ds_ks', tmpA)
                        self.rope_apply(kT, tmpA, C, Sg)
                        S.barrier(); S.emit()
                    with ExitStack() as s3:
                        C, Sg = self.rope_tables(s3, 'rope_di', 'rdi')
                        for t2 in range(2):
                            cols = slice(t2 * 128, (t2 + 1) * 128)
                            up_proj(2, cols, qiT[t2]); up_proj(3, cols, tmpA)
                            self.rope_apply(qiT[t2], tmpA, C, Sg)
                        in_proj('ds_ki', kiT); in_proj('ds_kis', tmpA)
                        self.rope_apply(kiT, tmpA, C, Sg)
                        S.barrier(); S.emit()
                self.issue_weight_cast(l)
                with ExitStack() as s2:
                    score = self.sb(s2, "ds_score", [128, SEQ], F32)
                    mask = self.sb(s2, "ds_mask", [128, SEQ], BF16)
                    junk = self.sb(s2, "ds_junk", [128, SEQ], BF16)
                    relb = [self.sb(s2, f"ds_rel{i}", [128, 512], F32) for i in range(2)]
                    negm = self.load_const(s2, 'negmask', F32)
                    mT = [self.sb(s2, f"ds_mT{i}", [128, NT, 128], BF16) for i in range(2)]
                    sc = {n: self.sb(s2, "ds_" + n, [128, 1], F32) for n in ('lo', 'hi', 'mid', 'cnt', 'ge', 'd')}
                    cntr = 0
                    qm = [self.sb(s2, f"ds_qm{i}", [128, 8, 128], BF16) for i in range(2)]
                    for tb in range(NT):
                        Sc = (tb + 1) * 128
                        tsl = slice(tb * 128, (tb + 1) * 128)
                        qm_ = qm[tb % 2]
                        for ih in range(8):
                            S.op('pool', lambda e, ih=ih, qm_=qm_, tsl=tsl: e.tensor_scalar(out=qm_[:, ih, :], in0=qiT[ih // 4][:, tsl], scalar1=hm4[:, ih % 4:ih % 4 + 1], scalar2=None, op0=ALU.mult),
                                 R=[qiT[ih // 4], hm4], W=[qm_])
                        for sg in range((Sc + 511) // 512):
                            w = min(512, Sc - sg * 512)
                            ssl = slice(sg * 512, sg * 512 + w)
                            for ih in range(8):
                                t2, j = ih // 4, ih % 4
                                pf = PF[cntr % 4]; rl = relb[cntr % 2]; cntr += 1
                                S.op('pe', lambda e, pf=pf, ih=ih, qm_=qm_, ssl=ssl, w=w: e.matmul(pf[:, 0:w], lhsT=qm_[:, ih, :], rhs=kiT[:, ssl], start=True, stop=True),
                                     R=[qm_, kiT], W=[pf])
                                S.op('act', lambda e, pf=pf, rl=rl, w=w: e.activation(out=rl[:, 0:w], in_=pf[:, 0:w], func=AF.Relu), R=[pf], W=[rl])
                                if ih == 0:
                                    S.op('dve', lambda e, rl=rl, w=w, ssl=ssl, tb=tb, ih=ih: e.tensor_scalar(out=score[:, ssl], in0=rl[:, 0:w], scalar1=wsc[:, tb, ih:ih + 1], scalar2=None, op0=ALU.mult),
                                         R=[rl, wsc], W=[score])
                                else:
                                    S.op('dve', lambda e, rl=rl, w=w, ssl=ssl, tb=tb, ih=ih: e.scalar_tensor_tensor(out=score[:, ssl], in0=rl[:, 0:w], scalar=wsc[:, tb, ih:ih + 1], in1=score[:, ssl],
                                                                                                              op0=ALU.mult, op1=ALU.add), R=[rl, wsc, score], W=[score])
                        S.op('dve', lambda e, tsl=tsl: e.tensor_tensor(out=score[:, tsl], in0=score[:, tsl], in1=negm[:], op=ALU.add), R=[score, negm], W=[score])
                        if tb >= 2:
                            S.op('dve', lambda e, Sc=Sc: e.tensor_reduce(out=sc['hi'][:], in_=score[:, 0:Sc], axis=AX.X, op=ALU.max), R=[score], W=[sc['hi']])
                            S.op('dve', lambda e: e.tensor_reduce(out=sc['lo'][:], in_=score[:, 0:256], axis=AX.X, op=ALU.min), R=[score], W=[sc['lo']])
                            S.op('dve', lambda e: e.tensor_tensor(out=sc['mid'][:], in0=sc['lo'][:], in1=sc['hi'][:], op=ALU.add), R=[sc['lo'], sc['hi']], W=[sc['mid']])
                            S.op('dve', lambda e: e.tensor_scalar(out=sc['mid'][:], in0=sc['mid'][:], scalar1=0.5, scalar2=None, op0=ALU.mult), R=[sc['mid']], W=[sc['mid']])
                            S.op('dve', lambda e: e.tensor_tensor(out=sc['d'][:], in0=sc['hi'][:], in1=sc['lo'][:], op=ALU.subtract), R=[sc['lo'], sc['hi']], W=[sc['d']])
                            S.op('dve', lambda e: e.tensor_scalar(out=sc['d'][:], in0=sc['d'][:], scalar1=0.25, scalar2=None, op0=ALU.mult), R=[sc['d']], W=[sc['d']])
                            for it in range(NBIS):
                                S.op('dve', lambda e, Sc=Sc: e.tensor_scalar(out=junk[:, 0:Sc], in0=score[:, 0:Sc], scalar1=sc['mid'][:, 0:1], scalar2=None, op0=ALU.is_ge, op1=ALU.add,
                                                                            accum_out=sc['cnt'][:]), R=[score, sc['mid']], W=[junk, sc['cnt']])
                                S.op('dve', lambda e: e.tensor_scalar(out=sc['ge'][:], in0=sc['cnt'][:], scalar1=255.5, scalar2=2.0, op0=ALU.is_ge, op1=ALU.mult), R=[sc['cnt']], W=[sc['ge']])
                                S.op('dve', lambda e: e.scalar_tensor_tensor(out=sc['ge'][:], in0=sc['ge'][:], scalar=-1.0, in1=sc['d'][:], op0=ALU.add, op1=ALU.mult), R=[sc['ge'], sc['d']], W=[sc['ge']])
                                S.op('dve', lambda e: e.tensor_tensor(out=sc['mid'][:], in0=sc['mid'][:], in1=sc['ge'][:], op=ALU.add), R=[sc['mid'], sc['ge']], W=[sc['mid']])
                                S.op('dve', lambda e: e.tensor_scalar(out=sc['d'][:], in0=sc['d'][:], scalar1=0.5, scalar2=None, op0=ALU.mult), R=[sc['d']], W=[sc['d']])
                            S.op('dve', lambda e: e.scalar_tensor_tensor(out=sc['lo'][:], in0=sc['d'][:], scalar=-2.0, in1=sc['mid'][:], op0=ALU.mult, op1=ALU.add), R=[sc['d'], sc['mid']], W=[sc['lo']])
                        else:
                            S.op('dve', lambda e: e.memset(sc['lo'][:], -1e29), W=[sc['lo']])
                        S.op('dve', lambda e, Sc=Sc: e.tensor_scalar(out=mask[:, 0:Sc], in0=score[:, 0:Sc], scalar1=sc['lo'][:, 0:1], scalar2=None, op0=ALU.is_ge), R=[score, sc['lo']], W=[mask])
                        mt = mT[tb % 2]
                        for s0 in range(0, tb + 1, 8):
                            nb_ = min(8, tb + 1 - s0)
                            pb = self.PB[(s0 // 8) % 2]
                            for q in range(nb_):
                                S.op('pe', lambda e, pb=pb, q=q, s0=s0: e.transpose(out=pb[:, q * 128:(q + 1) * 128], in_=mask[:, (s0 + q) * 128:(s0 + q + 1) * 128], identity=self.ident[:]),
                                     R=[mask, self.ident], W=[pb])
                            S.op('act', lambda e, pb=pb, mt=mt, s0=s0, nb_=nb_: e.activation(out=mt[:, s0:s0 + nb_, :].rearrange("p b t -> p (b t)"), in_=pb[:, 0:nb_ * 128], func=AF.Copy), R=[pb], W=[mt])
                        self.dma('sp', self.maskT_d[tb, :, 0:tb + 1, :], mt[:, 0:tb + 1, :], R=[mt], W=[TB()])
                    S.barrier(); S.emit()
            with ExitStack() as s2:
                onb = self.bcast_row(s2, "ds_onb", self.dsa_onorm[l, :], 256)
                o_all = self.sb(s2, "ds_oall", [128, NT, 256], BF16)
                mT = [self.sb(s2, f"ds_mT2{i}", [128, NT, 128], BF16) for i in range(2)]
                ebuf = [self.sb(s2, f"ds_e{i}", [128, 4, 128], BF16) for i in range(2)]
                pbuf = [self.sb(s2, f"ds_p{i}", [128, 4, 128], BF16) for i in range(2)]
                zer = self.sb(s2, "ds_zero", [128, 260], BF16)
                S.op('pool', lambda e: e.memset(zer[:], 0.0), W=[zer])
                osb = self.sb(s2, "ds_osb", [128, 4, 65], F32); rden = self.sb(s2, "ds_rden", [128, 4], F32)
                cnt = 0
                QM = [self.sb(s2, f"ds_QM{i}", [128, 4, 128], BF16) for i in range(2)]
                for tb in range(NT):
                    tsl = slice(tb * 128, (tb + 1) * 128)
                    mt = mT[tb % 2]
                    QM_ = QM[tb % 2]
                    for h in range(4):
                        S.op('pool', lambda e, h=h, QM_=QM_, tsl=tsl: e.tensor_scalar(out=QM_[:, h, :], in0=QT[h // 2][:, tsl], scalar1=hm2[:, h % 2:h % 2 + 1], scalar2=None, op0=ALU.mult),
                             R=[QT[h // 2], hm2], W=[QM_])
                    self.dma('act', mt[:, 0:tb + 1, :], self.maskT_d[tb, :, 0:tb + 1, :], W=[mt])
                    po = PF[4 + tb % 2]
                    S.op('pe', lambda e, po=po: e.matmul(po[:, 0:260], lhsT=zer[:, 0:128], rhs=zer[:, 0:260], start=True, stop=False), R=[zer], W=[po])
                    for sb_ in range(tb + 1):
                        b2 = cnt % 2; cnt += 1
                        ssl = slice(sb_ * 128, (sb_ + 1) * 128)
                        pl = PF[b2 * 2]
                        S.op('pe', lambda e, pl=pl, ssl=ssl, QM_=QM_: e.matmul(pl[:], lhsT=kT[:, ssl], rhs=QM_[:].rearrange("p h t -> p (h t)"), start=True, stop=True),
                             R=[kT, QM_], W=[pl])
                        S.op('act', lambda e, pl=pl, b2=b2: e.activation(out=ebuf[b2][:].rearrange("p h t -> p (h t)"), in_=pl[:], func=AF.Exp, scale=0.125), R=[pl], W=[ebuf[b2]])
                        S.op('dve', lambda e, b2=b2, mt=mt, sb_=sb_: e.tensor_tensor(out=pbuf[b2][:], in0=ebuf[b2][:], in1=mt[:, sb_, :].unsqueeze(1).broadcast_to([128, 4, 128]), op=ALU.mult),
                             R=[ebuf[b2], mt], W=[pbuf[b2]])
                        for h in range(4):
                            S.op('pe', lambda e, h=h, po=po, b2=b2, sb_=sb_, tb=tb: e.matmul(po[:, h * 65:(h + 1) * 65], lhsT=pbuf[b2][:, h, :], rhs=vext[:, sb_, :], start=False, stop=(sb_ == tb and h == 3)),
                                 R=[pbuf[b2], vext], W=[po])
                    S.op('act', lambda e, po=po: e.activation(out=osb[:].rearrange("p h d -> p (h d)"), in_=po[:, 0:260], func=AF.Copy), R=[po], W=[osb])
                    S.op('dve', lambda e: e.reciprocal(out=rden[:], in_=osb[:, :, 64]), R=[osb], W=[rden])
                    S.op('dve', lambda e, tb=tb: e.tensor_tensor(out=o_all[:, tb, :].rearrange("p (h d) -> p h d", d=64), in0=osb[:, :, 0:64], in1=rden[:].unsqueeze(2).broadcast_to([128, 4, 64]), op=ALU.mult),
                         R=[osb, rden], W=[o_all])
                S.barrier(); S.emit()
                self.rms_finalize(s2, o_all, onb, 4, "dsf")
                S.barrier(); S.emit()

    def head_norm(self, o_sb, cen, sq, st4, nh, eps):
        S = self.S
        v3 = lambda b: b[:, 0:nh * 64].rearrange("p (h d) -> p h d", d=64)
        S.op('dve', lambda e: e.tensor_reduce(out=st4[:, 0:nh], in_=v3(o_sb), axis=AX.X, op=ALU.add), R=[o_sb], W=[st4])
        S.op('dve', lambda e: e.tensor_scalar(out=st4[:, 0:nh], in0=st4[:, 0:nh], scalar1=1.0 / 64, scalar2=None, op0=ALU.mult), R=[st4], W=[st4])
        S.op('dve', lambda e: e.tensor_tensor(out=v3(cen), in0=v3(o_sb), in1=st4[:, 0:nh].unsqueeze(2).broadcast_to([128, nh, 64]), op=ALU.subtract), R=[o_sb, st4], W=[cen])
        S.op('dve', lambda e: e.tensor_tensor(out=v3(sq), in0=v3(cen), in1=v3(cen), op=ALU.mult), R=[cen], W=[sq])
        S.op('dve', lambda e: e.tensor_reduce(out=st4[:, 0:nh], in_=v3(sq), axis=AX.X, op=ALU.add), R=[sq], W=[st4])
        S.op('dve', lambda e: e.tensor_scalar(out=st4[:, 0:nh], in0=st4[:, 0:nh], scalar1=1.0 / 64, scalar2=eps, op0=ALU.mult, op1=ALU.add), R=[st4], W=[st4])
        S.op('act', lambda e: e.activation(out=st4[:, 0:nh], in_=st4[:, 0:nh], func=AF.Sqrt), R=[st4], W=[st4])
        S.op('dve', lambda e: e.reciprocal(out=st4[:, 0:nh], in_=st4[:, 0:nh]), R=[st4], W=[st4])
        S.op('dve', lambda e: e.tensor_tensor(out=v3(cen), in0=v3(cen), in1=st4[:, 0:nh].unsqueeze(2).broadcast_to([128, nh, 64]), op=ALU.mult), R=[cen, st4], W=[cen])

    def wout_phase(self, l):
        S = self.S
        src = self.x if l == 0 else self.xres
        with ExitStack() as st:
            YT = self.sb(st, "YT", [128, 8, SEQ], BF16)
            for j in range(8):
                self.dma('sp' if j % 2 == 0 else 'act', YT[:, j, :], self.yT_d[j], W=[YT])
            wo_sb = self.sb(st, "wo_sb", [128, 8, D], BF16)
            for f in range(8):
                self.dma('pool', wo_sb[:, f, :], self.w_out[l, f * 128:(f + 1) * 128, :], W=[wo_sb])
            xt = [self.sb(st, f"wx{i}", [128, D], F32) for i in range(2)]
            tmp = self.sb(st, "wtmp", [128, D], F32)
            for i in range(NT):
                x_ = xt[i % 2]
                self.dma('sp' if i % 2 == 0 else 'act', x_[:], src[i * 128:(i + 1) * 128, :], W=[x_])
                for hf in range(2):
                    pf = self.PF[(i % 2) * 2 + hf]
                    for f in range(8):
                        S.op('pe', lambda e, f=f, pf=pf, i=i, hf=hf: e.matmul(pf[:], lhsT=YT[:, f, i * 128:(i + 1) * 128], rhs=wo_sb[:, f, hf * 512:(hf + 1) * 512], start=(f == 0), stop=(f == 7)),
                             R=[YT, wo_sb], W=[pf])
                    S.op('dve', lambda e, pf=pf, hf=hf: e.tensor_tensor(out=tmp[:, hf * 512:(hf + 1) * 512], in0=pf[:], in1=self.gb[0][:, hf * 512:(hf + 1) * 512], op=ALU.mult),
                         R=[pf, self.gb[0]], W=[tmp])
                S.op('pool', lambda e, x_=x_: e.tensor_tensor(out=x_[:], in0=x_[:], in1=tmp[:], op=ALU.add), R=[x_, tmp], W=[x_])
                self.dma('sp', self.xres[i * 128:(i + 1) * 128, :], x_[:], R=[x_], W=[TB()])
            S.barrier(); S.emit()

    def moe_phase(self, l):
        S = self.S; PF = self.PF; nc = self.nc
        with ExitStack() as st:
            off_all = self.sb(st, "mo_off", [128, NT, 4], I32)
            gsel_all = self.sb(st, "mo_gsel", [128, NT, 4], F32)
            widx = self.sb(st, "mo_widx", [128, NBLK, 8], I32)
            OH = self.sb(st, "mo_OH", [32, NBLK], F32)
            ones_bf = self.sb(st, "mo_ones", [128, 512], BF16)
            S.op('dve', lambda e: e.memset(ones_bf[:], 1.0), W=[ones_bf])
            with ExitStack() as s1:
                self.hT = self.sb(s1, "hT2", [128, 8, SEQ + 1], BF16)
                self.norm_phase(l, 1)
                rw = self.sb(s1, "mo_rw", [128, 8, 32], BF16)
                self.dma('pool', rw[:], self.router_w[l].rearrange("(k p) e -> p k e", p=128), W=[rw])
                rbb = self.bcast_row(s1, "mo_rbb", self.router_b[l, :], 32)
                M_bf = self.sb(s1, "mo_Mbf", [128, NT, 32], BF16); M32 = self.sb(s1, "mo_M32", [128, NT, 32], F32)
                G_all = self.sb(s1, "mo_G", [128, NT, 32], F32)
                lg = self.sb(s1, "mo_lg", [128, 32], F32); ex = self.sb(s1, "mo_ex", [128, 32], F32); junk32 = self.sb(s1, "mo_junk", [128, 32], F32)
                top8 = self.sb(s1, "mo_top8", [128, 8], F32); sc1 = self.sb(s1, "mo_sc1", [128, 2], F32)
                for i in range(NT):
                    pf = PF[i % 2]
                    for k in range(8):
                        S.op('pe', lambda e, k=k, pf=pf, i=i: e.matmul(pf[:, 0:32], lhsT=self.hT[:, k, 1 + i * 128: 1 + (i + 1) * 128], rhs=rw[:, k, :], start=(k == 0), stop=(k == 7)),
                             R=[self.hT, rw], W=[pf])
                    S.op('dve', lambda e, pf=pf: e.tensor_tensor(out=lg[:], in0=pf[:, 0:32], in1=rbb[:], op=ALU.add), R=[pf, rbb], W=[lg])
                    S.op('dve', lambda e: e.max(out=top8[:], in_=lg[:]), R=[lg], W=[top8])
                    S.op('dve', lambda e, i=i: e.tensor_scalar(out=M32[:, i, :], in0=lg[:], scalar1=top8[:, 3:4], scalar2=None, op0=ALU.is_ge), R=[lg, top8], W=[M32])
                    S.op('pool', lambda e, i=i: e.tensor_copy(out=M_bf[:, i, :], in_=M32[:, i, :]), R=[M32], W=[M_bf])
                    S.op('dve', lambda e: e.tensor_scalar(out=sc1[:, 0:1], in0=top8[:, 0:1], scalar1=-1.0, scalar2=None, op0=ALU.mult), R=[top8], W=[sc1])
                    S.op('act', lambda e: e.activation(out=ex[:], in_=lg[:], func=AF.Exp, bias=sc1[:, 0:1]), R=[lg, sc1], W=[ex])
                    S.op('dve', lambda e, i=i: e.scalar_tensor_tensor(out=ex[:], in0=ex[:], scalar=1.0, in1=M32[:, i, :], op0=ALU.mult, op1=ALU.mult, accum_out=sc1[:, 1:2]),
                         R=[ex, M32], W=[ex, sc1])
                    S.op('dve', lambda e: e.reciprocal(out=sc1[:, 1:2], in_=sc1[:, 1:2]), R=[sc1], W=[sc1])
                    S.op('dve', lambda e, i=i: e.tensor_scalar(out=G_all[:, i, :], in0=ex[:], scalar1=sc1[:, 1:2], scalar2=None, op0=ALU.mult), R=[ex, sc1], W=[G_all])
                pc = PF[2]
                for i in range(NT):
                    S.op('pe', lambda e, i=i: e.matmul(pc[0:1, 0:32], lhsT=ones_bf[:, 0:1], rhs=M_bf[:, i, :], start=(i == 0), stop=(i == NT - 1)), R=[ones_bf, M_bf], W=[pc])
                row = lambda n, w=32, d=F32: self.sb(s1, "mo_" + n, [1, w], d)
                cnt = row('cnt'); nbf = row('nbf'); nbi = row('nbi', 32, I32); endr = row('end'); baser = row('base'); onesr = row('onesr')
                iota = self.load_const(s1, 'iota_blk', F32); kp = self.load_const(s1, 'kp', F32)
                cmp3 = self.sb(s1, "mo_cmp3", [1, NBLK, 32], F32); ebf = row('ebf', NBLK); chgf = row('chgf', NBLK)
                S.op('dve', lambda e: e.memset(onesr[:], 1.0), W=[onesr])
                S.op('dve', lambda e: e.tensor_scalar(out=cnt[:], in0=pc[0:1, 0:32], scalar1=float(MB - 1), scalar2=1.0 / MB, op0=ALU.add, op1=ALU.mult), R=[pc], W=[cnt])
                S.op('dve', lambda e: e.tensor_scalar(out=cnt[:], in0=cnt[:], scalar1=-0.5 + 0.5 / MB, scalar2=None, op0=ALU.add), R=[cnt], W=[cnt])
                S.op('dve', lambda e: e.tensor_copy(out=nbi[:], in_=cnt[:]), R=[cnt], W=[nbi])
                S.op('dve', lambda e: e.tensor_copy(out=nbf[:], in_=nbi[:]), R=[nbi], W=[nbf])
                S.op('dve', lambda e: e.tensor_tensor_scan(out=endr[:], data0=onesr[:], data1=nbf[:], initial=0.0, op0=ALU.mult, op1=ALU.add), R=[onesr, nbf], W=[endr])
                S.op('dve', lambda e: e.tensor_tensor(out=baser[:], in0=endr[:], in1=nbf[:], op=ALU.subtract), R=[endr, nbf], W=[baser])
                S.op('dve', lambda e: e.tensor_scalar(out=baser[:], in0=baser[:], scalar1=float(MB), scalar2=None, op0=ALU.mult), R=[baser], W=[baser])
                basebc = self.sb(s1, "mo_basebc", [128, 32], F32)
                onescol = self.sb(s1, "mo_ones1", [1, 128], F32)
                S.op('dve', lambda e: e.memset(onescol[:], 1.0), W=[onescol])
                S.op('pe', lambda e: e.matmul(PF[3][:, 0:32], lhsT=onescol[0:1, :], rhs=baser[0:1, :], start=True, stop=True), R=[onescol, baser], W=[PF[3]])
                S.op('act', lambda e: e.activation(out=basebc[:], in_=PF[3][:, 0:32], func=AF.Copy), R=[PF[3]], W=[basebc])
                S.op('dve', lambda e: e.tensor_tensor(out=cmp3[:], in0=endr[:].unsqueeze(1).broadcast_to([1, NBLK, 32]), in1=iota[0:1, :].unsqueeze(2).broadcast_to([1, NBLK, 32]), op=ALU.is_le),
                     R=[endr, iota], W=[cmp3])
                S.op('dve', lambda e: e.tensor_reduce(out=ebf[:], in_=cmp3[:], axis=AX.X, op=ALU.add), R=[cmp3], W=[ebf])
                S.op('dve', lambda e: e.tensor_scalar(out=ebf[:], in0=ebf[:], scalar1=31.0, scalar2=None, op0=ALU.min), R=[ebf], W=[ebf])
                needf = row('needf', NBLK); ebrow = row('ebrow', NBLK)
                S.op('dve', lambda e: e.memset(needf[:], 1.0), W=[needf])
                S.op('dve', lambda e: e.tensor_tensor(out=needf[0:1, 2:NBLK], in0=ebf[0:1, 2:NBLK], in1=ebf[0:1, 0:NBLK - 2], op=ALU.not_equal), R=[ebf, needf], W=[needf])
                S.op('dve', lambda e: e.tensor_scalar(out=needf[:], in0=needf[:], scalar1=-1.0e6, scalar2=1.0e6, op0=ALU.mult, op1=ALU.add), R=[needf], W=[needf])
                S.op('dve', lambda e: e.tensor_scalar(out=ebrow[:], in0=ebf[:], scalar1=float(l * 32), scalar2=1024.0, op0=ALU.add, op1=ALU.mult), R=[ebf], W=[ebrow])
                S.op('dve', lambda e: e.tensor_tensor(out=ebrow[:], in0=ebrow[:], in1=needf[:], op=ALU.add), R=[ebrow, needf], W=[ebrow])
                ebbc = self.sb(s1, "mo_ebbc", [128, NBLK], F32); wf = self.sb(s1, "mo_wf", [128, NBLK, 8], F32)
                S.op('pe', lambda e: e.matmul(PF[3][:, 0:NBLK], lhsT=onescol[0:1, :], rhs=ebrow[0:1, :], start=True, stop=True), R=[onescol, ebrow], W=[PF[3]])
                S.op('act', lambda e: e.activation(out=ebbc[:], in_=PF[3][:, 0:NBLK], func=AF.Copy), R=[PF[3]], W=[ebbc])
                S.op('dve', lambda e: e.tensor_tensor(out=wf[:], in0=ebbc[:].unsqueeze(2).broadcast_to([128, NBLK, 8]), in1=kp[:].unsqueeze(1).broadcast_to([128, NBLK, 8]), op=ALU.add),
                     R=[ebbc, kp], W=[wf])
                S.op('dve', lambda e: e.tensor_copy(out=widx[:], in_=wf[:]), R=[wf], W=[widx])
                pcol = self.load_const(s1, 'pcol', F32)
                S.op('pe', lambda e: e.matmul(PF[3][0:32, 0:NBLK], lhsT=onescol[0:1, 0:32], rhs=ebf[0:1, :], start=True, stop=True), R=[onescol, ebf], W=[PF[3]])
                S.op('dve', lambda e: e.tensor_scalar(out=OH[:], in0=PF[3][0:32, 0:NBLK], scalar1=pcol[0:32, 0:1], scalar2=None, op0=ALU.is_equal), R=[PF[3], pcol], W=[OH])
                zt = self.sb(s1, "mo_zt", [128, NSLOT * 2 // 128], I32)
                S.op('dve', lambda e: e.memset(zt[:], 0), W=[zt])
                tokz = TB()
                self.dma('sp', self.tokidx_d.rearrange("(p b) o -> p (b o)", p=128), zt[:], R=[zt], W=[tokz])
                tidx = self.sb(s1, "mo_tidx", [128, NT, 2], I32)
                S.op('pool', lambda e: e.iota(tidx[:], pattern=[[128, NT], [0, 2]], base=0, channel_multiplier=1), W=[tidx])
                tri = self.load_const(s1, 'tri_lt', BF16)
                a1 = self.sb(s1, "mo_a1", [128, 32], F32); A8 = self.sb(s1, "mo_A8", [128, 8], F32); offf = self.sb(s1, "mo_offf", [128, 4], F32)
                for i in range(NT):
                    pp = PF[i % 2]
                    S.op('pe', lambda e, i=i, pp=pp: e.matmul(pp[:, 0:32], lhsT=tri[:], rhs=M_bf[:, i, :], start=True, stop=(i == 0)), R=[tri, M_bf], W=[pp])
                    for j in range(i):
                        S.op('pe', lambda e, i=i, j=j, pp=pp: e.matmul(pp[:, 0:32], lhsT=ones_bf[:, 0:128], rhs=M_bf[:, j, :], start=False, stop=(j == i - 1)), R=[ones_bf, M_bf], W=[pp])
                    S.op('dve', lambda e, pp=pp: e.tensor_tensor(out=a1[:], in0=pp[:, 0:32], in1=basebc[:], op=ALU.add), R=[pp, basebc], W=[a1])
                    S.op('dve', lambda e, i=i: e.scalar_tensor_tensor(out=a1[:], in0=a1[:], scalar=1.0, in1=M32[:, i, :], op0=ALU.add, op1=ALU.mult), R=[a1, M32], W=[a1])
                    S.op('dve', lambda e: e.max(out=A8[:], in_=a1[:]), R=[a1], W=[A8])
                    S.op('dve', lambda e: e.tensor_scalar(out=offf[:], in0=A8[:, 0:4], scalar1=-1.0, scalar2=None, op0=ALU.add), R=[A8], W=[offf])
                    S.op('dve', lambda e, i=i: e.tensor_copy(out=off_all[:, i, :], in_=offf[:]), R=[offf], W=[off_all])
                    for j in range(4):
                        S.op('dve', lambda e, i=i, j=j: e.scalar_tensor_tensor(out=junk32[:], in0=a1[:], scalar=A8[:, j:j + 1], in1=G_all[:, i, :], op0=ALU.is_equal, op1=ALU.mult,
                                                                              accum_out=gsel_all[:, i, j:j + 1]), R=[a1, A8, G_all], W=[junk32, gsel_all])
                    for j in range(4):
                        S.op('pool', lambda e, i=i, j=j: e.indirect_dma_start(out=self.tokidx_d[:, :], out_offset=bass.IndirectOffsetOnAxis(ap=off_all[:, i, j:j + 1], axis=0),
                                                                               in_=tidx[:, i, :], in_offset=None),
                             R=[off_all, tidx, tokz], W=[TB()], dma=True)
                S.barrier(); S.emit()
            if self.flags.get('moe_stop') == 2:
                return
            self.issue_weight_cast(l)
            w1v = self.w1b_d; w2v = self.w2b_d; convtb = self.conv_tb[l]
            b1v = self.moe_b1.rearrange("l e f -> (l e) f"); b2v = self.moe_b2.rearrange("l e d -> (l e) d")
            IO = bass.IndirectOffsetOnAxis
            with ExitStack() as s1:
                W1 = [self.sb(s1, f"mo_W1{i}", [128, 8, 2 * D], BF16) for i in range(2)]; W2 = [self.sb(s1, f"mo_W2{i}", [128, 8, D], BF16) for i in range(2)]
                b1all = self.sb(s1, "mo_b1all", [32, 2 * D], BF16); b2all = self.sb(s1, "mo_b2all", [32, D], BF16)
                self.dma('pool', b1all[:], self.moe_b1[l], W=[b1all]); self.dma('pool', b2all[:], self.moe_b2[l], W=[b2all])
                sel = [self.sb(s1, f"mo_sel{i}", [32, MB], BF16) for i in range(2)]
                bcdone = self.sb(s1, "mo_bcd", [1, 1], F32)

                def setbc(e):
                    e.reg_mov(self.reg_bc, 64 * 1024 - 1)
                    return e.memset(bcdone[:], 0.0)
                S.op('pool', setbc, W=[bcdone])
                wts = [TB(), TB()]
                idx = [self.sb(s1, f"mo_idx{i}", [128, 4, 2], I32) for i in range(2)]
                X = [self.sb(s1, f"mo_X{i}", [128, D], BF16) for i in range(2)]
                XT = [self.sb(s1, f"mo_XT{i}", [128, 8, MB], BF16) for i in range(2)]
                AT = self.sb(s1, "mo_AT", [128, 8, MB], BF16)
                g_ = self.sb(s1, "mo_g", [128, 512], F32); sg_ = self.sb(s1, "mo_sg", [128, 512], F32); ln_ = self.sb(s1, "mo_ln", [128, 512], F32)
                yrow = [self.sb(s1, f"mo_y{i}", [128, D], F32) for i in range(2)]
                xc = 0

                def issue_weights(b):
                    b2_ = b % 2
                    W1_, W2_, wt_ = W1[b2_], W2[b2_], wts[b2_]
                    for k in range(8):
                        S.op('pool', lambda e, k=k, b=b, W1_=W1_: e.indirect_dma_start(out=W1_[:, k, :], out_offset=None, in_=w1v[:, :], in_offset=IO(ap=widx[:, b, k:k + 1], axis=0),
                                                                                  bounds_check=self.reg_bc, oob_is_err=False), R=[widx, bcdone, convtb], W=[wt_], dma=True)
                        S.op('pool', lambda e, k=k, b=b, W2_=W2_: e.indirect_dma_start(out=W2_[:, k, :], out_offset=None, in_=w2v[:, :], in_offset=IO(ap=widx[:, b, k:k + 1], axis=0),
                                                                                  bounds_check=self.reg_bc, oob_is_err=False), R=[widx, bcdone, convtb], W=[wt_], dma=True)
                X4 = [self.sb(s1, f"mo_X4{i}", [128, D], BF16) for i in range(4)]

                def issue_gathers(b):
                    b2_ = b % 2
                    self.dma('sp', idx[b2_][:], self.tokidx_d[b * MB:(b + 1) * MB, :].rearrange("(q p) o -> p q o", p=128), W=[idx[b2_]])
                    for q in range(MB // 128):
                        x_ = X4[q]
                        S.op('pool', lambda e, b2_=b2_, q=q, x_=x_: e.indirect_dma_start(out=x_[:], out_offset=None, in_=self.hrow_d[:, :], in_offset=IO(ap=idx[b2_][:, q, 0:1], axis=0)),
                             R=[idx[b2_]], W=[x_], dma=True)

                def issue_transposes(b):
                    xt_ = XT[b % 2]
                    for q in range(MB // 128):
                        x_ = X4[q]; pb = self.PB[q % 2]
                        for k in range(8):
                            S.op('pe', lambda e, k=k, pb=pb, x_=x_: e.transpose(out=pb[:, k * 128:(k + 1) * 128], in_=x_[:, k * 128:(k + 1) * 128], identity=self.ident[:]), R=[x_, self.ident], W=[pb])
                        S.op('act', lambda e, pb=pb, xt_=xt_, q=q: e.activation(out=xt_[:, :, q * 128:(q + 1) * 128], in_=pb[:].rearrange("p (k t) -> p k t", k=8), func=AF.Copy), R=[pb], W=[xt_])
                issue_weights(0)
                issue_gathers(0)
                issue_transposes(0)
                for b in range(NBLK):
                    b2_ = b % 2
                    W1_, W2_, wt_ = W1[b2_], W2[b2_], wts[b2_]
                    sel_ = sel[b2_]
                    xt_ = XT[b2_]
                    S.op('dve', lambda e, b=b, sel_=sel_: e.tensor_copy(out=sel_[:], in_=OH[:, b:b + 1].broadcast_to([32, MB])), R=[OH], W=[sel_])
                    if b + 1 < NBLK:
                        issue_weights(b + 1)
                        issue_gathers(b + 1)
                    for c in range(8):
                        pg, pl = PF[(c % 2) * 2], PF[(c % 2) * 2 + 1]
                        for (pf, cbase) in ((pg, 0), (pl, D)):
                            for k in range(8):
                                S.op('pe', lambda e, k=k, pf=pf, c=c, cbase=cbase, W1_=W1_, xt_=xt_: e.matmul(pf[:], lhsT=W1_[:, k, cbase + c * 128: cbase + (c + 1) * 128], rhs=xt_[:, k, :], start=(k == 0), stop=False),
                                     R=[wt_, xt_], W=[pf])
                            S.op('pe', lambda e, pf=pf, c=c, cbase=cbase, sel_=sel_: e.matmul(pf[:], lhsT=b1all[0:32, cbase + c * 128: cbase + (c + 1) * 128], rhs=sel_[0:32, :], start=False, stop=True), R=[b1all, sel_], W=[pf])
                        S.op('dve', lambda e, pg=pg: e.tensor_scalar(out=g_[:], in0=pg[:], scalar1=7.0, scalar2=None, op0=ALU.min), R=[pg], W=[g_])
                        S.op('act', lambda e: e.activation(out=sg_[:], in_=g_[:], func=AF.Sigmoid, scale=1.702), R=[g_], W=[sg_])
                        S.op('dve', lambda e, pl=pl: e.tensor_scalar(out=ln_[:], in0=pl[:], scalar1=7.0, scalar2=-7.0, op0=ALU.min, op1=ALU.max), R=[pl], W=[ln_])
                        S.op('dve', lambda e: e.tensor_tensor(out=sg_[:], in0=sg_[:], in1=g_[:], op=ALU.mult), R=[sg_, g_], W=[sg_])
                        S.op('dve', lambda e, c=c: e.scalar_tensor_tensor(out=AT[:, c, :], in0=ln_[:], scalar=1.0, in1=sg_[:], op0=ALU.add, op1=ALU.mult), R=[ln_, sg_], W=[AT])
                    if b + 1 < NBLK:
                        issue_transposes(b + 1)
                    for q in range(MB // 128):
                        y_ = yrow[q % 2]
                        for hf in range(2):
                            py = PF[4 + hf]
                            for c in range(8):
                                S.op('pe', lambda e, c=c, py=py, hf=hf, q=q, W2_=W2_: e.matmul(py[:], lhsT=AT[:, c, q * 128:(q + 1) * 128], rhs=W2_[:, c, hf * 512:(hf + 1) * 512], start=(c == 0), stop=False), R=[AT, wt_], W=[py])
                            S.op('pe', lambda e, py=py, hf=hf, sel_=sel_: e.matmul(py[:], lhsT=sel_[0:32, 0:128], rhs=b2all[0:32, hf * 512:(hf + 1) * 512], start=False, stop=True), R=[sel_, b2all], W=[py])
                            if hf == 0:
                                S.op('act', lambda e, py=py, y_=y_: e.activation(out=y_[:, 0:512], in_=py[:], func=AF.Copy), R=[py], W=[y_])
                            else:
                                S.op('dve', lambda e, py=py, y_=y_: e.tensor_copy(out=y_[:, 512:1024], in_=py[:]), R=[py], W=[y_])
                        self.dma('sp', self.yslot_d[b * MB + q * 128: b * MB + (q + 1) * 128, :], y_[:], R=[y_], W=[TB()])
                S.barrier(); S.emit()
            if self.flags.get('moe_stop') == 3:
                return
            with ExitStack() as s1:
                Y = [self.sb(s1, f"mo_Y{j}", [128, D], F32) for j in range(4)]
                xt = [self.sb(s1, f"mo_x{i}", [128, D], F32) for i in range(2)]
                acc = self.sb(s1, "mo_acc", [128, D], F32)
                for i in range(NT):
                    x_ = xt[i % 2]
                    self.dma('sp', x_[:], self.xres[i * 128:(i + 1) * 128, :], W=[x_])
                    for j in range(4):
                        S.op('pool', lambda e, i=i, j=j: e.indirect_dma_start(out=Y[j][:], out_offset=None, in_=self.yslot_d[:, :], in_offset=bass.IndirectOffsetOnAxis(ap=off_all[:, i, j:j + 1], axis=0)),
                             R=[off_all], W=[Y[j]], dma=True)
                    S.op('dve', lambda e, i=i: e.tensor_scalar(out=acc[:], in0=Y[0][:], scalar1=gsel_all[:, i, 0:1], scalar2=None, op0=ALU.mult), R=[Y[0], gsel_all], W=[acc])
                    for j in range(1, 4):
                        S.op('dve', lambda e, i=i, j=j: e.scalar_tensor_tensor(out=acc[:], in0=Y[j][:], scalar=gsel_all[:, i, j:j + 1], in1=acc[:], op0=ALU.mult, op1=ALU.add), R=[Y[j], gsel_all, acc], W=[acc])
                    S.op('pool', lambda e: e.tensor_tensor(out=acc[:], in0=acc[:], in1=self.gb[1][:], op=ALU.mult), R=[acc, self.gb[1]], W=[acc])
                    S.op('dve', lambda e, x_=x_: e.tensor_tensor(out=x_[:], in0=x_[:], in1=acc[:], op=ALU.add), R=[x_, acc], W=[x_])
                    self.dma('sp', self.xres[i * 128:(i + 1) * 128, :], x_[:], R=[x_], W=[TB()])
                S.barrier(); S.emit()

    def final_norm(self):
        S = self.S
        with ExitStack() as st:
            gfb = self.bcast_row(st, "gfb", self.norm_final[0, :], D)
            xt = [self.sb(st, f"fx{i}", [128, D], F32) for i in range(2)]
            junk = self.sb(st, "fjunk", [128, D], F32)
            ss = [self.sb(st, f"fss{i}", [128, 1], F32) for i in range(2)]
            for i in range(NT):
                x_, s_ = xt[i % 2], ss[i % 2]
                self.dma('sp' if i % 2 == 0 else 'act', x_[:], self.xres[i * 128:(i + 1) * 128, :], W=[x_])
                S.op('act', lambda e, x_=x_, s_=s_: e.activation(out=junk[:], in_=x_[:], func=AF.Square, accum_out=s_[:]), R=[x_], W=[junk, s_])
                S.op('dve', lambda e, s_=s_: e.tensor_scalar(out=s_[:], in0=s_[:], scalar1=1.0 / D, scalar2=1e-5, op0=ALU.mult, op1=ALU.add), R=[s_], W=[s_])
                S.op('act', lambda e, s_=s_: e.activation(out=s_[:], in_=s_[:], func=AF.Sqrt), R=[s_], W=[s_])
                S.op('dve', lambda e, s_=s_: e.reciprocal(out=s_[:], in_=s_[:]), R=[s_], W=[s_])
                S.op('dve', lambda e, x_=x_, s_=s_: e.scalar_tensor_tensor(out=x_[:], in0=x_[:], scalar=s_[:, 0:1], in1=gfb[:], op0=ALU.mult, op1=ALU.mult), R=[x_, s_, gfb], W=[x_])
                t = TB()
                self.dma('sp', self.out[i * 128:(i + 1) * 128, :], x_[:], R=[x_], W=[t])
                self.final_tbs.append(t)
            S.barrier(); S.emit()


class _Stop(Exception):
    pass


class _View:
    def __init__(self, buf, i):
        self.buf = buf; self.i = i; self.tb = buf.tb

    def __getitem__(self, k):
        return self.buf.t[:, self.i, :][k]


def make_in_maps(inputs):
    f = lambda a: np.ascontiguousarray(np.asarray(a, dtype=np.float32))
    w_ext = np.ascontiguousarray(np.asarray(inputs['w_in'], np.float32)[:, :, WCOLS])
    lw = np.ascontiguousarray(np.concatenate([inputs['rwkv_w2'], inputs['rwkv_a2'], inputs['rwkv_g2']], axis=1).astype(np.float32))
    qs = np.array(_swap_cols(0, 4, 64, 8)); qis = np.array(_swap_cols(0, 8, 32, 4))
    shared = dict(
        cst=CST, ada_w=f(inputs['ada_w']), ada_b=f(inputs['ada_b']), norm_mix=f(inputs['norm_mix']), norm_ffn=f(inputs['norm_ffn']),
        w_ext=w_ext, ret_gn=f(inputs['ret_gn']), rwkv_mu=f(inputs['rwkv_mu']), rwkv_w0=f(inputs['rwkv_w0']), rwkv_lw=lw,
        rwkv_a0=f(inputs['rwkv_a0']), rwkv_kk=f(inputs['rwkv_kk']), rwkv_ka=f(inputs['rwkv_ka']),
        rwkv_rk=f(np.asarray(inputs['rwkv_rk']).reshape(L, 256)), rwkv_ln=f(inputs['rwkv_ln']),
        dsa_qnorm=f(inputs['dsa_qnorm']), dsa_wq_up=f(inputs['dsa_wq_up']), dsa_wqs_up=f(np.asarray(inputs['dsa_wq_up'])[:, :, qs]),
        dsa_wqi_up=f(inputs['dsa_wqi_up']), dsa_wqis_up=f(np.asarray(inputs['dsa_wqi_up'])[:, :, qis]),
        dsa_onorm=f(inputs['dsa_onorm']), sb_onorm=f(inputs['sb_onorm']), w_out=f(inputs['w_out']),
        router_w=f(inputs['router_w']), router_b=f(inputs['router_b']), moe_w1=f(inputs['moe_w1']), moe_b1=f(inputs['moe_b1']),
        moe_w2=f(inputs['moe_w2']), moe_b2=f(inputs['moe_b2']), norm_final=f(np.asarray(inputs['norm_final']).reshape(1, D)),
    )
    maps = []
    x = np.asarray(inputs['x'], np.float32); c = np.asarray(inputs['c'], np.float32); pos = np.asarray(inputs['positions'], np.int32)
    for b in range(x.shape[0]):
        m = dict(shared)
        m['x'] = np.ascontiguousarray(x[b]); m['c'] = np.ascontiguousarray(c[b:b + 1]); m['pos'] = np.ascontiguousarray(pos[b:b + 1])
        maps.append(m)
    return maps


def kernel(**inputs):
    maps = make_in_maps(inputs)
    nc = Prog().build()
    res = run_bass_kernel_spmd(nc, maps, core_ids=list(range(NB)))
    return np.stack([np.asarray(r['out'], dtype=np.float32) for r in res.results], axis=0)
```

```python
import numpy as np
from contextlib import ExitStack
import concourse.bass as bass
import concourse.mybir as mybir
from concourse.bass_utils import run_bass_kernel_spmd

F32 = mybir.dt.float32; BF16 = mybir.dt.bfloat16; I32 = mybir.dt.int32; U32 = mybir.dt.uint32
AF = mybir.ActivationFunctionType; ALU = mybir.AluOpType; AX = mybir.AxisListType

D = 1024; SEQ = 4096; NB = 8; L = 2; NT = SEQ // 128
MB = 512; NBLK = 64; NSLOT = NBLK * MB
IN_COLS = 2984
ENGS = ['pe', 'act', 'dve', 'pool', 'sp']
LIMIT = 30000


class TB:
    __slots__ = ('w', 'wd', 'r', 'excl')

    def __init__(self):
        self.w = None; self.wd = {}; self.r = {}; self.excl = False


class Buf:
    def __init__(self, t):
        self.t = t; self.tb = TB()

    def __getitem__(self, k):
        return self.t[k]


def _tb(x):
    return getattr(x, 'tb', x)


class Sched:
    def __init__(self, nc, stack, ndsem=32):
        self.nc = nc; self.stack = stack
        self.ops = {e: [] for e in ENGS}
        self.epoch = {e: 0 for e in ENGS}
        self.cnt = {e: 0 for e in ENGS}
        self.sems = {}
        for e in ENGS:
            self.sems[(e, 0)] = stack.enter_context(nc.semaphore(f"s_{e}_0"))
        self.dsems = [stack.enter_context(nc.semaphore(f"sd_{i}")) for i in range(ndsem)]
        self.dcnt = [0] * ndsem; self.dnext = {'hw': 0, 'sw': 0}
        self.nhw = ndsem // 2
        self.seen = {e: {} for e in ENGS}
        self.nops = 0

    def semof(self, key):
        if key[0] == 'd':
            return self.dsems[key[1]]
        return self.sems[key]

    def op(self, eng, fn, R=(), W=(), dma=False):
        need = {}

        def nd(k, v):
            if need.get(k, 0) < v:
                need[k] = v
        R = [_tb(x) for x in R]; W = [_tb(x) for x in W]
        W = W + [t for t in R if t.excl and t not in W]
        R = [t for t in R if not t.excl]
        for t in R:
            if t.w:
                nd(*t.w)
            for k, v in t.wd.items():
                nd(k, v)
        for t in W:
            if t.w:
                nd(*t.w)
            if not dma:
                for k, v in t.wd.items():
                    nd(k, v)
            for k, v in t.r.items():
                nd(k, v)
        waits = []
        for k, v in need.items():
            if k[0] == 'd':
                v = self.dcnt[k[1]]
            elif k[0] == 'pe' and eng == 'pe':
                continue
            if self.seen[eng].get(k, 0) < v:
                waits.append((k, v)); self.seen[eng][k] = v
        if dma:
            kind = 'sw' if eng == 'pool' else 'hw'
            n = self.nhw if kind == 'hw' else len(self.dsems) - self.nhw
            i = self.dnext[kind] + (0 if kind == 'hw' else self.nhw)
            self.dnext[kind] = (self.dnext[kind] + 1) % n
            self.dcnt[i] += 16; key = ('d', i); val = self.dcnt[i]; inc = 16
        else:
            if self.cnt[eng] >= LIMIT:
                self.epoch[eng] += 1; self.cnt[eng] = 0
                self.sems[(eng, self.epoch[eng])] = self.stack.enter_context(
                    self.nc.semaphore(f"s_{eng}_{self.epoch[eng]}"))
            self.cnt[eng] += 1; key = (eng, self.epoch[eng]); val = self.cnt[eng]; inc = 1
        self.ops[eng].append((waits, fn, key, inc))
        for t in R:
            t.r[key] = val
        for t in W:
            if dma:
                t.wd[key] = val
            else:
                t.w = (key, val); t.wd = {}
                t.r = {}
        self.nops += 1

    def barrier(self):
        for e in ENGS:
            waits = []
            for f in ENGS:
                if f == e:
                    continue
                k = (f, self.epoch[f]); v = self.cnt[f]
                if v > 0 and self.seen[e].get(k, 0) < v:
                    waits.append((k, v)); self.seen[e][k] = v
            for i in range(len(self.dsems)):
                k = ('d', i); v = self.dcnt[i]
                if v > 0 and self.seen[e].get(k, 0) < v:
                    waits.append((k, v)); self.seen[e][k] = v
            self.ops[e].append((waits, None, None, 0))

    def emit(self):
        nc = self.nc
        names = {'pe': 'tensor', 'act': 'scalar', 'dve': 'vector', 'pool': 'gpsimd', 'sp': 'sync'}
        with nc.Block() as block:
            for e in ENGS:
                lst = self.ops[e]

                def body(engine, lst=lst):
                    for waits, fn, key, inc in lst:
                        for k, v in waits:
                            engine.wait_ge(self.semof(k), v)
                        if fn is not None:
                            ins = fn(engine)
                            ins.then_inc(self.semof(key), inc)
                getattr(block, names[e])(body)
        self.ops = {e: [] for e in ENGS}


RET0, RWKV0, DSA0, SB0 = 0, 1024, 1920, 2216


def _swap_cols(base, nheads, hd, half):
    cols = []
    for h in range(nheads):
        for f in range(hd):
            if f < half:
                g = f + half
            elif f < 2 * half:
                g = f - half
            else:
                g = f
            cols.append(base + h * hd + g)
    return cols


def build_wext_cols():
    blocks = {}
    r = lambda a, n: list(range(a, a + n))
    qs = _swap_cols(RET0, 4, 64, 32); ks = _swap_cols(RET0 + 256, 4, 64, 32)
    for hp in range(2):
        blocks[f'ret_q{hp}'] = r(RET0 + hp * 128, 128)
        blocks[f'ret_qs{hp}'] = qs[hp * 128:(hp + 1) * 128]
        blocks[f'ret_k{hp}'] = r(RET0 + 256 + hp * 128, 128)
        blocks[f'ret_ks{hp}'] = ks[hp * 128:(hp + 1) * 128]
        blocks[f'ret_vg{hp}'] = r(RET0 + 512 + hp * 128, 128) + r(RET0 + 768 + hp * 128, 128)
    for hp in range(2):
        blocks[f'sb_q{hp}'] = r(SB0 + hp * 128, 128)
        blocks[f'sb_k{hp}'] = r(SB0 + 256 + hp * 128, 128)
    blocks['sb_v'] = r(SB0 + 512, 256)
    blocks['rw_rkv'] = r(RWKV0, 768)
    blocks['rw_lora'] = r(RWKV0 + 768, 128)
    blocks['ds_cq'] = r(DSA0, 128)
    kcols = r(DSA0 + 128, 64); kscols = _swap_cols(DSA0 + 128, 1, 64, 8)
    blocks['ds_k'] = kcols + kcols
    blocks['ds_ks'] = kscols + kscols
    icols = r(DSA0 + 256, 32); iscols = _swap_cols(DSA0 + 256, 1, 32, 4)
    blocks['ds_ki'] = icols * 4
    blocks['ds_kis'] = iscols * 4
    blocks['ds_vw'] = r(DSA0 + 192, 64) + r(DSA0 + 288, 8)
    off = {}; cols = []
    for k, v in blocks.items():
        off[k] = (len(cols), len(v)); cols += v
    return off, np.array(cols, dtype=np.int64)


WOFF, WCOLS = build_wext_cols()
NEXT = len(WCOLS)


def build_consts():
    c = {}
    p = np.arange(128)
    c['ident'] = np.eye(128, dtype=np.float32)
    sbm = np.zeros((4, 128, 512), np.float32)
    for rr in range(4):
        for qb in range(4):
            if qb > rr:
                sbm[rr, :, qb * 128:(qb + 1) * 128] = 1.0
            elif qb == rr:
                sbm[rr, :, qb * 128:(qb + 1) * 128] = (p[:, None] < p[None, :]).astype(np.float32)
    c['sbmask'] = sbm.transpose(1, 0, 2).reshape(128, 4 * 512)
    c['tri_ge'] = (p[:, None] >= p[None, :]).astype(np.float32)
    c['tri_le'] = (p[:, None] <= p[None, :]).astype(np.float32)
    c['tri_lt'] = (p[:, None] < p[None, :]).astype(np.float32)
    c['tri_gt'] = (p[:, None] > p[None, :]).astype(np.float32)
    c['ntri_ge'] = -c['tri_ge']
    c['nones'] = -np.ones((128, 128), np.float32)
    c['hm4'] = (p[:, None] // 32 == np.arange(4)[None, :]).astype(np.float32)
    c['hm2'] = (p[:, None] // 64 == np.arange(2)[None, :]).astype(np.float32)
    c['iota_blk'] = np.tile(np.arange(64, dtype=np.float32)[None, :], (128, 1))
    c['kp'] = (np.arange(8)[None, :] * 128 + p[:, None]).astype(np.float32)
    c['pcol'] = p.astype(np.float32)[:, None]
    c['last'] = (p == 127).astype(np.float32)[:, None]
    c['negmask'] = np.where(p[None, :] > p[:, None], -1e30, 0.0).astype(np.float32)
    lg = np.log(1.0 - 2.0 ** (-5.0 - np.arange(4, dtype=np.float64)))
    idx = np.arange(128, dtype=np.float64)
    for hp in range(2):
        hs = [2 * hp, 2 * hp + 1]
        hp_of_p = np.array([hs[q // 64] for q in range(128)])
        c[f'ret_qdec{hp}'] = np.exp(lg[hp_of_p][:, None] * (idx[None, :] + 1.0)).astype(np.float32)
        kd = np.zeros((128, 128)); dm = np.zeros((128, 2, 128))
        for j, h in enumerate(hs):
            kd[:, j * 64:(j + 1) * 64] = (np.exp(lg[h] * (127.0 - idx)) / 8.0)[:, None]
            diff = idx[None, :] - idx[:, None]
            dm[:, j, :] = np.where(diff >= 0, np.exp(lg[h] * np.maximum(diff, 0.0)), 0.0) / 8.0
        c[f'ret_kdec{hp}'] = kd.astype(np.float32)
        c[f'ret_dmask{hp}'] = dm.reshape(128, 256).astype(np.float32)
        c[f'ret_cd{hp}'] = np.exp(lg[hp_of_p] * 128.0).astype(np.float32)[:, None]
    f64 = p % 64
    c['rope_ret'] = np.stack([10000.0 ** (-(f64 % 32) / 32.0) / (2 * np.pi), np.where(f64 < 32, -1.0, 1.0)], 1).astype(np.float32)
    inv = np.where(f64 < 16, 500000.0 ** (-(f64 % 8) / 8.0), 0.0) / (2 * np.pi)
    sg = np.where(f64 < 8, -1.0, np.where(f64 < 16, 1.0, 0.0))
    c['rope_dq'] = np.stack([inv, sg], 1).astype(np.float32)
    f32 = p % 32
    inv = np.where(f32 < 8, 500000.0 ** (-(f32 % 4) / 4.0), 0.0) / (2 * np.pi)
    sg = np.where(f32 < 4, -1.0, np.where(f32 < 8, 1.0, 0.0))
    c['rope_di'] = np.stack([inv, sg], 1).astype(np.float32)
    off = {}; n = 0; arrs = []
    for k, v in c.items():
        off[k] = (n, v.shape[1]); n += v.shape[1]; arrs.append(v.astype(np.float32))
    return off, np.ascontiguousarray(np.concatenate(arrs, 1))


COFF, CST = build_consts()


class Prog:
    def __init__(self, debug=None, nlayers=L, flags=None):
        self.debug = debug or {}
        self.flags = flags or {}
        self.nlayers = nlayers
        nc = self.nc = bass.Bass("TRN2", target_bir_lowering=False)
        dt = lambda name, shape, dtype, kind="ExternalInput": nc.dram_tensor(name, shape, dtype, kind=kind).ap()
        self.x = dt("x", [SEQ, D], F32)
        self.c = dt("c", [1, D], F32)
        self.pos = dt("pos", [1, SEQ], I32)
        self.cst = dt("cst", [128, CST.shape[1]], F32)
        self.ada_w = dt("ada_w", [L, D, 6 * D], F32)
        self.ada_b = dt("ada_b", [L, 6 * D], F32)
        self.norm_mix = dt("norm_mix", [L, D], F32)
        self.norm_ffn = dt("norm_ffn", [L, D], F32)
        self.w_ext = dt("w_ext", [L, D, NEXT], F32)
        self.ret_gn = dt("ret_gn", [L, 256], F32)
        self.rwkv_mu = dt("rwkv_mu", [L, 896], F32)
        self.rwkv_w0 = dt("rwkv_w0", [L, 256], F32)
        self.rwkv_lw = dt("rwkv_lw", [L, 128, 256], F32)
        self.rwkv_a0 = dt("rwkv_a0", [L, 256], F32)
        self.rwkv_kk = dt("rwkv_kk", [L, 256], F32)
        self.rwkv_ka = dt("rwkv_ka", [L, 256], F32)
        self.rwkv_rk = dt("rwkv_rk", [L, 256], F32)
        self.rwkv_ln = dt("rwkv_ln", [L, 256], F32)
        self.dsa_qnorm = dt("dsa_qnorm", [L, 128], F32)
        self.dsa_wq_up = dt("dsa_wq_up", [L, 128, 256], F32)
        self.dsa_wqs_up = dt("dsa_wqs_up", [L, 128, 256], F32)
        self.dsa_wqi_up = dt("dsa_wqi_up", [L, 128, 256], F32)
        self.dsa_wqis_up = dt("dsa_wqis_up", [L, 128, 256], F32)
        self.dsa_onorm = dt("dsa_onorm", [L, 256], F32)
        self.sb_onorm = dt("sb_onorm", [L, 256], F32)
        self.w_out = dt("w_out", [L, D, D], F32)
        self.router_w = dt("router_w", [L, D, 32], F32)
        self.router_b = dt("router_b", [L, 32], F32)
        if not self.flags.get('nomoe'):
            self.moe_w1 = dt("moe_w1", [L, 32, D, 2 * D], F32)
            self.moe_b1 = dt("moe_b1", [L, 32, 2 * D], F32)
            self.moe_w2 = dt("moe_w2", [L, 32, D, D], F32)
            self.moe_b2 = dt("moe_b2", [L, 32, D], F32)
        self.norm_final = dt("norm_final", [1, D], F32)
        self.out = dt("out", [SEQ, D], F32, kind="ExternalOutput")
        self.xres = dt("xres", [SEQ, D], F32, kind="Internal")
        self.yT_d = dt("yT_d", [8, 128, SEQ], BF16, kind="Internal")
        self.maskT_d = dt("maskT_d", [NT, 128, NT, 128], BF16, kind="Internal")
        self.hrow_d = dt("hrow_d", [SEQ, D], BF16, kind="Internal")
        if not self.flags.get('nomoe'):
            self.w1b_d = dt("w1b_d", [L * 32 * 128, 8 * 2 * D], BF16, kind="Internal")
            self.w2b_d = dt("w2b_d", [L * 32 * 128, 8 * D], BF16, kind="Internal")
        self.conv_tb = {}
        self.tokidx_d = dt("tokidx_d", [NSLOT, 2], I32, kind="Internal")
        self.yslot_d = dt("yslot_d", [NSLOT, D], F32, kind="Internal")
        self.dbg = {}
        for name, (shape, dtype) in self.debug.items():
            self.dbg[name] = dt("dbg_" + name, shape, dtype, kind="ExternalOutput")
        self.final_tbs = []

    def sb(self, st, name, shape, dtype):
        self._uid = getattr(self, '_uid', 0) + 1
        return Buf(st.enter_context(self.nc.sbuf_tensor(f"{name}_{self._uid}", shape, dtype)))

    def ps(self, st, name, shape, dtype):
        self._uid = getattr(self, '_uid', 0) + 1
        b = Buf(st.enter_context(self.nc.psum_tensor(f"{name}_{self._uid}", shape, dtype)))
        b.tb.excl = True
        return b

    def dma(self, eng, out, in_, R=(), W=(), **kw):
        self.S.op(eng, lambda e: e.dma_start(out=out, in_=in_, **kw), R=R, W=W, dma=True)

    def load_const(self, st, name, dtype=F32, eng='sp'):
        o, n = COFF[name]
        b = self.sb(st, "c_" + name, [128, n], dtype)
        kw = dict(allow_slow_non_contiguous=True) if n < 8 else {}
        self.dma('pool' if dtype != F32 else eng, b[:], self.cst[:, o:o + n], W=[b], **kw)
        return b

    def bcast_row(self, st, name, row_ap, n, eng='sp'):
        b = self.sb(st, name, [128, n], F32)
        self.dma(eng, b[:], row_ap.partition_broadcast(128), W=[b])
        return b

    def dbg_out(self, name, src_ap, R, dst=None):
        if name in self.dbg:
            t = TB()
            d = self.dbg[name] if dst is None else dst
            self.dma('sp', d, src_ap, R=R, W=[t])
            self.final_tbs.append(t)

    def build(self):
        nc = self.nc
        with ExitStack() as top:
            S = self.S = Sched(nc, top)
            self.ident = self.load_const(top, 'ident', BF16)
            self.identf = self.load_const(top, 'ident', F32)
            self.modT = self.sb(top, "modT", [128, 48], F32)
            self.gb = [self.sb(top, f"gb{i}", [128, D], F32) for i in range(2)]
            self.ab2 = self.sb(top, "ab2", [128, D], F32); self.shb2 = self.sb(top, "shb2", [128, D], F32)
            self.reg_bc = top.enter_context(nc.gpsimd.register("reg_bc"))

            self.ones_row = self.sb(top, "ones_row", [1, 512], F32)
            self.condT = self.sb(top, "condT", [128, 8], F32)
            self.PF = [self.ps(top, f"pf{i}", [128, 512], F32) for i in range(6)]
            self.PB = [self.ps(top, f"pb{i}", [128, 1024], BF16) for i in range(2)]
            S.op('dve', lambda e: e.memset(self.ones_row[:], 1.0), W=[self.ones_row])
            with ExitStack() as st:
                self.cond_phase(st)
                S.barrier(); S.emit()
            for l in range(self.nlayers):
                self.layer(l)
            if 'stop' not in self.flags:
                self.final_norm()
            need = {}
            for t in self.final_tbs:
                for k in t.wd:
                    need[k] = max(need.get(k, 0), S.dcnt[k[1]])
            S.ops['sp'].append((list(need.items()), None, None, 0))
            S.barrier(); S.emit()
        return nc

    def cond_phase(self, st):
        S = self.S
        crow = self.sb(st, "crow", [1, D], F32)
        self.dma('sp', crow[:], self.c[0:1, :], W=[crow])
        pf = self.PF[0]
        for j in range(8):
            S.op('pe', lambda e, j=j: e.matmul(pf[:, j:j + 1], lhsT=crow[0:1, j * 128:(j + 1) * 128], rhs=self.ones_row[0:1, 0:1],
                                              start=True, stop=True), R=[crow, self.ones_row], W=[pf])
        S.op('act', lambda e: e.activation(out=self.condT[:], in_=pf[:, 0:8], func=AF.Silu), R=[pf], W=[self.condT])

    def row_to_cols(self, row_buf, row_ap_fn, ncols, out_ap, extra_R=()):
        S = self.S; pf = self.PF[0]
        for j in range(ncols):
            S.op('pe', lambda e, j=j: e.matmul(pf[:, j:j + 1], lhsT=row_ap_fn(j), rhs=self.ones_row[0:1, 0:1], start=True, stop=True),
                 R=[row_buf, self.ones_row], W=[pf])
        return pf

    def layer(self, l):
        S = self.S
        self.mod_phase(l)
        if self.flags.get('upto') == 'mod':
            return
        with ExitStack() as hs:
            self.hT = self.sb(hs, "hT", [128, 8, SEQ + 1], BF16)
            S.op('dve', lambda e: e.memset(self.hT[:, :, 0:1], 0.0), W=[self.hT])
            self.norm_phase(l, 0)
            if self.flags.get('upto') == 'norm':
                return
            only = self.flags.get('only')
            if only in (None, 'ret'):
                self.retention_phase(l)
            if only in (None, 'rwkv'):
                self.rwkv_phase(l)
            if only in (None, 'dsa'):
                self.dsa_phase(l)
            if only in (None, 'sb'):
                self.sb_phase(l)
            S.barrier(); S.emit()
        if 'yT' in self.dbg:
            for j in range(8):
                self.dbg_out('yT', self.yT_d[j], [], dst=self.dbg['yT'][j])
            S.barrier(); S.emit()
        if self.flags.get('upto') == 'mix':
            return
        self.wout_phase(l)
        if f'xmix{l}' in self.dbg:
            self.dbg_out(f'xmix{l}', self.xres[:, :], [])
            S.barrier(); S.emit()
        if self.flags.get('upto') == 'wout':
            return
        self.moe_phase(l)
        if f'x{l}' in self.dbg:
            self.dbg_out(f'x{l}', self.xres[:, :], [])
            S.barrier(); S.emit()

    def mod_phase(self, l):
        S = self.S
        with ExitStack() as st:
            self.modrow = self.sb(st, "modrow", [1, 6 * D], F32)
            wt = [self.sb(st, f"adaw{i}", [128, 8, 512], F32) for i in range(2)]
            brow = self.sb(st, "adab", [1, 6 * D], F32)
            self.dma('sp', brow[:], self.ada_b[l:l + 1, :], W=[brow])
            for cg in range(12):
                w = wt[cg % 2]
                self.dma('sp' if cg % 2 == 0 else 'act', w[:], self.ada_w[l, :, cg * 512:(cg + 1) * 512].rearrange("(k p) c -> p k c", p=128), W=[w])
                pf = self.PF[1 + cg % 2]
                for k in range(8):
                    S.op('pe', lambda e, k=k, w=w, pf=pf: e.matmul(pf[0:1, :], lhsT=self.condT[:, k:k + 1], rhs=w[:, k, :], start=(k == 0), stop=(k == 7)),
                         R=[self.condT, w], W=[pf])
                S.op('dve', lambda e, cg=cg, pf=pf: e.tensor_tensor(out=self.modrow[0:1, cg * 512:(cg + 1) * 512], in0=pf[0:1, :],
                                                                  in1=brow[0:1, cg * 512:(cg + 1) * 512], op=ALU.add), R=[pf, brow], W=[self.modrow])
            pf = self.row_to_cols(self.modrow, lambda j: self.modrow[0:1, j * 128:(j + 1) * 128], 48, None)
            S.op('dve', lambda e: e.tensor_copy(out=self.modT[:], in_=pf[:, 0:48]), R=[pf], W=[self.modT])
            onescol = self.sb(st, "ones1", [1, 128], F32)
            S.op('dve', lambda e: e.memset(onescol[:], 1.0), W=[onescol])
            for gi, base in enumerate((2 * D, 5 * D)):
                for hf in range(2):
                    pf2 = self.PF[3 + hf]
                    S.op('pe', lambda e, pf2=pf2, base=base, hf=hf: e.matmul(pf2[:], lhsT=onescol[0:1, :], rhs=self.modrow[0:1, base + hf * 512: base + (hf + 1) * 512],
                                                                            start=True, stop=True), R=[onescol, self.modrow], W=[pf2])
                    S.op('act', lambda e, pf2=pf2, gi=gi, hf=hf: e.activation(out=self.gb[gi][:, hf * 512:(hf + 1) * 512], in_=pf2[:], func=AF.Copy), R=[pf2], W=[self.gb[gi]])
            grow2 = self.sb(st, "grow2", [1, D], F32); a2row = self.sb(st, "a2row", [1, D], F32)
            self.dma('sp', grow2[:], self.norm_ffn[l:l + 1, :], W=[grow2])
            S.op('dve', lambda e: e.scalar_tensor_tensor(out=a2row[:], in0=self.modrow[0:1, 4 * D:5 * D], scalar=1.0, in1=grow2[:], op0=ALU.add, op1=ALU.mult),
                 R=[self.modrow, grow2], W=[a2row])
            for dstb, rowfn, rb in ((self.ab2, lambda hf: a2row[0:1, hf * 512:(hf + 1) * 512], a2row), (self.shb2, lambda hf: self.modrow[0:1, 3 * D + hf * 512: 3 * D + (hf + 1) * 512], self.modrow)):
                for hf in range(2):
                    pf2 = self.PF[3 + hf]
                    S.op('pe', lambda e, pf2=pf2, rowfn=rowfn, hf=hf: e.matmul(pf2[:], lhsT=onescol[0:1, :], rhs=rowfn(hf), start=True, stop=True), R=[onescol, rb], W=[pf2])
                    S.op('act', lambda e, pf2=pf2, dstb=dstb, hf=hf: e.activation(out=dstb[:, hf * 512:(hf + 1) * 512], in_=pf2[:], func=AF.Copy), R=[pf2], W=[dstb])
            self.dbg_out(f'mod{l}', self.modrow[0:1, :], [self.modrow])
            S.barrier(); S.emit()

    def norm_phase(self, l, which):
        S = self.S
        src = self.x if (l == 0 and which == 0) else self.xres
        gsrc = self.norm_mix if which == 0 else self.norm_ffn
        shc, scc = (0, 8) if which == 0 else (24, 32)
        with ExitStack() as st:
            grow = self.sb(st, "grow", [1, D], F32)
            self.dma('sp', grow[:], gsrc[l:l + 1, :], W=[grow])
            pf = self.row_to_cols(grow, lambda j: grow[0:1, j * 128:(j + 1) * 128], 8, None)
            acol = self.sb(st, "acol", [128, 8], F32)
            S.op('dve', lambda e: e.scalar_tensor_tensor(out=acol[:], in0=self.modT[:, scc:scc + 8], scalar=1.0, in1=pf[:, 0:8], op0=ALU.add, op1=ALU.mult),
                 R=[self.modT, pf], W=[acol])
            xt = [self.sb(st, f"xt{i}", [128, D], F32) for i in range(2)]
            xn = [self.sb(st, f"xn{i}", [128, D], BF16) for i in range(2)]
            junk = self.sb(st, "junk", [128, D], BF16)
            hrow = [self.sb(st, f"hrow{i}", [128, D], BF16) for i in range(2)] if which == 1 else None
            ssq = [self.sb(st, f"ssq{i}", [128, 1], F32) for i in range(2)]
            for i in range(NT):
                x_, xn_, ss = xt[i % 2], xn[i % 2], ssq[i % 2]
                self.dma('sp' if i % 2 == 0 else 'act', x_[:], src[i * 128:(i + 1) * 128, :], W=[x_])
                S.op('act', lambda e, x_=x_, ss=ss: e.activation(out=junk[:], in_=x_[:], func=AF.Square, accum_out=ss[:]), R=[x_], W=[junk, ss])
                S.op('dve', lambda e, ss=ss: e.tensor_scalar(out=ss[:], in0=ss[:], scalar1=1.0 / D, scalar2=1e-5, op0=ALU.mult, op1=ALU.add), R=[ss], W=[ss])
                S.op('act', lambda e, ss=ss: e.activation(out=ss[:], in_=ss[:], func=AF.Sqrt), R=[ss], W=[ss])
                S.op('dve', lambda e, ss=ss: e.reciprocal(out=ss[:], in_=ss[:]), R=[ss], W=[ss])
                S.op('dve', lambda e, x_=x_, xn_=xn_, ss=ss: e.tensor_scalar(out=xn_[:], in0=x_[:], scalar1=ss[:, 0:1], scalar2=None, op0=ALU.mult), R=[x_, ss], W=[xn_])
                pb = self.PB[i % 2]
                for j in range(8):
                    S.op('pe', lambda e, j=j, xn_=xn_, pb=pb: e.transpose(out=pb[:, j * 128:(j + 1) * 128], in_=xn_[:, j * 128:(j + 1) * 128], identity=self.ident[:]),
                         R=[xn_, self.ident], W=[pb])
                if which == 1:
                    hr = hrow[i % 2]
                    S.op('pool', lambda e, xn_=xn_, hr=hr: e.tensor_tensor(out=hr[:], in0=xn_[:], in1=self.ab2[:], op=ALU.mult), R=[xn_, self.ab2], W=[hr])
                    S.op('pool', lambda e, hr=hr: e.tensor_tensor(out=hr[:], in0=hr[:], in1=self.shb2[:], op=ALU.add), R=[hr, self.shb2], W=[hr])
                    self.dma('sp', self.hrow_d[i * 128:(i + 1) * 128, :], hr[:], R=[hr], W=[TB()])
                for j in range(8):
                    eng = 'dve' if j % 2 == 0 else 'pool'
                    if eng == 'pool':
                        eng = 'act'
                        S.op('act', lambda e, j=j, pb=pb, i=i: e.activation(out=self.hT[:, j, 1 + i * 128: 1 + (i + 1) * 128], in_=pb[:, j * 128:(j + 1) * 128], func=AF.Identity,
                                                                           scale=acol[:, j:j + 1], bias=self.modT[:, shc + j: shc + j + 1]), R=[pb, acol, self.modT], W=[self.hT])
                    else:
                        S.op('dve', lambda e, j=j, pb=pb, i=i: e.tensor_scalar(out=self.hT[:, j, 1 + i * 128: 1 + (i + 1) * 128], in0=pb[:, j * 128:(j + 1) * 128],
                                                                              scalar1=acol[:, j:j + 1], scalar2=self.modT[:, shc + j: shc + j + 1], op0=ALU.mult, op1=ALU.add),
                             R=[pb, acol, self.modT], W=[self.hT])
            if f'hT{l}' in self.dbg and which == 0:
                for j in range(8):
                    self.dbg_out(f'hT{l}', self.hT[:, j, 1:], [self.hT], dst=self.dbg[f'hT{l}'][j])
            S.barrier(); S.emit()

    def load_w(self, st, name, l, blocks):
        n = sum(WOFF[b][1] for b in blocks)
        wm = self.sb(st, name, [128, 8, n], BF16)
        o = 0; offs = {}
        for b in blocks:
            c0, cn = WOFF[b]
            for k in range(8):
                self.dma('pool', wm[:, k, o:o + cn], self.w_ext[l, k * 128:(k + 1) * 128, c0:c0 + cn], W=[wm])
            offs[b] = o; o += cn
        return wm, offs

    def proj_T(self, wm, c0, pf, tg, shift=0, start=True, stop=True, M=128):
        S = self.S
        for k in range(8):
            S.op('pe', lambda e, k=k: e.matmul(pf[0:M, :], lhsT=wm[:, k, c0:c0 + M], rhs=self.hT[:, k, 1 - shift + tg * 512: 1 - shift + (tg + 1) * 512],
                                              start=(start and k == 0), stop=(stop and k == 7)), R=[wm, self.hT], W=[pf])

    def proj_tok(self, wm, c0, n, pf_ap, pf, i, shift=0, start=True, stop=True):
        S = self.S
        for k in range(8):
            S.op('pe', lambda e, k=k: e.matmul(pf_ap, lhsT=self.hT[:, k, 1 - shift + i * 128: 1 - shift + (i + 1) * 128], rhs=wm[:, k, c0:c0 + n],
                                              start=(start and k == 0), stop=(stop and k == 7)), R=[wm, self.hT], W=[pf])

    def rope_tables(self, st, cname, name):
        S = self.S
        rc = self.load_const(st, cname, F32)
        C = self.sb(st, name + "C", [128, SEQ], BF16); Sg = self.sb(st, name + "S", [128, SEQ], BF16)
        CH = 512
        with ExitStack() as s2:
            posi = self.sb(s2, name + "pi", [128, CH], I32); posf = self.sb(s2, name + "pf", [128, CH], F32)
            u = self.sb(s2, name + "u", [128, CH], F32); ui = self.sb(s2, name + "ui", [128, CH], I32)
            uf = self.sb(s2, name + "uf", [128, CH], F32)
            for ch in range(SEQ // CH):
                cs = slice(ch * CH, (ch + 1) * CH)
                self.dma('sp', posi[:], self.pos[0:1, cs].partition_broadcast(128), W=[posi])
                S.op('dve', lambda e: e.tensor_copy(out=posf[:], in_=posi[:]), R=[posi], W=[posf])
                for phase, dst in ((0.0, Sg), (0.25, C)):
                    S.op('dve', lambda e, phase=phase: e.tensor_scalar(out=u[:], in0=posf[:], scalar1=rc[:, 0:1], scalar2=phase, op0=ALU.mult, op1=ALU.add),
                         R=[posf, rc], W=[u])
                    S.op('dve', lambda e: e.tensor_copy(out=ui[:], in_=u[:]), R=[u], W=[ui])
                    S.op('dve', lambda e: e.tensor_copy(out=uf[:], in_=ui[:]), R=[ui], W=[uf])
                    S.op('dve', lambda e: e.tensor_tensor(out=u[:], in0=u[:], in1=uf[:], op=ALU.subtract), R=[u, uf], W=[u])
                    S.op('dve', lambda e: e.tensor_scalar(out=u[:], in0=u[:], scalar1=0.5, scalar2=-0.5, op0=ALU.min, op1=ALU.max), R=[u], W=[u])
                    if dst is Sg:
                        S.op('act', lambda e: e.activation(out=uf[:], in_=u[:], func=AF.Sin, scale=2 * np.pi), R=[u], W=[uf])
                        S.op('dve', lambda e, cs=cs: e.tensor_scalar(out=Sg[:, cs], in0=uf[:], scalar1=rc[:, 1:2], scalar2=None, op0=ALU.mult), R=[uf, rc], W=[Sg])
                    else:
                        S.op('act', lambda e, cs=cs: e.activation(out=C[:, cs], in_=u[:], func=AF.Sin, scale=2 * np.pi), R=[u], W=[C])
            self.S.barrier(); self.S.emit()
        return C, Sg

    def y_store(self, ytile_idx, i, ytok, ystage, R):
        S = self.S
        pb = self.PB[i % 2]
        for f in range(2):
            S.op('pe', lambda e, f=f: e.transpose(out=pb[:, f * 128:(f + 1) * 128], in_=ytok[:, f * 128:(f + 1) * 128], identity=self.ident[:]),
                 R=[ytok, self.ident] + list(R), W=[pb])
        S.op('act', lambda e: e.activation(out=ystage[:, :, (i % 4) * 128:(i % 4 + 1) * 128], in_=pb[:, 0:256].rearrange("p (f t) -> p f t", f=2), func=AF.Copy),
             R=[pb], W=[ystage])
        if i % 4 == 3:
            g = i // 4
            for f in range(2):
                t = TB()
                self.dma('sp', self.yT_d[ytile_idx + f, :, g * 512:(g + 1) * 512], ystage[:, f, :], R=[ystage], W=[t])

    def retention_phase(self, l):
        S = self.S
        with ExitStack() as st:
            C, Sg = self.rope_tables(st, 'rope_ret', 'rr')
            gnb = self.bcast_row(st, "ret_gnb", self.ret_gn[l, :], 256)
            ytok_all = self.sb(st, "ret_ytok", [128, NT, 256], BF16)
            for hp in range(2):
                with ExitStack() as s2:
                    wm, wo = self.load_w(s2, "wm_ret", l, [f'ret_q{hp}', f'ret_qs{hp}', f'ret_k{hp}', f'ret_ks{hp}', f'ret_vg{hp}'])
                    QT = self.sb(s2, "QT", [128, SEQ], BF16); QS = self.sb(s2, "QS", [128, SEQ], BF16)
                    KT = self.sb(s2, "KT", [128, SEQ], BF16); KS = self.sb(s2, "KS", [128, SEQ], BF16)
                    qdec = self.load_const(s2, f'ret_qdec{hp}', BF16); kdec = self.load_const(s2, f'ret_kdec{hp}', F32)
                    dmask = self.load_const(s2, f'ret_dmask{hp}', F32); cd = self.load_const(s2, f'ret_cd{hp}', F32)
                    for name, dst in ((f'ret_q{hp}', QT), (f'ret_qs{hp}', QS), (f'ret_k{hp}', KT), (f'ret_ks{hp}', KS)):
                        for tg in range(8):
                            pf = self.PF[tg % 4]
                            self.proj_T(wm, wo[name], pf, tg)
                            eng = 'act' if tg % 2 == 0 else 'dve'
                            if eng == 'act':
                                S.op('act', lambda e, pf=pf, dst=dst, tg=tg: e.activation(out=dst[:, tg * 512:(tg + 1) * 512], in_=pf[:], func=AF.Copy), R=[pf], W=[dst])
                            else:
                                S.op('dve', lambda e, pf=pf, dst=dst, tg=tg: e.tensor_copy(out=dst[:, tg * 512:(tg + 1) * 512], in_=pf[:]), R=[pf], W=[dst])
                    for A, B_ in ((QT, QS), (KT, KS)):
                        S.op('dve', lambda e, A=A: e.tensor_tensor(out=A[:], in0=A[:], in1=C[:], op=ALU.mult), R=[A, C], W=[A])
                        S.op('pool', lambda e, B_=B_: e.tensor_tensor(out=B_[:], in0=B_[:], in1=Sg[:], op=ALU.mult), R=[B_, Sg], W=[B_])
                        S.op('dve', lambda e, A=A, B_=B_: e.tensor_tensor(out=A[:], in0=A[:], in1=B_[:], op=ALU.add), R=[A, B_], W=[A])
                    QD = QS
                    S.op('dve', lambda e: e.tensor_tensor(out=QD[:].rearrange("p (c n) -> p c n", n=128), in0=QT[:].rearrange("p (c n) -> p c n", n=128),
                                                         in1=qdec[:].unsqueeze(1).broadcast_to([128, NT, 128]), op=ALU.mult), R=[QT, qdec], W=[QD])
                    state = self.sb(s2, "rstate", [128, 128], F32); state_bf = self.sb(s2, "rstate_bf", [128, 128], BF16)
                    qbd = [self.sb(s2, f"qbd{i}", [128, 256], BF16) for i in range(2)]
                    for i in range(2):
                        S.op('pool', lambda e, i=i: e.memset(qbd[i][:], 0.0), W=[qbd[i]])
                    S.op('dve', lambda e: e.memset(state[:], 0.0), W=[state])
                    S.op('dve', lambda e: e.memset(state_bf[:], 0.0), W=[state_bf])
                    vg = [self.sb(s2, f"rvg{i}", [128, 128], BF16) for i in range(2)]
                    sg = [self.sb(s2, f"rsg{i}", [128, 128], F32) for i in range(2)]
                    kd = [self.sb(s2, f"rkd{i}", [128, 128], BF16) for i in range(2)]
                    pT = [self.sb(s2, f"rpT{i}", [128, 256], BF16) for i in range(2)]
                    o_sb = self.sb(s2, "ro", [128, 128], F32); cen = self.sb(s2, "rcen", [128, 128], F32); sq = self.sb(s2, "rsq", [128, 128], F32)
                    st4 = self.sb(s2, "rst4", [128, 4], F32)
                    for c in range(NT):
                        i2 = c % 2
                        tok = slice(c * 128, (c + 1) * 128)
                        pfv = self.PF[0]
                        self.proj_tok(wm, wo[f'ret_vg{hp}'], 256, pfv[:, 0:256], pfv, c)
                        S.op('dve', lambda e, i2=i2: e.tensor_copy(out=vg[i2][:], in_=pfv[:, 0:128]), R=[pfv], W=[vg[i2]])
                        S.op('act', lambda e, i2=i2: e.activation(out=sg[i2][:], in_=pfv[:, 128:256], func=AF.Silu), R=[pfv], W=[sg[i2]])
                        pb = self.PB[0]
                        S.op('pe', lambda e, tok=tok: e.transpose(out=pb[:, 0:128], in_=KT[:, tok], identity=self.ident[:]), R=[KT, self.ident], W=[pb])
                        S.op('dve', lambda e, i2=i2: e.tensor_tensor(out=kd[i2][:], in0=pb[:, 0:128], in1=kdec[:], op=ALU.mult), R=[pb, kdec], W=[kd[i2]])
                        pfs = self.PF[1]
                        for hh in range(2):
                            pr = slice(hh * 64, (hh + 1) * 64)
                            S.op('pool', lambda e, hh=hh, pr=pr, tok=tok, i2=i2: e.tensor_copy(out=qbd[i2][pr, hh * 128:(hh + 1) * 128], in_=QT[pr, tok]), R=[QT, qbd[i2]], W=[qbd[i2]])
                        S.op('pe', lambda e, tok=tok, i2=i2: e.matmul(pfs[:, 0:256], lhsT=KT[:, tok], rhs=qbd[i2][:], start=True, stop=True), R=[KT, qbd[i2]], W=[pfs])
                        S.op('dve', lambda e, i2=i2: e.tensor_tensor(out=pT[i2][:], in0=pfs[:, 0:256], in1=dmask[:], op=ALU.mult), R=[pfs, dmask], W=[pT[i2]])
                        pfo = self.PF[2]
                        S.op('pe', lambda e, tok=tok: e.matmul(pfo[:, 0:128], lhsT=QD[:, tok], rhs=state_bf[:, :], start=True, stop=False), R=[QD, state_bf], W=[pfo])
                        for hh in range(2):
                            S.op('pe', lambda e, hh=hh, i2=i2: e.matmul(pfo[:, hh * 64:(hh + 1) * 64], lhsT=pT[i2][:, hh * 128:(hh + 1) * 128], rhs=vg[i2][:, hh * 64:(hh + 1) * 64],
                                                                      start=False, stop=(hh == 1)), R=[pT[i2], vg[i2]], W=[pfo])
                        pfu = self.PF[3]
                        S.op('pe', lambda e, i2=i2: e.matmul(pfu[:, 0:128], lhsT=kd[i2][:], rhs=vg[i2][:], start=True, stop=True), R=[kd[i2], vg[i2]], W=[pfu])
                        for hh in range(2):
                            pr = slice(hh * 64, (hh + 1) * 64)
                            cs = slice(hh * 64, (hh + 1) * 64)
                            S.op('dve', lambda e, hh=hh, pr=pr, cs=cs: e.scalar_tensor_tensor(out=state[pr, cs], in0=state[pr, cs], scalar=cd[pr, 0:1], in1=pfu[pr, cs],
                                                                                            op0=ALU.mult, op1=ALU.add), R=[state, cd, pfu], W=[state])
                        S.op('act', lambda e: e.activation(out=state_bf[:], in_=state[:], func=AF.Copy), R=[state], W=[state_bf])
                        S.op('act', lambda e: e.activation(out=o_sb[:], in_=pfo[:, 0:128], func=AF.Copy), R=[pfo], W=[o_sb])
                        self.head_norm(o_sb, cen, sq, st4, 2, 1e-5)
                        S.op('dve', lambda e, hp=hp: e.tensor_tensor(out=cen[:], in0=cen[:], in1=gnb[:, hp * 128:(hp + 1) * 128], op=ALU.mult), R=[cen, gnb], W=[cen])
                        S.op('dve', lambda e, i2=i2, c=c, hp=hp: e.tensor_tensor(out=ytok_all[:, c, hp * 128:(hp + 1) * 128], in0=cen[:], in1=sg[i2][:], op=ALU.mult),
                             R=[cen, sg[i2]], W=[ytok_all])
                    S.barrier(); S.emit()
            ystage = self.sb(st, "ystage", [128, 2, 512], BF16)
            for i in range(NT):
                self.y_store(0, i, _View(ytok_all, i), ystage, [])
            S.barrier(); S.emit()


    def rms_finalize(self, st, o_all, gain_b, ytile_idx, name):
        S = self.S
        ystage = self.sb(st, name + "ystage", [128, 2, 512], BF16)
        junk = self.sb(st, name + "junk", [128, 256], F32)
        ss = [self.sb(st, f"{name}ss{i}", [128, 1], F32) for i in range(2)]
        yt = [self.sb(st, f"{name}yt{i}", [128, 256], BF16) for i in range(2)]
        for i in range(NT):
            s_, y_ = ss[i % 2], yt[i % 2]
            S.op('act', lambda e, i=i, s_=s_: e.activation(out=junk[:], in_=o_all[:, i, :], func=AF.Square, accum_out=s_[:]), R=[o_all], W=[junk, s_])
            S.op('dve', lambda e, s_=s_: e.tensor_scalar(out=s_[:], in0=s_[:], scalar1=1.0 / 256, scalar2=1e-5, op0=ALU.mult, op1=ALU.add), R=[s_], W=[s_])
            S.op('act', lambda e, s_=s_: e.activation(out=s_[:], in_=s_[:], func=AF.Sqrt), R=[s_], W=[s_])
            S.op('dve', lambda e, s_=s_: e.reciprocal(out=s_[:], in_=s_[:]), R=[s_], W=[s_])
            S.op('dve', lambda e, i=i, s_=s_, y_=y_: e.scalar_tensor_tensor(out=y_[:], in0=o_all[:, i, :], scalar=s_[:, 0:1], in1=gain_b[:], op0=ALU.mult, op1=ALU.mult),
                 R=[o_all, s_, gain_b], W=[y_])
            self.y_store(ytile_idx, i, y_, ystage, [])

    def sb_phase(self, l):
        S = self.S
        with ExitStack() as st:
            ntri = self.load_const(st, 'ntri_ge', BF16); nones = self.load_const(st, 'nones', BF16)
            sbmask = self.load_const(st, 'sbmask', BF16)
            zer = self.sb(st, "sbzero", [128, 256], BF16)
            S.op('pool', lambda e: e.memset(zer[:], 0.0), W=[zer])
            onb = self.bcast_row(st, "sb_onb", self.sb_onorm[l, :], 256)
            o_all = self.sb(st, "sb_oall", [128, NT, 256], BF16)
            for hp in range(2):
                with ExitStack() as s2:
                    wm, wo = self.load_w(s2, "wm_sb", l, [f'sb_q{hp}', f'sb_k{hp}', 'sb_v'])
                    QT = self.sb(s2, "sbQT", [128, SEQ], BF16)
                    KM = [self.sb(s2, f"sbKM{i}", [128, SEQ], BF16) for i in range(2)]
                    V = self.sb(s2, "sbV", [128, NT, 128], BF16)
                    for i in range(2):
                        S.op('pool', lambda e, i=i: e.memset(KM[i][:], 0.0), W=[KM[i]])
                    for tg in range(8):
                        pf = self.PF[tg % 2]
                        self.proj_T(wm, wo[f'sb_q{hp}'], pf, tg)
                        S.op('act', lambda e, pf=pf, tg=tg: e.activation(out=QT[:, tg * 512:(tg + 1) * 512], in_=pf[:], func=AF.Copy, scale=0.125), R=[pf], W=[QT])
                        pf2 = self.PF[2 + tg % 2]
                        self.proj_T(wm, wo[f'sb_k{hp}'], pf2, tg)
                        S.op('dve', lambda e, pf2=pf2, tg=tg: e.tensor_copy(out=KM[0][0:64, tg * 512:(tg + 1) * 512], in_=pf2[0:64, :]), R=[pf2], W=[KM[0]])
                        S.op('act', lambda e, pf2=pf2, tg=tg: e.activation(out=KM[1][64:128, tg * 512:(tg + 1) * 512], in_=pf2[64:128, :], func=AF.Copy), R=[pf2], W=[KM[1]])
                    for i in range(NT):
                        pf = self.PF[i % 2]
                        self.proj_tok(wm, wo['sb_v'] + hp * 128, 128, pf[:, 0:128], pf, i)
                        S.op('dve', lambda e, pf=pf, i=i: e.tensor_copy(out=V[:, i, :], in_=pf[:, 0:128]), R=[pf], W=[V])
                    ebuf = [[self.sb(s2, f"sbe{h}{i}", [128, 512], F32) for i in range(2)] for h in range(2)]
                    spm = [[self.sb(s2, f"sbsp{h}{i}", [128, 512], BF16) for i in range(2)] for h in range(2)]
                    tbuf = [[self.sb(s2, f"sbt{h}{i}", [128, 512], F32) for i in range(2)] for h in range(2)]
                    abuf = [[self.sb(s2, f"sba{h}{i}", [128, 512], BF16) for i in range(2)] for h in range(2)]
                    racc = [self.sb(s2, f"sbracc{h}", [128, 512], F32) for h in range(2)]
                    cnt = 0
                    for g in range(8):
                        qs = slice(g * 512, (g + 1) * 512)
                        for hh in range(2):
                            po = self.PF[4 + hh]
                            S.op('pool', lambda e, hh=hh: e.memset(racc[hh][:], 0.0), W=[racc[hh]])
                            S.op('pe', lambda e, po=po: e.matmul(po[:, 0:256], lhsT=zer[:, 0:128], rhs=zer[:, 0:256], start=True, stop=False), R=[zer], W=[po])
                        nkb = 4 * g + 4
                        for kb in reversed(range(nkb)):
                            b2 = cnt % 2; cnt += 1
                            r = kb - 4 * g
                            ks = slice(kb * 128, (kb + 1) * 128)
                            HH = (0, 1)
                            E_ = [ebuf[h][b2] for h in HH]; SP_ = [spm[h][b2] for h in HH]; T_ = [tbuf[h][b2] for h in HH]; A_ = [abuf[h][b2] for h in HH]
                            PZ = [self.PF[0], self.PF[1]]; PC = [self.PF[2], self.PF[3]]; PO = [self.PF[4], self.PF[5]]
                            for hh in HH:
                                S.op('pe', lambda e, hh=hh, ks=ks, qs=qs: e.matmul(PZ[hh][:], lhsT=KM[hh][:, ks], rhs=QT[:, qs], start=True, stop=True), R=[KM[hh], QT], W=[PZ[hh]])
                            for hh in HH:
                                S.op('act', lambda e, hh=hh, E_=E_: e.activation(out=E_[hh][:], in_=PZ[hh][:], func=AF.Exp), R=[PZ[hh]], W=[E_[hh]])
                            for hh in HH:
                                S.op('act', lambda e, hh=hh, E_=E_, SP_=SP_: e.activation(out=SP_[hh][:], in_=E_[hh][:], func=AF.Ln, bias=1.0), R=[E_[hh]], W=[SP_[hh]])
                            if r >= 0:
                                for hh in HH:
                                    S.op('pool', lambda e, hh=hh, SP_=SP_, r=r: e.tensor_tensor(out=SP_[hh][:], in0=SP_[hh][:], in1=sbmask[:, r * 512:(r + 1) * 512], op=ALU.mult), R=[SP_[hh], sbmask], W=[SP_[hh]])
                            for hh in HH:
                                S.op('pe', lambda e, hh=hh, ks=ks, qs=qs: e.matmul(PC[hh][:], lhsT=KM[hh][:, ks], rhs=QT[:, qs], start=True, stop=False), R=[KM[hh], QT], W=[PC[hh]])
                                S.op('pe', lambda e, hh=hh, SP_=SP_: e.matmul(PC[hh][:], lhsT=ntri[:], rhs=SP_[hh][:], start=False, stop=True), R=[ntri, SP_[hh]], W=[PC[hh]])
                            for hh in HH:
                                S.op('dve', lambda e, hh=hh, T_=T_: e.tensor_tensor(out=T_[hh][:], in0=PC[hh][:], in1=racc[hh][:], op=ALU.add), R=[PC[hh], racc[hh]], W=[T_[hh]])
                            for hh in HH:
                                S.op('act', lambda e, hh=hh, T_=T_, A_=A_: e.activation(out=A_[hh][:], in_=T_[hh][:], func=AF.Exp), R=[T_[hh]], W=[A_[hh]])
                            if r >= 0:
                                for hh in HH:
                                    S.op('pool', lambda e, hh=hh, A_=A_, r=r: e.tensor_tensor(out=A_[hh][:], in0=A_[hh][:], in1=sbmask[:, r * 512:(r + 1) * 512], op=ALU.mult), R=[A_[hh], sbmask], W=[A_[hh]])
                            if kb > 0:
                                for hh in HH:
                                    S.op('pe', lambda e, hh=hh, SP_=SP_: e.matmul(PZ[hh][:], lhsT=nones[:], rhs=SP_[hh][:], start=True, stop=True), R=[nones, SP_[hh]], W=[PZ[hh]])
                                for hh in HH:
                                    S.op('dve', lambda e, hh=hh: e.tensor_tensor(out=racc[hh][:], in0=PZ[hh][:], in1=racc[hh][:], op=ALU.add), R=[PZ[hh], racc[hh]], W=[racc[hh]])
                            for hh in HH:
                                for qb in range(4):
                                    if r >= 0 and qb < r:
                                        continue
                                    S.op('pe', lambda e, qb=qb, hh=hh, A_=A_, kb=kb: e.matmul(PO[hh][:, qb * 64:(qb + 1) * 64], lhsT=A_[hh][:, qb * 128:(qb + 1) * 128], rhs=V[:, kb, hh * 64:(hh + 1) * 64],
                                                                                         start=False, stop=(kb == 0 and qb == 3)), R=[A_[hh], V], W=[PO[hh]])
                        for hh in range(2):
                            hcol = (hp * 2 + hh) * 64
                            po = self.PF[4 + hh]
                            S.op('act', lambda e, g=g, hcol=hcol, po=po: e.activation(out=o_all[:, 4 * g:4 * g + 4, hcol:hcol + 64], in_=po[:, 0:256].rearrange("p (q d) -> p q d", d=64), func=AF.Copy),
                                 R=[po], W=[o_all])
                    S.barrier(); S.emit()
            self.rms_finalize(st, o_all, onb, 6, "sbf")
            S.barrier(); S.emit()


    def issue_weight_cast(self, l):
        if l in self.conv_tb or self.flags.get('nomoe'):
            return
        tb = self.conv_tb[l] = TB()
        for e_ in range(32):
            r0 = (l * 32 + e_) * 128
            self.dma('pool', self.w1b_d[r0:r0 + 128, :].rearrange("p (k f) -> p k f", k=8), self.moe_w1[l, e_].rearrange("(k p) f -> p k f", p=128), W=[tb])
            self.dma('pool', self.w2b_d[r0:r0 + 128, :].rearrange("p (k f) -> p k f", k=8), self.moe_w2[l, e_].rearrange("(k p) f -> p k f", p=128), W=[tb])

    def _chk(self, n):
        if self.flags.get('rw_stop') == n:
            raise _Stop()

    def rwkv_phase(self, l):
        try:
            self.rwkv_phase_(l)
        except _Stop:
            pass

    def rwkv_phase_(self, l):
        S = self.S
        V3 = lambda ap, d=64: ap.rearrange("p (h d) -> p h d", d=d)
        with ExitStack() as st:
          try:
              wm, wo = self.load_w(st, "wm_rw", l, ['rw_rkv', 'rw_lora'])
              wmu = self.sb(st, "rw_wmu", [128, 8, 896], BF16)
              mub = self.bcast_row(st, "rw_mub", self.rwkv_mu[l, :], 896)
              S.op('dve', lambda e: e.tensor_tensor(out=wmu[:], in0=wm[:], in1=mub[:].unsqueeze(1).broadcast_to([128, 8, 896]), op=ALU.mult), R=[wm, mub], W=[wmu])
              S.op('pool', lambda e: e.tensor_tensor(out=wm[:], in0=wm[:], in1=wmu[:], op=ALU.subtract), R=[wm, wmu], W=[wm])
              lwbd = self.sb(st, "rw_lwbd", [128, 768], BF16)
              S.op('pool', lambda e: e.memset(lwbd[:], 0.0), W=[lwbd])
              for (r0, r1, c0) in ((0, 32, 0), (32, 64, 256), (64, 128, 512)):
                  self.dma('pool', lwbd[r0:r1, c0:c0 + 256], self.rwkv_lw[l, r0:r1, :], R=[lwbd], W=[lwbd])
              w0b = self.bcast_row(st, "rw_w0b", self.rwkv_w0[l, :], 256); a0b = self.bcast_row(st, "rw_a0b", self.rwkv_a0[l, :], 256)
              kkb = self.bcast_row(st, "rw_kkb", self.rwkv_kk[l, :], 256); kab = self.bcast_row(st, "rw_kab", self.rwkv_ka[l, :], 256)
              rkb = self.bcast_row(st, "rw_rkb", self.rwkv_rk[l, :], 256); lnb = self.bcast_row(st, "rw_lnb", self.rwkv_ln[l, :], 256)
              tri_le = self.load_const(st, 'tri_le', F32); lastc = self.load_const(st, 'last', F32)
              m_lt = self.load_const(st, 'tri_lt', F32); m_le = self.load_const(st, 'tri_le', F32); m_gt = self.load_const(st, 'tri_gt', F32)
              LT = self.sb(st, "rw_LT", [128, SEQ], BF16)
              for tg in range(8):
                  pf = self.PF[tg % 2]
                  self.proj_T(wm, wo['rw_lora'], pf, tg, shift=0, start=True, stop=False)
                  self.proj_T(wmu, wo['rw_lora'], pf, tg, shift=1, start=False, stop=True)
                  sl = slice(tg * 512, (tg + 1) * 512)
                  S.op('act', lambda e, pf=pf, sl=sl: e.activation(out=LT[0:32, sl], in_=pf[0:32, :], func=AF.Tanh), R=[pf], W=[LT])
                  S.op('act', lambda e, pf=pf, sl=sl: e.activation(out=LT[32:64, sl], in_=pf[32:64, :], func=AF.Copy), R=[pf], W=[LT])
                  S.op('act', lambda e, pf=pf, sl=sl: e.activation(out=LT[64:128, sl], in_=pf[64:128, :], func=AF.Sigmoid), R=[pf], W=[LT])
              self._chk(1)
              f32t = lambda n: self.sb(st, "rw_" + n, [128, 256], F32)
              bft = lambda n: self.sb(st, "rw_" + n, [128, 256], BF16)
              r_sb, k_sb, v_sb, a_sb, kk_sb, k2_sb, lw_sb, cum_sb, t1, t2, t3 = [f32t(n) for n in ('r', 'k', 'v', 'a', 'kk', 'k2', 'lw', 'cum', 't1', 't2', 't3')]
              gate_sb = f32t('gate')
              rt_b, kt_b, bt_b, at_b, v_bf, G_bf, U_bf = [bft(n) for n in ('rt', 'kt', 'bt', 'at', 'vbf', 'G', 'U')]
              st4 = self.sb(st, "rw_st4", [128, 4], F32)
              fm = self.sb(st, "rw_fm", [128, 8, 128], BF16)
              artbd = [self.sb(st, f"rw_artbd{p}", [128, 2, 256], BF16) for p in range(2)]
              btbd = [self.sb(st, f"rw_btbd{p}", [128, 2, 128], BF16) for p in range(2)]
              import os
              SKIP = os.environ.get('RW_SKIP', '').split(',')
              for p in range(2):
                  if 'b' in SKIP: break
                  S.op('pool', lambda e, p=p: e.memset(artbd[p][:], 0.0), W=[artbd[p]])
                  S.op('pool', lambda e, p=p: e.memset(btbd[p][:], 0.0), W=[btbd[p]])
              NU = [self.sb(st, f"rw_NU{i}", [128, 4, 128], F32) for i in range(2)]
              LL = [self.sb(st, f"rw_LL{i}", [128, 4, 128], F32) for i in range(2)]
              XX = [self.sb(st, f"rw_X{i}", [128, 4, 128], F32) for i in range(2)]
              G_f = self.sb(st, "rw_Gf", [128, 256], F32)
              RBm = self.sb(st, "rw_RB", [128, 4, 128], BF16); RKm = self.sb(st, "rw_RK", [128, 4, 128], BF16); MKm = self.sb(st, "rw_MK", [128, 4, 128], BF16)
              ST = [self.sb(st, f"rw_ST{p}", [128, 128], F32) for p in range(2)]
              STb = [self.sb(st, f"rw_STb{p}", [128, 128], BF16) for p in range(2)]
              ecl = self.sb(st, "rw_ecl", [128, 2], F32)
              for p in range(2):
                  if 'c' in SKIP: break
                  S.op('dve', lambda e, p=p: e.memset(ST[p][:], 0.0), W=[ST[p]])
                  S.op('dve', lambda e, p=p: e.memset(STb[p][:], 0.0), W=[STb[p]])
              o_sb = f32t('o'); cen = f32t('cen'); sq = f32t('sq')
              ystage = self.sb(st, "rw_ystage", [128, 2, 512], BF16)
              ytok = [self.sb(st, f"rw_ytok{i}", [128, 256], BF16) for i in range(2)]
              PF = self.PF
              for c in range(NT):
                  tok = slice(1 + c * 128, 1 + (c + 1) * 128); tokp = slice(c * 128, (c + 1) * 128)
                  for (pf, c0, n) in ((PF[0], 0, 512), (PF[1], 512, 256)):
                      if 'd' in SKIP: break
                      for k in range(8):
                          S.op('pe', lambda e, k=k, pf=pf, c0=c0, n=n, tok=tok: e.matmul(pf[:, 0:n], lhsT=self.hT[:, k, tok], rhs=wm[:, k, c0:c0 + n], start=(k == 0), stop=False), R=[wm, self.hT], W=[pf])
                      for k in range(8):
                          S.op('pe', lambda e, k=k, pf=pf, c0=c0, n=n, tokp=tokp: e.matmul(pf[:, 0:n], lhsT=self.hT[:, k, tokp], rhs=wmu[:, k, c0:c0 + n], start=False, stop=(k == 7)), R=[wmu, self.hT], W=[pf])
                  if 'e' in SKIP: self._chk(2)
                  S.op('act', lambda e: e.activation(out=r_sb[:], in_=PF[0][:, 0:256], func=AF.Copy), R=[PF[0]], W=[r_sb])
                  S.op('dve', lambda e: e.tensor_copy(out=k_sb[:], in_=PF[0][:, 256:512]), R=[PF[0]], W=[k_sb])
                  S.op('act', lambda e: e.activation(out=v_sb[:], in_=PF[1][:, 0:256], func=AF.Copy), R=[PF[1]], W=[v_sb])
                  if 'f' not in SKIP:
                      S.op('pool', lambda e: e.tensor_copy(out=v_bf[:], in_=v_sb[:]), R=[v_sb], W=[v_bf])
                  else:
                      S.op('dve', lambda e: e.tensor_copy(out=v_bf[:], in_=v_sb[:]), R=[v_sb], W=[v_bf])
                  self._chk(2)
                  S.op('pe', lambda e, c=c: e.matmul(PF[2][:, 0:512], lhsT=LT[:, c * 128:(c + 1) * 128], rhs=lwbd[:, 0:512], start=True, stop=True), R=[LT, lwbd], W=[PF[2]])
                  S.op('pe', lambda e, c=c: e.matmul(PF[3][:, 0:256], lhsT=LT[:, c * 128:(c + 1) * 128], rhs=lwbd[:, 512:768], start=True, stop=True), R=[LT, lwbd], W=[PF[3]])
                  S.op('act', lambda e: e.activation(out=gate_sb[:], in_=PF[3][:, 0:256], func=AF.Copy), R=[PF[3]], W=[gate_sb])
                  self._chk(3)
                  S.op('dve', lambda e: e.tensor_tensor(out=t1[:], in0=PF[2][:, 0:256], in1=w0b[:], op=ALU.add), R=[PF[2], w0b], W=[t1])
                  S.op('act', lambda e: e.activation(out=t1[:], in_=t1[:], func=AF.Sigmoid), R=[t1], W=[t1])
                  S.op('dve', lambda e: e.tensor_scalar(out=lw_sb[:], in0=t1[:], scalar1=-0.6065306597126334, scalar2=None, op0=ALU.mult), R=[t1], W=[lw_sb])
                  S.op('dve', lambda e: e.tensor_tensor(out=t2[:], in0=PF[2][:, 256:512], in1=a0b[:], op=ALU.add), R=[PF[2], a0b], W=[t2])
                  S.op('act', lambda e: e.activation(out=a_sb[:], in_=t2[:], func=AF.Sigmoid), R=[t2], W=[a_sb])
                  S.op('dve', lambda e: e.tensor_tensor(out=kk_sb[:], in0=k_sb[:], in1=kkb[:], op=ALU.mult), R=[k_sb, kkb], W=[kk_sb])
                  S.op('pool', lambda e: e.tensor_tensor(out=t3[:], in0=kk_sb[:], in1=kk_sb[:], op=ALU.mult), R=[kk_sb], W=[t3])
                  S.op('dve', lambda e: e.tensor_reduce(out=st4[:], in_=V3(t3[:]), axis=AX.X, op=ALU.add), R=[t3], W=[st4])
                  S.op('act', lambda e: e.activation(out=st4[:], in_=st4[:], func=AF.Sqrt), R=[st4], W=[st4])
                  S.op('dve', lambda e: e.tensor_scalar(out=st4[:], in0=st4[:], scalar1=1e-12, scalar2=None, op0=ALU.max), R=[st4], W=[st4])
                  S.op('dve', lambda e: e.reciprocal(out=st4[:], in_=st4[:]), R=[st4], W=[st4])
                  S.op('dve', lambda e: e.tensor_tensor(out=V3(kk_sb[:]), in0=V3(kk_sb[:]), in1=st4[:].unsqueeze(2).broadcast_to([128, 4, 64]), op=ALU.mult), R=[kk_sb, st4], W=[kk_sb])
                  S.op('dve', lambda e: e.scalar_tensor_tensor(out=t2[:], in0=a_sb[:], scalar=-1.0, in1=kab[:], op0=ALU.add, op1=ALU.mult), R=[a_sb, kab], W=[t2])
                  S.op('dve', lambda e: e.scalar_tensor_tensor(out=k2_sb[:], in0=t2[:], scalar=1.0, in1=k_sb[:], op0=ALU.add, op1=ALU.mult), R=[t2, k_sb], W=[k2_sb])
                  self._chk(4)
                  S.op('pe', lambda e: e.matmul(PF[4][:, 0:256], lhsT=tri_le[:], rhs=lw_sb[:], start=True, stop=True), R=[tri_le, lw_sb], W=[PF[4]])
                  S.op('act', lambda e: e.activation(out=cum_sb[:], in_=PF[4][:, 0:256], func=AF.Copy), R=[PF[4]], W=[cum_sb])
                  S.op('act', lambda e: e.activation(out=t1[:], in_=PF[4][:, 0:256], func=AF.Exp), R=[PF[4]], W=[t1])
                  S.op('act', lambda e: e.activation(out=t2[:], in_=PF[4][:, 0:256], func=AF.Exp, scale=-1.0), R=[PF[4]], W=[t2])
                  S.op('dve', lambda e: e.tensor_tensor(out=t3[:], in0=cum_sb[:], in1=lw_sb[:], op=ALU.subtract), R=[cum_sb, lw_sb], W=[t3])
                  S.op('act', lambda e: e.activation(out=t3[:], in_=t3[:], func=AF.Exp), R=[t3], W=[t3])
                  S.op('dve', lambda e: e.tensor_tensor(out=rt_b[:], in0=r_sb[:], in1=t1[:], op=ALU.mult), R=[r_sb, t1], W=[rt_b])
                  S.op('pool', lambda e: e.tensor_tensor(out=kt_b[:], in0=k2_sb[:], in1=t2[:], op=ALU.mult), R=[k2_sb, t2], W=[kt_b])
                  S.op('dve', lambda e: e.scalar_tensor_tensor(out=at_b[:], in0=kk_sb[:], scalar=-1.0, in1=t3[:], op0=ALU.mult, op1=ALU.mult), R=[kk_sb, t3], W=[at_b])
                  S.op('dve', lambda e: e.tensor_tensor(out=t3[:], in0=kk_sb[:], in1=a_sb[:], op=ALU.mult), R=[kk_sb, a_sb], W=[t3])
                  S.op('dve', lambda e: e.tensor_tensor(out=bt_b[:], in0=t3[:], in1=t2[:], op=ALU.mult), R=[t3, t2], W=[bt_b])
                  self._chk(5)
                  for p in range(2):
                      S.op('pe', lambda e, p=p: e.matmul(PF[5][:, p:p + 1], lhsT=cum_sb[:, p * 128:(p + 1) * 128], rhs=lastc[:, 0:1], start=True, stop=True), R=[cum_sb, lastc], W=[PF[5]])
                  S.op('act', lambda e: e.activation(out=ecl[:], in_=PF[5][:, 0:2], func=AF.Exp), R=[PF[5]], W=[ecl])
                  self._chk(6)
                  pb = self.PB[0]
                  for xi, src_ in enumerate((at_b, rt_b, bt_b, kt_b)):
                      for p in range(2):
                          j = xi * 2 + p
                          S.op('pe', lambda e, j=j, src_=src_, p=p: e.transpose(out=pb[:, j * 128:(j + 1) * 128], in_=src_[:, p * 128:(p + 1) * 128], identity=self.ident[:]), R=[src_, self.ident], W=[pb])
                  S.op('act', lambda e: e.activation(out=fm[:].rearrange("p j t -> p (j t)"), in_=pb[:], func=AF.Copy), R=[pb], W=[fm])
                  for p in range(2):
                      for hh in range(2):
                          pr = slice(hh * 64, (hh + 1) * 64)
                          S.op('pool', lambda e, p=p, hh=hh, pr=pr: e.tensor_copy(out=artbd[p][pr, hh, 0:128], in_=fm[pr, 0 + p, :]), R=[fm, artbd[p]], W=[artbd[p]])
                          S.op('pool', lambda e, p=p, hh=hh, pr=pr: e.tensor_copy(out=artbd[p][pr, hh, 128:256], in_=fm[pr, 2 + p, :]), R=[fm, artbd[p]], W=[artbd[p]])
                          S.op('pool', lambda e, p=p, hh=hh, pr=pr: e.tensor_copy(out=btbd[p][pr, hh, :], in_=fm[pr, 4 + p, :]), R=[fm, btbd[p]], W=[btbd[p]])
                  self._chk(7)
                  for p in range(2):
                      P1, P2, P3 = PF[0], PF[1], PF[2]
                      S.op('pe', lambda e, p=p: e.matmul(P1[:, 0:512], lhsT=fm[:, 4 + p, :], rhs=artbd[p][:].rearrange("p h c -> p (h c)"), start=True, stop=True), R=[fm, artbd[p]], W=[P1])
                      S.op('pe', lambda e, p=p: e.matmul(P2[:, 0:512], lhsT=fm[:, 6 + p, :], rhs=artbd[p][:].rearrange("p h c -> p (h c)"), start=True, stop=True), R=[fm, artbd[p]], W=[P2])
                      S.op('pe', lambda e, p=p: e.matmul(P3[:, 0:256], lhsT=fm[:, 0 + p, :], rhs=btbd[p][:].rearrange("p h c -> p (h c)"), start=True, stop=True), R=[fm, btbd[p]], W=[P3])
                      hs = slice(2 * p, 2 * p + 2)
                      v4 = lambda pf_: pf_[:, 0:512].rearrange("p (h w t) -> p h w t", h=2, w=2)
                      bc = lambda m: m[:].unsqueeze(1).broadcast_to([128, 2, 128])
                      S.op('dve', lambda e, hs=hs: e.tensor_tensor(out=NU[0][:, hs, :], in0=v4(P1)[:, :, 0, :], in1=bc(m_lt), op=ALU.mult), R=[P1, m_lt], W=[NU[0]])
                      S.op('dve', lambda e, hs=hs: e.tensor_tensor(out=RBm[:, hs, :], in0=v4(P1)[:, :, 1, :], in1=bc(m_le), op=ALU.mult), R=[P1, m_le], W=[RBm])
                      S.op('dve', lambda e, hs=hs: e.tensor_tensor(out=MKm[:, hs, :], in0=v4(P2)[:, :, 0, :], in1=bc(m_lt), op=ALU.mult), R=[P2, m_lt], W=[MKm])
                      S.op('dve', lambda e, hs=hs: e.tensor_tensor(out=RKm[:, hs, :], in0=v4(P2)[:, :, 1, :], in1=bc(m_le), op=ALU.mult), R=[P2, m_le], W=[RKm])
                      S.op('dve', lambda e, hs=hs: e.tensor_tensor(out=LL[0][:, hs, :], in0=P3[:, 0:256].rearrange("p (h t) -> p h t", h=2), in1=bc(m_gt), op=ALU.mult), R=[P3, m_gt], W=[LL[0]])
                  self._chk(8)
                  S.op('dve', lambda e: e.tensor_tensor(out=XX[0][:], in0=NU[0][:], in1=self.identf[:].unsqueeze(1).broadcast_to([128, 4, 128]), op=ALU.add), R=[NU[0], self.identf], W=[XX[0]])
                  cur = 0
                  for it in range(6):
                      nxt = 1 - cur
                      PL, PN, PX = PF[3], PF[4], PF[5]
                      for h in range(4):
                          S.op('pe', lambda e, h=h, cur=cur: e.matmul(PL[:, h * 128:(h + 1) * 128], lhsT=NU[cur][:, h, :], rhs=LL[cur][:, h, :], start=True, stop=True), R=[NU[cur], LL[cur]], W=[PL])
                      if it < 5:
                          for h in range(4):
                              S.op('pe', lambda e, h=h, cur=cur: e.matmul(PN[:, h * 128:(h + 1) * 128], lhsT=LL[cur][:, h, :], rhs=NU[cur][:, h, :], start=True, stop=True), R=[NU[cur], LL[cur]], W=[PN])
                      S.op('act', lambda e, nxt=nxt: e.activation(out=LL[nxt][:].rearrange("p h t -> p (h t)"), in_=PL[:], func=AF.Copy), R=[PL], W=[LL[nxt]])
                      if it < 5:
                          S.op('dve', lambda e, nxt=nxt: e.tensor_copy(out=NU[nxt][:].rearrange("p h t -> p (h t)"), in_=PN[:]), R=[PN], W=[NU[nxt]])
                      for h in range(4):
                          S.op('pe', lambda e, h=h, cur=cur, nxt=nxt: e.matmul(PX[:, h * 128:(h + 1) * 128], lhsT=LL[nxt][:, h, :], rhs=XX[cur][:, h, :], start=True, stop=True), R=[LL[nxt], XX[cur]], W=[PX])
                      S.op('dve', lambda e, cur=cur, nxt=nxt: e.tensor_tensor(out=XX[nxt][:].rearrange("p h t -> p (h t)"), in0=PX[:], in1=XX[cur][:].rearrange("p h t -> p (h t)"), op=ALU.add),
                           R=[PX, XX[cur]], W=[XX[nxt]])
                      cur = nxt
                  X = XX[cur]
                  self._chk(9)
                  PG, PU, PY, PS_ = PF[0], PF[1], PF[2], PF[3]
                  for p in range(2):
                      S.op('pe', lambda e, p=p: e.matmul(PG[:, p * 128:(p + 1) * 128], lhsT=fm[:, 0 + p, :], rhs=STb[p][:], start=True, stop=False), R=[fm, STb[p]], W=[PG])
                      for hh in range(2):
                          h = 2 * p + hh
                          S.op('pe', lambda e, h=h, hh=hh: e.matmul(PG[:, h * 64:(h + 1) * 64], lhsT=MKm[:, h, :], rhs=v_bf[:, h * 64:(h + 1) * 64], start=False, stop=(hh == 1)), R=[MKm, v_bf], W=[PG])
                  S.op('act', lambda e: e.activation(out=G_f[:], in_=PG[:, 0:256], func=AF.Copy), R=[PG], W=[G_f])
                  for h in range(4):
                      S.op('pe', lambda e, h=h, X=X: e.matmul(PU[:, h * 64:(h + 1) * 64], lhsT=X[:, h, :], rhs=G_f[:, h * 64:(h + 1) * 64], start=True, stop=True), R=[X, G_f], W=[PU])
                  S.op('dve', lambda e: e.tensor_copy(out=U_bf[:], in_=PU[:, 0:256]), R=[PU], W=[U_bf])
                  for p in range(2):
                      S.op('pe', lambda e, p=p: e.matmul(PY[:, p * 128:(p + 1) * 128], lhsT=fm[:, 2 + p, :], rhs=STb[p][:], start=True, stop=False), R=[fm, STb[p]], W=[PY])
                      for hh in range(2):
                          h = 2 * p + hh
                          S.op('pe', lambda e, h=h: e.matmul(PY[:, h * 64:(h + 1) * 64], lhsT=RBm[:, h, :], rhs=U_bf[:, h * 64:(h + 1) * 64], start=False, stop=False), R=[RBm, U_bf], W=[PY])
                          S.op('pe', lambda e, h=h, hh=hh: e.matmul(PY[:, h * 64:(h + 1) * 64], lhsT=RKm[:, h, :], rhs=v_bf[:, h * 64:(h + 1) * 64], start=False, stop=(hh == 1)), R=[RKm, v_bf], W=[PY])
                  S.op('act', lambda e: e.activation(out=o_sb[:], in_=PY[:, 0:256], func=AF.Copy), R=[PY], W=[o_sb])
                  for p in range(2):
                      cs_ = slice(p * 128, (p + 1) * 128)
                      S.op('pe', lambda e, cs_=cs_: e.matmul(PS_[:, cs_], lhsT=bt_b[:, cs_], rhs=U_bf[:, cs_], start=True, stop=False), R=[bt_b, U_bf], W=[PS_])
                      S.op('pe', lambda e, cs_=cs_: e.matmul(PS_[:, cs_], lhsT=kt_b[:, cs_], rhs=v_bf[:, cs_], start=False, stop=True), R=[kt_b, v_bf], W=[PS_])
                      for hh in range(2):
                          pr = slice(hh * 64, (hh + 1) * 64); cc = slice(hh * 64, (hh + 1) * 64); pc_ = slice(p * 128 + hh * 64, p * 128 + (hh + 1) * 64)
                          S.op('dve', lambda e, p=p, pr=pr, cc=cc, pc_=pc_: e.scalar_tensor_tensor(out=ST[p][pr, cc], in0=ST[p][pr, cc], scalar=1.0, in1=PS_[pr, pc_], op0=ALU.mult, op1=ALU.add),
                               R=[ST[p], PS_], W=[ST[p]])
                          S.op('dve', lambda e, p=p, pr=pr, cc=cc: e.tensor_scalar(out=ST[p][pr, cc], in0=ST[p][pr, cc], scalar1=ecl[pr, p:p + 1], scalar2=None, op0=ALU.mult), R=[ST[p], ecl], W=[ST[p]])
                      S.op('act', lambda e, p=p: e.activation(out=STb[p][:], in_=ST[p][:], func=AF.Copy), R=[ST[p]], W=[STb[p]])
                  self._chk(10)
                  self.head_norm(o_sb, cen, sq, st4, 4, 64e-5)
                  S.op('dve', lambda e: e.tensor_tensor(out=cen[:], in0=cen[:], in1=lnb[:], op=ALU.mult), R=[cen, lnb], W=[cen])
                  S.op('dve', lambda e: e.tensor_tensor(out=t1[:], in0=r_sb[:], in1=k2_sb[:], op=ALU.mult), R=[r_sb, k2_sb], W=[t1])
                  S.op('dve', lambda e: e.tensor_tensor(out=t1[:], in0=t1[:], in1=rkb[:], op=ALU.mult), R=[t1, rkb], W=[t1])
                  S.op('dve', lambda e: e.tensor_reduce(out=st4[:], in_=V3(t1[:]), axis=AX.X, op=ALU.add), R=[t1], W=[st4])
                  S.op('dve', lambda e: e.tensor_tensor(out=V3(t1[:]), in0=V3(v_sb[:]), in1=st4[:].unsqueeze(2).broadcast_to([128, 4, 64]), op=ALU.mult), R=[v_sb, st4], W=[t1])
                  S.op('dve', lambda e: e.tensor_tensor(out=cen[:], in0=cen[:], in1=t1[:], op=ALU.add), R=[cen, t1], W=[cen])
                  yt_ = ytok[c % 2]
                  S.op('dve', lambda e, yt_=yt_: e.tensor_tensor(out=yt_[:], in0=cen[:], in1=gate_sb[:], op=ALU.mult), R=[cen, gate_sb], W=[yt_])
                  self.y_store(2, c, yt_, ystage, [])
              S.barrier(); S.emit()
          except _Stop:
            S.barrier(); S.emit()


    def rope_apply(self, A, B_, C, Sg):
        S = self.S
        S.op('dve', lambda e: e.tensor_tensor(out=A[:], in0=A[:], in1=C[:], op=ALU.mult), R=[A, C], W=[A])
        S.op('pool', lambda e: e.tensor_tensor(out=B_[:], in0=B_[:], in1=Sg[:], op=ALU.mult), R=[B_, Sg], W=[B_])
        S.op('dve', lambda e: e.tensor_tensor(out=A[:], in0=A[:], in1=B_[:], op=ALU.add), R=[A, B_], W=[A])

    def dsa_phase(self, l):
        S = self.S
        PF = self.PF
        NBIS = 14
        with ExitStack() as st:
            QT = [self.sb(st, f"dsQT{i}", [128, SEQ], BF16) for i in range(2)]
            kT = self.sb(st, "dskT", [128, SEQ], BF16)
            hm2 = self.load_const(st, 'hm2', F32); hm4 = self.load_const(st, 'hm4', F32)
            vext = self.sb(st, "ds_vext", [128, NT, 65], BF16)
            wsc = self.sb(st, "ds_wsc", [128, NT, 8], F32)
            with ExitStack() as s1:
                qiT = [self.sb(s1, f"dsqiT{i}", [128, SEQ], BF16) for i in range(2)]
                kiT = self.sb(s1, "dskiT", [128, SEQ], BF16)
                with ExitStack() as s2:
                    wm, wo = self.load_w(s2, "wm_ds", l, ['ds_cq', 'ds_k', 'ds_ks', 'ds_ki', 'ds_kis', 'ds_vw'])
                    wup = self.sb(s2, "ds_wup", [128, 4, 256], BF16)
                    for j, src_ in enumerate((self.dsa_wq_up, self.dsa_wqs_up, self.dsa_wqi_up, self.dsa_wqis_up)):
                        self.dma('pool', wup[:, j, :], src_[l], W=[wup])
                    qn = self.sb(s2, "ds_qn", [128, 1], F32)
                    self.dma('sp', qn[:], self.dsa_qnorm[l, :].rearrange("(p o) -> p o", o=1), W=[qn], allow_slow_non_contiguous=True)
                    onesf = self.sb(s2, "ds_ones", [128, 128], F32)
                    S.op('dve', lambda e: e.memset(onesf[:], 1.0), W=[onesf])
                    cqn = self.sb(s2, "ds_cqn", [128, SEQ], BF16)
                    cqf = self.sb(s2, "ds_cqf", [128, 512], F32); cq2 = self.sb(s2, "ds_cq2", [128, 512], F32); rs = self.sb(s2, "ds_rs", [128, 512], F32)
                    for tg in range(8):
                        pf = PF[tg % 2]; pf2 = PF[2 + tg % 2]
                        self.proj_T(wm, wo['ds_cq'], pf, tg)
                        S.op('act', lambda e, pf=pf: e.activation(out=cqf[:], in_=pf[:], func=AF.Copy), R=[pf], W=[cqf])
                        S.op('dve', lambda e: e.tensor_tensor(out=cq2[:], in0=cqf[:], in1=cqf[:], op=ALU.mult), R=[cqf], W=[cq2])
                        S.op('pe', lambda e, pf2=pf2: e.matmul(pf2[:], lhsT=onesf[:], rhs=cq2[:], start=True, stop=True), R=[onesf, cq2], W=[pf2])
                        S.op('dve', lambda e, pf2=pf2: e.tensor_scalar(out=rs[:], in0=pf2[:], scalar1=1.0 / 128, scalar2=1e-5, op0=ALU.mult, op1=ALU.add), R=[pf2], W=[rs])
                        S.op('act', lambda e: e.activation(out=rs[:], in_=rs[:], func=AF.Sqrt), R=[rs], W=[rs])
                        S.op('dve', lambda e: e.reciprocal(out=rs[:], in_=rs[:]), R=[rs], W=[rs])
                        S.op('dve', lambda e, tg=tg: e.scalar_tensor_tensor(out=cqn[:, tg * 512:(tg + 1) * 512], in0=cqf[:], scalar=qn[:, 0:1], in1=rs[:], op0=ALU.mult, op1=ALU.mult),
                             R=[cqf, qn, rs], W=[cqn])
                    S.op('pool', lambda e: e.memset(vext[:, :, 64:65], 1.0), W=[vext])
                    for i in range(NT):
                        pf = PF[i % 2]
                        self.proj_tok(wm, wo['ds_vw'], 72, pf[:, 0:72], pf, i)
                        S.op('act', lambda e, pf=pf, i=i: e.activation(out=vext[:, i, 0:64], in_=pf[:, 0:64], func=AF.Copy), R=[pf], W=[vext])
                        S.op('dve', lambda e, pf=pf, i=i: e.tensor_scalar(out=wsc[:, i, :], in0=pf[:, 64:72], scalar1=1.0 / 16, scalar2=None, op0=ALU.mult), R=[pf], W=[wsc])
                    tmpA = self.sb(s2, "ds_tmpA", [128, SEQ], BF16)

                    def up_proj(j, cols, dst):
                        for tg in range(8):
                            pf = PF[tg % 2]
                            S.op('pe', lambda e, pf=pf, tg=tg: e.matmul(pf[:], lhsT=wup[:, j, cols], rhs=cqn[:, tg * 512:(tg + 1) * 512], start=True, stop=True), R=[wup, cqn], W=[pf])
                            S.op('act', lambda e, pf=pf, tg=tg: e.activation(out=dst[:, tg * 512:(tg + 1) * 512], in_=pf[:], func=AF.Copy), R=[pf], W=[dst])

                    def in_proj(name, dst):
                        for tg in range(8):
                            pf = PF[2 + tg % 2]
                            self.proj_T(wm, wo[name], pf, tg)
                            S.op('dve', lambda e, pf=pf, tg=tg: e.tensor_copy(out=dst[:, tg * 512:(tg + 1) * 512], in_=pf[:]), R=[pf], W=[dst])
                    with ExitStack() as s3:
                        C, Sg = self.rope_tables(s3, 'rope_dq', 'rdq')
                        for pr_ in range(2):
                            cols = slice(pr_ * 128, (pr_ + 1) * 128)
                            up_proj(0, cols, QT[pr_]); up_proj(1, cols, tmpA)
                            self.rope_apply(QT[pr_], tmpA, C, Sg)
                        in_proj('ds_k', kT); in_proj('ds_ks', tmpA)
                        self.rope_apply(kT, tmpA, C, Sg)
                        S.barrier(); S.emit()
                    with ExitStack() as s3:
                        C, Sg = self.rope_tables(s3, 'rope_di', 'rdi')
                        for t2 in range(2):
                            cols = slice(t2 * 128, (t2 + 1) * 128)
                            up_proj(2, cols, qiT[t2]); up_proj(3, cols, tmpA)
                            self.rope_apply(qiT[t2], tmpA, C, Sg)
                        in_proj('ds_ki', kiT); in_proj('ds_kis', tmpA)
                        self.rope_apply(kiT, tmpA, C, Sg)
                        S.barrier(); S.emit()
                self.issue_weight_cast(l)
                with ExitStack() as s2:
                    score = self.sb(s2, "ds_score", [128, SEQ], F32)
                    mask = self.sb(s2, "ds_mask", [128, SEQ], BF16)
                    junk = self.sb(s2, "ds_junk", [128, SEQ], BF16)
                    relb = [self.sb(s2, f"ds_rel{i}", [128, 512], F32) for i in range(2)]
                    negm = self.load_const(s2, 'negmask', F32)
                    mT = [self.sb(s2, f"ds_mT{i}", [128, NT, 128], BF16) for i in range(2)]
                    sc = {n: self.sb(s2, "ds_" + n, [128, 1], F32) for n in ('lo', 'hi', 'mid', 'cnt', 'ge', 'd')}
                    cntr = 0
                    qm = [self.sb(s2, f"ds_qm{i}", [128, 8, 128], BF16) for i in range(2)]
                    for tb in range(NT):
                        Sc = (tb + 1) * 128
                        tsl = slice(tb * 128, (tb + 1) * 128)
                        qm_ = qm[tb % 2]
                        for ih in range(8):
                            S.op('pool', lambda e, ih=ih, qm_=qm_, tsl=tsl: e.tensor_scalar(out=qm_[:, ih, :], in0=qiT[ih // 4][:, tsl], scalar1=hm4[:, ih % 4:ih % 4 + 1], scalar2=None, op0=ALU.mult),
                                 R=[qiT[ih // 4], hm4], W=[qm_])
                        for sg in range((Sc + 511) // 512):
                            w = min(512, Sc - sg * 512)
                            ssl = slice(sg * 512, sg * 512 + w)
                            for ih in range(8):
                                t2, j = ih // 4, ih % 4
                                pf = PF[cntr % 4]; rl = relb[cntr % 2]; cntr += 1
                                S.op('pe', lambda e, pf=pf, ih=ih, qm_=qm_, ssl=ssl, w=w: e.matmul(pf[:, 0:w], lhsT=qm_[:, ih, :], rhs=kiT[:, ssl], start=True, stop=True),
                                     R=[qm_, kiT], W=[pf])
                                S.op('act', lambda e, pf=pf, rl=rl, w=w: e.activation(out=rl[:, 0:w], in_=pf[:, 0:w], func=AF.Relu), R=[pf], W=[rl])
                                if ih == 0:
                                    S.op('dve', lambda e, rl=rl, w=w, ssl=ssl, tb=tb, ih=ih: e.tensor_scalar(out=score[:, ssl], in0=rl[:, 0:w], scalar1=wsc[:, tb, ih:ih + 1], scalar2=None, op0=ALU.mult),
                                         R=[rl, wsc], W=[score])
                                else:
                                    S.op('dve', lambda e, rl=rl, w=w, ssl=ssl, tb=tb, ih=ih: e.scalar_tensor_tensor(out=score[:, ssl], in0=rl[:, 0:w], scalar=wsc[:, tb, ih:ih + 1], in1=score[:, ssl],
                                                                                                              op0=ALU.mult, op1=ALU.add), R=[rl, wsc, score], W=[score])
                        S.op('dve', lambda e, tsl=tsl: e.tensor_tensor(out=score[:, tsl], in0=score[:, tsl], in1=negm[:], op=ALU.add), R=[score, negm], W=[score])
                        if tb >= 2:
                            S.op('dve', lambda e, Sc=Sc: e.tensor_reduce(out=sc['hi'][:], in_=score[:, 0:Sc], axis=AX.X, op=ALU.max), R=[score], W=[sc['hi']])
                            S.op('dve', lambda e: e.tensor_reduce(out=sc['lo'][:], in_=score[:, 0:256], axis=AX.X, op=ALU.min), R=[score], W=[sc['lo']])
                            S.op('dve', lambda e: e.tensor_tensor(out=sc['mid'][:], in0=sc['lo'][:], in1=sc['hi'][:], op=ALU.add), R=[sc['lo'], sc['hi']], W=[sc['mid']])
                            S.op('dve', lambda e: e.tensor_scalar(out=sc['mid'][:], in0=sc['mid'][:], scalar1=0.5, scalar2=None, op0=ALU.mult), R=[sc['mid']], W=[sc['mid']])
                            S.op('dve', lambda e: e.tensor_tensor(out=sc['d'][:], in0=sc['hi'][:], in1=sc['lo'][:], op=ALU.subtract), R=[sc['lo'], sc['hi']], W=[sc['d']])
                            S.op('dve', lambda e: e.tensor_scalar(out=sc['d'][:], in0=sc['d'][:], scalar1=0.25, scalar2=None, op0=ALU.mult), R=[sc['d']], W=[sc['d']])
                            for it in range(NBIS):
                                S.op('dve', lambda e, Sc=Sc: e.tensor_scalar(out=junk[:, 0:Sc], in0=score[:, 0:Sc], scalar1=sc['mid'][:, 0:1], scalar2=None, op0=ALU.is_ge, op1=ALU.add,
                                                                            accum_out=sc['cnt'][:]), R=[score, sc['mid']], W=[junk, sc['cnt']])
                                S.op('dve', lambda e: e.tensor_scalar(out=sc['ge'][:], in0=sc['cnt'][:], scalar1=255.5, scalar2=2.0, op0=ALU.is_ge, op1=ALU.mult), R=[sc['cnt']], W=[sc['ge']])
                                S.op('dve', lambda e: e.scalar_tensor_tensor(out=sc['ge'][:], in0=sc['ge'][:], scalar=-1.0, in1=sc['d'][:], op0=ALU.add, op1=ALU.mult), R=[sc['ge'], sc['d']], W=[sc['ge']])
                                S.op('dve', lambda e: e.tensor_tensor(out=sc['mid'][:], in0=sc['mid'][:], in1=sc['ge'][:], op=ALU.add), R=[sc['mid'], sc['ge']], W=[sc['mid']])
                                S.op('dve', lambda e: e.tensor_scalar(out=sc['d'][:], in0=sc['d'][:], scalar1=0.5, scalar2=None, op0=ALU.mult), R=[sc['d']], W=[sc['d']])
                            S.op('dve', lambda e: e.scalar_tensor_tensor(out=sc['lo'][:], in0=sc['d'][:], scalar=-2.0, in1=sc['mid'][:], op0=ALU.mult, op1=ALU.add), R=[sc['d'], sc['mid']], W=[sc['lo']])
                        else:
                            S.op('dve', lambda e: e.memset(sc['lo'][:], -1e29), W=[sc['lo']])
                        S.op('dve', lambda e, Sc=Sc: e.tensor_scalar(out=mask[:, 0:Sc], in0=score[:, 0:Sc], scalar1=sc['lo'][:, 0:1], scalar2=None, op0=ALU.is_ge), R=[score, sc['lo']], W=[mask])
                        mt = mT[tb % 2]
                        for s0 in range(0, tb + 1, 8):
                            nb_ = min(8, tb + 1 - s0)
                            pb = self.PB[(s0 // 8) % 2]
                            for q in range(nb_):
                                S.op('pe', lambda e, pb=pb, q=q, s0=s0: e.transpose(out=pb[:, q * 128:(q + 1) * 128], in_=mask[:, (s0 + q) * 128:(s0 + q + 1) * 128], identity=self.ident[:]),
                                     R=[mask, self.ident], W=[pb])
                            S.op('act', lambda e, pb=pb, mt=mt, s0=s0, nb_=nb_: e.activation(out=mt[:, s0:s0 + nb_, :].rearrange("p b t -> p (b t)"), in_=pb[:, 0:nb_ * 128], func=AF.Copy), R=[pb], W=[mt])
                        self.dma('sp', self.maskT_d[tb, :, 0:tb + 1, :], mt[:, 0:tb + 1, :], R=[mt], W=[TB()])
                    S.barrier(); S.emit()
            with ExitStack() as s2:
                onb = self.bcast_row(s2, "ds_onb", self.dsa_onorm[l, :], 256)
                o_all = self.sb(s2, "ds_oall", [128, NT, 256], BF16)
                mT = [self.sb(s2, f"ds_mT2{i}", [128, NT, 128], BF16) for i in range(2)]
                ebuf = [self.sb(s2, f"ds_e{i}", [128, 4, 128], BF16) for i in range(2)]
                pbuf = [self.sb(s2, f"ds_p{i}", [128, 4, 128], BF16) for i in range(2)]
                zer = self.sb(s2, "ds_zero", [128, 260], BF16)
                S.op('pool', lambda e: e.memset(zer[:], 0.0), W=[zer])
                osb = self.sb(s2, "ds_osb", [128, 4, 65], F32); rden = self.sb(s2, "ds_rden", [128, 4], F32)
                cnt = 0
                QM = [self.sb(s2, f"ds_QM{i}", [128, 4, 128], BF16) for i in range(2)]
                for tb in range(NT):
                    tsl = slice(tb * 128, (tb + 1) * 128)
                    mt = mT[tb % 2]
                    QM_ = QM[tb % 2]
                    for h in range(4):
                        S.op('pool', lambda e, h=h, QM_=QM_, tsl=tsl: e.tensor_scalar(out=QM_[:, h, :], in0=QT[h // 2][:, tsl], scalar1=hm2[:, h % 2:h % 2 + 1], scalar2=None, op0=ALU.mult),
                             R=[QT[h // 2], hm2], W=[QM_])
                    self.dma('act', mt[:, 0:tb + 1, :], self.maskT_d[tb, :, 0:tb + 1, :], W=[mt])
                    po = PF[4 + tb % 2]
                    S.op('pe', lambda e, po=po: e.matmul(po[:, 0:260], lhsT=zer[:, 0:128], rhs=zer[:, 0:260], start=True, stop=False), R=[zer], W=[po])
                    for sb_ in range(tb + 1):
                        b2 = cnt % 2; cnt += 1
                        ssl = slice(sb_ * 128, (sb_ + 1) * 128)
                        pl = PF[b2 * 2]
                        S.op('pe', lambda e, pl=pl, ssl=ssl, QM_=QM_: e.matmul(pl[:], lhsT=kT[:, ssl], rhs=QM_[:].rearrange("p h t -> p (h t)"), start=True, stop=True),
                             R=[kT, QM_], W=[pl])
                        S.op('act', lambda e, pl=pl, b2=b2: e.activation(out=ebuf[b2][:].rearrange("p h t -> p (h t)"), in_=pl[:], func=AF.Exp, scale=0.125), R=[pl], W=[ebuf[b2]])
                        S.op('dve', lambda e, b2=b2, mt=mt, sb_=sb_: e.tensor_tensor(out=pbuf[b2][:], in0=ebuf[b2][:], in1=mt[:, sb_, :].unsqueeze(1).broadcast_to([128, 4, 128]), op=ALU.mult),
                             R=[ebuf[b2], mt], W=[pbuf[b2]])
                        for h in range(4):
                            S.op('pe', lambda e, h=h, po=po, b2=b2, sb_=sb_, tb=tb: e.matmul(po[:, h * 65:(h + 1) * 65], lhsT=pbuf[b2][:, h, :], rhs=vext[:, sb_, :], start=False, stop=(sb_ == tb and h == 3)),
                                 R=[pbuf[b2], vext], W=[po])
                    S.op('act', lambda e, po=po: e.activation(out=osb[:].rearrange("p h d -> p (h d)"), in_=po[:, 0:260], func=AF.Copy), R=[po], W=[osb])
                    S.op('dve', lambda e: e.reciprocal(out=rden[:], in_=osb[:, :, 64]), R=[osb], W=[rden])
                    S.op('dve', lambda e, tb=tb: e.tensor_tensor(out=o_all[:, tb, :].rearrange("p (h d) -> p h d", d=64), in0=osb[:, :, 0:64], in1=rden[:].unsqueeze(2).broadcast_to([128, 4, 64]), op=ALU.mult),
                         R=[osb, rden], W=[o_all])
                S.barrier(); S.emit()
                self.rms_finalize(s2, o_all, onb, 4, "dsf")
                S.barrier(); S.emit()

    def head_norm(self, o_sb, cen, sq, st4, nh, eps):
        S = self.S
        v3 = lambda b: b[:, 0:nh * 64].rearrange("p (h d) -> p h d", d=64)
        S.op('dve', lambda e: e.tensor_reduce(out=st4[:, 0:nh], in_=v3(o_sb), axis=AX.X, op=ALU.add), R=[o_sb], W=[st4])
        S.op('dve', lambda e: e.tensor_scalar(out=st4[:, 0:nh], in0=st4[:, 0:nh], scalar1=1.0 / 64, scalar2=None, op0=ALU.mult), R=[st4], W=[st4])
        S.op('dve', lambda e: e.tensor_tensor(out=v3(cen), in0=v3(o_sb), in1=st4[:, 0:nh].unsqueeze(2).broadcast_to([128, nh, 64]), op=ALU.subtract), R=[o_sb, st4], W=[cen])
        S.op('dve', lambda e: e.tensor_tensor(out=v3(sq), in0=v3(cen), in1=v3(cen), op=ALU.mult), R=[cen], W=[sq])
        S.op('dve', lambda e: e.tensor_reduce(out=st4[:, 0:nh], in_=v3(sq), axis=AX.X, op=ALU.add), R=[sq], W=[st4])
        S.op('dve', lambda e: e.tensor_scalar(out=st4[:, 0:nh], in0=st4[:, 0:nh], scalar1=1.0 / 64, scalar2=eps, op0=ALU.mult, op1=ALU.add), R=[st4], W=[st4])
        S.op('act', lambda e: e.activation(out=st4[:, 0:nh], in_=st4[:, 0:nh], func=AF.Sqrt), R=[st4], W=[st4])
        S.op('dve', lambda e: e.reciprocal(out=st4[:, 0:nh], in_=st4[:, 0:nh]), R=[st4], W=[st4])
        S.op('dve', lambda e: e.tensor_tensor(out=v3(cen), in0=v3(cen), in1=st4[:, 0:nh].unsqueeze(2).broadcast_to([128, nh, 64]), op=ALU.mult), R=[cen, st4], W=[cen])

    def wout_phase(self, l):
        S = self.S
        src = self.x if l == 0 else self.xres
        with ExitStack() as st:
            YT = self.sb(st, "YT", [128, 8, SEQ], BF16)
            for j in range(8):
                self.dma('sp' if j % 2 == 0 else 'act', YT[:, j, :], self.yT_d[j], W=[YT])
            wo_sb = self.sb(st, "wo_sb", [128, 8, D], BF16)
            for f in range(8):
                self.dma('pool', wo_sb[:, f, :], self.w_out[l, f * 128:(f + 1) * 128, :], W=[wo_sb])
            xt = [self.sb(st, f"wx{i}", [128, D], F32) for i in range(2)]
            tmp = self.sb(st, "wtmp", [128, D], F32)
            for i in range(NT):
                x_ = xt[i % 2]
                self.dma('sp' if i % 2 == 0 else 'act', x_[:], src[i * 128:(i + 1) * 128, :], W=[x_])
                for hf in range(2):
                    pf = self.PF[(i % 2) * 2 + hf]
                    for f in range(8):
                        S.op('pe', lambda e, f=f, pf=pf, i=i, hf=hf: e.matmul(pf[:], lhsT=YT[:, f, i * 128:(i + 1) * 128], rhs=wo_sb[:, f, hf * 512:(hf + 1) * 512], start=(f == 0), stop=(f == 7)),
                             R=[YT, wo_sb], W=[pf])
                    S.op('dve', lambda e, pf=pf, hf=hf: e.tensor_tensor(out=tmp[:, hf * 512:(hf + 1) * 512], in0=pf[:], in1=self.gb[0][:, hf * 512:(hf + 1) * 512], op=ALU.mult),
                         R=[pf, self.gb[0]], W=[tmp])
                S.op('pool', lambda e, x_=x_: e.tensor_tensor(out=x_[:], in0=x_[:], in1=tmp[:], op=ALU.add), R=[x_, tmp], W=[x_])
                self.dma('sp', self.xres[i * 128:(i + 1) * 128, :], x_[:], R=[x_], W=[TB()])
            S.barrier(); S.emit()

    def moe_phase(self, l):
        S = self.S; PF = self.PF; nc = self.nc
        with ExitStack() as st:
            off_all = self.sb(st, "mo_off", [128, NT, 4], I32)
            gsel_all = self.sb(st, "mo_gsel", [128, NT, 4], F32)
            widx = self.sb(st, "mo_widx", [128, NBLK, 8], I32)
            OH = self.sb(st, "mo_OH", [32, NBLK], F32)
            ones_bf = self.sb(st, "mo_ones", [128, 512], BF16)
            S.op('dve', lambda e: e.memset(ones_bf[:], 1.0), W=[ones_bf])
            with ExitStack() as s1:
                self.hT = self.sb(s1, "hT2", [128, 8, SEQ + 1], BF16)
                self.norm_phase(l, 1)
                rw = self.sb(s1, "mo_rw", [128, 8, 32], BF16)
                self.dma('pool', rw[:], self.router_w[l].rearrange("(k p) e -> p k e", p=128), W=[rw])
                rbb = self.bcast_row(s1, "mo_rbb", self.router_b[l, :], 32)
                M_bf = self.sb(s1, "mo_Mbf", [128, NT, 32], BF16); M32 = self.sb(s1, "mo_M32", [128, NT, 32], F32)
                G_all = self.sb(s1, "mo_G", [128, NT, 32], F32)
                lg = self.sb(s1, "mo_lg", [128, 32], F32); ex = self.sb(s1, "mo_ex", [128, 32], F32); junk32 = self.sb(s1, "mo_junk", [128, 32], F32)
                top8 = self.sb(s1, "mo_top8", [128, 8], F32); sc1 = self.sb(s1, "mo_sc1", [128, 2], F32)
                for i in range(NT):
                    pf = PF[i % 2]
                    for k in range(8):
                        S.op('pe', lambda e, k=k, pf=pf, i=i: e.matmul(pf[:, 0:32], lhsT=self.hT[:, k, 1 + i * 128: 1 + (i + 1) * 128], rhs=rw[:, k, :], start=(k == 0), stop=(k == 7)),
                             R=[self.hT, rw], W=[pf])
                    S.op('dve', lambda e, pf=pf: e.tensor_tensor(out=lg[:], in0=pf[:, 0:32], in1=rbb[:], op=ALU.add), R=[pf, rbb], W=[lg])
                    S.op('dve', lambda e: e.max(out=top8[:], in_=lg[:]), R=[lg], W=[top8])
                    S.op('dve', lambda e, i=i: e.tensor_scalar(out=M32[:, i, :], in0=lg[:], scalar1=top8[:, 3:4], scalar2=None, op0=ALU.is_ge), R=[lg, top8], W=[M32])
                    S.op('pool', lambda e, i=i: e.tensor_copy(out=M_bf[:, i, :], in_=M32[:, i, :]), R=[M32], W=[M_bf])
                    S.op('dve', lambda e: e.tensor_scalar(out=sc1[:, 0:1], in0=top8[:, 0:1], scalar1=-1.0, scalar2=None, op0=ALU.mult), R=[top8], W=[sc1])
                    S.op('act', lambda e: e.activation(out=ex[:], in_=lg[:], func=AF.Exp, bias=sc1[:, 0:1]), R=[lg, sc1], W=[ex])
                    S.op('dve', lambda e, i=i: e.scalar_tensor_tensor(out=ex[:], in0=ex[:], scalar=1.0, in1=M32[:, i, :], op0=ALU.mult, op1=ALU.mult, accum_out=sc1[:, 1:2]),
                         R=[ex, M32], W=[ex, sc1])
                    S.op('dve', lambda e: e.reciprocal(out=sc1[:, 1:2], in_=sc1[:, 1:2]), R=[sc1], W=[sc1])
                    S.op('dve', lambda e, i=i: e.tensor_scalar(out=G_all[:, i, :], in0=ex[:], scalar1=sc1[:, 1:2], scalar2=None, op0=ALU.mult), R=[ex, sc1], W=[G_all])
                pc = PF[2]
                for i in range(NT):
                    S.op('pe', lambda e, i=i: e.matmul(pc[0:1, 0:32], lhsT=ones_bf[:, 0:1], rhs=M_bf[:, i, :], start=(i == 0), stop=(i == NT - 1)), R=[ones_bf, M_bf], W=[pc])
                row = lambda n, w=32, d=F32: self.sb(s1, "mo_" + n, [1, w], d)
                cnt = row('cnt'); nbf = row('nbf'); nbi = row('nbi', 32, I32); endr = row('end'); baser = row('base'); onesr = row('onesr')
                iota = self.load_const(s1, 'iota_blk', F32); kp = self.load_const(s1, 'kp', F32)
                cmp3 = self.sb(s1, "mo_cmp3", [1, NBLK, 32], F32); ebf = row('ebf', NBLK); chgf = row('chgf', NBLK)
                S.op('dve', lambda e: e.memset(onesr[:], 1.0), W=[onesr])
                S.op('dve', lambda e: e.tensor_scalar(out=cnt[:], in0=pc[0:1, 0:32], scalar1=float(MB - 1), scalar2=1.0 / MB, op0=ALU.add, op1=ALU.mult), R=[pc], W=[cnt])
                S.op('dve', lambda e: e.tensor_scalar(out=cnt[:], in0=cnt[:], scalar1=-0.5 + 0.5 / MB, scalar2=None, op0=ALU.add), R=[cnt], W=[cnt])
                S.op('dve', lambda e: e.tensor_copy(out=nbi[:], in_=cnt[:]), R=[cnt], W=[nbi])
                S.op('dve', lambda e: e.tensor_copy(out=nbf[:], in_=nbi[:]), R=[nbi], W=[nbf])
                S.op('dve', lambda e: e.tensor_tensor_scan(out=endr[:], data0=onesr[:], data1=nbf[:], initial=0.0, op0=ALU.mult, op1=ALU.add), R=[onesr, nbf], W=[endr])
                S.op('dve', lambda e: e.tensor_tensor(out=baser[:], in0=endr[:], in1=nbf[:], op=ALU.subtract), R=[endr, nbf], W=[baser])
                S.op('dve', lambda e: e.tensor_scalar(out=baser[:], in0=baser[:], scalar1=float(MB), scalar2=None, op0=ALU.mult), R=[baser], W=[baser])
                basebc = self.sb(s1, "mo_basebc", [128, 32], F32)
                onescol = self.sb(s1, "mo_ones1", [1, 128], F32)
                S.op('dve', lambda e: e.memset(onescol[:], 1.0), W=[onescol])
                S.op('pe', lambda e: e.matmul(PF[3][:, 0:32], lhsT=onescol[0:1, :], rhs=baser[0:1, :], start=True, stop=True), R=[onescol, baser], W=[PF[3]])
                S.op('act', lambda e: e.activation(out=basebc[:], in_=PF[3][:, 0:32], func=AF.Copy), R=[PF[3]], W=[basebc])
                S.op('dve', lambda e: e.tensor_tensor(out=cmp3[:], in0=endr[:].unsqueeze(1).broadcast_to([1, NBLK, 32]), in1=iota[0:1, :].unsqueeze(2).broadcast_to([1, NBLK, 32]), op=ALU.is_le),
                     R=[endr, iota], W=[cmp3])
                S.op('dve', lambda e: e.tensor_reduce(out=ebf[:], in_=cmp3[:], axis=AX.X, op=ALU.add), R=[cmp3], W=[ebf])
                S.op('dve', lambda e: e.tensor_scalar(out=ebf[:], in0=ebf[:], scalar1=31.0, scalar2=None, op0=ALU.min), R=[ebf], W=[ebf])
                needf = row('needf', NBLK); ebrow = row('ebrow', NBLK)
                S.op('dve', lambda e: e.memset(needf[:], 1.0), W=[needf])
                S.op('dve', lambda e: e.tensor_tensor(out=needf[0:1, 2:NBLK], in0=ebf[0:1, 2:NBLK], in1=ebf[0:1, 0:NBLK - 2], op=ALU.not_equal), R=[ebf, needf], W=[needf])
                S.op('dve', lambda e: e.tensor_scalar(out=needf[:], in0=needf[:], scalar1=-1.0e6, scalar2=1.0e6, op0=ALU.mult, op1=ALU.add), R=[needf], W=[needf])
                S.op('dve', lambda e: e.tensor_scalar(out=ebrow[:], in0=ebf[:], scalar1=float(l * 32), scalar2=128.0, op0=ALU.add, op1=ALU.mult), R=[ebf], W=[ebrow])
                S.op('dve', lambda e: e.tensor_tensor(out=ebrow[:], in0=ebrow[:], in1=needf[:], op=ALU.add), R=[ebrow, needf], W=[ebrow])
                ebbc = self.sb(s1, "mo_ebbc", [128, NBLK], F32); wf = self.sb(s1, "mo_wf", [128, NBLK, 8], F32)
                S.op('pe', lambda e: e.matmul(PF[3][:, 0:NBLK], lhsT=onescol[0:1, :], rhs=ebrow[0:1, :], start=True, stop=True), R=[onescol, ebrow], W=[PF[3]])
                S.op('act', lambda e: e.activation(out=ebbc[:], in_=PF[3][:, 0:NBLK], func=AF.Copy), R=[PF[3]], W=[ebbc])
                S.op('dve', lambda e: e.tensor_tensor(out=wf[:], in0=ebbc[:].unsqueeze(2).broadcast_to([128, NBLK, 8]), in1=kp[:].unsqueeze(1).broadcast_to([128, NBLK, 8]), op=ALU.add),
                     R=[ebbc, kp], W=[wf])
                S.op('dve', lambda e: e.tensor_copy(out=widx[:], in_=wf[:]), R=[wf], W=[widx])
                pcol = self.load_const(s1, 'pcol', F32)
                S.op('pe', lambda e: e.matmul(PF[3][0:32, 0:NBLK], lhsT=onescol[0:1, 0:32], rhs=ebf[0:1, :], start=True, stop=True), R=[onescol, ebf], W=[PF[3]])
                S.op('dve', lambda e: e.tensor_scalar(out=OH[:], in0=PF[3][0:32, 0:NBLK], scalar1=pcol[0:32, 0:1], scalar2=None, op0=ALU.is_equal), R=[PF[3], pcol], W=[OH])
                zt = self.sb(s1, "mo_zt", [128, NSLOT * 2 // 128], I32)
                S.op('dve', lambda e: e.memset(zt[:], 0), W=[zt])
                tokz = TB()
                self.dma('sp', self.tokidx_d.rearrange("(p b) o -> p (b o)", p=128), zt[:], R=[zt], W=[tokz])
                tidx = self.sb(s1, "mo_tidx", [128, NT, 2], I32)
                S.op('pool', lambda e: e.iota(tidx[:], pattern=[[128, NT], [0, 2]], base=0, channel_multiplier=1), W=[tidx])
                tri = self.load_const(s1, 'tri_lt', BF16)
                a1 = self.sb(s1, "mo_a1", [128, 32], F32); A8 = self.sb(s1, "mo_A8", [128, 8], F32); offf = self.sb(s1, "mo_offf", [128, 4], F32)
                for i in range(NT):
                    pp = PF[i % 2]
                    S.op('pe', lambda e, i=i, pp=pp: e.matmul(pp[:, 0:32], lhsT=tri[:], rhs=M_bf[:, i, :], start=True, stop=(i == 0)), R=[tri, M_bf], W=[pp])
                    for j in range(i):
                        S.op('pe', lambda e, i=i, j=j, pp=pp: e.matmul(pp[:, 0:32], lhsT=ones_bf[:, 0:128], rhs=M_bf[:, j, :], start=False, stop=(j == i - 1)), R=[ones_bf, M_bf], W=[pp])
                    S.op('dve', lambda e, pp=pp: e.tensor_tensor(out=a1[:], in0=pp[:, 0:32], in1=basebc[:], op=ALU.add), R=[pp, basebc], W=[a1])
                    S.op('dve', lambda e, i=i: e.scalar_tensor_tensor(out=a1[:], in0=a1[:], scalar=1.0, in1=M32[:, i, :], op0=ALU.add, op1=ALU.mult), R=[a1, M32], W=[a1])
                    S.op('dve', lambda e: e.max(out=A8[:], in_=a1[:]), R=[a1], W=[A8])
                    S.op('dve', lambda e: e.tensor_scalar(out=offf[:], in0=A8[:, 0:4], scalar1=-1.0, scalar2=None, op0=ALU.add), R=[A8], W=[offf])
                    S.op('dve', lambda e, i=i: e.tensor_copy(out=off_all[:, i, :], in_=offf[:]), R=[offf], W=[off_all])
                    for j in range(4):
                        S.op('dve', lambda e, i=i, j=j: e.scalar_tensor_tensor(out=junk32[:], in0=a1[:], scalar=A8[:, j:j + 1], in1=G_all[:, i, :], op0=ALU.is_equal, op1=ALU.mult,
                                                                              accum_out=gsel_all[:, i, j:j + 1]), R=[a1, A8, G_all], W=[junk32, gsel_all])
                    for j in range(4):
                        S.op('pool', lambda e, i=i, j=j: e.indirect_dma_start(out=self.tokidx_d[:, :], out_offset=bass.IndirectOffsetOnAxis(ap=off_all[:, i, j:j + 1], axis=0),
                                                                               in_=tidx[:, i, :], in_offset=None),
                             R=[off_all, tidx, tokz], W=[TB()], dma=True)
                S.barrier(); S.emit()
            if self.flags.get('moe_stop') == 2:
                return
            self.issue_weight_cast(l)
            w1v = self.w1b_d; w2v = self.w2b_d; convtb = self.conv_tb[l]
            b1v = self.moe_b1.rearrange("l e f -> (l e) f"); b2v = self.moe_b2.rearrange("l e d -> (l e) d")
            IO = bass.IndirectOffsetOnAxis
            with ExitStack() as s1:
                W1 = [self.sb(s1, f"mo_W1{i}", [128, 8, 2 * D], BF16) for i in range(2)]; W2 = [self.sb(s1, f"mo_W2{i}", [128, 8, D], BF16) for i in range(2)]
                b1all = self.sb(s1, "mo_b1all", [32, 2 * D], BF16); b2all = self.sb(s1, "mo_b2all", [32, D], BF16)
                self.dma('pool', b1all[:], self.moe_b1[l], W=[b1all]); self.dma('pool', b2all[:], self.moe_b2[l], W=[b2all])
                sel = [self.sb(s1, f"mo_sel{i}", [32, MB], BF16) for i in range(2)]
                bcdone = self.sb(s1, "mo_bcd", [1, 1], F32)

                def setbc(e):
                    e.reg_mov(self.reg_bc, 64 * 128 - 1)
                    return e.memset(bcdone[:], 0.0)
                S.op('pool', setbc, W=[bcdone])
                wts = [TB(), TB()]
                idx = [self.sb(s1, f"mo_idx{i}", [128, 4, 2], I32) for i in range(2)]
                X = [self.sb(s1, f"mo_X{i}", [128, D], BF16) for i in range(2)]
                XT = [self.sb(s1, f"mo_XT{i}", [128, 8, MB], BF16) for i in range(2)]
                AT = self.sb(s1, "mo_AT", [128, 8, MB], BF16)
                g_ = self.sb(s1, "mo_g", [128, 512], F32); sg_ = self.sb(s1, "mo_sg", [128, 512], F32); ln_ = self.sb(s1, "mo_ln", [128, 512], F32)
                yrow = [self.sb(s1, f"mo_y{i}", [128, D], F32) for i in range(2)]
                xc = 0

                def issue_weights(b):
                    b2_ = b % 2
                    W1_, W2_, wt_ = W1[b2_], W2[b2_], wts[b2_]
                    S.op('pool', lambda e, b=b, W1_=W1_: e.indirect_dma_start(out=W1_[:].rearrange("p k f -> p (k f)"), out_offset=None, in_=w1v[:, :], in_offset=IO(ap=widx[:, b, 0:1], axis=0),
                                                                         bounds_check=self.reg_bc, oob_is_err=False), R=[widx, bcdone, convtb], W=[wt_], dma=True)
                    S.op('pool', lambda e, b=b, W2_=W2_: e.indirect_dma_start(out=W2_[:].rearrange("p k f -> p (k f)"), out_offset=None, in_=w2v[:, :], in_offset=IO(ap=widx[:, b, 0:1], axis=0),
                                                                         bounds_check=self.reg_bc, oob_is_err=False), R=[widx, bcdone, convtb], W=[wt_], dma=True)
                X4 = [self.sb(s1, f"mo_X4{i}", [128, D], BF16) for i in range(4)]

                def issue_gathers(b):
                    b2_ = b % 2
                    self.dma('sp', idx[b2_][:], self.tokidx_d[b * MB:(b + 1) * MB, :].rearrange("(q p) o -> p q o", p=128), W=[idx[b2_]])
                    for q in range(MB // 128):
                        x_ = X4[q]
                        S.op('pool', lambda e, b2_=b2_, q=q, x_=x_: e.indirect_dma_start(out=x_[:], out_offset=None, in_=self.hrow_d[:, :], in_offset=IO(ap=idx[b2_][:, q, 0:1], axis=0)),
                             R=[idx[b2_]], W=[x_], dma=True)

                def issue_transposes(b):
                    xt_ = XT[b % 2]
                    for q in range(MB // 128):
                        x_ = X4[q]; pb = self.PB[q % 2]
                        for k in range(8):
                            S.op('pe', lambda e, k=k, pb=pb, x_=x_: e.transpose(out=pb[:, k * 128:(k + 1) * 128], in_=x_[:, k * 128:(k + 1) * 128], identity=self.ident[:]), R=[x_, self.ident], W=[pb])
                        S.op('act', lambda e, pb=pb, xt_=xt_, q=q: e.activation(out=xt_[:, :, q * 128:(q + 1) * 128], in_=pb[:].rearrange("p (k t) -> p k t", k=8), func=AF.Copy), R=[pb], W=[xt_])
                issue_weights(0)
                issue_gathers(0)
                issue_transposes(0)
                for b in range(NBLK):
                    b2_ = b % 2
                    W1_, W2_, wt_ = W1[b2_], W2[b2_], wts[b2_]
                    sel_ = sel[b2_]
                    xt_ = XT[b2_]
                    S.op('dve', lambda e, b=b, sel_=sel_: e.tensor_copy(out=sel_[:], in_=OH[:, b:b + 1].broadcast_to([32, MB])), R=[OH], W=[sel_])
                    if b + 1 < NBLK:
                        issue_weights(b + 1)
                        issue_gathers(b + 1)
                    for c in range(8):
                        pg, pl = PF[(c % 2) * 2], PF[(c % 2) * 2 + 1]
                        for (pf, cbase) in ((pg, 0), (pl, D)):
                            for k in range(8):
                                S.op('pe', lambda e, k=k, pf=pf, c=c, cbase=cbase, W1_=W1_, xt_=xt_: e.matmul(pf[:], lhsT=W1_[:, k, cbase + c * 128: cbase + (c + 1) * 128], rhs=xt_[:, k, :], start=(k == 0), stop=False),
                                     R=[wt_, xt_], W=[pf])
                            S.op('pe', lambda e, pf=pf, c=c, cbase=cbase, sel_=sel_: e.matmul(pf[:], lhsT=b1all[0:32, cbase + c * 128: cbase + (c + 1) * 128], rhs=sel_[0:32, :], start=False, stop=True), R=[b1all, sel_], W=[pf])
                        S.op('dve', lambda e, pg=pg: e.tensor_scalar(out=g_[:], in0=pg[:], scalar1=7.0, scalar2=None, op0=ALU.min), R=[pg], W=[g_])
                        S.op('act', lambda e: e.activation(out=sg_[:], in_=g_[:], func=AF.Sigmoid, scale=1.702), R=[g_], W=[sg_])
                        S.op('dve', lambda e, pl=pl: e.tensor_scalar(out=ln_[:], in0=pl[:], scalar1=7.0, scalar2=-7.0, op0=ALU.min, op1=ALU.max), R=[pl], W=[ln_])
                        S.op('dve', lambda e: e.tensor_tensor(out=sg_[:], in0=sg_[:], in1=g_[:], op=ALU.mult), R=[sg_, g_], W=[sg_])
                        S.op('dve', lambda e, c=c: e.scalar_tensor_tensor(out=AT[:, c, :], in0=ln_[:], scalar=1.0, in1=sg_[:], op0=ALU.add, op1=ALU.mult), R=[ln_, sg_], W=[AT])
                    if b + 1 < NBLK:
                        issue_transposes(b + 1)
                    for q in range(MB // 128):
                        y_ = yrow[q % 2]
                        for hf in range(2):
                            py = PF[4 + hf]
                            for c in range(8):
                                S.op('pe', lambda e, c=c, py=py, hf=hf, q=q, W2_=W2_: e.matmul(py[:], lhsT=AT[:, c, q * 128:(q + 1) * 128], rhs=W2_[:, c, hf * 512:(hf + 1) * 512], start=(c == 0), stop=False), R=[AT, wt_], W=[py])
                            S.op('pe', lambda e, py=py, hf=hf, sel_=sel_: e.matmul(py[:], lhsT=sel_[0:32, 0:128], rhs=b2all[0:32, hf * 512:(hf + 1) * 512], start=False, stop=True), R=[sel_, b2all], W=[py])
                            if hf == 0:
                                S.op('act', lambda e, py=py, y_=y_: e.activation(out=y_[:, 0:512], in_=py[:], func=AF.Copy), R=[py], W=[y_])
                            else:
                                S.op('dve', lambda e, py=py, y_=y_: e.tensor_copy(out=y_[:, 512:1024], in_=py[:]), R=[py], W=[y_])
                        self.dma('sp', self.yslot_d[b * MB + q * 128: b * MB + (q + 1) * 128, :], y_[:], R=[y_], W=[TB()])
                S.barrier(); S.emit()
            if self.flags.get('moe_stop') == 3:
                return
            with ExitStack() as s1:
                Y = [self.sb(s1, f"mo_Y{j}", [128, D], F32) for j in range(4)]
                xt = [self.sb(s1, f"mo_x{i}", [128, D], F32) for i in range(2)]
                acc = self.sb(s1, "mo_acc", [128, D], F32)
                for i in range(NT):
                    x_ = xt[i % 2]
                    self.dma('sp', x_[:], self.xres[i * 128:(i + 1) * 128, :], W=[x_])
                    for j in range(4):
                        S.op('pool', lambda e, i=i, j=j: e.indirect_dma_start(out=Y[j][:], out_offset=None, in_=self.yslot_d[:, :], in_offset=bass.IndirectOffsetOnAxis(ap=off_all[:, i, j:j + 1], axis=0)),
                             R=[off_all], W=[Y[j]], dma=True)
                    S.op('dve', lambda e, i=i: e.tensor_scalar(out=acc[:], in0=Y[0][:], scalar1=gsel_all[:, i, 0:1], scalar2=None, op0=ALU.mult), R=[Y[0], gsel_all], W=[acc])
                    for j in range(1, 4):
                        S.op('dve', lambda e, i=i, j=j: e.scalar_tensor_tensor(out=acc[:], in0=Y[j][:], scalar=gsel_all[:, i, j:j + 1], in1=acc[:], op0=ALU.mult, op1=ALU.add), R=[Y[j], gsel_all, acc], W=[acc])
                    S.op('pool', lambda e: e.tensor_tensor(out=acc[:], in0=acc[:], in1=self.gb[1][:], op=ALU.mult), R=[acc, self.gb[1]], W=[acc])
                    S.op('dve', lambda e, x_=x_: e.tensor_tensor(out=x_[:], in0=x_[:], in1=acc[:], op=ALU.add), R=[x_, acc], W=[x_])
                    self.dma('sp', self.xres[i * 128:(i + 1) * 128, :], x_[:], R=[x_], W=[TB()])
                S.barrier(); S.emit()

    def final_norm(self):
        S = self.S
        with ExitStack() as st:
            gfb = self.bcast_row(st, "gfb", self.norm_final[0, :], D)
            xt = [self.sb(st, f"fx{i}", [128, D], F32) for i in range(2)]
            junk = self.sb(st, "fjunk", [128, D], F32)
            ss = [self.sb(st, f"fss{i}", [128, 1], F32) for i in range(2)]
            for i in range(NT):
                x_, s_ = xt[i % 2], ss[i % 2]
                self.dma('sp' if i % 2 == 0 else 'act', x_[:], self.xres[i * 128:(i + 1) * 128, :], W=[x_])
                S.op('act', lambda e, x_=x_, s_=s_: e.activation(out=junk[:], in_=x_[:], func=AF.Square, accum_out=s_[:]), R=[x_], W=[junk, s_])
                S.op('dve', lambda e, s_=s_: e.tensor_scalar(out=s_[:], in0=s_[:], scalar1=1.0 / D, scalar2=1e-5, op0=ALU.mult, op1=ALU.add), R=[s_], W=[s_])
                S.op('act', lambda e, s_=s_: e.activation(out=s_[:], in_=s_[:], func=AF.Sqrt), R=[s_], W=[s_])
                S.op('dve', lambda e, s_=s_: e.reciprocal(out=s_[:], in_=s_[:]), R=[s_], W=[s_])
                S.op('dve', lambda e, x_=x_, s_=s_: e.scalar_tensor_tensor(out=x_[:], in0=x_[:], scalar=s_[:, 0:1], in1=gfb[:], op0=ALU.mult, op1=ALU.mult), R=[x_, s_, gfb], W=[x_])
                t = TB()
                self.dma('sp', self.out[i * 128:(i + 1) * 128, :], x_[:], R=[x_], W=[t])
                self.final_tbs.append(t)
            S.barrier(); S.emit()


class _Stop(Exception):
    pass


class _View:
    def __init__(self, buf, i):
        self.buf = buf; self.i = i; self.tb = buf.tb

    def __getitem__(self, k):
        return self.buf.t[:, self.i, :][k]


def make_in_maps(inputs):
    f = lambda a: np.ascontiguousarray(np.asarray(a, dtype=np.float32))
    w_ext = np.ascontiguousarray(np.asarray(inputs['w_in'], np.float32)[:, :, WCOLS])
    lw = np.ascontiguousarray(np.concatenate([inputs['rwkv_w2'], inputs['rwkv_a2'], inputs['rwkv_g2']], axis=1).astype(np.float32))
    qs = np.array(_swap_cols(0, 4, 64, 8)); qis = np.array(_swap_cols(0, 8, 32, 4))
    shared = dict(
        cst=CST, ada_w=f(inputs['ada_w']), ada_b=f(inputs['ada_b']), norm_mix=f(inputs['norm_mix']), norm_ffn=f(inputs['norm_ffn']),
        w_ext=w_ext, ret_gn=f(inputs['ret_gn']), rwkv_mu=f(inputs['rwkv_mu']), rwkv_w0=f(inputs['rwkv_w0']), rwkv_lw=lw,
        rwkv_a0=f(inputs['rwkv_a0']), rwkv_kk=f(inputs['rwkv_kk']), rwkv_ka=f(inputs['rwkv_ka']),
        rwkv_rk=f(np.asarray(inputs['rwkv_rk']).reshape(L, 256)), rwkv_ln=f(inputs['rwkv_ln']),
        dsa_qnorm=f(inputs['dsa_qnorm']), dsa_wq_up=f(inputs['dsa_wq_up']), dsa_wqs_up=f(np.asarray(inputs['dsa_wq_up'])[:, :, qs]),
        dsa_wqi_up=f(inputs['dsa_wqi_up']), dsa_wqis_up=f(np.asarray(inputs['dsa_wqi_up'])[:, :, qis]),
        dsa_onorm=f(inputs['dsa_onorm']), sb_onorm=f(inputs['sb_onorm']), w_out=f(inputs['w_out']),
        router_w=f(inputs['router_w']), router_b=f(inputs['router_b']), moe_w1=f(inputs['moe_w1']), moe_b1=f(inputs['moe_b1']),
        moe_w2=f(inputs['moe_w2']), moe_b2=f(inputs['moe_b2']), norm_final=f(np.asarray(inputs['norm_final']).reshape(1, D)),
    )
    maps = []
    x = np.asarray(inputs['x'], np.float32); c = np.asarray(inputs['c'], np.float32); pos = np.asarray(inputs['positions'], np.int32)
    for b in range(x.shape[0]):
        m = dict(shared)
        m['x'] = np.ascontiguousarray(x[b]); m['c'] = np.ascontiguousarray(c[b:b + 1]); m['pos'] = np.ascontiguousarray(pos[b:b + 1])
        maps.append(m)
    return maps


def kernel(**inputs):
    maps = make_in_maps(inputs)
    nc = Prog().build()
    res = run_bass_kernel_spmd(nc, maps, core_ids=list(range(NB)))
    return np.stack([np.asarray(r['out'], dtype=np.float32) for r in res.results], axis=0)
```

```python
import numpy as np
from contextlib import ExitStack
import concourse.bass as bass
import concourse.mybir as mybir
from concourse.bass_utils import run_bass_kernel_spmd

F32 = mybir.dt.float32; BF16 = mybir.dt.bfloat16; I32 = mybir.dt.int32; U32 = mybir.dt.uint32
AF = mybir.ActivationFunctionType; ALU = mybir.AluOpType; AX = mybir.AxisListType

D = 1024; SEQ = 4096; NB = 8; L = 2; NT = SEQ // 128
MB = 512; NBLK = 64; NSLOT = NBLK * MB
IN_COLS = 2984
ENGS = ['pe', 'act', 'dve', 'pool', 'sp']
LIMIT = 30000


class TB:
    __slots__ = ('w', 'wd', 'r', 'excl')

    def __init__(self):
        self.w = None; self.wd = {}; self.r = {}; self.excl = False


class Buf:
    def __init__(self, t):
        self.t = t; self.tb = TB()

    def __getitem__(self, k):
        return self.t[k]


def _tb(x):
    return getattr(x, 'tb', x)


class Sched:
    def __init__(self, nc, stack, ndsem=32):
        self.nc = nc; self.stack = stack
        self.ops = {e: [] for e in ENGS}
        self.epoch = {e: 0 for e in ENGS}
        self.cnt = {e: 0 for e in ENGS}
        self.sems = {}
        for e in ENGS:
            self.sems[(e, 0)] = stack.enter_context(nc.semaphore(f"s_{e}_0"))
        self.dsems = [stack.enter_context(nc.semaphore(f"sd_{i}")) for i in range(ndsem)]
        self.dcnt = [0] * ndsem; self.dnext = {'hw': 0, 'sw': 0}
        self.nhw = ndsem // 2
        self.seen = {e: {} for e in ENGS}
        self.nops = 0

    def semof(self, key):
        if key[0] == 'd':
            return self.dsems[key[1]]
        return self.sems[key]

    def op(self, eng, fn, R=(), W=(), dma=False):
        need = {}

        def nd(k, v):
            if need.get(k, 0) < v:
                need[k] = v
        R = [_tb(x) for x in R]; W = [_tb(x) for x in W]
        W = W + [t for t in R if t.excl and t not in W]
        R = [t for t in R if not t.excl]
        for t in R:
            if t.w:
                nd(*t.w)
            for k, v in t.wd.items():
                nd(k, v)
        for t in W:
            if t.w and (dma or t.w[0][0] != eng):
                nd(*t.w)
            if not dma:
                for k, v in t.wd.items():
                    nd(k, v)
            for k, v in t.r.items():
                if not dma and k[0] == eng:
                    continue
                nd(k, v)
        waits = []
        for k, v in need.items():
            if k[0] == 'd':
                v = self.dcnt[k[1]]
            elif k[0] == 'pe' and eng == 'pe':
                continue
            if self.seen[eng].get(k, 0) < v:
                waits.append((k, v)); self.seen[eng][k] = v
        if dma:
            kind = 'sw' if eng == 'pool' else 'hw'
            n = self.nhw if kind == 'hw' else len(self.dsems) - self.nhw
            i = self.dnext[kind] + (0 if kind == 'hw' else self.nhw)
            self.dnext[kind] = (self.dnext[kind] + 1) % n
            self.dcnt[i] += 16; key = ('d', i); val = self.dcnt[i]; inc = 16
        else:
            if self.cnt[eng] >= LIMIT:
                self.epoch[eng] += 1; self.cnt[eng] = 0
                self.sems[(eng, self.epoch[eng])] = self.stack.enter_context(
                    self.nc.semaphore(f"s_{eng}_{self.epoch[eng]}"))
            self.cnt[eng] += 1; key = (eng, self.epoch[eng]); val = self.cnt[eng]; inc = 1
        self.ops[eng].append((waits, fn, key, inc))
        for t in R:
            t.r[key] = val
        for t in W:
            if dma:
                t.wd[key] = val
            else:
                t.w = (key, val); t.wd = {}
                t.r = {}
        self.nops += 1

    def barrier(self):
        for e in ENGS:
            waits = []
            for f in ENGS:
                if f == e:
                    continue
                k = (f, self.epoch[f]); v = self.cnt[f]
                if v > 0 and self.seen[e].get(k, 0) < v:
                    waits.append((k, v)); self.seen[e][k] = v
            for i in range(len(self.dsems)):
                k = ('d', i); v = self.dcnt[i]
                if v > 0 and self.seen[e].get(k, 0) < v:
                    waits.append((k, v)); self.seen[e][k] = v
            self.ops[e].append((waits, None, None, 0))

    def emit(self):
        nc = self.nc
        names = {'pe': 'tensor', 'act': 'scalar', 'dve': 'vector', 'pool': 'gpsimd', 'sp': 'sync'}
        with nc.Block() as block:
            for e in ENGS:
                lst = self.ops[e]

                def body(engine, lst=lst):
                    for waits, fn, key, inc in lst:
                        for k, v in waits:
                            engine.wait_ge(self.semof(k), v)
                        if fn is not None:
                            ins = fn(engine)
                            ins.then_inc(self.semof(key), inc)
                getattr(block, names[e])(body)
        self.ops = {e: [] for e in ENGS}


RET0, RWKV0, DSA0, SB0 = 0, 1024, 1920, 2216


def _swap_cols(base, nheads, hd, half):
    cols = []
    for h in range(nheads):
        for f in range(hd):
            if f < half:
                g = f + half
            elif f < 2 * half:
                g = f - half
            else:
                g = f
            cols.append(base + h * hd + g)
    return cols


def build_wext_cols():
    blocks = {}
    r = lambda a, n: list(range(a, a + n))
    qs = _swap_cols(RET0, 4, 64, 32); ks = _swap_cols(RET0 + 256, 4, 64, 32)
    for hp in range(2):
        blocks[f'ret_q{hp}'] = r(RET0 + hp * 128, 128)
        blocks[f'ret_qs{hp}'] = qs[hp * 128:(hp + 1) * 128]
        blocks[f'ret_k{hp}'] = r(RET0 + 256 + hp * 128, 128)
        blocks[f'ret_ks{hp}'] = ks[hp * 128:(hp + 1) * 128]
        blocks[f'ret_vg{hp}'] = r(RET0 + 512 + hp * 128, 128) + r(RET0 + 768 + hp * 128, 128)
    for hp in range(2):
        blocks[f'sb_q{hp}'] = r(SB0 + hp * 128, 128)
        blocks[f'sb_k{hp}'] = r(SB0 + 256 + hp * 128, 128)
    blocks['sb_v'] = r(SB0 + 512, 256)
    blocks['rw_rkv'] = r(RWKV0, 768)
    blocks['rw_lora'] = r(RWKV0 + 768, 128)
    blocks['ds_cq'] = r(DSA0, 128)
    kcols = r(DSA0 + 128, 64); kscols = _swap_cols(DSA0 + 128, 1, 64, 8)
    blocks['ds_k'] = kcols + kcols
    blocks['ds_ks'] = kscols + kscols
    icols = r(DSA0 + 256, 32); iscols = _swap_cols(DSA0 + 256, 1, 32, 4)
    blocks['ds_ki'] = icols * 4
    blocks['ds_kis'] = iscols * 4
    blocks['ds_vw'] = r(DSA0 + 192, 64) + r(DSA0 + 288, 8)
    off = {}; cols = []
    for k, v in blocks.items():
        off[k] = (len(cols), len(v)); cols += v
    return off, np.array(cols, dtype=np.int64)


WOFF, WCOLS = build_wext_cols()
NEXT = len(WCOLS)


def build_consts():
    c = {}
    p = np.arange(128)
    c['ident'] = np.eye(128, dtype=np.float32)
    sbm = np.zeros((4, 128, 512), np.float32)
    for rr in range(4):
        for qb in range(4):
            if qb > rr:
                sbm[rr, :, qb * 128:(qb + 1) * 128] = 1.0
            elif qb == rr:
                sbm[rr, :, qb * 128:(qb + 1) * 128] = (p[:, None] < p[None, :]).astype(np.float32)
    c['sbmask'] = sbm.transpose(1, 0, 2).reshape(128, 4 * 512)
    c['tri_ge'] = (p[:, None] >= p[None, :]).astype(np.float32)
    c['tri_le'] = (p[:, None] <= p[None, :]).astype(np.float32)
    c['tri_lt'] = (p[:, None] < p[None, :]).astype(np.float32)
    c['tri_gt'] = (p[:, None] > p[None, :]).astype(np.float32)
    c['ntri_ge'] = -c['tri_ge']
    c['nones'] = -np.ones((128, 128), np.float32)
    c['hm4'] = (p[:, None] // 32 == np.arange(4)[None, :]).astype(np.float32)
    c['hm2'] = (p[:, None] // 64 == np.arange(2)[None, :]).astype(np.float32)
    c['iota_blk'] = np.tile(np.arange(64, dtype=np.float32)[None, :], (128, 1))
    c['kp'] = (np.arange(8)[None, :] * 128 + p[:, None]).astype(np.float32)
    c['pcol'] = p.astype(np.float32)[:, None]
    c['last'] = (p == 127).astype(np.float32)[:, None]
    c['negmask'] = np.where(p[None, :] > p[:, None], -1e30, 0.0).astype(np.float32)
    lg = np.log(1.0 - 2.0 ** (-5.0 - np.arange(4, dtype=np.float64)))
    idx = np.arange(128, dtype=np.float64)
    for hp in range(2):
        hs = [2 * hp, 2 * hp + 1]
        hp_of_p = np.array([hs[q // 64] for q in range(128)])
        c[f'ret_qdec{hp}'] = np.exp(lg[hp_of_p][:, None] * (idx[None, :] + 1.0)).astype(np.float32)
        kd = np.zeros((128, 128)); dm = np.zeros((128, 2, 128))
        for j, h in enumerate(hs):
            kd[:, j * 64:(j + 1) * 64] = (np.exp(lg[h] * (127.0 - idx)) / 8.0)[:, None]
            diff = idx[None, :] - idx[:, None]
            dm[:, j, :] = np.where(diff >= 0, np.exp(lg[h] * np.maximum(diff, 0.0)), 0.0) / 8.0
        c[f'ret_kdec{hp}'] = kd.astype(np.float32)
        c[f'ret_dmask{hp}'] = dm.reshape(128, 256).astype(np.float32)
        c[f'ret_cd{hp}'] = np.exp(lg[hp_of_p] * 128.0).astype(np.float32)[:, None]
    f64 = p % 64
    c['rope_ret'] = np.stack([10000.0 ** (-(f64 % 32) / 32.0) / (2 * np.pi), np.where(f64 < 32, -1.0, 1.0)], 1).astype(np.float32)
    inv = np.where(f64 < 16, 500000.0 ** (-(f64 % 8) / 8.0), 0.0) / (2 * np.pi)
    sg = np.where(f64 < 8, -1.0, np.where(f64 < 16, 1.0, 0.0))
    c['rope_dq'] = np.stack([inv, sg], 1).astype(np.float32)
    f32 = p % 32
    inv = np.where(f32 < 8, 500000.0 ** (-(f32 % 4) / 4.0), 0.0) / (2 * np.pi)
    sg = np.where(f32 < 4, -1.0, np.where(f32 < 8, 1.0, 0.0))
    c['rope_di'] = np.stack([inv, sg], 1).astype(np.float32)
    off = {}; n = 0; arrs = []
    for k, v in c.items():
        off[k] = (n, v.shape[1]); n += v.shape[1]; arrs.append(v.astype(np.float32))
    return off, np.ascontiguousarray(np.concatenate(arrs, 1))


COFF, CST = build_consts()


class Prog:
    def __init__(self, debug=None, nlayers=L, flags=None):
        self.debug = debug or {}
        self.flags = flags or {}
        self.nlayers = nlayers
        nc = self.nc = bass.Bass("TRN2", target_bir_lowering=False)
        dt = lambda name, shape, dtype, kind="ExternalInput": nc.dram_tensor(name, shape, dtype, kind=kind).ap()
        self.x = dt("x", [SEQ, D], F32)
        self.c = dt("c", [1, D], F32)
        self.pos = dt("pos", [1, SEQ], I32)
        self.cst = dt("cst", [128, CST.shape[1]], F32)
        self.ada_w = dt("ada_w", [L, D, 6 * D], F32)
        self.ada_b = dt("ada_b", [L, 6 * D], F32)
        self.norm_mix = dt("norm_mix", [L, D], F32)
        self.norm_ffn = dt("norm_ffn", [L, D], F32)
        self.w_ext = dt("w_ext", [L, D, NEXT], F32)
        self.ret_gn = dt("ret_gn", [L, 256], F32)
        self.rwkv_mu = dt("rwkv_mu", [L, 896], F32)
        self.rwkv_w0 = dt("rwkv_w0", [L, 256], F32)
        self.rwkv_lw = dt("rwkv_lw", [L, 128, 256], F32)
        self.rwkv_a0 = dt("rwkv_a0", [L, 256], F32)
        self.rwkv_kk = dt("rwkv_kk", [L, 256], F32)
        self.rwkv_ka = dt("rwkv_ka", [L, 256], F32)
        self.rwkv_rk = dt("rwkv_rk", [L, 256], F32)
        self.rwkv_ln = dt("rwkv_ln", [L, 256], F32)
        self.dsa_qnorm = dt("dsa_qnorm", [L, 128], F32)
        self.dsa_wq_up = dt("dsa_wq_up", [L, 128, 256], F32)
        self.dsa_wqs_up = dt("dsa_wqs_up", [L, 128, 256], F32)
        self.dsa_wqi_up = dt("dsa_wqi_up", [L, 128, 256], F32)
        self.dsa_wqis_up = dt("dsa_wqis_up", [L, 128, 256], F32)
        self.dsa_onorm = dt("dsa_onorm", [L, 256], F32)
        self.sb_onorm = dt("sb_onorm", [L, 256], F32)
        self.w_out = dt("w_out", [L, D, D], F32)
        self.router_w = dt("router_w", [L, D, 32], F32)
        self.router_b = dt("router_b", [L, 32], F32)
        if not self.flags.get('nomoe'):
            self.moe_w1 = dt("moe_w1", [L, 32, D, 2 * D], F32)
            self.moe_b1 = dt("moe_b1", [L, 32, 2 * D], F32)
            self.moe_w2 = dt("moe_w2", [L, 32, D, D], F32)
            self.moe_b2 = dt("moe_b2", [L, 32, D], F32)
        self.norm_final = dt("norm_final", [1, D], F32)
        self.out = dt("out", [SEQ, D], F32, kind="ExternalOutput")
        self.xres = dt("xres", [SEQ, D], F32, kind="Internal")
        self.yT_d = dt("yT_d", [8, 128, SEQ], BF16, kind="Internal")
        self.maskT_d = dt("maskT_d", [NT, 128, NT, 128], BF16, kind="Internal")
        self.hrow_d = dt("hrow_d", [SEQ, D], BF16, kind="Internal")
        self.tokidx_d = dt("tokidx_d", [NSLOT, 2], I32, kind="Internal")
        self.yslot_d = dt("yslot_d", [NSLOT, D], F32, kind="Internal")
        self.dbg = {}
        for name, (shape, dtype) in self.debug.items():
            self.dbg[name] = dt("dbg_" + name, shape, dtype, kind="ExternalOutput")
        self.final_tbs = []

    def sb(self, st, name, shape, dtype):
        self._uid = getattr(self, '_uid', 0) + 1
        return Buf(st.enter_context(self.nc.sbuf_tensor(f"{name}_{self._uid}", shape, dtype)))

    def ps(self, st, name, shape, dtype):
        self._uid = getattr(self, '_uid', 0) + 1
        b = Buf(st.enter_context(self.nc.psum_tensor(f"{name}_{self._uid}", shape, dtype)))
        b.tb.excl = True
        return b

    def dma(self, eng, out, in_, R=(), W=(), **kw):
        self.S.op(eng, lambda e: e.dma_start(out=out, in_=in_, **kw), R=R, W=W, dma=True)

    def load_const(self, st, name, dtype=F32, eng='sp'):
        o, n = COFF[name]
        b = self.sb(st, "c_" + name, [128, n], dtype)
        kw = dict(allow_slow_non_contiguous=True) if n < 8 else {}
        self.dma('pool' if dtype != F32 else eng, b[:], self.cst[:, o:o + n], W=[b], **kw)
        return b

    def bcast_row(self, st, name, row_ap, n, eng='sp'):
        b = self.sb(st, name, [128, n], F32)
        self.dma(eng, b[:], row_ap.partition_broadcast(128), W=[b])
        return b

    def dbg_out(self, name, src_ap, R, dst=None):
        if name in self.dbg:
            t = TB()
            d = self.dbg[name] if dst is None else dst
            self.dma('sp', d, src_ap, R=R, W=[t])
            self.final_tbs.append(t)

    def build(self):
        nc = self.nc
        with ExitStack() as top:
            S = self.S = Sched(nc, top)
            self.ident = self.load_const(top, 'ident', BF16)
            self.identf = self.load_const(top, 'ident', F32)
            self.modT = self.sb(top, "modT", [128, 48], F32)
            self.gb = [self.sb(top, f"gb{i}", [128, D], F32) for i in range(2)]
            self.ab2 = self.sb(top, "ab2", [128, D], F32); self.shb2 = self.sb(top, "shb2", [128, D], F32)
            self.reg_bc = top.enter_context(nc.gpsimd.register("reg_bc"))

            self.ones_row = self.sb(top, "ones_row", [1, 512], F32)
            self.condT = self.sb(top, "condT", [128, 8], F32)
            self.PF = [self.ps(top, f"pf{i}", [128, 512], F32) for i in range(6)]
            self.PB = [self.ps(top, f"pb{i}", [128, 1024], BF16) for i in range(2)]
            S.op('dve', lambda e: e.memset(self.ones_row[:], 1.0), W=[self.ones_row])
            with ExitStack() as st:
                self.cond_phase(st)
                S.barrier(); S.emit()
            for l in range(self.nlayers):
                self.layer(l)
            if 'stop' not in self.flags:
                self.final_norm()
            need = {}
            for t in self.final_tbs:
                for k in t.wd:
                    need[k] = max(need.get(k, 0), S.dcnt[k[1]])
            S.ops['sp'].append((list(need.items()), None, None, 0))
            S.barrier(); S.emit()
        return nc

    def cond_phase(self, st):
        S = self.S
        crow = self.sb(st, "crow", [1, D], F32)
        self.dma('sp', crow[:], self.c[0:1, :], W=[crow])
        pf = self.PF[0]
        for j in range(8):
            S.op('pe', lambda e, j=j: e.matmul(pf[:, j:j + 1], lhsT=crow[0:1, j * 128:(j + 1) * 128], rhs=self.ones_row[0:1, 0:1],
                                              start=True, stop=True), R=[crow, self.ones_row], W=[pf])
        S.op('act', lambda e: e.activation(out=self.condT[:], in_=pf[:, 0:8], func=AF.Silu), R=[pf], W=[self.condT])

    def row_to_cols(self, row_buf, row_ap_fn, ncols, out_ap, extra_R=()):
        S = self.S; pf = self.PF[0]
        for j in range(ncols):
            S.op('pe', lambda e, j=j: e.matmul(pf[:, j:j + 1], lhsT=row_ap_fn(j), rhs=self.ones_row[0:1, 0:1], start=True, stop=True),
                 R=[row_buf, self.ones_row], W=[pf])
        return pf

    def layer(self, l):
        S = self.S
        self.mod_phase(l)
        if self.flags.get('upto') == 'mod':
            return
        with ExitStack() as hs:
            self.hT = self.sb(hs, "hT", [128, 8, SEQ + 1], BF16)
            S.op('dve', lambda e: e.memset(self.hT[:, :, 0:1], 0.0), W=[self.hT])
            self.norm_phase(l, 0)
            if self.flags.get('upto') == 'norm':
                return
            only = self.flags.get('only')
            if only in (None, 'ret'):
                self.retention_phase(l)
            if only in (None, 'rwkv'):
                self.rwkv_phase(l)
            if only in (None, 'dsa'):
                self.dsa_phase(l)
            if only in (None, 'sb'):
                self.sb_phase(l)
            S.barrier(); S.emit()
        if 'yT' in self.dbg:
            for j in range(8):
                self.dbg_out('yT', self.yT_d[j], [], dst=self.dbg['yT'][j])
            S.barrier(); S.emit()
        if self.flags.get('upto') == 'mix':
            return
        self.wout_phase(l)
        if f'xmix{l}' in self.dbg:
            self.dbg_out(f'xmix{l}', self.xres[:, :], [])
            S.barrier(); S.emit()
        if self.flags.get('upto') == 'wout':
            return
        self.moe_phase(l)
        if f'x{l}' in self.dbg:
            self.dbg_out(f'x{l}', self.xres[:, :], [])
            S.barrier(); S.emit()

    def mod_phase(self, l):
        S = self.S
        with ExitStack() as st:
            self.modrow = self.sb(st, "modrow", [1, 6 * D], F32)
            wt = [self.sb(st, f"adaw{i}", [128, 8, 512], F32) for i in range(2)]
            brow = self.sb(st, "adab", [1, 6 * D], F32)
            self.dma('sp', brow[:], self.ada_b[l:l + 1, :], W=[brow])
            for cg in range(12):
                w = wt[cg % 2]
                self.dma('sp' if cg % 2 == 0 else 'act', w[:], self.ada_w[l, :, cg * 512:(cg + 1) * 512].rearrange("(k p) c -> p k c", p=128), W=[w])
                pf = self.PF[1 + cg % 2]
                for k in range(8):
                    S.op('pe', lambda e, k=k, w=w, pf=pf: e.matmul(pf[0:1, :], lhsT=self.condT[:, k:k + 1], rhs=w[:, k, :], start=(k == 0), stop=(k == 7)),
                         R=[self.condT, w], W=[pf])
                S.op('dve', lambda e, cg=cg, pf=pf: e.tensor_tensor(out=self.modrow[0:1, cg * 512:(cg + 1) * 512], in0=pf[0:1, :],
                                                                  in1=brow[0:1, cg * 512:(cg + 1) * 512], op=ALU.add), R=[pf, brow], W=[self.modrow])
            pf = self.row_to_cols(self.modrow, lambda j: self.modrow[0:1, j * 128:(j + 1) * 128], 48, None)
            S.op('dve', lambda e: e.tensor_copy(out=self.modT[:], in_=pf[:, 0:48]), R=[pf], W=[self.modT])
            onescol = self.sb(st, "ones1", [1, 128], F32)
            S.op('dve', lambda e: e.memset(onescol[:], 1.0), W=[onescol])
            for gi, base in enumerate((2 * D, 5 * D)):
                for hf in range(2):
                    pf2 = self.PF[3 + hf]
                    S.op('pe', lambda e, pf2=pf2, base=base, hf=hf: e.matmul(pf2[:], lhsT=onescol[0:1, :], rhs=self.modrow[0:1, base + hf * 512: base + (hf + 1) * 512],
                                                                            start=True, stop=True), R=[onescol, self.modrow], W=[pf2])
                    S.op('act', lambda e, pf2=pf2, gi=gi, hf=hf: e.activation(out=self.gb[gi][:, hf * 512:(hf + 1) * 512], in_=pf2[:], func=AF.Copy), R=[pf2], W=[self.gb[gi]])
            grow2 = self.sb(st, "grow2", [1, D], F32); a2row = self.sb(st, "a2row", [1, D], F32)
            self.dma('sp', grow2[:], self.norm_ffn[l:l + 1, :], W=[grow2])
            S.op('dve', lambda e: e.scalar_tensor_tensor(out=a2row[:], in0=self.modrow[0:1, 4 * D:5 * D], scalar=1.0, in1=grow2[:], op0=ALU.add, op1=ALU.mult),
                 R=[self.modrow, grow2], W=[a2row])
            for dstb, rowfn, rb in ((self.ab2, lambda hf: a2row[0:1, hf * 512:(hf + 1) * 512], a2row), (self.shb2, lambda hf: self.modrow[0:1, 3 * D + hf * 512: 3 * D + (hf + 1) * 512], self.modrow)):
                for hf in range(2):
                    pf2 = self.PF[3 + hf]
                    S.op('pe', lambda e, pf2=pf2, rowfn=rowfn, hf=hf: e.matmul(pf2[:], lhsT=onescol[0:1, :], rhs=rowfn(hf), start=True, stop=True), R=[onescol, rb], W=[pf2])
                    S.op('act', lambda e, pf2=pf2, dstb=dstb, hf=hf: e.activation(out=dstb[:, hf * 512:(hf + 1) * 512], in_=pf2[:], func=AF.Copy), R=[pf2], W=[dstb])
            self.dbg_out(f'mod{l}', self.modrow[0:1, :], [self.modrow])
            S.barrier(); S.emit()

    def norm_phase(self, l, which):
        S = self.S
        src = self.x if (l == 0 and which == 0) else self.xres
        gsrc = self.norm_mix if which == 0 else self.norm_ffn
        shc, scc = (0, 8) if which == 0 else (24, 32)
        with ExitStack() as st:
            grow = self.sb(st, "grow", [1, D], F32)
            self.dma('sp', grow[:], gsrc[l:l + 1, :], W=[grow])
            pf = self.row_to_cols(grow, lambda j: grow[0:1, j * 128:(j + 1) * 128], 8, None)
            acol = self.sb(st, "acol", [128, 8], F32)
            S.op('dve', lambda e: e.scalar_tensor_tensor(out=acol[:], in0=self.modT[:, scc:scc + 8], scalar=1.0, in1=pf[:, 0:8], op0=ALU.add, op1=ALU.mult),
                 R=[self.modT, pf], W=[acol])
            xt = [self.sb(st, f"xt{i}", [128, D], F32) for i in range(2)]
            xn = [self.sb(st, f"xn{i}", [128, D], BF16) for i in range(2)]
            junk = self.sb(st, "junk", [128, D], BF16)
            hrow = [self.sb(st, f"hrow{i}", [128, D], BF16) for i in range(2)] if which == 1 else None
            ssq = [self.sb(st, f"ssq{i}", [128, 1], F32) for i in range(2)]
            for i in range(NT):
                x_, xn_, ss = xt[i % 2], xn[i % 2], ssq[i % 2]
                self.dma('sp' if i % 2 == 0 else 'act', x_[:], src[i * 128:(i + 1) * 128, :], W=[x_])
                S.op('act', lambda e, x_=x_, ss=ss: e.activation(out=junk[:], in_=x_[:], func=AF.Square, accum_out=ss[:]), R=[x_], W=[junk, ss])
                S.op('dve', lambda e, ss=ss: e.tensor_scalar(out=ss[:], in0=ss[:], scalar1=1.0 / D, scalar2=1e-5, op0=ALU.mult, op1=ALU.add), R=[ss], W=[ss])
                S.op('act', lambda e, ss=ss: e.activation(out=ss[:], in_=ss[:], func=AF.Sqrt), R=[ss], W=[ss])
                S.op('dve', lambda e, ss=ss: e.reciprocal(out=ss[:], in_=ss[:]), R=[ss], W=[ss])
                S.op('dve', lambda e, x_=x_, xn_=xn_, ss=ss: e.tensor_scalar(out=xn_[:], in0=x_[:], scalar1=ss[:, 0:1], scalar2=None, op0=ALU.mult), R=[x_, ss], W=[xn_])
                pb = self.PB[i % 2]
                for j in range(8):
                    S.op('pe', lambda e, j=j, xn_=xn_, pb=pb: e.transpose(out=pb[:, j * 128:(j + 1) * 128], in_=xn_[:, j * 128:(j + 1) * 128], identity=self.ident[:]),
                         R=[xn_, self.ident], W=[pb])
                if which == 1:
                    hr = hrow[i % 2]
                    S.op('pool', lambda e, xn_=xn_, hr=hr: e.tensor_tensor(out=hr[:], in0=xn_[:], in1=self.ab2[:], op=ALU.mult), R=[xn_, self.ab2], W=[hr])
                    S.op('pool', lambda e, hr=hr: e.tensor_tensor(out=hr[:], in0=hr[:], in1=self.shb2[:], op=ALU.add), R=[hr, self.shb2], W=[hr])
                    self.dma('sp', self.hrow_d[i * 128:(i + 1) * 128, :], hr[:], R=[hr], W=[TB()])
                for j in range(8):
                    eng = 'dve' if j % 2 == 0 else 'pool'
                    if eng == 'pool':
                        eng = 'act'
                        S.op('act', lambda e, j=j, pb=pb, i=i: e.activation(out=self.hT[:, j, 1 + i * 128: 1 + (i + 1) * 128], in_=pb[:, j * 128:(j + 1) * 128], func=AF.Identity,
                                                                           scale=acol[:, j:j + 1], bias=self.modT[:, shc + j: shc + j + 1]), R=[pb, acol, self.modT], W=[self.hT])
                    else:
                        S.op('dve', lambda e, j=j, pb=pb, i=i: e.tensor_scalar(out=self.hT[:, j, 1 + i * 128: 1 + (i + 1) * 128], in0=pb[:, j * 128:(j + 1) * 128],
                                                                              scalar1=acol[:, j:j + 1], scalar2=self.modT[:, shc + j: shc + j + 1], op0=ALU.mult, op1=ALU.add),
                             R=[pb, acol, self.modT], W=[self.hT])
            if f'hT{l}' in self.dbg and which == 0:
                for j in range(8):
                    self.dbg_out(f'hT{l}', self.hT[:, j, 1:], [self.hT], dst=self.dbg[f'hT{l}'][j])
            S.barrier(); S.emit()

    def load_w(self, st, name, l, blocks):
        n = sum(WOFF[b][1] for b in blocks)
        wm = self.sb(st, name, [128, 8, n], BF16)
        o = 0; offs = {}
        for b in blocks:
            c0, cn = WOFF[b]
            for k in range(8):
                self.dma('pool', wm[:, k, o:o + cn], self.w_ext[l, k * 128:(k + 1) * 128, c0:c0 + cn], W=[wm])
            offs[b] = o; o += cn
        return wm, offs

    def proj_T(self, wm, c0, pf, tg, shift=0, start=True, stop=True, M=128):
        S = self.S
        for k in range(8):
            S.op('pe', lambda e, k=k: e.matmul(pf[0:M, :], lhsT=wm[:, k, c0:c0 + M], rhs=self.hT[:, k, 1 - shift + tg * 512: 1 - shift + (tg + 1) * 512],
                                              start=(start and k == 0), stop=(stop and k == 7)), R=[wm, self.hT], W=[pf])

    def proj_tok(self, wm, c0, n, pf_ap, pf, i, shift=0, start=True, stop=True):
        S = self.S
        for k in range(8):
            S.op('pe', lambda e, k=k: e.matmul(pf_ap, lhsT=self.hT[:, k, 1 - shift + i * 128: 1 - shift + (i + 1) * 128], rhs=wm[:, k, c0:c0 + n],
                                              start=(start and k == 0), stop=(stop and k == 7)), R=[wm, self.hT], W=[pf])

    def rope_tables(self, st, cname, name):
        S = self.S
        rc = self.load_const(st, cname, F32)
        C = self.sb(st, name + "C", [128, SEQ], BF16); Sg = self.sb(st, name + "S", [128, SEQ], BF16)
        CH = 512
        with ExitStack() as s2:
            posi = self.sb(s2, name + "pi", [128, CH], I32); posf = self.sb(s2, name + "pf", [128, CH], F32)
            u = self.sb(s2, name + "u", [128, CH], F32); ui = self.sb(s2, name + "ui", [128, CH], I32)
            uf = self.sb(s2, name + "uf", [128, CH], F32)
            for ch in range(SEQ // CH):
                cs = slice(ch * CH, (ch + 1) * CH)
                self.dma('sp', posi[:], self.pos[0:1, cs].partition_broadcast(128), W=[posi])
                S.op('dve', lambda e: e.tensor_copy(out=posf[:], in_=posi[:]), R=[posi], W=[posf])
                for phase, dst in ((0.0, Sg), (0.25, C)):
                    S.op('dve', lambda e, phase=phase: e.tensor_scalar(out=u[:], in0=posf[:], scalar1=rc[:, 0:1], scalar2=phase, op0=ALU.mult, op1=ALU.add),
                         R=[posf, rc], W=[u])
                    S.op('dve', lambda e: e.tensor_copy(out=ui[:], in_=u[:]), R=[u], W=[ui])
                    S.op('dve', lambda e: e.tensor_copy(out=uf[:], in_=ui[:]), R=[ui], W=[uf])
                    S.op('dve', lambda e: e.tensor_tensor(out=u[:], in0=u[:], in1=uf[:], op=ALU.subtract), R=[u, uf], W=[u])
                    S.op('dve', lambda e: e.tensor_scalar(out=u[:], in0=u[:], scalar1=0.5, scalar2=-0.5, op0=ALU.min, op1=ALU.max), R=[u], W=[u])
                    if dst is Sg:
                        S.op('act', lambda e: e.activation(out=uf[:], in_=u[:], func=AF.Sin, scale=2 * np.pi), R=[u], W=[uf])
                        S.op('dve', lambda e, cs=cs: e.tensor_scalar(out=Sg[:, cs], in0=uf[:], scalar1=rc[:, 1:2], scalar2=None, op0=ALU.mult), R=[uf, rc], W=[Sg])
                    else:
                        S.op('act', lambda e, cs=cs: e.activation(out=C[:, cs], in_=u[:], func=AF.Sin, scale=2 * np.pi), R=[u], W=[C])
            self.S.barrier(); self.S.emit()
        return C, Sg

    def y_store(self, ytile_idx, i, ytok, ystage, R):
        S = self.S
        pb = self.PB[i % 2]
        for f in range(2):
            S.op('pe', lambda e, f=f: e.transpose(out=pb[:, f * 128:(f + 1) * 128], in_=ytok[:, f * 128:(f + 1) * 128], identity=self.ident[:]),
                 R=[ytok, self.ident] + list(R), W=[pb])
        S.op('act', lambda e: e.activation(out=ystage[:, :, (i % 4) * 128:(i % 4 + 1) * 128], in_=pb[:, 0:256].rearrange("p (f t) -> p f t", f=2), func=AF.Copy),
             R=[pb], W=[ystage])
        if i % 4 == 3:
            g = i // 4
            for f in range(2):
                t = TB()
                self.dma('sp', self.yT_d[ytile_idx + f, :, g * 512:(g + 1) * 512], ystage[:, f, :], R=[ystage], W=[t])

    def retention_phase(self, l):
        S = self.S
        with ExitStack() as st:
            C, Sg = self.rope_tables(st, 'rope_ret', 'rr')
            gnb = self.bcast_row(st, "ret_gnb", self.ret_gn[l, :], 256)
            ytok_all = self.sb(st, "ret_ytok", [128, NT, 256], BF16)
            for hp in range(2):
                with ExitStack() as s2:
                    wm, wo = self.load_w(s2, "wm_ret", l, [f'ret_q{hp}', f'ret_qs{hp}', f'ret_k{hp}', f'ret_ks{hp}', f'ret_vg{hp}'])
                    QT = self.sb(s2, "QT", [128, SEQ], BF16); QS = self.sb(s2, "QS", [128, SEQ], BF16)
                    KT = self.sb(s2, "KT", [128, SEQ], BF16); KS = self.sb(s2, "KS", [128, SEQ], BF16)
                    qdec = self.load_const(s2, f'ret_qdec{hp}', BF16); kdec = self.load_const(s2, f'ret_kdec{hp}', F32)
                    dmask = self.load_const(s2, f'ret_dmask{hp}', F32); cd = self.load_const(s2, f'ret_cd{hp}', F32)
                    for name, dst in ((f'ret_q{hp}', QT), (f'ret_qs{hp}', QS), (f'ret_k{hp}', KT), (f'ret_ks{hp}', KS)):
                        for tg in range(8):
                            pf = self.PF[tg % 4]
                            self.proj_T(wm, wo[name], pf, tg)
                            eng = 'act' if tg % 2 == 0 else 'dve'
                            if eng == 'act':
                                S.op('act', lambda e, pf=pf, dst=dst, tg=tg: e.activation(out=dst[:, tg * 512:(tg + 1) * 512], in_=pf[:], func=AF.Copy), R=[pf], W=[dst])
                            else:
                                S.op('dve', lambda e, pf=pf, dst=dst, tg=tg: e.tensor_copy(out=dst[:, tg * 512:(tg + 1) * 512], in_=pf[:]), R=[pf], W=[dst])
                    for A, B_ in ((QT, QS), (KT, KS)):
                        S.op('dve', lambda e, A=A: e.tensor_tensor(out=A[:], in0=A[:], in1=C[:], op=ALU.mult), R=[A, C], W=[A])
                        S.op('pool', lambda e, B_=B_: e.tensor_tensor(out=B_[:], in0=B_[:], in1=Sg[:], op=ALU.mult), R=[B_, Sg], W=[B_])
                        S.op('dve', lambda e, A=A, B_=B_: e.tensor_tensor(out=A[:], in0=A[:], in1=B_[:], op=ALU.add), R=[A, B_], W=[A])
                    QD = QS
                    S.op('dve', lambda e: e.tensor_tensor(out=QD[:].rearrange("p (c n) -> p c n", n=128), in0=QT[:].rearrange("p (c n) -> p c n", n=128),
                                                         in1=qdec[:].unsqueeze(1).broadcast_to([128, NT, 128]), op=ALU.mult), R=[QT, qdec], W=[QD])
                    state = self.sb(s2, "rstate", [128, 128], F32); state_bf = self.sb(s2, "rstate_bf", [128, 128], BF16)
                    qbd = [self.sb(s2, f"qbd{i}", [128, 256], BF16) for i in range(2)]
                    for i in range(2):
                        S.op('pool', lambda e, i=i: e.memset(qbd[i][:], 0.0), W=[qbd[i]])
                    S.op('dve', lambda e: e.memset(state[:], 0.0), W=[state])
                    S.op('dve', lambda e: e.memset(state_bf[:], 0.0), W=[state_bf])
                    vg = [self.sb(s2, f"rvg{i}", [128, 128], BF16) for i in range(2)]
                    sg = [self.sb(s2, f"rsg{i}", [128, 128], F32) for i in range(2)]
                    kd = [self.sb(s2, f"rkd{i}", [128, 128], BF16) for i in range(2)]
                    pT = [self.sb(s2, f"rpT{i}", [128, 256], BF16) for i in range(2)]
                    o_sb = self.sb(s2, "ro", [128, 128], F32); cen = self.sb(s2, "rcen", [128, 128], F32); sq = self.sb(s2, "rsq", [128, 128], F32)
                    st4 = self.sb(s2, "rst4", [128, 4], F32)
                    for c in range(NT):
                        i2 = c % 2
                        tok = slice(c * 128, (c + 1) * 128)
                        pfv = self.PF[0]
                        self.proj_tok(wm, wo[f'ret_vg{hp}'], 256, pfv[:, 0:256], pfv, c)
                        S.op('dve', lambda e, i2=i2: e.tensor_copy(out=vg[i2][:], in_=pfv[:, 0:128]), R=[pfv], W=[vg[i2]])
                        S.op('act', lambda e, i2=i2: e.activation(out=sg[i2][:], in_=pfv[:, 128:256], func=AF.Silu), R=[pfv], W=[sg[i2]])
                        pb = self.PB[0]
                        S.op('pe', lambda e, tok=tok: e.transpose(out=pb[:, 0:128], in_=KT[:, tok], identity=self.ident[:]), R=[KT, self.ident], W=[pb])
                        S.op('dve', lambda e, i2=i2: e.tensor_tensor(out=kd[i2][:], in0=pb[:, 0:128], in1=kdec[:], op=ALU.mult), R=[pb, kdec], W=[kd[i2]])
                        pfs = self.PF[1]
                        for hh in range(2):
                            pr = slice(hh * 64, (hh + 1) * 64)
                            S.op('pool', lambda e, hh=hh, pr=pr, tok=tok, i2=i2: e.tensor_copy(out=qbd[i2][pr, hh * 128:(hh + 1) * 128], in_=QT[pr, tok]), R=[QT, qbd[i2]], W=[qbd[i2]])
                        S.op('pe', lambda e, tok=tok, i2=i2: e.matmul(pfs[:, 0:256], lhsT=KT[:, tok], rhs=qbd[i2][:], start=True, stop=True), R=[KT, qbd[i2]], W=[pfs])
                        S.op('dve', lambda e, i2=i2: e.tensor_tensor(out=pT[i2][:], in0=pfs[:, 0:256], in1=dmask[:], op=ALU.mult), R=[pfs, dmask], W=[pT[i2]])
                        pfo = self.PF[2]
                        S.op('pe', lambda e, tok=tok: e.matmul(pfo[:, 0:128], lhsT=QD[:, tok], rhs=state_bf[:, :], start=True, stop=False), R=[QD, state_bf], W=[pfo])
                        for hh in range(2):
                            S.op('pe', lambda e, hh=hh, i2=i2: e.matmul(pfo[:, hh * 64:(hh + 1) * 64], lhsT=pT[i2][:, hh * 128:(hh + 1) * 128], rhs=vg[i2][:, hh * 64:(hh + 1) * 64],
                                                                      start=False, stop=(hh == 1)), R=[pT[i2], vg[i2]], W=[pfo])
                        pfu = self.PF[3]
                        S.op('pe', lambda e, i2=i2: e.matmul(pfu[:, 0:128], lhsT=kd[i2][:], rhs=vg[i2][:], start=True, stop=True), R=[kd[i2], vg[i2]], W=[pfu])
                        for hh in range(2):
                            pr = slice(hh * 64, (hh + 1) * 64)
                            cs = slice(hh * 64, (hh + 1) * 64)
                            S.op('dve', lambda e, hh=hh, pr=pr, cs=cs: e.scalar_tensor_tensor(out=state[pr, cs], in0=state[pr, cs], scalar=cd[pr, 0:1], in1=pfu[pr, cs],
                                                                                            op0=ALU.mult, op1=ALU.add), R=[state, cd, pfu], W=[state])
                        S.op('act', lambda e: e.activation(out=state_bf[:], in_=state[:], func=AF.Copy), R=[state], W=[state_bf])
                        S.op('act', lambda e: e.activation(out=o_sb[:], in_=pfo[:, 0:128], func=AF.Copy), R=[pfo], W=[o_sb])
                        self.head_norm(o_sb, cen, sq, st4, 2, 1e-5)
                        S.op('dve', lambda e, hp=hp: e.tensor_tensor(out=cen[:], in0=cen[:], in1=gnb[:, hp * 128:(hp + 1) * 128], op=ALU.mult), R=[cen, gnb], W=[cen])
                        S.op('dve', lambda e, i2=i2, c=c, hp=hp: e.tensor_tensor(out=ytok_all[:, c, hp * 128:(hp + 1) * 128], in0=cen[:], in1=sg[i2][:], op=ALU.mult),
                             R=[cen, sg[i2]], W=[ytok_all])
                    S.barrier(); S.emit()
            ystage = self.sb(st, "ystage", [128, 2, 512], BF16)
            for i in range(NT):
                self.y_store(0, i, _View(ytok_all, i), ystage, [])
            S.barrier(); S.emit()


    def rms_finalize(self, st, o_all, gain_b, ytile_idx, name):
        S = self.S
        ystage = self.sb(st, name + "ystage", [128, 2, 512], BF16)
        junk = self.sb(st, name + "junk", [128, 256], F32)
        ss = [self.sb(st, f"{name}ss{i}", [128, 1], F32) for i in range(2)]
        yt = [self.sb(st, f"{name}yt{i}", [128, 256], BF16) for i in range(2)]
        for i in range(NT):
            s_, y_ = ss[i % 2], yt[i % 2]
            S.op('act', lambda e, i=i, s_=s_: e.activation(out=junk[:], in_=o_all[:, i, :], func=AF.Square, accum_out=s_[:]), R=[o_all], W=[junk, s_])
            S.op('dve', lambda e, s_=s_: e.tensor_scalar(out=s_[:], in0=s_[:], scalar1=1.0 / 256, scalar2=1e-5, op0=ALU.mult, op1=ALU.add), R=[s_], W=[s_])
            S.op('act', lambda e, s_=s_: e.activation(out=s_[:], in_=s_[:], func=AF.Sqrt), R=[s_], W=[s_])
            S.op('dve', lambda e, s_=s_: e.reciprocal(out=s_[:], in_=s_[:]), R=[s_], W=[s_])
            S.op('dve', lambda e, i=i, s_=s_, y_=y_: e.scalar_tensor_tensor(out=y_[:], in0=o_all[:, i, :], scalar=s_[:, 0:1], in1=gain_b[:], op0=ALU.mult, op1=ALU.mult),
                 R=[o_all, s_, gain_b], W=[y_])
            self.y_store(ytile_idx, i, y_, ystage, [])

    def sb_phase(self, l):
        S = self.S
        with ExitStack() as st:
            ntri = self.load_const(st, 'ntri_ge', BF16); nones = self.load_const(st, 'nones', BF16)
            sbmask = self.load_const(st, 'sbmask', BF16)
            zer = self.sb(st, "sbzero", [128, 256], BF16)
            S.op('pool', lambda e: e.memset(zer[:], 0.0), W=[zer])
            onb = self.bcast_row(st, "sb_onb", self.sb_onorm[l, :], 256)
            o_all = self.sb(st, "sb_oall", [128, NT, 256], BF16)
            for hp in range(2):
                with ExitStack() as s2:
                    wm, wo = self.load_w(s2, "wm_sb", l, [f'sb_q{hp}', f'sb_k{hp}', 'sb_v'])
                    QT = self.sb(s2, "sbQT", [128, SEQ], BF16)
                    KM = [self.sb(s2, f"sbKM{i}", [128, SEQ], BF16) for i in range(2)]
                    V = self.sb(s2, "sbV", [128, NT, 128], BF16)
                    for i in range(2):
                        S.op('pool', lambda e, i=i: e.memset(KM[i][:], 0.0), W=[KM[i]])
                    for tg in range(8):
                        pf = self.PF[tg % 2]
                        self.proj_T(wm, wo[f'sb_q{hp}'], pf, tg)
                        S.op('act', lambda e, pf=pf, tg=tg: e.activation(out=QT[:, tg * 512:(tg + 1) * 512], in_=pf[:], func=AF.Copy, scale=0.125), R=[pf], W=[QT])
                        pf2 = self.PF[2 + tg % 2]
                        self.proj_T(wm, wo[f'sb_k{hp}'], pf2, tg)
                        S.op('dve', lambda e, pf2=pf2, tg=tg: e.tensor_copy(out=KM[0][0:64, tg * 512:(tg + 1) * 512], in_=pf2[0:64, :]), R=[pf2], W=[KM[0]])
                        S.op('act', lambda e, pf2=pf2, tg=tg: e.activation(out=KM[1][64:128, tg * 512:(tg + 1) * 512], in_=pf2[64:128, :], func=AF.Copy), R=[pf2], W=[KM[1]])
                    for i in range(NT):
                        pf = self.PF[i % 2]
                        self.proj_tok(wm, wo['sb_v'] + hp * 128, 128, pf[:, 0:128], pf, i)
                        S.op('dve', lambda e, pf=pf, i=i: e.tensor_copy(out=V[:, i, :], in_=pf[:, 0:128]), R=[pf], W=[V])
                    ebuf = [[self.sb(s2, f"sbe{h}{i}", [128, 512], F32) for i in range(2)] for h in range(2)]
                    spm = [[self.sb(s2, f"sbsp{h}{i}", [128, 512], BF16) for i in range(2)] for h in range(2)]
                    tbuf = [[self.sb(s2, f"sbt{h}{i}", [128, 512], F32) for i in range(2)] for h in range(2)]
                    abuf = [[self.sb(s2, f"sba{h}{i}", [128, 512], BF16) for i in range(2)] for h in range(2)]
                    racc = [self.sb(s2, f"sbracc{h}", [128, 512], F32) for h in range(2)]
                    cnt = 0
                    for g in range(8):
                        qs = slice(g * 512, (g + 1) * 512)
                        for hh in range(2):
                            po = self.PF[4 + hh]
                            S.op('pool', lambda e, hh=hh: e.memset(racc[hh][:], 0.0), W=[racc[hh]])
                            S.op('pe', lambda e, po=po: e.matmul(po[:, 0:256], lhsT=zer[:, 0:128], rhs=zer[:, 0:256], start=True, stop=False), R=[zer], W=[po])
                        nkb = 4 * g + 4
                        for kb in reversed(range(nkb)):
                            b2 = cnt % 2; cnt += 1
                            r = kb - 4 * g
                            ks = slice(kb * 128, (kb + 1) * 128)
                            HH = (0, 1)
                            E_ = [ebuf[h][b2] for h in HH]; SP_ = [spm[h][b2] for h in HH]; T_ = [tbuf[h][b2] for h in HH]; A_ = [abuf[h][b2] for h in HH]
                            PZ = [self.PF[0], self.PF[1]]; PC = [self.PF[2], self.PF[3]]; PO = [self.PF[4], self.PF[5]]
                            for hh in HH:
                                S.op('pe', lambda e, hh=hh, ks=ks, qs=qs: e.matmul(PZ[hh][:], lhsT=KM[hh][:, ks], rhs=QT[:, qs], start=True, stop=True), R=[KM[hh], QT], W=[PZ[hh]])
                            for hh in HH:
                                S.op('act', lambda e, hh=hh, E_=E_: e.activation(out=E_[hh][:], in_=PZ[hh][:], func=AF.Exp), R=[PZ[hh]], W=[E_[hh]])
                            for hh in HH:
                                S.op('act', lambda e, hh=hh, E_=E_, SP_=SP_: e.activation(out=SP_[hh][:], in_=E_[hh][:], func=AF.Ln, bias=1.0), R=[E_[hh]], W=[SP_[hh]])
                            if r >= 0:
                                for hh in HH:
                                    S.op('pool', lambda e, hh=hh, SP_=SP_, r=r: e.tensor_tensor(out=SP_[hh][:], in0=SP_[hh][:], in1=sbmask[:, r * 512:(r + 1) * 512], op=ALU.mult), R=[SP_[hh], sbmask], W=[SP_[hh]])
                            for hh in HH:
                                S.op('pe', lambda e, hh=hh, ks=ks, qs=qs: e.matmul(PC[hh][:], lhsT=KM[hh][:, ks], rhs=QT[:, qs], start=True, stop=False), R=[KM[hh], QT], W=[PC[hh]])
                                S.op('pe', lambda e, hh=hh, SP_=SP_: e.matmul(PC[hh][:], lhsT=ntri[:], rhs=SP_[hh][:], start=False, stop=True), R=[ntri, SP_[hh]], W=[PC[hh]])
                            for hh in HH:
                                S.op('dve', lambda e, hh=hh, T_=T_: e.tensor_tensor(out=T_[hh][:], in0=PC[hh][:], in1=racc[hh][:], op=ALU.add), R=[PC[hh], racc[hh]], W=[T_[hh]])
                            for hh in HH:
                                S.op('act', lambda e, hh=hh, T_=T_, A_=A_: e.activation(out=A_[hh][:], in_=T_[hh][:], func=AF.Exp), R=[T_[hh]], W=[A_[hh]])
                            if r >= 0:
                                for hh in HH:
                                    S.op('pool', lambda e, hh=hh, A_=A_, r=r: e.tensor_tensor(out=A_[hh][:], in0=A_[hh][:], in1=sbmask[:, r * 512:(r + 1) * 512], op=ALU.mult), R=[A_[hh], sbmask], W=[A_[hh]])
                            if kb > 0:
                                for hh in HH:
                                    S.op('pe', lambda e, hh=hh, SP_=SP_: e.matmul(PZ[hh][:], lhsT=nones[:], rhs=SP_[hh][:], start=True, stop=True), R=[nones, SP_[hh]], W=[PZ[hh]])
                                for hh in HH:
                                    S.op('dve', lambda e, hh=hh: e.tensor_tensor(out=racc[hh][:], in0=PZ[hh][:], in1=racc[hh][:], op=ALU.add), R=[PZ[hh], racc[hh]], W=[racc[hh]])
                            for hh in HH:
                                for qb in range(4):
                                    if r >= 0 and qb < r:
                                        continue
                                    S.op('pe', lambda e, qb=qb, hh=hh, A_=A_, kb=kb: e.matmul(PO[hh][:, qb * 64:(qb + 1) * 64], lhsT=A_[hh][:, qb * 128:(qb + 1) * 128], rhs=V[:, kb, hh * 64:(hh + 1) * 64],
                                                                                         start=False, stop=(kb == 0 and qb == 3)), R=[A_[hh], V], W=[PO[hh]])
                        for hh in range(2):
                            hcol = (hp * 2 + hh) * 64
                            po = self.PF[4 + hh]
                            S.op('act', lambda e, g=g, hcol=hcol, po=po: e.activation(out=o_all[:, 4 * g:4 * g + 4, hcol:hcol + 64], in_=po[:, 0:256].rearrange("p (q d) -> p q d", d=64), func=AF.Copy),
                                 R=[po], W=[o_all])
                    S.barrier(); S.emit()
            self.rms_finalize(st, o_all, onb, 6, "sbf")
            S.barrier(); S.emit()


    def _chk(self, n):
        if self.flags.get('rw_stop') == n:
            raise _Stop()

    def rwkv_phase(self, l):
        try:
            self.rwkv_phase_(l)
        except _Stop:
            pass

    def rwkv_phase_(self, l):
        S = self.S
        V3 = lambda ap, d=64: ap.rearrange("p (h d) -> p h d", d=d)
        with ExitStack() as st:
          try:
              wm, wo = self.load_w(st, "wm_rw", l, ['rw_rkv', 'rw_lora'])
              wmu = self.sb(st, "rw_wmu", [128, 8, 896], BF16)
              mub = self.bcast_row(st, "rw_mub", self.rwkv_mu[l, :], 896)
              S.op('dve', lambda e: e.tensor_tensor(out=wmu[:], in0=wm[:], in1=mub[:].unsqueeze(1).broadcast_to([128, 8, 896]), op=ALU.mult), R=[wm, mub], W=[wmu])
              S.op('pool', lambda e: e.tensor_tensor(out=wm[:], in0=wm[:], in1=wmu[:], op=ALU.subtract), R=[wm, wmu], W=[wm])
              lwbd = self.sb(st, "rw_lwbd", [128, 768], BF16)
              S.op('pool', lambda e: e.memset(lwbd[:], 0.0), W=[lwbd])
              for (r0, r1, c0) in ((0, 32, 0), (32, 64, 256), (64, 128, 512)):
                  self.dma('pool', lwbd[r0:r1, c0:c0 + 256], self.rwkv_lw[l, r0:r1, :], R=[lwbd], W=[lwbd])
              w0b = self.bcast_row(st, "rw_w0b", self.rwkv_w0[l, :], 256); a0b = self.bcast_row(st, "rw_a0b", self.rwkv_a0[l, :], 256)
              kkb = self.bcast_row(st, "rw_kkb", self.rwkv_kk[l, :], 256); kab = self.bcast_row(st, "rw_kab", self.rwkv_ka[l, :], 256)
              rkb = self.bcast_row(st, "rw_rkb", self.rwkv_rk[l, :], 256); lnb = self.bcast_row(st, "rw_lnb", self.rwkv_ln[l, :], 256)
              tri_le = self.load_const(st, 'tri_le', F32); lastc = self.load_const(st, 'last', F32)
              m_lt = self.load_const(st, 'tri_lt', F32); m_le = self.load_const(st, 'tri_le', F32); m_gt = self.load_const(st, 'tri_gt', F32)
              LT = self.sb(st, "rw_LT", [128, SEQ], BF16)
              for tg in range(8):
                  pf = self.PF[tg % 2]
                  self.proj_T(wm, wo['rw_lora'], pf, tg, shift=0, start=True, stop=False)
                  self.proj_T(wmu, wo['rw_lora'], pf, tg, shift=1, start=False, stop=True)
                  sl = slice(tg * 512, (tg + 1) * 512)
                  S.op('act', lambda e, pf=pf, sl=sl: e.activation(out=LT[0:32, sl], in_=pf[0:32, :], func=AF.Tanh), R=[pf], W=[LT])
                  S.op('act', lambda e, pf=pf, sl=sl: e.activation(out=LT[32:64, sl], in_=pf[32:64, :], func=AF.Copy), R=[pf], W=[LT])
                  S.op('act', lambda e, pf=pf, sl=sl: e.activation(out=LT[64:128, sl], in_=pf[64:128, :], func=AF.Sigmoid), R=[pf], W=[LT])
              self._chk(1)
              f32t = lambda n: self.sb(st, "rw_" + n, [128, 256], F32)
              bft = lambda n: self.sb(st, "rw_" + n, [128, 256], BF16)
              r_sb, k_sb, v_sb, a_sb, kk_sb, k2_sb, lw_sb, cum_sb, t1, t2, t3 = [f32t(n) for n in ('r', 'k', 'v', 'a', 'kk', 'k2', 'lw', 'cum', 't1', 't2', 't3')]
              gate_sb = f32t('gate')
              rt_b, kt_b, bt_b, at_b, v_bf, G_bf, U_bf = [bft(n) for n in ('rt', 'kt', 'bt', 'at', 'vbf', 'G', 'U')]
              st4 = self.sb(st, "rw_st4", [128, 4], F32)
              fm = self.sb(st, "rw_fm", [128, 8, 128], BF16)
              artbd = [self.sb(st, f"rw_artbd{p}", [128, 2, 256], BF16) for p in range(2)]
              btbd = [self.sb(st, f"rw_btbd{p}", [128, 2, 128], BF16) for p in range(2)]
              import os
              SKIP = os.environ.get('RW_SKIP', '').split(',')
              for p in range(2):
                  if 'b' in SKIP: break
                  S.op('pool', lambda e, p=p: e.memset(artbd[p][:], 0.0), W=[artbd[p]])
                  S.op('pool', lambda e, p=p: e.memset(btbd[p][:], 0.0), W=[btbd[p]])
              NU = [self.sb(st, f"rw_NU{i}", [128, 4, 128], F32) for i in range(2)]
              LL = [self.sb(st, f"rw_LL{i}", [128, 4, 128], F32) for i in range(2)]
              XX = [self.sb(st, f"rw_X{i}", [128, 4, 128], F32) for i in range(2)]
              G_f = self.sb(st, "rw_Gf", [128, 256], F32)
              RBm = self.sb(st, "rw_RB", [128, 4, 128], BF16); RKm = self.sb(st, "rw_RK", [128, 4, 128], BF16); MKm = self.sb(st, "rw_MK", [128, 4, 128], BF16)
              ST = [self.sb(st, f"rw_ST{p}", [128, 128], F32) for p in range(2)]
              STb = [self.sb(st, f"rw_STb{p}", [128, 128], BF16) for p in range(2)]
              ecl = self.sb(st, "rw_ecl", [128, 2], F32)
              for p in range(2):
                  if 'c' in SKIP: break
                  S.op('dve', lambda e, p=p: e.memset(ST[p][:], 0.0), W=[ST[p]])
                  S.op('dve', lambda e, p=p: e.memset(STb[p][:], 0.0), W=[STb[p]])
              o_sb = f32t('o'); cen = f32t('cen'); sq = f32t('sq')
              ystage = self.sb(st, "rw_ystage", [128, 2, 512], BF16)
              ytok = [self.sb(st, f"rw_ytok{i}", [128, 256], BF16) for i in range(2)]
              PF = self.PF
              for c in range(NT):
                  tok = slice(1 + c * 128, 1 + (c + 1) * 128); tokp = slice(c * 128, (c + 1) * 128)
                  for (pf, c0, n) in ((PF[0], 0, 512), (PF[1], 512, 256)):
                      if 'd' in SKIP: break
                      for k in range(8):
                          S.op('pe', lambda e, k=k, pf=pf, c0=c0, n=n, tok=tok: e.matmul(pf[:, 0:n], lhsT=self.hT[:, k, tok], rhs=wm[:, k, c0:c0 + n], start=(k == 0), stop=False), R=[wm, self.hT], W=[pf])
                      for k in range(8):
                          S.op('pe', lambda e, k=k, pf=pf, c0=c0, n=n, tokp=tokp: e.matmul(pf[:, 0:n], lhsT=self.hT[:, k, tokp], rhs=wmu[:, k, c0:c0 + n], start=False, stop=(k == 7)), R=[wmu, self.hT], W=[pf])
                  if 'e' in SKIP: self._chk(2)
                  S.op('act', lambda e: e.activation(out=r_sb[:], in_=PF[0][:, 0:256], func=AF.Copy), R=[PF[0]], W=[r_sb])
                  S.op('dve', lambda e: e.tensor_copy(out=k_sb[:], in_=PF[0][:, 256:512]), R=[PF[0]], W=[k_sb])
                  S.op('act', lambda e: e.activation(out=v_sb[:], in_=PF[1][:, 0:256], func=AF.Copy), R=[PF[1]], W=[v_sb])
                  if 'f' not in SKIP:
                      S.op('pool', lambda e: e.tensor_copy(out=v_bf[:], in_=v_sb[:]), R=[v_sb], W=[v_bf])
                  else:
                      S.op('dve', lambda e: e.tensor_copy(out=v_bf[:], in_=v_sb[:]), R=[v_sb], W=[v_bf])
                  self._chk(2)
                  S.op('pe', lambda e, c=c: e.matmul(PF[2][:, 0:512], lhsT=LT[:, c * 128:(c + 1) * 128], rhs=lwbd[:, 0:512], start=True, stop=True), R=[LT, lwbd], W=[PF[2]])
                  S.op('pe', lambda e, c=c: e.matmul(PF[3][:, 0:256], lhsT=LT[:, c * 128:(c + 1) * 128], rhs=lwbd[:, 512:768], start=True, stop=True), R=[LT, lwbd], W=[PF[3]])
                  S.op('act', lambda e: e.activation(out=gate_sb[:], in_=PF[3][:, 0:256], func=AF.Copy), R=[PF[3]], W=[gate_sb])
                  self._chk(3)
                  S.op('dve', lambda e: e.tensor_tensor(out=t1[:], in0=PF[2][:, 0:256], in1=w0b[:], op=ALU.add), R=[PF[2], w0b], W=[t1])
                  S.op('act', lambda e: e.activation(out=t1[:], in_=t1[:], func=AF.Sigmoid), R=[t1], W=[t1])
                  S.op('dve', lambda e: e.tensor_scalar(out=lw_sb[:], in0=t1[:], scalar1=-0.6065306597126334, scalar2=None, op0=ALU.mult), R=[t1], W=[lw_sb])
                  S.op('dve', lambda e: e.tensor_tensor(out=t2[:], in0=PF[2][:, 256:512], in1=a0b[:], op=ALU.add), R=[PF[2], a0b], W=[t2])
                  S.op('act', lambda e: e.activation(out=a_sb[:], in_=t2[:], func=AF.Sigmoid), R=[t2], W=[a_sb])
                  S.op('dve', lambda e: e.tensor_tensor(out=kk_sb[:], in0=k_sb[:], in1=kkb[:], op=ALU.mult), R=[k_sb, kkb], W=[kk_sb])
                  S.op('pool', lambda e: e.tensor_tensor(out=t3[:], in0=kk_sb[:], in1=kk_sb[:], op=ALU.mult), R=[kk_sb], W=[t3])
                  S.op('dve', lambda e: e.tensor_reduce(out=st4[:], in_=V3(t3[:]), axis=AX.X, op=ALU.add), R=[t3], W=[st4])
                  S.op('act', lambda e: e.activation(out=st4[:], in_=st4[:], func=AF.Sqrt), R=[st4], W=[st4])
                  S.op('dve', lambda e: e.tensor_scalar(out=st4[:], in0=st4[:], scalar1=1e-12, scalar2=None, op0=ALU.max), R=[st4], W=[st4])
                  S.op('dve', lambda e: e.reciprocal(out=st4[:], in_=st4[:]), R=[st4], W=[st4])
                  S.op('dve', lambda e: e.tensor_tensor(out=V3(kk_sb[:]), in0=V3(kk_sb[:]), in1=st4[:].unsqueeze(2).broadcast_to([128, 4, 64]), op=ALU.mult), R=[kk_sb, st4], W=[kk_sb])
                  S.op('dve', lambda e: e.scalar_tensor_tensor(out=t2[:], in0=a_sb[:], scalar=-1.0, in1=kab[:], op0=ALU.add, op1=ALU.mult), R=[a_sb, kab], W=[t2])
                  S.op('dve', lambda e: e.scalar_tensor_tensor(out=k2_sb[:], in0=t2[:], scalar=1.0, in1=k_sb[:], op0=ALU.add, op1=ALU.mult), R=[t2, k_sb], W=[k2_sb])
                  self._chk(4)
                  S.op('pe', lambda e: e.matmul(PF[4][:, 0:256], lhsT=tri_le[:], rhs=lw_sb[:], start=True, stop=True), R=[tri_le, lw_sb], W=[PF[4]])
                  S.op('act', lambda e: e.activation(out=cum_sb[:], in_=PF[4][:, 0:256], func=AF.Copy), R=[PF[4]], W=[cum_sb])
                  S.op('act', lambda e: e.activation(out=t1[:], in_=PF[4][:, 0:256], func=AF.Exp), R=[PF[4]], W=[t1])
                  S.op('act', lambda e: e.activation(out=t2[:], in_=PF[4][:, 0:256], func=AF.Exp, scale=-1.0), R=[PF[4]], W=[t2])
                  S.op('dve', lambda e: e.tensor_tensor(out=t3[:], in0=cum_sb[:], in1=lw_sb[:], op=ALU.subtract), R=[cum_sb, lw_sb], W=[t3])
                  S.op('act', lambda e: e.activation(out=t3[:], in_=t3[:], func=AF.Exp), R=[t3], W=[t3])
                  S.op('dve', lambda e: e.tensor_tensor(out=rt_b[:], in0=r_sb[:], in1=t1[:], op=ALU.mult), R=[r_sb, t1], W=[rt_b])
                  S.op('pool', lambda e: e.tensor_tensor(out=kt_b[:], in0=k2_sb[:], in1=t2[:], op=ALU.mult), R=[k2_sb, t2], W=[kt_b])
                  S.op('dve', lambda e: e.scalar_tensor_tensor(out=at_b[:], in0=kk_sb[:], scalar=-1.0, in1=t3[:], op0=ALU.mult, op1=ALU.mult), R=[kk_sb, t3], W=[at_b])
                  S.op('dve', lambda e: e.tensor_tensor(out=t3[:], in0=kk_sb[:], in1=a_sb[:], op=ALU.mult), R=[kk_sb, a_sb], W=[t3])
                  S.op('dve', lambda e: e.tensor_tensor(out=bt_b[:], in0=t3[:], in1=t2[:], op=ALU.mult), R=[t3, t2], W=[bt_b])
                  self._chk(5)
                  for p in range(2):
                      S.op('pe', lambda e, p=p: e.matmul(PF[5][:, p:p + 1], lhsT=cum_sb[:, p * 128:(p + 1) * 128], rhs=lastc[:, 0:1], start=True, stop=True), R=[cum_sb, lastc], W=[PF[5]])
                  S.op('act', lambda e: e.activation(out=ecl[:], in_=PF[5][:, 0:2], func=AF.Exp), R=[PF[5]], W=[ecl])
                  self._chk(6)
                  pb = self.PB[0]
                  for xi, src_ in enumerate((at_b, rt_b, bt_b, kt_b)):
                      for p in range(2):
                          j = xi * 2 + p
                          S.op('pe', lambda e, j=j, src_=src_, p=p: e.transpose(out=pb[:, j * 128:(j + 1) * 128], in_=src_[:, p * 128:(p + 1) * 128], identity=self.ident[:]), R=[src_, self.ident], W=[pb])
                  S.op('act', lambda e: e.activation(out=fm[:].rearrange("p j t -> p (j t)"), in_=pb[:], func=AF.Copy), R=[pb], W=[fm])
                  for p in range(2):
                      for hh in range(2):
                          pr = slice(hh * 64, (hh + 1) * 64)
                          S.op('pool', lambda e, p=p, hh=hh, pr=pr: e.tensor_copy(out=artbd[p][pr, hh, 0:128], in_=fm[pr, 0 + p, :]), R=[fm, artbd[p]], W=[artbd[p]])
                          S.op('pool', lambda e, p=p, hh=hh, pr=pr: e.tensor_copy(out=artbd[p][pr, hh, 128:256], in_=fm[pr, 2 + p, :]), R=[fm, artbd[p]], W=[artbd[p]])
                          S.op('pool', lambda e, p=p, hh=hh, pr=pr: e.tensor_copy(out=btbd[p][pr, hh, :], in_=fm[pr, 4 + p, :]), R=[fm, btbd[p]], W=[btbd[p]])
                  self._chk(7)
                  for p in range(2):
                      P1, P2, P3 = PF[0], PF[1], PF[2]
                      S.op('pe', lambda e, p=p: e.matmul(P1[:, 0:512], lhsT=fm[:, 4 + p, :], rhs=artbd[p][:].rearrange("p h c -> p (h c)"), start=True, stop=True), R=[fm, artbd[p]], W=[P1])
                      S.op('pe', lambda e, p=p: e.matmul(P2[:, 0:512], lhsT=fm[:, 6 + p, :], rhs=artbd[p][:].rearrange("p h c -> p (h c)"), start=True, stop=True), R=[fm, artbd[p]], W=[P2])
                      S.op('pe', lambda e, p=p: e.matmul(P3[:, 0:256], lhsT=fm[:, 0 + p, :], rhs=btbd[p][:].rearrange("p h c -> p (h c)"), start=True, stop=True), R=[fm, btbd[p]], W=[P3])
                      hs = slice(2 * p, 2 * p + 2)
                      v4 = lambda pf_: pf_[:, 0:512].rearrange("p (h w t) -> p h w t", h=2, w=2)
                      bc = lambda m: m[:].unsqueeze(1).broadcast_to([128, 2, 128])
                      S.op('dve', lambda e, hs=hs: e.tensor_tensor(out=NU[0][:, hs, :], in0=v4(P1)[:, :, 0, :], in1=bc(m_lt), op=ALU.mult), R=[P1, m_lt], W=[NU[0]])
                      S.op('dve', lambda e, hs=hs: e.tensor_tensor(out=RBm[:, hs, :], in0=v4(P1)[:, :, 1, :], in1=bc(m_le), op=ALU.mult), R=[P1, m_le], W=[RBm])
                      S.op('dve', lambda e, hs=hs: e.tensor_tensor(out=MKm[:, hs, :], in0=v4(P2)[:, :, 0, :], in1=bc(m_lt), op=ALU.mult), R=[P2, m_lt], W=[MKm])
                      S.op('dve', lambda e, hs=hs: e.tensor_tensor(out=RKm[:, hs, :], in0=v4(P2)[:, :, 1, :], in1=bc(m_le), op=ALU.mult), R=[P2, m_le], W=[RKm])
                      S.op('dve', lambda e, hs=hs: e.tensor_tensor(out=LL[0][:, hs, :], in0=P3[:, 0:256].rearrange("p (h t) -> p h t", h=2), in1=bc(m_gt), op=ALU.mult), R=[P3, m_gt], W=[LL[0]])
                  self._chk(8)
                  S.op('dve', lambda e: e.tensor_tensor(out=XX[0][:], in0=NU[0][:], in1=self.identf[:].unsqueeze(1).broadcast_to([128, 4, 128]), op=ALU.add), R=[NU[0], self.identf], W=[XX[0]])
                  cur = 0
                  for it in range(6):
                      nxt = 1 - cur
                      PL, PN, PX = PF[3], PF[4], PF[5]
                      for h in range(4):
                          S.op('pe', lambda e, h=h, cur=cur: e.matmul(PL[:, h * 128:(h + 1) * 128], lhsT=NU[cur][:, h, :], rhs=LL[cur][:, h, :], start=True, stop=True), R=[NU[cur], LL[cur]], W=[PL])
                      if it < 5:
                          for h in range(4):
                              S.op('pe', lambda e, h=h, cur=cur: e.matmul(PN[:, h * 128:(h + 1) * 128], lhsT=LL[cur][:, h, :], rhs=NU[cur][:, h, :], start=True, stop=True), R=[NU[cur], LL[cur]], W=[PN])
                      S.op('act', lambda e, nxt=nxt: e.activation(out=LL[nxt][:].rearrange("p h t -> p (h t)"), in_=PL[:], func=AF.Copy), R=[PL], W=[LL[nxt]])
                      if it < 5:
                          S.op('dve', lambda e, nxt=nxt: e.tensor_copy(out=NU[nxt][:].rearrange("p h t -> p (h t)"), in_=PN[:]), R=[PN], W=[NU[nxt]])
                      for h in range(4):
                          S.op('pe', lambda e, h=h, cur=cur, nxt=nxt: e.matmul(PX[:, h * 128:(h + 1) * 128], lhsT=LL[nxt][:, h, :], rhs=XX[cur][:, h, :], start=True, stop=True), R=[LL[nxt], XX[cur]], W=[PX])
                      S.op('dve', lambda e, cur=cur, nxt=nxt: e.tensor_tensor(out=XX[nxt][:].rearrange("p h t -> p (h t)"), in0=PX[:], in1=XX[cur][:].rearrange("p h t -> p (h t)"), op=ALU.add),
                           R=[PX, XX[cur]], W=[XX[nxt]])
                      cur = nxt
                  X = XX[cur]
                  self._chk(9)
                  PG, PU, PY, PS_ = PF[0], PF[1], PF[2], PF[3]
                  for p in range(2):
                      S.op('pe', lambda e, p=p: e.matmul(PG[:, p * 128:(p + 1) * 128], lhsT=fm[:, 0 + p, :], rhs=STb[p][:], start=True, stop=False), R=[fm, STb[p]], W=[PG])
                      for hh in range(2):
                          h = 2 * p + hh
                          S.op('pe', lambda e, h=h, hh=hh: e.matmul(PG[:, h * 64:(h + 1) * 64], lhsT=MKm[:, h, :], rhs=v_bf[:, h * 64:(h + 1) * 64], start=False, stop=(hh == 1)), R=[MKm, v_bf], W=[PG])
                  S.op('act', lambda e: e.activation(out=G_f[:], in_=PG[:, 0:256], func=AF.Copy), R=[PG], W=[G_f])
                  for h in range(4):
                      S.op('pe', lambda e, h=h, X=X: e.matmul(PU[:, h * 64:(h + 1) * 64], lhsT=X[:, h, :], rhs=G_f[:, h * 64:(h + 1) * 64], start=True, stop=True), R=[X, G_f], W=[PU])
                  S.op('dve', lambda e: e.tensor_copy(out=U_bf[:], in_=PU[:, 0:256]), R=[PU], W=[U_bf])
                  for p in range(2):
                      S.op('pe', lambda e, p=p: e.matmul(PY[:, p * 128:(p + 1) * 128], lhsT=fm[:, 2 + p, :], rhs=STb[p][:], start=True, stop=False), R=[fm, STb[p]], W=[PY])
                      for hh in range(2):
                          h = 2 * p + hh
                          S.op('pe', lambda e, h=h: e.matmul(PY[:, h * 64:(h + 1) * 64], lhsT=RBm[:, h, :], rhs=U_bf[:, h * 64:(h + 1) * 64], start=False, stop=False), R=[RBm, U_bf], W=[PY])
                          S.op('pe', lambda e, h=h, hh=hh: e.matmul(PY[:, h * 64:(h + 1) * 64], lhsT=RKm[:, h, :], rhs=v_bf[:, h * 64:(h + 1) * 64], start=False, stop=(hh == 1)), R=[RKm, v_bf], W=[PY])
                  S.op('act', lambda e: e.activation(out=o_sb[:], in_=PY[:, 0:256], func=AF.Copy), R=[PY], W=[o_sb])
                  for p in range(2):
                      cs_ = slice(p * 128, (p + 1) * 128)
                      S.op('pe', lambda e, cs_=cs_: e.matmul(PS_[:, cs_], lhsT=bt_b[:, cs_], rhs=U_bf[:, cs_], start=True, stop=False), R=[bt_b, U_bf], W=[PS_])
                      S.op('pe', lambda e, cs_=cs_: e.matmul(PS_[:, cs_], lhsT=kt_b[:, cs_], rhs=v_bf[:, cs_], start=False, stop=True), R=[kt_b, v_bf], W=[PS_])
                      for hh in range(2):
                          pr = slice(hh * 64, (hh + 1) * 64); cc = slice(hh * 64, (hh + 1) * 64); pc_ = slice(p * 128 + hh * 64, p * 128 + (hh + 1) * 64)
                          S.op('dve', lambda e, p=p, pr=pr, cc=cc, pc_=pc_: e.scalar_tensor_tensor(out=ST[p][pr, cc], in0=ST[p][pr, cc], scalar=1.0, in1=PS_[pr, pc_], op0=ALU.mult, op1=ALU.add),
                               R=[ST[p], PS_], W=[ST[p]])
                          S.op('dve', lambda e, p=p, pr=pr, cc=cc: e.tensor_scalar(out=ST[p][pr, cc], in0=ST[p][pr, cc], scalar1=ecl[pr, p:p + 1], scalar2=None, op0=ALU.mult), R=[ST[p], ecl], W=[ST[p]])
                      S.op('act', lambda e, p=p: e.activation(out=STb[p][:], in_=ST[p][:], func=AF.Copy), R=[ST[p]], W=[STb[p]])
                  self._chk(10)
                  self.head_norm(o_sb, cen, sq, st4, 4, 64e-5)
                  S.op('dve', lambda e: e.tensor_tensor(out=cen[:], in0=cen[:], in1=lnb[:], op=ALU.mult), R=[cen, lnb], W=[cen])
                  S.op('dve', lambda e: e.tensor_tensor(out=t1[:], in0=r_sb[:], in1=k2_sb[:], op=ALU.mult), R=[r_sb, k2_sb], W=[t1])
                  S.op('dve', lambda e: e.tensor_tensor(out=t1[:], in0=t1[:], in1=rkb[:], op=ALU.mult), R=[t1, rkb], W=[t1])
                  S.op('dve', lambda e: e.tensor_reduce(out=st4[:], in_=V3(t1[:]), axis=AX.X, op=ALU.add), R=[t1], W=[st4])
                  S.op('dve', lambda e: e.tensor_tensor(out=V3(t1[:]), in0=V3(v_sb[:]), in1=st4[:].unsqueeze(2).broadcast_to([128, 4, 64]), op=ALU.mult), R=[v_sb, st4], W=[t1])
                  S.op('dve', lambda e: e.tensor_tensor(out=cen[:], in0=cen[:], in1=t1[:], op=ALU.add), R=[cen, t1], W=[cen])
                  yt_ = ytok[c % 2]
                  S.op('dve', lambda e, yt_=yt_: e.tensor_tensor(out=yt_[:], in0=cen[:], in1=gate_sb[:], op=ALU.mult), R=[cen, gate_sb], W=[yt_])
                  self.y_store(2, c, yt_, ystage, [])
              S.barrier(); S.emit()
          except _Stop:
            S.barrier(); S.emit()


    def rope_apply(self, A, B_, C, Sg):
        S = self.S
        S.op('dve', lambda e: e.tensor_tensor(out=A[:], in0=A[:], in1=C[:], op=ALU.mult), R=[A, C], W=[A])
        S.op('pool', lambda e: e.tensor_tensor(out=B_[:], in0=B_[:], in1=Sg[:], op=ALU.mult), R=[B_, Sg], W=[B_])
        S.op('dve', lambda e: e.tensor_tensor(out=A[:], in0=A[:], in1=B_[:], op=ALU.add), R=[A, B_], W=[A])

    def dsa_phase(self, l):
        S = self.S
        PF = self.PF
        NBIS = 13
        with ExitStack() as st:
            QT = [self.sb(st, f"dsQT{i}", [128, SEQ], BF16) for i in range(2)]
            kT = self.sb(st, "dskT", [128, SEQ], BF16)
            hm2 = self.load_const(st, 'hm2', F32); hm4 = self.load_const(st, 'hm4', F32)
            vext = self.sb(st, "ds_vext", [128, NT, 65], BF16)
            wsc = self.sb(st, "ds_wsc", [128, NT, 8], F32)
            with ExitStack() as s1:
                qiT = [self.sb(s1, f"dsqiT{i}", [128, SEQ], BF16) for i in range(2)]
                kiT = self.sb(s1, "dskiT", [128, SEQ], BF16)
                with ExitStack() as s2:
                    wm, wo = self.load_w(s2, "wm_ds", l, ['ds_cq', 'ds_k', 'ds_ks', 'ds_ki', 'ds_kis', 'ds_vw'])
                    wup = self.sb(s2, "ds_wup", [128, 4, 256], BF16)
                    for j, src_ in enumerate((self.dsa_wq_up, self.dsa_wqs_up, self.dsa_wqi_up, self.dsa_wqis_up)):
                        self.dma('pool', wup[:, j, :], src_[l], W=[wup])
                    qn = self.sb(s2, "ds_qn", [128, 1], F32)
                    self.dma('sp', qn[:], self.dsa_qnorm[l, :].rearrange("(p o) -> p o", o=1), W=[qn], allow_slow_non_contiguous=True)
                    onesf = self.sb(s2, "ds_ones", [128, 128], F32)
                    S.op('dve', lambda e: e.memset(onesf[:], 1.0), W=[onesf])
                    cqn = self.sb(s2, "ds_cqn", [128, SEQ], BF16)
                    cqf = self.sb(s2, "ds_cqf", [128, 512], F32); cq2 = self.sb(s2, "ds_cq2", [128, 512], F32); rs = self.sb(s2, "ds_rs", [128, 512], F32)
                    for tg in range(8):
                        pf = PF[tg % 2]; pf2 = PF[2 + tg % 2]
                        self.proj_T(wm, wo['ds_cq'], pf, tg)
                        S.op('act', lambda e, pf=pf: e.activation(out=cqf[:], in_=pf[:], func=AF.Copy), R=[pf], W=[cqf])
                        S.op('dve', lambda e: e.tensor_tensor(out=cq2[:], in0=cqf[:], in1=cqf[:], op=ALU.mult), R=[cqf], W=[cq2])
                        S.op('pe', lambda e, pf2=pf2: e.matmul(pf2[:], lhsT=onesf[:], rhs=cq2[:], start=True, stop=True), R=[onesf, cq2], W=[pf2])
                        S.op('dve', lambda e, pf2=pf2: e.tensor_scalar(out=rs[:], in0=pf2[:], scalar1=1.0 / 128, scalar2=1e-5, op0=ALU.mult, op1=ALU.add), R=[pf2], W=[rs])
                        S.op('act', lambda e: e.activation(out=rs[:], in_=rs[:], func=AF.Sqrt), R=[rs], W=[rs])
                        S.op('dve', lambda e: e.reciprocal(out=rs[:], in_=rs[:]), R=[rs], W=[rs])
                        S.op('dve', lambda e, tg=tg: e.scalar_tensor_tensor(out=cqn[:, tg * 512:(tg + 1) * 512], in0=cqf[:], scalar=qn[:, 0:1], in1=rs[:], op0=ALU.mult, op1=ALU.mult),
                             R=[cqf, qn, rs], W=[cqn])
                    S.op('pool', lambda e: e.memset(vext[:, :, 64:65], 1.0), W=[vext])
                    for i in range(NT):
                        pf = PF[i % 2]
                        self.proj_tok(wm, wo['ds_vw'], 72, pf[:, 0:72], pf, i)
                        S.op('act', lambda e, pf=pf, i=i: e.activation(out=vext[:, i, 0:64], in_=pf[:, 0:64], func=AF.Copy), R=[pf], W=[vext])
                        S.op('dve', lambda e, pf=pf, i=i: e.tensor_scalar(out=wsc[:, i, :], in0=pf[:, 64:72], scalar1=1.0 / 16, scalar2=None, op0=ALU.mult), R=[pf], W=[wsc])
                    tmpA = self.sb(s2, "ds_tmpA", [128, SEQ], BF16)

                    def up_proj(j, cols, dst):
                        for tg in range(8):
                            pf = PF[tg % 2]
                            S.op('pe', lambda e, pf=pf, tg=tg: e.matmul(pf[:], lhsT=wup[:, j, cols], rhs=cqn[:, tg * 512:(tg + 1) * 512], start=True, stop=True), R=[wup, cqn], W=[pf])
                            S.op('act', lambda e, pf=pf, tg=tg: e.activation(out=dst[:, tg * 512:(tg + 1) * 512], in_=pf[:], func=AF.Copy), R=[pf], W=[dst])

                    def in_proj(name, dst):
                        for tg in range(8):
                            pf = PF[2 + tg % 2]
                            self.proj_T(wm, wo[name], pf, tg)
                            S.op('dve', lambda e, pf=pf, tg=tg: e.tensor_copy(out=dst[:, tg * 512:(tg + 1) * 512], in_=pf[:]), R=[pf], W=[dst])
                    with ExitStack() as s3:
                        C, Sg = self.rope_tables(s3, 'rope_dq', 'rdq')
                        for pr_ in range(2):
                            cols = slice(pr_ * 128, (pr_ + 1) * 128)
                            up_proj(0, cols, QT[pr_]); up_proj(1, cols, tmpA)
                            self.rope_apply(QT[pr_], tmpA, C, Sg)
                        in_proj('ds_k', kT); in_proj('ds_ks', tmpA)
                        self.rope_apply(kT, tmpA, C, Sg)
                        S.barrier(); S.emit()
                    with ExitStack() as s3:
                        C, Sg = self.rope_tables(s3, 'rope_di', 'rdi')
                        for t2 in range(2):
                            cols = slice(t2 * 128, (t2 + 1) * 128)
                            up_proj(2, cols, qiT[t2]); up_proj(3, cols, tmpA)
                            self.rope_apply(qiT[t2], tmpA, C, Sg)
                        in_proj('ds_ki', kiT); in_proj('ds_kis', tmpA)
                        self.rope_apply(kiT, tmpA, C, Sg)
                        S.barrier(); S.emit()
                with ExitStack() as s2:
                    score = self.sb(s2, "ds_score", [128, SEQ], F32)
                    mask = self.sb(s2, "ds_mask", [128, SEQ], BF16)
                    junk = self.sb(s2, "ds_junk", [128, SEQ], BF16)
                    relb = [self.sb(s2, f"ds_rel{i}", [128, 512], F32) for i in range(2)]
                    negm = self.load_const(s2, 'negmask', F32)
                    mT = [self.sb(s2, f"ds_mT{i}", [128, NT, 128], BF16) for i in range(2)]
                    sc = {n: self.sb(s2, "ds_" + n, [128, 1], F32) for n in ('lo', 'hi', 'mid', 'cnt', 'ge', 'd')}
                    cntr = 0
                    qm = [self.sb(s2, f"ds_qm{i}", [128, 8, 128], BF16) for i in range(2)]
                    for tb in range(NT):
                        Sc = (tb + 1) * 128
                        tsl = slice(tb * 128, (tb + 1) * 128)
                        qm_ = qm[tb % 2]
                        for ih in range(8):
                            S.op('pool', lambda e, ih=ih, qm_=qm_, tsl=tsl: e.tensor_scalar(out=qm_[:, ih, :], in0=qiT[ih // 4][:, tsl], scalar1=hm4[:, ih % 4:ih % 4 + 1], scalar2=None, op0=ALU.mult),
                                 R=[qiT[ih // 4], hm4], W=[qm_])
                        for sg in range((Sc + 511) // 512):
                            w = min(512, Sc - sg * 512)
                            ssl = slice(sg * 512, sg * 512 + w)
                            for ih in range(8):
                                t2, j = ih // 4, ih % 4
                                pf = PF[cntr % 4]; rl = relb[cntr % 2]; cntr += 1
                                S.op('pe', lambda e, pf=pf, ih=ih, qm_=qm_, ssl=ssl, w=w: e.matmul(pf[:, 0:w], lhsT=qm_[:, ih, :], rhs=kiT[:, ssl], start=True, stop=True),
                                     R=[qm_, kiT], W=[pf])
                                S.op('act', lambda e, pf=pf, rl=rl, w=w: e.activation(out=rl[:, 0:w], in_=pf[:, 0:w], func=AF.Relu), R=[pf], W=[rl])
                                if ih == 0:
                                    S.op('dve', lambda e, rl=rl, w=w, ssl=ssl, tb=tb, ih=ih: e.tensor_scalar(out=score[:, ssl], in0=rl[:, 0:w], scalar1=wsc[:, tb, ih:ih + 1], scalar2=None, op0=ALU.mult),
                                         R=[rl, wsc], W=[score])
                                else:
                                    S.op('dve', lambda e, rl=rl, w=w, ssl=ssl, tb=tb, ih=ih: e.scalar_tensor_tensor(out=score[:, ssl], in0=rl[:, 0:w], scalar=wsc[:, tb, ih:ih + 1], in1=score[:, ssl],
                                                                                                              op0=ALU.mult, op1=ALU.add), R=[rl, wsc, score], W=[score])
                        S.op('dve', lambda e, tsl=tsl: e.tensor_tensor(out=score[:, tsl], in0=score[:, tsl], in1=negm[:], op=ALU.add), R=[score, negm], W=[score])
                        if tb >= 2:
                            S.op('dve', lambda e, Sc=Sc: e.tensor_reduce(out=sc['hi'][:], in_=score[:, 0:Sc], axis=AX.X, op=ALU.max), R=[score], W=[sc['hi']])
                            S.op('dve', lambda e: e.tensor_reduce(out=sc['lo'][:], in_=score[:, 0:256], axis=AX.X, op=ALU.min), R=[score], W=[sc['lo']])
                            S.op('dve', lambda e: e.tensor_tensor(out=sc['mid'][:], in0=sc['lo'][:], in1=sc['hi'][:], op=ALU.add), R=[sc['lo'], sc['hi']], W=[sc['mid']])
                            S.op('dve', lambda e: e.tensor_scalar(out=sc['mid'][:], in0=sc['mid'][:], scalar1=0.5, scalar2=None, op0=ALU.mult), R=[sc['mid']], W=[sc['mid']])
                            S.op('dve', lambda e: e.tensor_tensor(out=sc['d'][:], in0=sc['hi'][:], in1=sc['lo'][:], op=ALU.subtract), R=[sc['lo'], sc['hi']], W=[sc['d']])
                            S.op('dve', lambda e: e.tensor_scalar(out=sc['d'][:], in0=sc['d'][:], scalar1=0.25, scalar2=None, op0=ALU.mult), R=[sc['d']], W=[sc['d']])
                            for it in range(NBIS):
                                S.op('dve', lambda e, Sc=Sc: e.tensor_scalar(out=junk[:, 0:Sc], in0=score[:, 0:Sc], scalar1=sc['mid'][:, 0:1], scalar2=None, op0=ALU.is_ge, op1=ALU.add,
                                                                            accum_out=sc['cnt'][:]), R=[score, sc['mid']], W=[junk, sc['cnt']])
                                S.op('dve', lambda e: e.tensor_scalar(out=sc['ge'][:], in0=sc['cnt'][:], scalar1=255.5, scalar2=2.0, op0=ALU.is_ge, op1=ALU.mult), R=[sc['cnt']], W=[sc['ge']])
                                S.op('dve', lambda e: e.scalar_tensor_tensor(out=sc['ge'][:], in0=sc['ge'][:], scalar=-1.0, in1=sc['d'][:], op0=ALU.add, op1=ALU.mult), R=[sc['ge'], sc['d']], W=[sc['ge']])
                                S.op('dve', lambda e: e.tensor_tensor(out=sc['mid'][:], in0=sc['mid'][:], in1=sc['ge'][:], op=ALU.add), R=[sc['mid'], sc['ge']], W=[sc['mid']])
                                S.op('dve', lambda e: e.tensor_scalar(out=sc['d'][:], in0=sc['d'][:], scalar1=0.5, scalar2=None, op0=ALU.mult), R=[sc['d']], W=[sc['d']])
                            S.op('dve', lambda e: e.scalar_tensor_tensor(out=sc['lo'][:], in0=sc['d'][:], scalar=-2.0, in1=sc['mid'][:], op0=ALU.mult, op1=ALU.add), R=[sc['d'], sc['mid']], W=[sc['lo']])
                        else:
                            S.op('dve', lambda e: e.memset(sc['lo'][:], -1e29), W=[sc['lo']])
                        S.op('dve', lambda e, Sc=Sc: e.tensor_scalar(out=mask[:, 0:Sc], in0=score[:, 0:Sc], scalar1=sc['lo'][:, 0:1], scalar2=None, op0=ALU.is_ge), R=[score, sc['lo']], W=[mask])
                        mt = mT[tb % 2]
                        for s0 in range(0, tb + 1, 8):
                            nb_ = min(8, tb + 1 - s0)
                            pb = self.PB[(s0 // 8) % 2]
                            for q in range(nb_):
                                S.op('pe', lambda e, pb=pb, q=q, s0=s0: e.transpose(out=pb[:, q * 128:(q + 1) * 128], in_=mask[:, (s0 + q) * 128:(s0 + q + 1) * 128], identity=self.ident[:]),
                                     R=[mask, self.ident], W=[pb])
                            S.op('act', lambda e, pb=pb, mt=mt, s0=s0, nb_=nb_: e.activation(out=mt[:, s0:s0 + nb_, :].rearrange("p b t -> p (b t)"), in_=pb[:, 0:nb_ * 128], func=AF.Copy), R=[pb], W=[mt])
                        self.dma('sp', self.maskT_d[tb, :, 0:tb + 1, :], mt[:, 0:tb + 1, :], R=[mt], W=[TB()])
                    S.barrier(); S.emit()
            with ExitStack() as s2:
                onb = self.bcast_row(s2, "ds_onb", self.dsa_onorm[l, :], 256)
                o_all = self.sb(s2, "ds_oall", [128, NT, 256], BF16)
                mT = [self.sb(s2, f"ds_mT2{i}", [128, NT, 128], BF16) for i in range(2)]
                ebuf = [self.sb(s2, f"ds_e{i}", [128, 4, 128], BF16) for i in range(2)]
                pbuf = [self.sb(s2, f"ds_p{i}", [128, 4, 128], BF16) for i in range(2)]
                zer = self.sb(s2, "ds_zero", [128, 260], BF16)
                S.op('pool', lambda e: e.memset(zer[:], 0.0), W=[zer])
                osb = self.sb(s2, "ds_osb", [128, 4, 65], F32); rden = self.sb(s2, "ds_rden", [128, 4], F32)
                cnt = 0
                QM = [self.sb(s2, f"ds_QM{i}", [128, 4, 128], BF16) for i in range(2)]
                for tb in range(NT):
                    tsl = slice(tb * 128, (tb + 1) * 128)
                    mt = mT[tb % 2]
                    QM_ = QM[tb % 2]
                    for h in range(4):
                        S.op('pool', lambda e, h=h, QM_=QM_, tsl=tsl: e.tensor_scalar(out=QM_[:, h, :], in0=QT[h // 2][:, tsl], scalar1=hm2[:, h % 2:h % 2 + 1], scalar2=None, op0=ALU.mult),
                             R=[QT[h // 2], hm2], W=[QM_])
                    self.dma('act', mt[:, 0:tb + 1, :], self.maskT_d[tb, :, 0:tb + 1, :], W=[mt])
                    po = PF[4 + tb % 2]
                    S.op('pe', lambda e, po=po: e.matmul(po[:, 0:260], lhsT=zer[:, 0:128], rhs=zer[:, 0:260], start=True, stop=False), R=[zer], W=[po])
                    for sb_ in range(tb + 1):
                        b2 = cnt % 2; cnt += 1
                        ssl = slice(sb_ * 128, (sb_ + 1) * 128)
                        pl = PF[b2 * 2]
                        S.op('pe', lambda e, pl=pl, ssl=ssl, QM_=QM_: e.matmul(pl[:], lhsT=kT[:, ssl], rhs=QM_[:].rearrange("p h t -> p (h t)"), start=True, stop=True),
                             R=[kT, QM_], W=[pl])
                        S.op('act', lambda e, pl=pl, b2=b2: e.activation(out=ebuf[b2][:].rearrange("p h t -> p (h t)"), in_=pl[:], func=AF.Exp, scale=0.125), R=[pl], W=[ebuf[b2]])
                        S.op('dve', lambda e, b2=b2, mt=mt, sb_=sb_: e.tensor_tensor(out=pbuf[b2][:], in0=ebuf[b2][:], in1=mt[:, sb_, :].unsqueeze(1).broadcast_to([128, 4, 128]), op=ALU.mult),
                             R=[ebuf[b2], mt], W=[pbuf[b2]])
                        for h in range(4):
                            S.op('pe', lambda e, h=h, po=po, b2=b2, sb_=sb_, tb=tb: e.matmul(po[:, h * 65:(h + 1) * 65], lhsT=pbuf[b2][:, h, :], rhs=vext[:, sb_, :], start=False, stop=(sb_ == tb and h == 3)),
                                 R=[pbuf[b2], vext], W=[po])
                    S.op('act', lambda e, po=po: e.activation(out=osb[:].rearrange("p h d -> p (h d)"), in_=po[:, 0:260], func=AF.Copy), R=[po], W=[osb])
                    S.op('dve', lambda e: e.reciprocal(out=rden[:], in_=osb[:, :, 64]), R=[osb], W=[rden])
                    S.op('dve', lambda e, tb=tb: e.tensor_tensor(out=o_all[:, tb, :].rearrange("p (h d) -> p h d", d=64), in0=osb[:, :, 0:64], in1=rden[:].unsqueeze(2).broadcast_to([128, 4, 64]), op=ALU.mult),
                         R=[osb, rden], W=[o_all])
                S.barrier(); S.emit()
                self.rms_finalize(s2, o_all, onb, 4, "dsf")
                S.barrier(); S.emit()

    def head_norm(self, o_sb, cen, sq, st4, nh, eps):
        S = self.S
        v3 = lambda b: b[:, 0:nh * 64].rearrange("p (h d) -> p h d", d=64)
        S.op('dve', lambda e: e.tensor_reduce(out=st4[:, 0:nh], in_=v3(o_sb), axis=AX.X, op=ALU.add), R=[o_sb], W=[st4])
        S.op('dve', lambda e: e.tensor_scalar(out=st4[:, 0:nh], in0=st4[:, 0:nh], scalar1=1.0 / 64, scalar2=None, op0=ALU.mult), R=[st4], W=[st4])
        S.op('dve', lambda e: e.tensor_tensor(out=v3(cen), in0=v3(o_sb), in1=st4[:, 0:nh].unsqueeze(2).broadcast_to([128, nh, 64]), op=ALU.subtract), R=[o_sb, st4], W=[cen])
        S.op('dve', lambda e: e.tensor_tensor(out=v3(sq), in0=v3(cen), in1=v3(cen), op=ALU.mult), R=[cen], W=[sq])
        S.op('dve', lambda e: e.tensor_reduce(out=st4[:, 0:nh], in_=v3(sq), axis=AX.X, op=ALU.add), R=[sq], W=[st4])
        S.op('dve', lambda e: e.tensor_scalar(out=st4[:, 0:nh], in0=st4[:, 0:nh], scalar1=1.0 / 64, scalar2=eps, op0=ALU.mult, op1=ALU.add), R=[st4], W=[st4])
        S.op('act', lambda e: e.activation(out=st4[:, 0:nh], in_=st4[:, 0:nh], func=AF.Sqrt), R=[st4], W=[st4])
        S.op('dve', lambda e: e.reciprocal(out=st4[:, 0:nh], in_=st4[:, 0:nh]), R=[st4], W=[st4])
        S.op('dve', lambda e: e.tensor_tensor(out=v3(cen), in0=v3(cen), in1=st4[:, 0:nh].unsqueeze(2).broadcast_to([128, nh, 64]), op=ALU.mult), R=[cen, st4], W=[cen])

    def wout_phase(self, l):
        S = self.S
        src = self.x if l == 0 else self.xres
        with ExitStack() as st:
            YT = self.sb(st, "YT", [128, 8, SEQ], BF16)
            for j in range(8):
                self.dma('sp' if j % 2 == 0 else 'act', YT[:, j, :], self.yT_d[j], W=[YT])
            wo_sb = self.sb(st, "wo_sb", [128, 8, D], BF16)
            for f in range(8):
                self.dma('pool', wo_sb[:, f, :], self.w_out[l, f * 128:(f + 1) * 128, :], W=[wo_sb])
            xt = [self.sb(st, f"wx{i}", [128, D], F32) for i in range(2)]
            tmp = self.sb(st, "wtmp", [128, D], F32)
            for i in range(NT):
                x_ = xt[i % 2]
                self.dma('sp' if i % 2 == 0 else 'act', x_[:], src[i * 128:(i + 1) * 128, :], W=[x_])
                for hf in range(2):
                    pf = self.PF[(i % 2) * 2 + hf]
                    for f in range(8):
                        S.op('pe', lambda e, f=f, pf=pf, i=i, hf=hf: e.matmul(pf[:], lhsT=YT[:, f, i * 128:(i + 1) * 128], rhs=wo_sb[:, f, hf * 512:(hf + 1) * 512], start=(f == 0), stop=(f == 7)),
                             R=[YT, wo_sb], W=[pf])
                    S.op('dve', lambda e, pf=pf, hf=hf: e.tensor_tensor(out=tmp[:, hf * 512:(hf + 1) * 512], in0=pf[:], in1=self.gb[0][:, hf * 512:(hf + 1) * 512], op=ALU.mult),
                         R=[pf, self.gb[0]], W=[tmp])
                S.op('pool', lambda e, x_=x_: e.tensor_tensor(out=x_[:], in0=x_[:], in1=tmp[:], op=ALU.add), R=[x_, tmp], W=[x_])
                self.dma('sp', self.xres[i * 128:(i + 1) * 128, :], x_[:], R=[x_], W=[TB()])
            S.barrier(); S.emit()

    def moe_phase(self, l):
        S = self.S; PF = self.PF; nc = self.nc
        with ExitStack() as st:
            off_all = self.sb(st, "mo_off", [128, NT, 4], I32)
            gsel_all = self.sb(st, "mo_gsel", [128, NT, 4], F32)
            widx = self.sb(st, "mo_widx", [128, NBLK, 8], I32)
            OH = self.sb(st, "mo_OH", [32, NBLK], F32)
            ones_bf = self.sb(st, "mo_ones", [128, 512], BF16)
            S.op('dve', lambda e: e.memset(ones_bf[:], 1.0), W=[ones_bf])
            with ExitStack() as s1:
                self.hT = self.sb(s1, "hT2", [128, 8, SEQ + 1], BF16)
                self.norm_phase(l, 1)
                rw = self.sb(s1, "mo_rw", [128, 8, 32], BF16)
                self.dma('pool', rw[:], self.router_w[l].rearrange("(k p) e -> p k e", p=128), W=[rw])
                rbb = self.bcast_row(s1, "mo_rbb", self.router_b[l, :], 32)
                M_bf = self.sb(s1, "mo_Mbf", [128, NT, 32], BF16); M32 = self.sb(s1, "mo_M32", [128, NT, 32], F32)
                G_all = self.sb(s1, "mo_G", [128, NT, 32], F32)
                lg = self.sb(s1, "mo_lg", [128, 32], F32); ex = self.sb(s1, "mo_ex", [128, 32], F32); junk32 = self.sb(s1, "mo_junk", [128, 32], F32)
                top8 = self.sb(s1, "mo_top8", [128, 8], F32); sc1 = self.sb(s1, "mo_sc1", [128, 2], F32)
                for i in range(NT):
                    pf = PF[i % 2]
                    for k in range(8):
                        S.op('pe', lambda e, k=k, pf=pf, i=i: e.matmul(pf[:, 0:32], lhsT=self.hT[:, k, 1 + i * 128: 1 + (i + 1) * 128], rhs=rw[:, k, :], start=(k == 0), stop=(k == 7)),
                             R=[self.hT, rw], W=[pf])
                    S.op('dve', lambda e, pf=pf: e.tensor_tensor(out=lg[:], in0=pf[:, 0:32], in1=rbb[:], op=ALU.add), R=[pf, rbb], W=[lg])
                    S.op('dve', lambda e: e.max(out=top8[:], in_=lg[:]), R=[lg], W=[top8])
                    S.op('dve', lambda e, i=i: e.tensor_scalar(out=M32[:, i, :], in0=lg[:], scalar1=top8[:, 3:4], scalar2=None, op0=ALU.is_ge), R=[lg, top8], W=[M32])
                    S.op('pool', lambda e, i=i: e.tensor_copy(out=M_bf[:, i, :], in_=M32[:, i, :]), R=[M32], W=[M_bf])
                    S.op('dve', lambda e: e.tensor_scalar(out=sc1[:, 0:1], in0=top8[:, 0:1], scalar1=-1.0, scalar2=None, op0=ALU.mult), R=[top8], W=[sc1])
                    S.op('act', lambda e: e.activation(out=ex[:], in_=lg[:], func=AF.Exp, bias=sc1[:, 0:1]), R=[lg, sc1], W=[ex])
                    S.op('dve', lambda e, i=i: e.scalar_tensor_tensor(out=ex[:], in0=ex[:], scalar=1.0, in1=M32[:, i, :], op0=ALU.mult, op1=ALU.mult, accum_out=sc1[:, 1:2]),
                         R=[ex, M32], W=[ex, sc1])
                    S.op('dve', lambda e: e.reciprocal(out=sc1[:, 1:2], in_=sc1[:, 1:2]), R=[sc1], W=[sc1])
                    S.op('dve', lambda e, i=i: e.tensor_scalar(out=G_all[:, i, :], in0=ex[:], scalar1=sc1[:, 1:2], scalar2=None, op0=ALU.mult), R=[ex, sc1], W=[G_all])
                pc = PF[2]
                for i in range(NT):
                    S.op('pe', lambda e, i=i: e.matmul(pc[0:1, 0:32], lhsT=ones_bf[:, 0:1], rhs=M_bf[:, i, :], start=(i == 0), stop=(i == NT - 1)), R=[ones_bf, M_bf], W=[pc])
                row = lambda n, w=32, d=F32: self.sb(s1, "mo_" + n, [1, w], d)
                cnt = row('cnt'); nbf = row('nbf'); nbi = row('nbi', 32, I32); endr = row('end'); baser = row('base'); onesr = row('onesr')
                iota = self.load_const(s1, 'iota_blk', F32); kp = self.load_const(s1, 'kp', F32)
                cmp3 = self.sb(s1, "mo_cmp3", [1, NBLK, 32], F32); ebf = row('ebf', NBLK); chgf = row('chgf', NBLK)
                S.op('dve', lambda e: e.memset(onesr[:], 1.0), W=[onesr])
                S.op('dve', lambda e: e.tensor_scalar(out=cnt[:], in0=pc[0:1, 0:32], scalar1=float(MB - 1), scalar2=1.0 / MB, op0=ALU.add, op1=ALU.mult), R=[pc], W=[cnt])
                S.op('dve', lambda e: e.tensor_scalar(out=cnt[:], in0=cnt[:], scalar1=-0.5 + 0.5 / MB, scalar2=None, op0=ALU.add), R=[cnt], W=[cnt])
                S.op('dve', lambda e: e.tensor_copy(out=nbi[:], in_=cnt[:]), R=[cnt], W=[nbi])
                S.op('dve', lambda e: e.tensor_copy(out=nbf[:], in_=nbi[:]), R=[nbi], W=[nbf])
                S.op('dve', lambda e: e.tensor_tensor_scan(out=endr[:], data0=onesr[:], data1=nbf[:], initial=0.0, op0=ALU.mult, op1=ALU.add), R=[onesr, nbf], W=[endr])
                S.op('dve', lambda e: e.tensor_tensor(out=baser[:], in0=endr[:], in1=nbf[:], op=ALU.subtract), R=[endr, nbf], W=[baser])
                S.op('dve', lambda e: e.tensor_scalar(out=baser[:], in0=baser[:], scalar1=float(MB), scalar2=None, op0=ALU.mult), R=[baser], W=[baser])
                basebc = self.sb(s1, "mo_basebc", [128, 32], F32)
                onescol = self.sb(s1, "mo_ones1", [1, 128], F32)
                S.op('dve', lambda e: e.memset(onescol[:], 1.0), W=[onescol])
                S.op('pe', lambda e: e.matmul(PF[3][:, 0:32], lhsT=onescol[0:1, :], rhs=baser[0:1, :], start=True, stop=True), R=[onescol, baser], W=[PF[3]])
                S.op('act', lambda e: e.activation(out=basebc[:], in_=PF[3][:, 0:32], func=AF.Copy), R=[PF[3]], W=[basebc])
                S.op('dve', lambda e: e.tensor_tensor(out=cmp3[:], in0=endr[:].unsqueeze(1).broadcast_to([1, NBLK, 32]), in1=iota[0:1, :].unsqueeze(2).broadcast_to([1, NBLK, 32]), op=ALU.is_le),
                     R=[endr, iota], W=[cmp3])
                S.op('dve', lambda e: e.tensor_reduce(out=ebf[:], in_=cmp3[:], axis=AX.X, op=ALU.add), R=[cmp3], W=[ebf])
                S.op('dve', lambda e: e.tensor_scalar(out=ebf[:], in0=ebf[:], scalar1=31.0, scalar2=None, op0=ALU.min), R=[ebf], W=[ebf])
                needf = row('needf', NBLK); ebrow = row('ebrow', NBLK)
                S.op('dve', lambda e: e.memset(needf[:], 1.0), W=[needf])
                S.op('dve', lambda e: e.tensor_tensor(out=needf[0:1, 2:NBLK], in0=ebf[0:1, 2:NBLK], in1=ebf[0:1, 0:NBLK - 2], op=ALU.not_equal), R=[ebf, needf], W=[needf])
                S.op('dve', lambda e: e.tensor_scalar(out=needf[:], in0=needf[:], scalar1=-1.0e6, scalar2=1.0e6, op0=ALU.mult, op1=ALU.add), R=[needf], W=[needf])
                S.op('dve', lambda e: e.tensor_scalar(out=ebrow[:], in0=ebf[:], scalar1=float(l * 32), scalar2=1024.0, op0=ALU.add, op1=ALU.mult), R=[ebf], W=[ebrow])
                S.op('dve', lambda e: e.tensor_tensor(out=ebrow[:], in0=ebrow[:], in1=needf[:], op=ALU.add), R=[ebrow, needf], W=[ebrow])
                ebbc = self.sb(s1, "mo_ebbc", [128, NBLK], F32); wf = self.sb(s1, "mo_wf", [128, NBLK, 8], F32)
                S.op('pe', lambda e: e.matmul(PF[3][:, 0:NBLK], lhsT=onescol[0:1, :], rhs=ebrow[0:1, :], start=True, stop=True), R=[onescol, ebrow], W=[PF[3]])
                S.op('act', lambda e: e.activation(out=ebbc[:], in_=PF[3][:, 0:NBLK], func=AF.Copy), R=[PF[3]], W=[ebbc])
                S.op('dve', lambda e: e.tensor_tensor(out=wf[:], in0=ebbc[:].unsqueeze(2).broadcast_to([128, NBLK, 8]), in1=kp[:].unsqueeze(1).broadcast_to([128, NBLK, 8]), op=ALU.add),
                     R=[ebbc, kp], W=[wf])
                S.op('dve', lambda e: e.tensor_copy(out=widx[:], in_=wf[:]), R=[wf], W=[widx])
                pcol = self.load_const(s1, 'pcol', F32)
                S.op('pe', lambda e: e.matmul(PF[3][0:32, 0:NBLK], lhsT=onescol[0:1, 0:32], rhs=ebf[0:1, :], start=True, stop=True), R=[onescol, ebf], W=[PF[3]])
                S.op('dve', lambda e: e.tensor_scalar(out=OH[:], in0=PF[3][0:32, 0:NBLK], scalar1=pcol[0:32, 0:1], scalar2=None, op0=ALU.is_equal), R=[PF[3], pcol], W=[OH])
                zt = self.sb(s1, "mo_zt", [128, NSLOT * 2 // 128], I32)
                S.op('dve', lambda e: e.memset(zt[:], 0), W=[zt])
                tokz = TB()
                self.dma('sp', self.tokidx_d.rearrange("(p b) o -> p (b o)", p=128), zt[:], R=[zt], W=[tokz])
                tidx = self.sb(s1, "mo_tidx", [128, NT, 2], I32)
                S.op('pool', lambda e: e.iota(tidx[:], pattern=[[128, NT], [0, 2]], base=0, channel_multiplier=1), W=[tidx])
                tri = self.load_const(s1, 'tri_lt', BF16)
                a1 = self.sb(s1, "mo_a1", [128, 32], F32); A8 = self.sb(s1, "mo_A8", [128, 8], F32); offf = self.sb(s1, "mo_offf", [128, 4], F32)
                for i in range(NT):
                    pp = PF[i % 2]
                    S.op('pe', lambda e, i=i, pp=pp: e.matmul(pp[:, 0:32], lhsT=tri[:], rhs=M_bf[:, i, :], start=True, stop=(i == 0)), R=[tri, M_bf], W=[pp])
                    for j in range(i):
                        S.op('pe', lambda e, i=i, j=j, pp=pp: e.matmul(pp[:, 0:32], lhsT=ones_bf[:, 0:128], rhs=M_bf[:, j, :], start=False, stop=(j == i - 1)), R=[ones_bf, M_bf], W=[pp])
                    S.op('dve', lambda e, pp=pp: e.tensor_tensor(out=a1[:], in0=pp[:, 0:32], in1=basebc[:], op=ALU.add), R=[pp, basebc], W=[a1])
                    S.op('dve', lambda e, i=i: e.scalar_tensor_tensor(out=a1[:], in0=a1[:], scalar=1.0, in1=M32[:, i, :], op0=ALU.add, op1=ALU.mult), R=[a1, M32], W=[a1])
                    S.op('dve', lambda e: e.max(out=A8[:], in_=a1[:]), R=[a1], W=[A8])
                    S.op('dve', lambda e: e.tensor_scalar(out=offf[:], in0=A8[:, 0:4], scalar1=-1.0, scalar2=None, op0=ALU.add), R=[A8], W=[offf])
                    S.op('dve', lambda e, i=i: e.tensor_copy(out=off_all[:, i, :], in_=offf[:]), R=[offf], W=[off_all])
                    for j in range(4):
                        S.op('dve', lambda e, i=i, j=j: e.scalar_tensor_tensor(out=junk32[:], in0=a1[:], scalar=A8[:, j:j + 1], in1=G_all[:, i, :], op0=ALU.is_equal, op1=ALU.mult,
                                                                              accum_out=gsel_all[:, i, j:j + 1]), R=[a1, A8, G_all], W=[junk32, gsel_all])
                    for j in range(4):
                        S.op('pool', lambda e, i=i, j=j: e.indirect_dma_start(out=self.tokidx_d[:, :], out_offset=bass.IndirectOffsetOnAxis(ap=off_all[:, i, j:j + 1], axis=0),
                                                                               in_=tidx[:, i, :], in_offset=None),
                             R=[off_all, tidx, tokz], W=[TB()], dma=True)
                S.barrier(); S.emit()
            if self.flags.get('moe_stop') == 2:
                return
            w1v = self.moe_w1.rearrange("l e d f -> (l e d) f"); w2v = self.moe_w2.rearrange("l e f d -> (l e f) d")
            b1v = self.moe_b1.rearrange("l e f -> (l e) f"); b2v = self.moe_b2.rearrange("l e d -> (l e) d")
            IO = bass.IndirectOffsetOnAxis
            with ExitStack() as s1:
                W1 = [self.sb(s1, f"mo_W1{i}", [128, 8, 2 * D], BF16) for i in range(2)]; W2 = [self.sb(s1, f"mo_W2{i}", [128, 8, D], BF16) for i in range(2)]
                b1all = self.sb(s1, "mo_b1all", [32, 2 * D], BF16); b2all = self.sb(s1, "mo_b2all", [32, D], BF16)
                self.dma('pool', b1all[:], self.moe_b1[l], W=[b1all]); self.dma('pool', b2all[:], self.moe_b2[l], W=[b2all])
                sel = [self.sb(s1, f"mo_sel{i}", [32, MB], BF16) for i in range(2)]
                bcdone = self.sb(s1, "mo_bcd", [1, 1], F32)

                def setbc(e):
                    e.reg_mov(self.reg_bc, 64 * 1024 - 1)
                    return e.memset(bcdone[:], 0.0)
                S.op('pool', setbc, W=[bcdone])
                wts = [TB(), TB()]
                idx = [self.sb(s1, f"mo_idx{i}", [128, 4, 2], I32) for i in range(2)]
                X = [self.sb(s1, f"mo_X{i}", [128, D], BF16) for i in range(2)]
                XT = [self.sb(s1, f"mo_XT{i}", [128, 8, MB], BF16) for i in range(2)]
                AT = self.sb(s1, "mo_AT", [128, 8, MB], BF16)
                g_ = self.sb(s1, "mo_g", [128, 512], F32); sg_ = self.sb(s1, "mo_sg", [128, 512], F32); ln_ = self.sb(s1, "mo_ln", [128, 512], F32)
                yrow = [self.sb(s1, f"mo_y{i}", [128, D], F32) for i in range(2)]
                xc = 0

                def issue_weights(b):
                    b2_ = b % 2
                    W1_, W2_, wt_ = W1[b2_], W2[b2_], wts[b2_]
                    for k in range(8):
                        S.op('pool', lambda e, k=k, b=b, W1_=W1_: e.indirect_dma_start(out=W1_[:, k, :], out_offset=None, in_=w1v[:, :], in_offset=IO(ap=widx[:, b, k:k + 1], axis=0),
                                                                                  bounds_check=self.reg_bc, oob_is_err=False), R=[widx, bcdone], W=[wt_], dma=True)
                        S.op('pool', lambda e, k=k, b=b, W2_=W2_: e.indirect_dma_start(out=W2_[:, k, :], out_offset=None, in_=w2v[:, :], in_offset=IO(ap=widx[:, b, k:k + 1], axis=0),
                                                                                  bounds_check=self.reg_bc, oob_is_err=False), R=[widx, bcdone], W=[wt_], dma=True)
                X4 = [self.sb(s1, f"mo_X4{i}", [128, D], BF16) for i in range(4)]

                def issue_gathers(b):
                    b2_ = b % 2
                    self.dma('sp', idx[b2_][:], self.tokidx_d[b * MB:(b + 1) * MB, :].rearrange("(q p) o -> p q o", p=128), W=[idx[b2_]])
                    for q in range(MB // 128):
                        x_ = X4[q]
                        S.op('pool', lambda e, b2_=b2_, q=q, x_=x_: e.indirect_dma_start(out=x_[:], out_offset=None, in_=self.hrow_d[:, :], in_offset=IO(ap=idx[b2_][:, q, 0:1], axis=0)),
                             R=[idx[b2_]], W=[x_], dma=True)

                def issue_transposes(b):
                    xt_ = XT[b % 2]
                    for q in range(MB // 128):
                        x_ = X4[q]; pb = self.PB[q % 2]
                        for k in range(8):
                            S.op('pe', lambda e, k=k, pb=pb, x_=x_: e.transpose(out=pb[:, k * 128:(k + 1) * 128], in_=x_[:, k * 128:(k + 1) * 128], identity=self.ident[:]), R=[x_, self.ident], W=[pb])
                        S.op('act', lambda e, pb=pb, xt_=xt_, q=q: e.activation(out=xt_[:, :, q * 128:(q + 1) * 128], in_=pb[:].rearrange("p (k t) -> p k t", k=8), func=AF.Copy), R=[pb], W=[xt_])
                issue_weights(0)
                issue_gathers(0)
                issue_transposes(0)
                for b in range(NBLK):
                    b2_ = b % 2
                    W1_, W2_, wt_ = W1[b2_], W2[b2_], wts[b2_]
                    sel_ = sel[b2_]
                    xt_ = XT[b2_]
                    S.op('dve', lambda e, b=b, sel_=sel_: e.tensor_copy(out=sel_[:], in_=OH[:, b:b + 1].broadcast_to([32, MB])), R=[OH], W=[sel_])
                    if b + 1 < NBLK:
                        issue_weights(b + 1)
                        issue_gathers(b + 1)
                    for c in range(8):
                        pg, pl = PF[(c % 2) * 2], PF[(c % 2) * 2 + 1]
                        for (pf, cbase) in ((pg, 0), (pl, D)):
                            for k in range(8):
                                S.op('pe', lambda e, k=k, pf=pf, c=c, cbase=cbase, W1_=W1_, xt_=xt_: e.matmul(pf[:], lhsT=W1_[:, k, cbase + c * 128: cbase + (c + 1) * 128], rhs=xt_[:, k, :], start=(k == 0), stop=False),
                                     R=[wt_, xt_], W=[pf])
                            S.op('pe', lambda e, pf=pf, c=c, cbase=cbase, sel_=sel_: e.matmul(pf[:], lhsT=b1all[0:32, cbase + c * 128: cbase + (c + 1) * 128], rhs=sel_[0:32, :], start=False, stop=True), R=[b1all, sel_], W=[pf])
                        S.op('dve', lambda e, pg=pg: e.tensor_scalar(out=g_[:], in0=pg[:], scalar1=7.0, scalar2=None, op0=ALU.min), R=[pg], W=[g_])
                        S.op('act', lambda e: e.activation(out=sg_[:], in_=g_[:], func=AF.Sigmoid, scale=1.702), R=[g_], W=[sg_])
                        S.op('dve', lambda e, pl=pl: e.tensor_scalar(out=ln_[:], in0=pl[:], scalar1=7.0, scalar2=-7.0, op0=ALU.min, op1=ALU.max), R=[pl], W=[ln_])
                        S.op('dve', lambda e: e.tensor_tensor(out=sg_[:], in0=sg_[:], in1=g_[:], op=ALU.mult), R=[sg_, g_], W=[sg_])
                        S.op('dve', lambda e, c=c: e.scalar_tensor_tensor(out=AT[:, c, :], in0=ln_[:], scalar=1.0, in1=sg_[:], op0=ALU.add, op1=ALU.mult), R=[ln_, sg_], W=[AT])
                    if b + 1 < NBLK:
                        issue_transposes(b + 1)
                    for q in range(MB // 128):
                        y_ = yrow[q % 2]
                        for hf in range(2):
                            py = PF[4 + hf]
                            for c in range(8):
                                S.op('pe', lambda e, c=c, py=py, hf=hf, q=q, W2_=W2_: e.matmul(py[:], lhsT=AT[:, c, q * 128:(q + 1) * 128], rhs=W2_[:, c, hf * 512:(hf + 1) * 512], start=(c == 0), stop=False), R=[AT, wt_], W=[py])
                            S.op('pe', lambda e, py=py, hf=hf, sel_=sel_: e.matmul(py[:], lhsT=sel_[0:32, 0:128], rhs=b2all[0:32, hf * 512:(hf + 1) * 512], start=False, stop=True), R=[sel_, b2all], W=[py])
                            if hf == 0:
                                S.op('act', lambda e, py=py, y_=y_: e.activation(out=y_[:, 0:512], in_=py[:], func=AF.Copy), R=[py], W=[y_])
                            else:
                                S.op('dve', lambda e, py=py, y_=y_: e.tensor_copy(out=y_[:, 512:1024], in_=py[:]), R=[py], W=[y_])
                        self.dma('sp', self.yslot_d[b * MB + q * 128: b * MB + (q + 1) * 128, :], y_[:], R=[y_], W=[TB()])
                S.barrier(); S.emit()
            if self.flags.get('moe_stop') == 3:
                return
            with ExitStack() as s1:
                YY = [[self.sb(s1, f"mo_Y{i}{j}", [128, D], F32) for j in range(4)] for i in range(2)]
                xt = [self.sb(s1, f"mo_x{i}", [128, D], F32) for i in range(2)]
                acc = self.sb(s1, "mo_acc", [128, D], F32)
                for i in range(NT):
                    x_ = xt[i % 2]
                    Y = YY[i % 2]
                    self.dma('sp', x_[:], self.xres[i * 128:(i + 1) * 128, :], W=[x_])
                    for j in range(4):
                        S.op('pool', lambda e, i=i, j=j, Y=Y: e.indirect_dma_start(out=Y[j][:], out_offset=None, in_=self.yslot_d[:, :], in_offset=bass.IndirectOffsetOnAxis(ap=off_all[:, i, j:j + 1], axis=0)),
                             R=[off_all], W=[Y[j]], dma=True)
                    S.op('dve', lambda e, i=i, Y=Y: e.tensor_scalar(out=acc[:], in0=Y[0][:], scalar1=gsel_all[:, i, 0:1], scalar2=None, op0=ALU.mult), R=[Y[0], gsel_all], W=[acc])
                    for j in range(1, 4):
                        S.op('dve', lambda e, i=i, j=j, Y=Y: e.scalar_tensor_tensor(out=acc[:], in0=Y[j][:], scalar=gsel_all[:, i, j:j + 1], in1=acc[:], op0=ALU.mult, op1=ALU.add), R=[Y[j], gsel_all, acc], W=[acc])
                    S.op('pool', lambda e: e.tensor_tensor(out=acc[:], in0=acc[:], in1=self.gb[1][:], op=ALU.mult), R=[acc, self.gb[1]], W=[acc])
                    S.op('dve', lambda e, x_=x_: e.tensor_tensor(out=x_[:], in0=x_[:], in1=acc[:], op=ALU.add), R=[x_, acc], W=[x_])
                    self.dma('sp', self.xres[i * 128:(i + 1) * 128, :], x_[:], R=[x_], W=[TB()])
                S.barrier(); S.emit()

    def final_norm(self):
        S = self.S
        with ExitStack() as st:
            gfb = self.bcast_row(st, "gfb", self.norm_final[0, :], D)
            xt = [self.sb(st, f"fx{i}", [128, D], F32) for i in range(2)]
            junk = self.sb(st, "fjunk", [128, D], F32)
            ss = [self.sb(st, f"fss{i}", [128, 1], F32) for i in range(2)]
            for i in range(NT):
                x_, s_ = xt[i % 2], ss[i % 2]
                self.dma('sp' if i % 2 == 0 else 'act', x_[:], self.xres[i * 128:(i + 1) * 128, :], W=[x_])
                S.op('act', lambda e, x_=x_, s_=s_: e.activation(out=junk[:], in_=x_[:], func=AF.Square, accum_out=s_[:]), R=[x_], W=[junk, s_])
                S.op('dve', lambda e, s_=s_: e.tensor_scalar(out=s_[:], in0=s_[:], scalar1=1.0 / D, scalar2=1e-5, op0=ALU.mult, op1=ALU.add), R=[s_], W=[s_])
                S.op('act', lambda e, s_=s_: e.activation(out=s_[:], in_=s_[:], func=AF.Sqrt), R=[s_], W=[s_])
                S.op('dve', lambda e, s_=s_: e.reciprocal(out=s_[:], in_=s_[:]), R=[s_], W=[s_])
                S.op('dve', lambda e, x_=x_, s_=s_: e.scalar_tensor_tensor(out=x_[:], in0=x_[:], scalar=s_[:, 0:1], in1=gfb[:], op0=ALU.mult, op1=ALU.mult), R=[x_, s_, gfb], W=[x_])
                t = TB()
                self.dma('sp', self.out[i * 128:(i + 1) * 128, :], x_[:], R=[x_], W=[t])
                self.final_tbs.append(t)
            S.barrier(); S.emit()


class _Stop(Exception):
    pass


class _View:
    def __init__(self, buf, i):
        self.buf = buf; self.i = i; self.tb = buf.tb

    def __getitem__(self, k):
        return self.buf.t[:, self.i, :][k]


def make_in_maps(inputs):
    f = lambda a: np.ascontiguousarray(np.asarray(a, dtype=np.float32))
    w_ext = np.ascontiguousarray(np.asarray(inputs['w_in'], np.float32)[:, :, WCOLS])
    lw = np.ascontiguousarray(np.concatenate([inputs['rwkv_w2'], inputs['rwkv_a2'], inputs['rwkv_g2']], axis=1).astype(np.float32))
    qs = np.array(_swap_cols(0, 4, 64, 8)); qis = np.array(_swap_cols(0, 8, 32, 4))
    shared = dict(
        cst=CST, ada_w=f(inputs['ada_w']), ada_b=f(inputs['ada_b']), norm_mix=f(inputs['norm_mix']), norm_ffn=f(inputs['norm_ffn']),
        w_ext=w_ext, ret_gn=f(inputs['ret_gn']), rwkv_mu=f(inputs['rwkv_mu']), rwkv_w0=f(inputs['rwkv_w0']), rwkv_lw=lw,
        rwkv_a0=f(inputs['rwkv_a0']), rwkv_kk=f(inputs['rwkv_kk']), rwkv_ka=f(inputs['rwkv_ka']),
        rwkv_rk=f(np.asarray(inputs['rwkv_rk']).reshape(L, 256)), rwkv_ln=f(inputs['rwkv_ln']),
        dsa_qnorm=f(inputs['dsa_qnorm']), dsa_wq_up=f(inputs['dsa_wq_up']), dsa_wqs_up=f(np.asarray(inputs['dsa_wq_up'])[:, :, qs]),
        dsa_wqi_up=f(inputs['dsa_wqi_up']), dsa_wqis_up=f(np.asarray(inputs['dsa_wqi_up'])[:, :, qis]),
        dsa_onorm=f(inputs['dsa_onorm']), sb_onorm=f(inputs['sb_onorm']), w_out=f(inputs['w_out']),
        router_w=f(inputs['router_w']), router_b=f(inputs['router_b']), moe_w1=f(inputs['moe_w1']), moe_b1=f(inputs['moe_b1']),
        moe_w2=f(inputs['moe_w2']), moe_b2=f(inputs['moe_b2']), norm_final=f(np.asarray(inputs['norm_final']).reshape(1, D)),
    )
    maps = []
    x = np.asarray(inputs['x'], np.float32); c = np.asarray(inputs['c'], np.float32); pos = np.asarray(inputs['positions'], np.int32)
    for b in range(x.shape[0]):
        m = dict(shared)
        m['x'] = np.ascontiguousarray(x[b]); m['c'] = np.ascontiguousarray(c[b:b + 1]); m['pos'] = np.ascontiguousarray(pos[b:b + 1])
        maps.append(m)
    return maps


def kernel(**inputs):
    maps = make_in_maps(inputs)
    nc = Prog().build()
    res = run_bass_kernel_spmd(nc, maps, core_ids=list(range(NB)))
    return np.stack([np.asarray(r['out'], dtype=np.float32) for r in res.results], axis=0)
```

```python
import numpy as np
from contextlib import ExitStack
import concourse.bass as bass
import concourse.mybir as mybir
from concourse.bass_utils import run_bass_kernel_spmd

F32 = mybir.dt.float32; BF16 = mybir.dt.bfloat16; I32 = mybir.dt.int32; U32 = mybir.dt.uint32
AF = mybir.ActivationFunctionType; ALU = mybir.AluOpType; AX = mybir.AxisListType

D = 1024; SEQ = 4096; NB = 8; L = 2; NT = SEQ // 128
MB = 512; NBLK = 64; NSLOT = NBLK * MB
IN_COLS = 2984
ENGS = ['pe', 'act', 'dve', 'pool', 'sp']
LIMIT = 30000


class TB:
    __slots__ = ('w', 'wd', 'r', 'excl')

    def __init__(self):
        self.w = None; self.wd = {}; self.r = {}; self.excl = False


class Buf:
    def __init__(self, t):
        self.t = t; self.tb = TB()

    def __getitem__(self, k):
        return self.t[k]


def _tb(x):
    return getattr(x, 'tb', x)


class Sched:
    def __init__(self, nc, stack, ndsem=32):
        self.nc = nc; self.stack = stack
        self.ops = {e: [] for e in ENGS}
        self.epoch = {e: 0 for e in ENGS}
        self.cnt = {e: 0 for e in ENGS}
        self.sems = {}
        for e in ENGS:
            self.sems[(e, 0)] = stack.enter_context(nc.semaphore(f"s_{e}_0"))
        self.dsems = [stack.enter_context(nc.semaphore(f"sd_{i}")) for i in range(ndsem)]
        self.dcnt = [0] * ndsem; self.dnext = {'hw': 0, 'sw': 0}
        self.nhw = ndsem // 2
        self.seen = {e: {} for e in ENGS}
        self.nops = 0

    def semof(self, key):
        if key[0] == 'd':
            return self.dsems[key[1]]
        return self.sems[key]

    def op(self, eng, fn, R=(), W=(), dma=False):
        need = {}

        def nd(k, v):
            if need.get(k, 0) < v:
                need[k] = v
        R = [_tb(x) for x in R]; W = [_tb(x) for x in W]
        W = W + [t for t in R if t.excl and t not in W]
        R = [t for t in R if not t.excl]
        for t in R:
            if t.w:
                nd(*t.w)
            for k, v in t.wd.items():
                nd(k, v)
        for t in W:
            if t.w and (dma or t.w[0][0] != eng):
                nd(*t.w)
            if not dma:
                for k, v in t.wd.items():
                    nd(k, v)
            for k, v in t.r.items():
                if not dma and k[0] == eng:
                    continue
                nd(k, v)
        waits = []
        for k, v in need.items():
            if k[0] == 'd':
                v = self.dcnt[k[1]]
            elif k[0] == 'pe' and eng == 'pe':
                continue
            if self.seen[eng].get(k, 0) < v:
                waits.append((k, v)); self.seen[eng][k] = v
        if dma:
            kind = 'sw' if eng == 'pool' else 'hw'
            n = self.nhw if kind == 'hw' else len(self.dsems) - self.nhw
            i = self.dnext[kind] + (0 if kind == 'hw' else self.nhw)
            self.dnext[kind] = (self.dnext[kind] + 1) % n
            self.dcnt[i] += 16; key = ('d', i); val = self.dcnt[i]; inc = 16
        else:
            if self.cnt[eng] >= LIMIT:
                self.epoch[eng] += 1; self.cnt[eng] = 0
                self.sems[(eng, self.epoch[eng])] = self.stack.enter_context(
                    self.nc.semaphore(f"s_{eng}_{self.epoch[eng]}"))
            self.cnt[eng] += 1; key = (eng, self.epoch[eng]); val = self.cnt[eng]; inc = 1
        self.ops[eng].append((waits, fn, key, inc))
        for t in R:
            t.r[key] = val
        for t in W:
            if dma:
                t.wd[key] = val
            else:
                t.w = (key, val); t.wd = {}
                t.r = {}
        self.nops += 1

    def barrier(self):
        for e in ENGS:
            waits = []
            for f in ENGS:
                if f == e:
                    continue
                k = (f, self.epoch[f]); v = self.cnt[f]
                if v > 0 and self.seen[e].get(k, 0) < v:
                    waits.append((k, v)); self.seen[e][k] = v
            for i in range(len(self.dsems)):
                k = ('d', i); v = self.dcnt[i]
                if v > 0 and self.seen[e].get(k, 0) < v:
                    waits.append((k, v)); self.seen[e][k] = v
            self.ops[e].append((waits, None, None, 0))

    def emit(self):
        nc = self.nc
        names = {'pe': 'tensor', 'act': 'scalar', 'dve': 'vector', 'pool': 'gpsimd', 'sp': 'sync'}
        with nc.Block() as block:
            for e in ENGS:
                lst = self.ops[e]

                def body(engine, lst=lst):
                    for waits, fn, key, inc in lst:
                        for k, v in waits:
                            engine.wait_ge(self.semof(k), v)
                        if fn is not None:
                            ins = fn(engine)
                            ins.then_inc(self.semof(key), inc)
                getattr(block, names[e])(body)
        self.ops = {e: [] for e in ENGS}


RET0, RWKV0, DSA0, SB0 = 0, 1024, 1920, 2216


def _swap_cols(base, nheads, hd, half):
    cols = []
    for h in range(nheads):
        for f in range(hd):
            if f < half:
                g = f + half
            elif f < 2 * half:
                g = f - half
            else:
                g = f
            cols.append(base + h * hd + g)
    return cols


def build_wext_cols():
    blocks = {}
    r = lambda a, n: list(range(a, a + n))
    qs = _swap_cols(RET0, 4, 64, 32); ks = _swap_cols(RET0 + 256, 4, 64, 32)
    for hp in range(2):
        blocks[f'ret_q{hp}'] = r(RET0 + hp * 128, 128)
        blocks[f'ret_qs{hp}'] = qs[hp * 128:(hp + 1) * 128]
        blocks[f'ret_k{hp}'] = r(RET0 + 256 + hp * 128, 128)
        blocks[f'ret_ks{hp}'] = ks[hp * 128:(hp + 1) * 128]
        blocks[f'ret_vg{hp}'] = r(RET0 + 512 + hp * 128, 128) + r(RET0 + 768 + hp * 128, 128)
    for hp in range(2):
        blocks[f'sb_q{hp}'] = r(SB0 + hp * 128, 128)
        blocks[f'sb_k{hp}'] = r(SB0 + 256 + hp * 128, 128)
    blocks['sb_v'] = r(SB0 + 512, 256)
    blocks['rw_rkv'] = r(RWKV0, 768)
    blocks['rw_lora'] = r(RWKV0 + 768, 128)
    blocks['ds_cq'] = r(DSA0, 128)
    kcols = r(DSA0 + 128, 64); kscols = _swap_cols(DSA0 + 128, 1, 64, 8)
    blocks['ds_k'] = kcols + kcols
    blocks['ds_ks'] = kscols + kscols
    icols = r(DSA0 + 256, 32); iscols = _swap_cols(DSA0 + 256, 1, 32, 4)
    blocks['ds_ki'] = icols * 4
    blocks['ds_kis'] = iscols * 4
    blocks['ds_vw'] = r(DSA0 + 192, 64) + r(DSA0 + 288, 8)
    off = {}; cols = []
    for k, v in blocks.items():
        off[k] = (len(cols), len(v)); cols += v
    return off, np.array(cols, dtype=np.int64)


WOFF, WCOLS = build_wext_cols()
NEXT = len(WCOLS)


def build_consts():
    c = {}
    p = np.arange(128)
    c['ident'] = np.eye(128, dtype=np.float32)
    sbm = np.zeros((4, 128, 512), np.float32)
    for rr in range(4):
        for qb in range(4):
            if qb > rr:
                sbm[rr, :, qb * 128:(qb + 1) * 128] = 1.0
            elif qb == rr:
                sbm[rr, :, qb * 128:(qb + 1) * 128] = (p[:, None] < p[None, :]).astype(np.float32)
    c['sbmask'] = sbm.transpose(1, 0, 2).reshape(128, 4 * 512)
    c['tri_ge'] = (p[:, None] >= p[None, :]).astype(np.float32)
    c['tri_le'] = (p[:, None] <= p[None, :]).astype(np.float32)
    c['tri_lt'] = (p[:, None] < p[None, :]).astype(np.float32)
    c['tri_gt'] = (p[:, None] > p[None, :]).astype(np.float32)
    c['ntri_ge'] = -c['tri_ge']
    c['nones'] = -np.ones((128, 128), np.float32)
    c['hm4'] = (p[:, None] // 32 == np.arange(4)[None, :]).astype(np.float32)
    c['hm2'] = (p[:, None] // 64 == np.arange(2)[None, :]).astype(np.float32)
    c['iota_blk'] = np.tile(np.arange(64, dtype=np.float32)[None, :], (128, 1))
    c['kp'] = (np.arange(8)[None, :] * 128 + p[:, None]).astype(np.float32)
    c['pcol'] = p.astype(np.float32)[:, None]
    c['last'] = (p == 127).astype(np.float32)[:, None]
    c['negmask'] = np.where(p[None, :] > p[:, None], -1e30, 0.0).astype(np.float32)
    lg = np.log(1.0 - 2.0 ** (-5.0 - np.arange(4, dtype=np.float64)))
    idx = np.arange(128, dtype=np.float64)
    for hp in range(2):
        hs = [2 * hp, 2 * hp + 1]
        hp_of_p = np.array([hs[q // 64] for q in range(128)])
        c[f'ret_qdec{hp}'] = np.exp(lg[hp_of_p][:, None] * (idx[None, :] + 1.0)).astype(np.float32)
        kd = np.zeros((128, 128)); dm = np.zeros((128, 2, 128))
        for j, h in enumerate(hs):
            kd[:, j * 64:(j + 1) * 64] = (np.exp(lg[h] * (127.0 - idx)) / 8.0)[:, None]
            diff = idx[None, :] - idx[:, None]
            dm[:, j, :] = np.where(diff >= 0, np.exp(lg[h] * np.maximum(diff, 0.0)), 0.0) / 8.0
        c[f'ret_kdec{hp}'] = kd.astype(np.float32)
        c[f'ret_dmask{hp}'] = dm.reshape(128, 256).astype(np.float32)
        c[f'ret_cd{hp}'] = np.exp(lg[hp_of_p] * 128.0).astype(np.float32)[:, None]
    f64 = p % 64
    c['rope_ret'] = np.stack([10000.0 ** (-(f64 % 32) / 32.0) / (2 * np.pi), np.where(f64 < 32, -1.0, 1.0)], 1).astype(np.float32)
    inv = np.where(f64 < 16, 500000.0 ** (-(f64 % 8) / 8.0), 0.0) / (2 * np.pi)
    sg = np.where(f64 < 8, -1.0, np.where(f64 < 16, 1.0, 0.0))
    c['rope_dq'] = np.stack([inv, sg], 1).astype(np.float32)
    f32 = p % 32
    inv = np.where(f32 < 8, 500000.0 ** (-(f32 % 4) / 4.0), 0.0) / (2 * np.pi)
    sg = np.where(f32 < 4, -1.0, np.where(f32 < 8, 1.0, 0.0))
    c['rope_di'] = np.stack([inv, sg], 1).astype(np.float32)
    off = {}; n = 0; arrs = []
    for k, v in c.items():
        off[k] = (n, v.shape[1]); n += v.shape[1]; arrs.append(v.astype(np.float32))
    return off, np.ascontiguousarray(np.concatenate(arrs, 1))


COFF, CST = build_consts()


class Prog:
    def __init__(self, debug=None, nlayers=L, flags=None):
        self.debug = debug or {}
        self.flags = flags or {}
        self.nlayers = nlayers
        nc = self.nc = bass.Bass("TRN2", target_bir_lowering=False)
        dt = lambda name, shape, dtype, kind="ExternalInput": nc.dram_tensor(name, shape, dtype, kind=kind).ap()
        self.x = dt("x", [SEQ, D], F32)
        self.c = dt("c", [1, D], F32)
        self.pos = dt("pos", [1, SEQ], I32)
        self.cst = dt("cst", [128, CST.shape[1]], F32)
        self.ada_w = dt("ada_w", [L, D, 6 * D], F32)
        self.ada_b = dt("ada_b", [L, 6 * D], F32)
        self.norm_mix = dt("norm_mix", [L, D], F32)
        self.norm_ffn = dt("norm_ffn", [L, D], F32)
        self.w_ext = dt("w_ext", [L, D, NEXT], F32)
        self.ret_gn = dt("ret_gn", [L, 256], F32)
        self.rwkv_mu = dt("rwkv_mu", [L, 896], F32)
        self.rwkv_w0 = dt("rwkv_w0", [L, 256], F32)
        self.rwkv_lw = dt("rwkv_lw", [L, 128, 256], F32)
        self.rwkv_a0 = dt("rwkv_a0", [L, 256], F32)
        self.rwkv_kk = dt("rwkv_kk", [L, 256], F32)
        self.rwkv_ka = dt("rwkv_ka", [L, 256], F32)
        self.rwkv_rk = dt("rwkv_rk", [L, 256], F32)
        self.rwkv_ln = dt("rwkv_ln", [L, 256], F32)
        self.dsa_qnorm = dt("dsa_qnorm", [L, 128], F32)
        self.dsa_wq_up = dt("dsa_wq_up", [L, 128, 256], F32)
        self.dsa_wqs_up = dt("dsa_wqs_up", [L, 128, 256], F32)
        self.dsa_wqi_up = dt("dsa_wqi_up", [L, 128, 256], F32)
        self.dsa_wqis_up = dt("dsa_wqis_up", [L, 128, 256], F32)
        self.dsa_onorm = dt("dsa_onorm", [L, 256], F32)
        self.sb_onorm = dt("sb_onorm", [L, 256], F32)
        self.w_out = dt("w_out", [L, D, D], F32)
        self.router_w = dt("router_w", [L, D, 32], F32)
        self.router_b = dt("router_b", [L, 32], F32)
        if not self.flags.get('nomoe'):
            self.moe_w1 = dt("moe_w1", [L, 32, D, 2 * D], F32)
            self.moe_b1 = dt("moe_b1", [L, 32, 2 * D], F32)
            self.moe_w2 = dt("moe_w2", [L, 32, D, D], F32)
            self.moe_b2 = dt("moe_b2", [L, 32, D], F32)
        self.norm_final = dt("norm_final", [1, D], F32)
        self.out = dt("out", [SEQ, D], F32, kind="ExternalOutput")
        self.xres = dt("xres", [SEQ, D], F32, kind="Internal")
        self.yT_d = dt("yT_d", [8, 128, SEQ], BF16, kind="Internal")
        self.maskT_d = dt("maskT_d", [NT, 128, NT, 128], BF16, kind="Internal")
        self.hrow_d = dt("hrow_d", [SEQ, D], BF16, kind="Internal")
        self.tokidx_d = dt("tokidx_d", [NSLOT, 2], I32, kind="Internal")
        self.yslot_d = dt("yslot_d", [NSLOT, D], F32, kind="Internal")
        self.dbg = {}
        for name, (shape, dtype) in self.debug.items():
            self.dbg[name] = dt("dbg_" + name, shape, dtype, kind="ExternalOutput")
        self.final_tbs = []

    def sb(self, st, name, shape, dtype):
        self._uid = getattr(self, '_uid', 0) + 1
        return Buf(st.enter_context(self.nc.sbuf_tensor(f"{name}_{self._uid}", shape, dtype)))

    def ps(self, st, name, shape, dtype):
        self._uid = getattr(self, '_uid', 0) + 1
        b = Buf(st.enter_context(self.nc.psum_tensor(f"{name}_{self._uid}", shape, dtype)))
        b.tb.excl = True
        return b

    def dma(self, eng, out, in_, R=(), W=(), **kw):
        self.S.op(eng, lambda e: e.dma_start(out=out, in_=in_, **kw), R=R, W=W, dma=True)

    def load_const(self, st, name, dtype=F32, eng='sp'):
        o, n = COFF[name]
        b = self.sb(st, "c_" + name, [128, n], dtype)
        kw = dict(allow_slow_non_contiguous=True) if n < 8 else {}
        self.dma('pool' if dtype != F32 else eng, b[:], self.cst[:, o:o + n], W=[b], **kw)
        return b

    def bcast_row(self, st, name, row_ap, n, eng='sp'):
        b = self.sb(st, name, [128, n], F32)
        self.dma(eng, b[:], row_ap.partition_broadcast(128), W=[b])
        return b

    def dbg_out(self, name, src_ap, R, dst=None):
        if name in self.dbg:
            t = TB()
            d = self.dbg[name] if dst is None else dst
            self.dma('sp', d, src_ap, R=R, W=[t])
            self.final_tbs.append(t)

    def build(self):
        nc = self.nc
        with ExitStack() as top:
            S = self.S = Sched(nc, top)
            self.ident = self.load_const(top, 'ident', BF16)
            self.identf = self.load_const(top, 'ident', F32)
            self.modT = self.sb(top, "modT", [128, 48], F32)
            self.gb = [self.sb(top, f"gb{i}", [128, D], F32) for i in range(2)]
            self.ab2 = self.sb(top, "ab2", [128, D], F32); self.shb2 = self.sb(top, "shb2", [128, D], F32)
            self.reg_bc = top.enter_context(nc.gpsimd.register("reg_bc"))

            self.ones_row = self.sb(top, "ones_row", [1, 512], F32)
            self.condT = self.sb(top, "condT", [128, 8], F32)
            self.PF = [self.ps(top, f"pf{i}", [128, 512], F32) for i in range(6)]
            self.PB = [self.ps(top, f"pb{i}", [128, 1024], BF16) for i in range(2)]
            S.op('dve', lambda e: e.memset(self.ones_row[:], 1.0), W=[self.ones_row])
            with ExitStack() as st:
                self.cond_phase(st)
                S.barrier(); S.emit()
            for l in range(self.nlayers):
                self.layer(l)
            if 'stop' not in self.flags:
                self.final_norm()
            need = {}
            for t in self.final_tbs:
                for k in t.wd:
                    need[k] = max(need.get(k, 0), S.dcnt[k[1]])
            S.ops['sp'].append((list(need.items()), None, None, 0))
            S.barrier(); S.emit()
        return nc

    def cond_phase(self, st):
        S = self.S
        crow = self.sb(st, "crow", [1, D], F32)
        self.dma('sp', crow[:], self.c[0:1, :], W=[crow])
        pf = self.PF[0]
        for j in range(8):
            S.op('pe', lambda e, j=j: e.matmul(pf[:, j:j + 1], lhsT=crow[0:1, j * 128:(j + 1) * 128], rhs=self.ones_row[0:1, 0:1],
                                              start=True, stop=True), R=[crow, self.ones_row], W=[pf])
        S.op('act', lambda e: e.activation(out=self.condT[:], in_=pf[:, 0:8], func=AF.Silu), R=[pf], W=[self.condT])

    def row_to_cols(self, row_buf, row_ap_fn, ncols, out_ap, extra_R=()):
        S = self.S; pf = self.PF[0]
        for j in range(ncols):
            S.op('pe', lambda e, j=j: e.matmul(pf[:, j:j + 1], lhsT=row_ap_fn(j), rhs=self.ones_row[0:1, 0:1], start=True, stop=True),
                 R=[row_buf, self.ones_row], W=[pf])
        return pf

    def layer(self, l):
        S = self.S
        self.mod_phase(l)
        if self.flags.get('upto') == 'mod':
            return
        with ExitStack() as hs:
            self.hT = self.sb(hs, "hT", [128, 8, SEQ + 1], BF16)
            S.op('dve', lambda e: e.memset(self.hT[:, :, 0:1], 0.0), W=[self.hT])
            self.norm_phase(l, 0)
            if self.flags.get('upto') == 'norm':
                return
            only = self.flags.get('only')
            if only in (None, 'ret'):
                self.retention_phase(l)
            if only in (None, 'rwkv'):
                self.rwkv_phase(l)
            if only in (None, 'dsa'):
                self.dsa_phase(l)
            if only in (None, 'sb'):
                self.sb_phase(l)
            S.barrier(); S.emit()
        if 'yT' in self.dbg:
            for j in range(8):
                self.dbg_out('yT', self.yT_d[j], [], dst=self.dbg['yT'][j])
            S.barrier(); S.emit()
        if self.flags.get('upto') == 'mix':
            return
        self.wout_phase(l)
        if f'xmix{l}' in self.dbg:
            self.dbg_out(f'xmix{l}', self.xres[:, :], [])
            S.barrier(); S.emit()
        if self.flags.get('upto') == 'wout':
            return
        self.moe_phase(l)
        if f'x{l}' in self.dbg:
            self.dbg_out(f'x{l}', self.xres[:, :], [])
            S.barrier(); S.emit()

    def mod_phase(self, l):
        S = self.S
        with ExitStack() as st:
            self.modrow = self.sb(st, "modrow", [1, 6 * D], F32)
            wt = [self.sb(st, f"adaw{i}", [128, 8, 512], F32) for i in range(2)]
            brow = self.sb(st, "adab", [1, 6 * D], F32)
            self.dma('sp', brow[:], self.ada_b[l:l + 1, :], W=[brow])
            for cg in range(12):
                w = wt[cg % 2]
                self.dma('sp' if cg % 2 == 0 else 'act', w[:], self.ada_w[l, :, cg * 512:(cg + 1) * 512].rearrange("(k p) c -> p k c", p=128), W=[w])
                pf = self.PF[1 + cg % 2]
                for k in range(8):
                    S.op('pe', lambda e, k=k, w=w, pf=pf: e.matmul(pf[0:1, :], lhsT=self.condT[:, k:k + 1], rhs=w[:, k, :], start=(k == 0), stop=(k == 7)),
                         R=[self.condT, w], W=[pf])
                S.op('dve', lambda e, cg=cg, pf=pf: e.tensor_tensor(out=self.modrow[0:1, cg * 512:(cg + 1) * 512], in0=pf[0:1, :],
                                                                  in1=brow[0:1, cg * 512:(cg + 1) * 512], op=ALU.add), R=[pf, brow], W=[self.modrow])
            pf = self.row_to_cols(self.modrow, lambda j: self.modrow[0:1, j * 128:(j + 1) * 128], 48, None)
            S.op('dve', lambda e: e.tensor_copy(out=self.modT[:], in_=pf[:, 0:48]), R=[pf], W=[self.modT])
            onescol = self.sb(st, "ones1", [1, 128], F32)
            S.op('dve', lambda e: e.memset(onescol[:], 1.0), W=[onescol])
            for gi, base in enumerate((2 * D, 5 * D)):
                for hf in range(2):
                    pf2 = self.PF[3 + hf]
                    S.op('pe', lambda e, pf2=pf2, base=base, hf=hf: e.matmul(pf2[:], lhsT=onescol[0:1, :], rhs=self.modrow[0:1, base + hf * 512: base + (hf + 1) * 512],
                                                                            start=True, stop=True), R=[onescol, self.modrow], W=[pf2])
                    S.op('act', lambda e, pf2=pf2, gi=gi, hf=hf: e.activation(out=self.gb[gi][:, hf * 512:(hf + 1) * 512], in_=pf2[:], func=AF.Copy), R=[pf2], W=[self.gb[gi]])
            grow2 = self.sb(st, "grow2", [1, D], F32); a2row = self.sb(st, "a2row", [1, D], F32)
            self.dma('sp', grow2[:], self.norm_ffn[l:l + 1, :], W=[grow2])
            S.op('dve', lambda e: e.scalar_tensor_tensor(out=a2row[:], in0=self.modrow[0:1, 4 * D:5 * D], scalar=1.0, in1=grow2[:], op0=ALU.add, op1=ALU.mult),
                 R=[self.modrow, grow2], W=[a2row])
            for dstb, rowfn, rb in ((self.ab2, lambda hf: a2row[0:1, hf * 512:(hf + 1) * 512], a2row), (self.shb2, lambda hf: self.modrow[0:1, 3 * D + hf * 512: 3 * D + (hf + 1) * 512], self.modrow)):
                for hf in range(2):
                    pf2 = self.PF[3 + hf]
                    S.op('pe', lambda e, pf2=pf2, rowfn=rowfn, hf=hf: e.matmul(pf2[:], lhsT=onescol[0:1, :], rhs=rowfn(hf), start=True, stop=True), R=[onescol, rb], W=[pf2])
                    S.op('act', lambda e, pf2=pf2, dstb=dstb, hf=hf: e.activation(out=dstb[:, hf * 512:(hf + 1) * 512], in_=pf2[:], func=AF.Copy), R=[pf2], W=[dstb])
            self.dbg_out(f'mod{l}', self.modrow[0:1, :], [self.modrow])
            S.barrier(); S.emit()

    def norm_phase(self, l, which):
        S = self.S
        src = self.x if (l == 0 and which == 0) else self.xres
        gsrc = self.norm_mix if which == 0 else self.norm_ffn
        shc, scc = (0, 8) if which == 0 else (24, 32)
        with ExitStack() as st:
            grow = self.sb(st, "grow", [1, D], F32)
            self.dma('sp', grow[:], gsrc[l:l + 1, :], W=[grow])
            pf = self.row_to_cols(grow, lambda j: grow[0:1, j * 128:(j + 1) * 128], 8, None)
            acol = self.sb(st, "acol", [128, 8], F32)
            S.op('dve', lambda e: e.scalar_tensor_tensor(out=acol[:], in0=self.modT[:, scc:scc + 8], scalar=1.0, in1=pf[:, 0:8], op0=ALU.add, op1=ALU.mult),
                 R=[self.modT, pf], W=[acol])
            xt = [self.sb(st, f"xt{i}", [128, D], F32) for i in range(2)]
            xn = [self.sb(st, f"xn{i}", [128, D], BF16) for i in range(2)]
            junk = self.sb(st, "junk", [128, D], BF16)
            hrow = [self.sb(st, f"hrow{i}", [128, D], BF16) for i in range(2)] if which == 1 else None
            ssq = [self.sb(st, f"ssq{i}", [128, 1], F32) for i in range(2)]
            for i in range(NT):
                x_, xn_, ss = xt[i % 2], xn[i % 2], ssq[i % 2]
                self.dma('sp' if i % 2 == 0 else 'act', x_[:], src[i * 128:(i + 1) * 128, :], W=[x_])
                S.op('act', lambda e, x_=x_, ss=ss: e.activation(out=junk[:], in_=x_[:], func=AF.Square, accum_out=ss[:]), R=[x_], W=[junk, ss])
                S.op('dve', lambda e, ss=ss: e.tensor_scalar(out=ss[:], in0=ss[:], scalar1=1.0 / D, scalar2=1e-5, op0=ALU.mult, op1=ALU.add), R=[ss], W=[ss])
                S.op('act', lambda e, ss=ss: e.activation(out=ss[:], in_=ss[:], func=AF.Sqrt), R=[ss], W=[ss])
                S.op('dve', lambda e, ss=ss: e.reciprocal(out=ss[:], in_=ss[:]), R=[ss], W=[ss])
                S.op('dve', lambda e, x_=x_, xn_=xn_, ss=ss: e.tensor_scalar(out=xn_[:], in0=x_[:], scalar1=ss[:, 0:1], scalar2=None, op0=ALU.mult), R=[x_, ss], W=[xn_])
                pb = self.PB[i % 2]
                for j in range(8):
                    S.op('pe', lambda e, j=j, xn_=xn_, pb=pb: e.transpose(out=pb[:, j * 128:(j + 1) * 128], in_=xn_[:, j * 128:(j + 1) * 128], identity=self.ident[:]),
                         R=[xn_, self.ident], W=[pb])
                if which == 1:
                    hr = hrow[i % 2]
                    S.op('pool', lambda e, xn_=xn_, hr=hr: e.tensor_tensor(out=hr[:], in0=xn_[:], in1=self.ab2[:], op=ALU.mult), R=[xn_, self.ab2], W=[hr])
                    S.op('pool', lambda e, hr=hr: e.tensor_tensor(out=hr[:], in0=hr[:], in1=self.shb2[:], op=ALU.add), R=[hr, self.shb2], W=[hr])
                    self.dma('sp', self.hrow_d[i * 128:(i + 1) * 128, :], hr[:], R=[hr], W=[TB()])
                for j in range(8):
                    eng = 'dve' if j % 2 == 0 else 'pool'
                    if eng == 'pool':
                        eng = 'act'
                        S.op('act', lambda e, j=j, pb=pb, i=i: e.activation(out=self.hT[:, j, 1 + i * 128: 1 + (i + 1) * 128], in_=pb[:, j * 128:(j + 1) * 128], func=AF.Identity,
                                                                           scale=acol[:, j:j + 1], bias=self.modT[:, shc + j: shc + j + 1]), R=[pb, acol, self.modT], W=[self.hT])
                    else:
                        S.op('dve', lambda e, j=j, pb=pb, i=i: e.tensor_scalar(out=self.hT[:, j, 1 + i * 128: 1 + (i + 1) * 128], in0=pb[:, j * 128:(j + 1) * 128],
                                                                              scalar1=acol[:, j:j + 1], scalar2=self.modT[:, shc + j: shc + j + 1], op0=ALU.mult, op1=ALU.add),
                             R=[pb, acol, self.modT], W=[self.hT])
            if f'hT{l}' in self.dbg and which == 0:
                for j in range(8):
                    self.dbg_out(f'hT{l}', self.hT[:, j, 1:], [self.hT], dst=self.dbg[f'hT{l}'][j])
            S.barrier(); S.emit()

    def load_w(self, st, name, l, blocks):
        n = sum(WOFF[b][1] for b in blocks)
        wm = self.sb(st, name, [128, 8, n], BF16)
        o = 0; offs = {}
        for b in blocks:
            c0, cn = WOFF[b]
            for k in range(8):
                self.dma('pool', wm[:, k, o:o + cn], self.w_ext[l, k * 128:(k + 1) * 128, c0:c0 + cn], W=[wm])
            offs[b] = o; o += cn
        return wm, offs

    def proj_T(self, wm, c0, pf, tg, shift=0, start=True, stop=True, M=128):
        S = self.S
        for k in range(8):
            S.op('pe', lambda e, k=k: e.matmul(pf[0:M, :], lhsT=wm[:, k, c0:c0 + M], rhs=self.hT[:, k, 1 - shift + tg * 512: 1 - shift + (tg + 1) * 512],
                                              start=(start and k == 0), stop=(stop and k == 7)), R=[wm, self.hT], W=[pf])

    def proj_tok(self, wm, c0, n, pf_ap, pf, i, shift=0, start=True, stop=True):
        S = self.S
        for k in range(8):
            S.op('pe', lambda e, k=k: e.matmul(pf_ap, lhsT=self.hT[:, k, 1 - shift + i * 128: 1 - shift + (i + 1) * 128], rhs=wm[:, k, c0:c0 + n],
                                              start=(start and k == 0), stop=(stop and k == 7)), R=[wm, self.hT], W=[pf])

    def rope_tables(self, st, cname, name):
        S = self.S
        rc = self.load_const(st, cname, F32)
        C = self.sb(st, name + "C", [128, SEQ], BF16); Sg = self.sb(st, name + "S", [128, SEQ], BF16)
        CH = 512
        with ExitStack() as s2:
            posi = self.sb(s2, name + "pi", [128, CH], I32); posf = self.sb(s2, name + "pf", [128, CH], F32)
            u = self.sb(s2, name + "u", [128, CH], F32); ui = self.sb(s2, name + "ui", [128, CH], I32)
            uf = self.sb(s2, name + "uf", [128, CH], F32)
            for ch in range(SEQ // CH):
                cs = slice(ch * CH, (ch + 1) * CH)
                self.dma('sp', posi[:], self.pos[0:1, cs].partition_broadcast(128), W=[posi])
                S.op('dve', lambda e: e.tensor_copy(out=posf[:], in_=posi[:]), R=[posi], W=[posf])
                for phase, dst in ((0.0, Sg), (0.25, C)):
                    S.op('dve', lambda e, phase=phase: e.tensor_scalar(out=u[:], in0=posf[:], scalar1=rc[:, 0:1], scalar2=phase, op0=ALU.mult, op1=ALU.add),
                         R=[posf, rc], W=[u])
                    S.op('dve', lambda e: e.tensor_copy(out=ui[:], in_=u[:]), R=[u], W=[ui])
                    S.op('dve', lambda e: e.tensor_copy(out=uf[:], in_=ui[:]), R=[ui], W=[uf])
                    S.op('dve', lambda e: e.tensor_tensor(out=u[:], in0=u[:], in1=uf[:], op=ALU.subtract), R=[u, uf], W=[u])
                    S.op('dve', lambda e: e.tensor_scalar(out=u[:], in0=u[:], scalar1=0.5, scalar2=-0.5, op0=ALU.min, op1=ALU.max), R=[u], W=[u])
                    if dst is Sg:
                        S.op('act', lambda e: e.activation(out=uf[:], in_=u[:], func=AF.Sin, scale=2 * np.pi), R=[u], W=[uf])
                        S.op('dve', lambda e, cs=cs: e.tensor_scalar(out=Sg[:, cs], in0=uf[:], scalar1=rc[:, 1:2], scalar2=None, op0=ALU.mult), R=[uf, rc], W=[Sg])
                    else:
                        S.op('act', lambda e, cs=cs: e.activation(out=C[:, cs], in_=u[:], func=AF.Sin, scale=2 * np.pi), R=[u], W=[C])
            self.S.barrier(); self.S.emit()
        return C, Sg

    def y_store(self, ytile_idx, i, ytok, ystage, R):
        S = self.S
        pb = self.PB[i % 2]
        for f in range(2):
            S.op('pe', lambda e, f=f: e.transpose(out=pb[:, f * 128:(f + 1) * 128], in_=ytok[:, f * 128:(f + 1) * 128], identity=self.ident[:]),
                 R=[ytok, self.ident] + list(R), W=[pb])
        S.op('act', lambda e: e.activation(out=ystage[:, :, (i % 4) * 128:(i % 4 + 1) * 128], in_=pb[:, 0:256].rearrange("p (f t) -> p f t", f=2), func=AF.Copy),
             R=[pb], W=[ystage])
        if i % 4 == 3:
            g = i // 4
            for f in range(2):
                t = TB()
                self.dma('sp', self.yT_d[ytile_idx + f, :, g * 512:(g + 1) * 512], ystage[:, f, :], R=[ystage], W=[t])

    def retention_phase(self, l):
        S = self.S
        with ExitStack() as st:
            C, Sg = self.rope_tables(st, 'rope_ret', 'rr')
            gnb = self.bcast_row(st, "ret_gnb", self.ret_gn[l, :], 256)
            ytok_all = self.sb(st, "ret_ytok", [128, NT, 256], BF16)
            for hp in range(2):
                with ExitStack() as s2:
                    wm, wo = self.load_w(s2, "wm_ret", l, [f'ret_q{hp}', f'ret_qs{hp}', f'ret_k{hp}', f'ret_ks{hp}', f'ret_vg{hp}'])
                    QT = self.sb(s2, "QT", [128, SEQ], BF16); QS = self.sb(s2, "QS", [128, SEQ], BF16)
                    KT = self.sb(s2, "KT", [128, SEQ], BF16); KS = self.sb(s2, "KS", [128, SEQ], BF16)
                    qdec = self.load_const(s2, f'ret_qdec{hp}', BF16); kdec = self.load_const(s2, f'ret_kdec{hp}', F32)
                    dmask = self.load_const(s2, f'ret_dmask{hp}', F32); cd = self.load_const(s2, f'ret_cd{hp}', F32)
                    for name, dst in ((f'ret_q{hp}', QT), (f'ret_qs{hp}', QS), (f'ret_k{hp}', KT), (f'ret_ks{hp}', KS)):
                        for tg in range(8):
                            pf = self.PF[tg % 4]
                            self.proj_T(wm, wo[name], pf, tg)
                            eng = 'act' if tg % 2 == 0 else 'dve'
                            if eng == 'act':
                                S.op('act', lambda e, pf=pf, dst=dst, tg=tg: e.activation(out=dst[:, tg * 512:(tg + 1) * 512], in_=pf[:], func=AF.Copy), R=[pf], W=[dst])
                            else:
                                S.op('dve', lambda e, pf=pf, dst=dst, tg=tg: e.tensor_copy(out=dst[:, tg * 512:(tg + 1) * 512], in_=pf[:]), R=[pf], W=[dst])
                    for A, B_ in ((QT, QS), (KT, KS)):
                        S.op('dve', lambda e, A=A: e.tensor_tensor(out=A[:], in0=A[:], in1=C[:], op=ALU.mult), R=[A, C], W=[A])
                        S.op('pool', lambda e, B_=B_: e.tensor_tensor(out=B_[:], in0=B_[:], in1=Sg[:], op=ALU.mult), R=[B_, Sg], W=[B_])
                        S.op('dve', lambda e, A=A, B_=B_: e.tensor_tensor(out=A[:], in0=A[:], in1=B_[:], op=ALU.add), R=[A, B_], W=[A])
                    QD = QS
                    S.op('dve', lambda e: e.tensor_tensor(out=QD[:].rearrange("p (c n) -> p c n", n=128), in0=QT[:].rearrange("p (c n) -> p c n", n=128),
                                                         in1=qdec[:].unsqueeze(1).broadcast_to([128, NT, 128]), op=ALU.mult), R=[QT, qdec], W=[QD])
                    state = self.sb(s2, "rstate", [128, 128], F32); state_bf = self.sb(s2, "rstate_bf", [128, 128], BF16)
                    qbd = [self.sb(s2, f"qbd{i}", [128, 256], BF16) for i in range(2)]
                    for i in range(2):
                        S.op('pool', lambda e, i=i: e.memset(qbd[i][:], 0.0), W=[qbd[i]])
                    S.op('dve', lambda e: e.memset(state[:], 0.0), W=[state])
                    S.op('dve', lambda e: e.memset(state_bf[:], 0.0), W=[state_bf])
                    vg = [self.sb(s2, f"rvg{i}", [128, 128], BF16) for i in range(2)]
                    sg = [self.sb(s2, f"rsg{i}", [128, 128], F32) for i in range(2)]
                    kd = [self.sb(s2, f"rkd{i}", [128, 128], BF16) for i in range(2)]
                    pT = [self.sb(s2, f"rpT{i}", [128, 256], BF16) for i in range(2)]
                    o_sb = self.sb(s2, "ro", [128, 128], F32); cen = self.sb(s2, "rcen", [128, 128], F32); sq = self.sb(s2, "rsq", [128, 128], F32)
                    st4 = self.sb(s2, "rst4", [128, 4], F32)
                    for c in range(NT):
                        i2 = c % 2
                        tok = slice(c * 128, (c + 1) * 128)
                        pfv = self.PF[0]
                        self.proj_tok(wm, wo[f'ret_vg{hp}'], 256, pfv[:, 0:256], pfv, c)
                        S.op('dve', lambda e, i2=i2: e.tensor_copy(out=vg[i2][:], in_=pfv[:, 0:128]), R=[pfv], W=[vg[i2]])
                        S.op('act', lambda e, i2=i2: e.activation(out=sg[i2][:], in_=pfv[:, 128:256], func=AF.Silu), R=[pfv], W=[sg[i2]])
                        pb = self.PB[0]
                        S.op('pe', lambda e, tok=tok: e.transpose(out=pb[:, 0:128], in_=KT[:, tok], identity=self.ident[:]), R=[KT, self.ident], W=[pb])
                        S.op('dve', lambda e, i2=i2: e.tensor_tensor(out=kd[i2][:], in0=pb[:, 0:128], in1=kdec[:], op=ALU.mult), R=[pb, kdec], W=[kd[i2]])
                        pfs = self.PF[1]
                        for hh in range(2):
                            pr = slice(hh * 64, (hh + 1) * 64)
                            S.op('pool', lambda e, hh=hh, pr=pr, tok=tok, i2=i2: e.tensor_copy(out=qbd[i2][pr, hh * 128:(hh + 1) * 128], in_=QT[pr, tok]), R=[QT, qbd[i2]], W=[qbd[i2]])
                        S.op('pe', lambda e, tok=tok, i2=i2: e.matmul(pfs[:, 0:256], lhsT=KT[:, tok], rhs=qbd[i2][:], start=True, stop=True), R=[KT, qbd[i2]], W=[pfs])
                        S.op('dve', lambda e, i2=i2: e.tensor_tensor(out=pT[i2][:], in0=pfs[:, 0:256], in1=dmask[:], op=ALU.mult), R=[pfs, dmask], W=[pT[i2]])
                        pfo = self.PF[2]
                        S.op('pe', lambda e, tok=tok: e.matmul(pfo[:, 0:128], lhsT=QD[:, tok], rhs=state_bf[:, :], start=True, stop=False), R=[QD, state_bf], W=[pfo])
                        for hh in range(2):
                            S.op('pe', lambda e, hh=hh, i2=i2: e.matmul(pfo[:, hh * 64:(hh + 1) * 64], lhsT=pT[i2][:, hh * 128:(hh + 1) * 128], rhs=vg[i2][:, hh * 64:(hh + 1) * 64],
                                                                      start=False, stop=(hh == 1)), R=[pT[i2], vg[i2]], W=[pfo])
                        pfu = self.PF[3]
                        S.op('pe', lambda e, i2=i2: e.matmul(pfu[:, 0:128], lhsT=kd[i2][:], rhs=vg[i2][:], start=True, stop=True), R=[kd[i2], vg[i2]], W=[pfu])
                        for hh in range(2):
                            pr = slice(hh * 64, (hh + 1) * 64)
                            cs = slice(hh * 64, (hh + 1) * 64)
                            S.op('dve', lambda e, hh=hh, pr=pr, cs=cs: e.scalar_tensor_tensor(out=state[pr, cs], in0=state[pr, cs], scalar=cd[pr, 0:1], in1=pfu[pr, cs],
                                                                                            op0=ALU.mult, op1=ALU.add), R=[state, cd, pfu], W=[state])
                        S.op('act', lambda e: e.activation(out=state_bf[:], in_=state[:], func=AF.Copy), R=[state], W=[state_bf])
                        S.op('act', lambda e: e.activation(out=o_sb[:], in_=pfo[:, 0:128], func=AF.Copy), R=[pfo], W=[o_sb])
                        self.head_norm(o_sb, cen, sq, st4, 2, 1e-5)
                        S.op('dve', lambda e, hp=hp: e.tensor_tensor(out=cen[:], in0=cen[:], in1=gnb[:, hp * 128:(hp + 1) * 128], op=ALU.mult), R=[cen, gnb], W=[cen])
                        S.op('dve', lambda e, i2=i2, c=c, hp=hp: e.tensor_tensor(out=ytok_all[:, c, hp * 128:(hp + 1) * 128], in0=cen[:], in1=sg[i2][:], op=ALU.mult),
                             R=[cen, sg[i2]], W=[ytok_all])
                    S.barrier(); S.emit()
            ystage = self.sb(st, "ystage", [128, 2, 512], BF16)
            for i in range(NT):
                self.y_store(0, i, _View(ytok_all, i), ystage, [])
            S.barrier(); S.emit()


    def rms_finalize(self, st, o_all, gain_b, ytile_idx, name):
        S = self.S
        ystage = self.sb(st, name + "ystage", [128, 2, 512], BF16)
        junk = self.sb(st, name + "junk", [128, 256], F32)
        ss = [self.sb(st, f"{name}ss{i}", [128, 1], F32) for i in range(2)]
        yt = [self.sb(st, f"{name}yt{i}", [128, 256], BF16) for i in range(2)]
        for i in range(NT):
            s_, y_ = ss[i % 2], yt[i % 2]
            S.op('act', lambda e, i=i, s_=s_: e.activation(out=junk[:], in_=o_all[:, i, :], func=AF.Square, accum_out=s_[:]), R=[o_all], W=[junk, s_])
            S.op('dve', lambda e, s_=s_: e.tensor_scalar(out=s_[:], in0=s_[:], scalar1=1.0 / 256, scalar2=1e-5, op0=ALU.mult, op1=ALU.add), R=[s_], W=[s_])
            S.op('act', lambda e, s_=s_: e.activation(out=s_[:], in_=s_[:], func=AF.Sqrt), R=[s_], W=[s_])
            S.op('dve', lambda e, s_=s_: e.reciprocal(out=s_[:], in_=s_[:]), R=[s_], W=[s_])
            S.op('dve', lambda e, i=i, s_=s_, y_=y_: e.scalar_tensor_tensor(out=y_[:], in0=o_all[:, i, :], scalar=s_[:, 0:1], in1=gain_b[:], op0=ALU.mult, op1=ALU.mult),
                 R=[o_all, s_, gain_b], W=[y_])
            self.y_store(ytile_idx, i, y_, ystage, [])

    def sb_phase(self, l):
        S = self.S
        with ExitStack() as st:
            ntri = self.load_const(st, 'ntri_ge', BF16); nones = self.load_const(st, 'nones', BF16)
            sbmask = self.load_const(st, 'sbmask', BF16)
            zer = self.sb(st, "sbzero", [128, 256], BF16)
            S.op('pool', lambda e: e.memset(zer[:], 0.0), W=[zer])
            onb = self.bcast_row(st, "sb_onb", self.sb_onorm[l, :], 256)
            o_all = self.sb(st, "sb_oall", [128, NT, 256], BF16)
            for hp in range(2):
                with ExitStack() as s2:
                    wm, wo = self.load_w(s2, "wm_sb", l, [f'sb_q{hp}', f'sb_k{hp}', 'sb_v'])
                    QT = self.sb(s2, "sbQT", [128, SEQ], BF16)
                    KM = [self.sb(s2, f"sbKM{i}", [128, SEQ], BF16) for i in range(2)]
                    V = self.sb(s2, "sbV", [128, NT, 128], BF16)
                    for i in range(2):
                        S.op('pool', lambda e, i=i: e.memset(KM[i][:], 0.0), W=[KM[i]])
                    for tg in range(8):
                        pf = self.PF[tg % 2]
                        self.proj_T(wm, wo[f'sb_q{hp}'], pf, tg)
                        S.op('act', lambda e, pf=pf, tg=tg: e.activation(out=QT[:, tg * 512:(tg + 1) * 512], in_=pf[:], func=AF.Copy, scale=0.125), R=[pf], W=[QT])
                        pf2 = self.PF[2 + tg % 2]
                        self.proj_T(wm, wo[f'sb_k{hp}'], pf2, tg)
                        S.op('dve', lambda e, pf2=pf2, tg=tg: e.tensor_copy(out=KM[0][0:64, tg * 512:(tg + 1) * 512], in_=pf2[0:64, :]), R=[pf2], W=[KM[0]])
                        S.op('act', lambda e, pf2=pf2, tg=tg: e.activation(out=KM[1][64:128, tg * 512:(tg + 1) * 512], in_=pf2[64:128, :], func=AF.Copy), R=[pf2], W=[KM[1]])
                    for i in range(NT):
                        pf = self.PF[i % 2]
                        self.proj_tok(wm, wo['sb_v'] + hp * 128, 128, pf[:, 0:128], pf, i)
                        S.op('dve', lambda e, pf=pf, i=i: e.tensor_copy(out=V[:, i, :], in_=pf[:, 0:128]), R=[pf], W=[V])
                    ebuf = [[self.sb(s2, f"sbe{h}{i}", [128, 512], F32) for i in range(2)] for h in range(2)]
                    spm = [[self.sb(s2, f"sbsp{h}{i}", [128, 512], BF16) for i in range(2)] for h in range(2)]
                    tbuf = [[self.sb(s2, f"sbt{h}{i}", [128, 512], F32) for i in range(2)] for h in range(2)]
                    abuf = [[self.sb(s2, f"sba{h}{i}", [128, 512], BF16) for i in range(2)] for h in range(2)]
                    racc = [self.sb(s2, f"sbracc{h}", [128, 512], F32) for h in range(2)]
                    cnt = 0
                    for g in range(8):
                        qs = slice(g * 512, (g + 1) * 512)
                        for hh in range(2):
                            po = self.PF[4 + hh]
                            S.op('pool', lambda e, hh=hh: e.memset(racc[hh][:], 0.0), W=[racc[hh]])
                            S.op('pe', lambda e, po=po: e.matmul(po[:, 0:256], lhsT=zer[:, 0:128], rhs=zer[:, 0:256], start=True, stop=False), R=[zer], W=[po])
                        nkb = 4 * g + 4
                        for kb in reversed(range(nkb)):
                            b2 = cnt % 2; cnt += 1
                            r = kb - 4 * g
                            ks = slice(kb * 128, (kb + 1) * 128)
                            HH = (0, 1)
                            E_ = [ebuf[h][b2] for h in HH]; SP_ = [spm[h][b2] for h in HH]; T_ = [tbuf[h][b2] for h in HH]; A_ = [abuf[h][b2] for h in HH]
                            PZ = [self.PF[0], self.PF[1]]; PC = [self.PF[2], self.PF[3]]; PO = [self.PF[4], self.PF[5]]
                            for hh in HH:
                                S.op('pe', lambda e, hh=hh, ks=ks, qs=qs: e.matmul(PZ[hh][:], lhsT=KM[hh][:, ks], rhs=QT[:, qs], start=True, stop=True), R=[KM[hh], QT], W=[PZ[hh]])
                            for hh in HH:
                                S.op('act', lambda e, hh=hh, E_=E_: e.activation(out=E_[hh][:], in_=PZ[hh][:], func=AF.Exp), R=[PZ[hh]], W=[E_[hh]])
                            for hh in HH:
                                S.op('act', lambda e, hh=hh, E_=E_, SP_=SP_: e.activation(out=SP_[hh][:], in_=E_[hh][:], func=AF.Ln, bias=1.0), R=[E_[hh]], W=[SP_[hh]])
                            if r >= 0:
                                for hh in HH:
                                    S.op('pool', lambda e, hh=hh, SP_=SP_, r=r: e.tensor_tensor(out=SP_[hh][:], in0=SP_[hh][:], in1=sbmask[:, r * 512:(r + 1) * 512], op=ALU.mult), R=[SP_[hh], sbmask], W=[SP_[hh]])
                            for hh in HH:
                                S.op('pe', lambda e, hh=hh, ks=ks, qs=qs: e.matmul(PC[hh][:], lhsT=KM[hh][:, ks], rhs=QT[:, qs], start=True, stop=False), R=[KM[hh], QT], W=[PC[hh]])
                                S.op('pe', lambda e, hh=hh, SP_=SP_: e.matmul(PC[hh][:], lhsT=ntri[:], rhs=SP_[hh][:], start=False, stop=True), R=[ntri, SP_[hh]], W=[PC[hh]])
                            for hh in HH:
                                S.op('dve', lambda e, hh=hh, T_=T_: e.tensor_tensor(out=T_[hh][:], in0=PC[hh][:], in1=racc[hh][:], op=ALU.add), R=[PC[hh], racc[hh]], W=[T_[hh]])
                            for hh in HH:
                                S.op('act', lambda e, hh=hh, T_=T_, A_=A_: e.activation(out=A_[hh][:], in_=T_[hh][:], func=AF.Exp), R=[T_[hh]], W=[A_[hh]])
                            if r >= 0:
                                for hh in HH:
                                    S.op('pool', lambda e, hh=hh, A_=A_, r=r: e.tensor_tensor(out=A_[hh][:], in0=A_[hh][:], in1=sbmask[:, r * 512:(r + 1) * 512], op=ALU.mult), R=[A_[hh], sbmask], W=[A_[hh]])
                            if kb > 0:
                                for hh in HH:
                                    S.op('pe', lambda e, hh=hh, SP_=SP_: e.matmul(PZ[hh][:], lhsT=nones[:], rhs=SP_[hh][:], start=True, stop=True), R=[nones, SP_[hh]], W=[PZ[hh]])
                                for hh in HH:
                                    S.op('dve', lambda e, hh=hh: e.tensor_tensor(out=racc[hh][:], in0=PZ[hh][:], in1=racc[hh][:], op=ALU.add), R=[PZ[hh], racc[hh]], W=[racc[hh]])
                            for hh in HH:
                                for qb in range(4):
                                    if r >= 0 and qb < r:
                                        continue
                                    S.op('pe', lambda e, qb=qb, hh=hh, A_=A_, kb=kb: e.matmul(PO[hh][:, qb * 64:(qb + 1) * 64], lhsT=A_[hh][:, qb * 128:(qb + 1) * 128], rhs=V[:, kb, hh * 64:(hh + 1) * 64],
                                                                                         start=False, stop=(kb == 0 and qb == 3)), R=[A_[hh], V], W=[PO[hh]])
                        for hh in range(2):
                            hcol = (hp * 2 + hh) * 64
                            po = self.PF[4 + hh]
                            S.op('act', lambda e, g=g, hcol=hcol, po=po: e.activation(out=o_all[:, 4 * g:4 * g + 4, hcol:hcol + 64], in_=po[:, 0:256].rearrange("p (q d) -> p q d", d=64), func=AF.Copy),
                                 R=[po], W=[o_all])
                    S.barrier(); S.emit()
            self.rms_finalize(st, o_all, onb, 6, "sbf")
            S.barrier(); S.emit()


    def _chk(self, n):
        if self.flags.get('rw_stop') == n:
            raise _Stop()

    def rwkv_phase(self, l):
        try:
            self.rwkv_phase_(l)
        except _Stop:
            pass

    def rwkv_phase_(self, l):
        S = self.S
        V3 = lambda ap, d=64: ap.rearrange("p (h d) -> p h d", d=d)
        with ExitStack() as st:
          try:
              wm, wo = self.load_w(st, "wm_rw", l, ['rw_rkv', 'rw_lora'])
              wmu = self.sb(st, "rw_wmu", [128, 8, 896], BF16)
              mub = self.bcast_row(st, "rw_mub", self.rwkv_mu[l, :], 896)
              S.op('dve', lambda e: e.tensor_tensor(out=wmu[:], in0=wm[:], in1=mub[:].unsqueeze(1).broadcast_to([128, 8, 896]), op=ALU.mult), R=[wm, mub], W=[wmu])
              S.op('pool', lambda e: e.tensor_tensor(out=wm[:], in0=wm[:], in1=wmu[:], op=ALU.subtract), R=[wm, wmu], W=[wm])
              lwbd = self.sb(st, "rw_lwbd", [128, 768], BF16)
              S.op('pool', lambda e: e.memset(lwbd[:], 0.0), W=[lwbd])
              for (r0, r1, c0) in ((0, 32, 0), (32, 64, 256), (64, 128, 512)):
                  self.dma('pool', lwbd[r0:r1, c0:c0 + 256], self.rwkv_lw[l, r0:r1, :], R=[lwbd], W=[lwbd])
              w0b = self.bcast_row(st, "rw_w0b", self.rwkv_w0[l, :], 256); a0b = self.bcast_row(st, "rw_a0b", self.rwkv_a0[l, :], 256)
              kkb = self.bcast_row(st, "rw_kkb", self.rwkv_kk[l, :], 256); kab = self.bcast_row(st, "rw_kab", self.rwkv_ka[l, :], 256)
              rkb = self.bcast_row(st, "rw_rkb", self.rwkv_rk[l, :], 256); lnb = self.bcast_row(st, "rw_lnb", self.rwkv_ln[l, :], 256)
              tri_le = self.load_const(st, 'tri_le', F32); lastc = self.load_const(st, 'last', F32)
              m_lt = self.load_const(st, 'tri_lt', F32); m_le = self.load_const(st, 'tri_le', F32); m_gt = self.load_const(st, 'tri_gt', F32)
              LT = self.sb(st, "rw_LT", [128, SEQ], BF16)
              for tg in range(8):
                  pf = self.PF[tg % 2]
                  self.proj_T(wm, wo['rw_lora'], pf, tg, shift=0, start=True, stop=False)
                  self.proj_T(wmu, wo['rw_lora'], pf, tg, shift=1, start=False, stop=True)
                  sl = slice(tg * 512, (tg + 1) * 512)
                  S.op('act', lambda e, pf=pf, sl=sl: e.activation(out=LT[0:32, sl], in_=pf[0:32, :], func=AF.Tanh), R=[pf], W=[LT])
                  S.op('act', lambda e, pf=pf, sl=sl: e.activation(out=LT[32:64, sl], in_=pf[32:64, :], func=AF.Copy), R=[pf], W=[LT])
                  S.op('act', lambda e, pf=pf, sl=sl: e.activation(out=LT[64:128, sl], in_=pf[64:128, :], func=AF.Sigmoid), R=[pf], W=[LT])
              self._chk(1)
              f32t = lambda n: self.sb(st, "rw_" + n, [128, 256], F32)
              bft = lambda n: self.sb(st, "rw_" + n, [128, 256], BF16)
              r_sb, k_sb, v_sb, a_sb, kk_sb, k2_sb, lw_sb, cum_sb, t1, t2, t3 = [f32t(n) for n in ('r', 'k', 'v', 'a', 'kk', 'k2', 'lw', 'cum', 't1', 't2', 't3')]
              gate_sb = f32t('gate')
              rt_b, kt_b, bt_b, at_b, v_bf, G_bf, U_bf = [bft(n) for n in ('rt', 'kt', 'bt', 'at', 'vbf', 'G', 'U')]
              st4 = self.sb(st, "rw_st4", [128, 4], F32)
              fm = self.sb(st, "rw_fm", [128, 8, 128], BF16)
              artbd = [self.sb(st, f"rw_artbd{p}", [128, 2, 256], BF16) for p in range(2)]
              btbd = [self.sb(st, f"rw_btbd{p}", [128, 2, 128], BF16) for p in range(2)]
              import os
              SKIP = os.environ.get('RW_SKIP', '').split(',')
              for p in range(2):
                  if 'b' in SKIP: break
                  S.op('pool', lambda e, p=p: e.memset(artbd[p][:], 0.0), W=[artbd[p]])
                  S.op('pool', lambda e, p=p: e.memset(btbd[p][:], 0.0), W=[btbd[p]])
              NU = [self.sb(st, f"rw_NU{i}", [128, 4, 128], F32) for i in range(2)]
              LL = [self.sb(st, f"rw_LL{i}", [128, 4, 128], F32) for i in range(2)]
              XX = [self.sb(st, f"rw_X{i}", [128, 4, 128], F32) for i in range(2)]
              G_f = self.sb(st, "rw_Gf", [128, 256], F32)
              RBm = self.sb(st, "rw_RB", [128, 4, 128], BF16); RKm = self.sb(st, "rw_RK", [128, 4, 128], BF16); MKm = self.sb(st, "rw_MK", [128, 4, 128], BF16)
              ST = [self.sb(st, f"rw_ST{p}", [128, 128], F32) for p in range(2)]
              STb = [self.sb(st, f"rw_STb{p}", [128, 128], BF16) for p in range(2)]
              ecl = self.sb(st, "rw_ecl", [128, 2], F32)
              for p in range(2):
                  if 'c' in SKIP: break
                  S.op('dve', lambda e, p=p: e.memset(ST[p][:], 0.0), W=[ST[p]])
                  S.op('dve', lambda e, p=p: e.memset(STb[p][:], 0.0), W=[STb[p]])
              o_sb = f32t('o'); cen = f32t('cen'); sq = f32t('sq')
              ystage = self.sb(st, "rw_ystage", [128, 2, 512], BF16)
              ytok = [self.sb(st, f"rw_ytok{i}", [128, 256], BF16) for i in range(2)]
              PF = self.PF
              for c in range(NT):
                  tok = slice(1 + c * 128, 1 + (c + 1) * 128); tokp = slice(c * 128, (c + 1) * 128)
                  for (pf, c0, n) in ((PF[0], 0, 512), (PF[1], 512, 256)):
                      if 'd' in SKIP: break
                      for k in range(8):
                          S.op('pe', lambda e, k=k, pf=pf, c0=c0, n=n, tok=tok: e.matmul(pf[:, 0:n], lhsT=self.hT[:, k, tok], rhs=wm[:, k, c0:c0 + n], start=(k == 0), stop=False), R=[wm, self.hT], W=[pf])
                      for k in range(8):
                          S.op('pe', lambda e, k=k, pf=pf, c0=c0, n=n, tokp=tokp: e.matmul(pf[:, 0:n], lhsT=self.hT[:, k, tokp], rhs=wmu[:, k, c0:c0 + n], start=False, stop=(k == 7)), R=[wmu, self.hT], W=[pf])
                  if 'e' in SKIP: self._chk(2)
                  S.op('act', lambda e: e.activation(out=r_sb[:], in_=PF[0][:, 0:256], func=AF.Copy), R=[PF[0]], W=[r_sb])
                  S.op('dve', lambda e: e.tensor_copy(out=k_sb[:], in_=PF[0][:, 256:512]), R=[PF[0]], W=[k_sb])
                  S.op('act', lambda e: e.activation(out=v_sb[:], in_=PF[1][:, 0:256], func=AF.Copy), R=[PF[1]], W=[v_sb])
                  if 'f' not in SKIP:
                      S.op('pool', lambda e: e.tensor_copy(out=v_bf[:], in_=v_sb[:]), R=[v_sb], W=[v_bf])
                  else:
                      S.op('dve', lambda e: e.tensor_copy(out=v_bf[:], in_=v_sb[:]), R=[v_sb], W=[v_bf])
                  self._chk(2)
                  S.op('pe', lambda e, c=c: e.matmul(PF[2][:, 0:512], lhsT=LT[:, c * 128:(c + 1) * 128], rhs=lwbd[:, 0:512], start=True, stop=True), R=[LT, lwbd], W=[PF[2]])
                  S.op('pe', lambda e, c=c: e.matmul(PF[3][:, 0:256], lhsT=LT[:, c * 128:(c + 1) * 128], rhs=lwbd[:, 512:768], start=True, stop=True), R=[LT, lwbd], W=[PF[3]])
                  S.op('act', lambda e: e.activation(out=gate_sb[:], in_=PF[3][:, 0:256], func=AF.Copy), R=[PF[3]], W=[gate_sb])
                  self._chk(3)
                  S.op('dve', lambda e: e.tensor_tensor(out=t1[:], in0=PF[2][:, 0:256], in1=w0b[:], op=ALU.add), R=[PF[2], w0b], W=[t1])
                  S.op('act', lambda e: e.activation(out=t1[:], in_=t1[:], func=AF.Sigmoid), R=[t1], W=[t1])
                  S.op('dve', lambda e: e.tensor_scalar(out=lw_sb[:], in0=t1[:], scalar1=-0.6065306597126334, scalar2=None, op0=ALU.mult), R=[t1], W=[lw_sb])
                  S.op('dve', lambda e: e.tensor_tensor(out=t2[:], in0=PF[2][:, 256:512], in1=a0b[:], op=ALU.add), R=[PF[2], a0b], W=[t2])
                  S.op('act', lambda e: e.activation(out=a_sb[:], in_=t2[:], func=AF.Sigmoid), R=[t2], W=[a_sb])
                  S.op('dve', lambda e: e.tensor_tensor(out=kk_sb[:], in0=k_sb[:], in1=kkb[:], op=ALU.mult), R=[k_sb, kkb], W=[kk_sb])
                  S.op('pool', lambda e: e.tensor_tensor(out=t3[:], in0=kk_sb[:], in1=kk_sb[:], op=ALU.mult), R=[kk_sb], W=[t3])
                  S.op('dve', lambda e: e.tensor_reduce(out=st4[:], in_=V3(t3[:]), axis=AX.X, op=ALU.add), R=[t3], W=[st4])
                  S.op('act', lambda e: e.activation(out=st4[:], in_=st4[:], func=AF.Sqrt), R=[st4], W=[st4])
                  S.op('dve', lambda e: e.tensor_scalar(out=st4[:], in0=st4[:], scalar1=1e-12, scalar2=None, op0=ALU.max), R=[st4], W=[st4])
                  S.op('dve', lambda e: e.reciprocal(out=st4[:], in_=st4[:]), R=[st4], W=[st4])
                  S.op('dve', lambda e: e.tensor_tensor(out=V3(kk_sb[:]), in0=V3(kk_sb[:]), in1=st4[:].unsqueeze(2).broadcast_to([128, 4, 64]), op=ALU.mult), R=[kk_sb, st4], W=[kk_sb])
                  S.op('dve', lambda e: e.scalar_tensor_tensor(out=t2[:], in0=a_sb[:], scalar=-1.0, in1=kab[:], op0=ALU.add, op1=ALU.mult), R=[a_sb, kab], W=[t2])
                  S.op('dve', lambda e: e.scalar_tensor_tensor(out=k2_sb[:], in0=t2[:], scalar=1.0, in1=k_sb[:], op0=ALU.add, op1=ALU.mult), R=[t2, k_sb], W=[k2_sb])
                  self._chk(4)
                  S.op('pe', lambda e: e.matmul(PF[4][:, 0:256], lhsT=tri_le[:], rhs=lw_sb[:], start=True, stop=True), R=[tri_le, lw_sb], W=[PF[4]])
                  S.op('act', lambda e: e.activation(out=cum_sb[:], in_=PF[4][:, 0:256], func=AF.Copy), R=[PF[4]], W=[cum_sb])
                  S.op('act', lambda e: e.activation(out=t1[:], in_=PF[4][:, 0:256], func=AF.Exp), R=[PF[4]], W=[t1])
                  S.op('act', lambda e: e.activation(out=t2[:], in_=PF[4][:, 0:256], func=AF.Exp, scale=-1.0), R=[PF[4]], W=[t2])
                  S.op('dve', lambda e: e.tensor_tensor(out=t3[:], in0=cum_sb[:], in1=lw_sb[:], op=ALU.subtract), R=[cum_sb, lw_sb], W=[t3])
                  S.op('act', lambda e: e.activation(out=t3[:], in_=t3[:], func=AF.Exp), R=[t3], W=[t3])
                  S.op('dve', lambda e: e.tensor_tensor(out=rt_b[:], in0=r_sb[:], in1=t1[:], op=ALU.mult), R=[r_sb, t1], W=[rt_b])
                  S.op('pool', lambda e: e.tensor_tensor(out=kt_b[:], in0=k2_sb[:], in1=t2[:], op=ALU.mult), R=[k2_sb, t2], W=[kt_b])
                  S.op('dve', lambda e: e.scalar_tensor_tensor(out=at_b[:], in0=kk_sb[:], scalar=-1.0, in1=t3[:], op0=ALU.mult, op1=ALU.mult), R=[kk_sb, t3], W=[at_b])
                  S.op('dve', lambda e: e.tensor_tensor(out=t3[:], in0=kk_sb[:], in1=a_sb[:], op=ALU.mult), R=[kk_sb, a_sb], W=[t3])
                  S.op('dve', lambda e: e.tensor_tensor(out=bt_b[:], in0=t3[:], in1=t2[:], op=ALU.mult), R=[t3, t2], W=[bt_b])
                  self._chk(5)
                  for p in range(2):
                      S.op('pe', lambda e, p=p: e.matmul(PF[5][:, p:p + 1], lhsT=cum_sb[:, p * 128:(p + 1) * 128], rhs=lastc[:, 0:1], start=True, stop=True), R=[cum_sb, lastc], W=[PF[5]])
                  S.op('act', lambda e: e.activation(out=ecl[:], in_=PF[5][:, 0:2], func=AF.Exp), R=[PF[5]], W=[ecl])
                  self._chk(6)
                  pb = self.PB[0]
                  for xi, src_ in enumerate((at_b, rt_b, bt_b, kt_b)):
                      for p in range(2):
                          j = xi * 2 + p
                          S.op('pe', lambda e, j=j, src_=src_, p=p: e.transpose(out=pb[:, j * 128:(j + 1) * 128], in_=src_[:, p * 128:(p + 1) * 128], identity=self.ident[:]), R=[src_, self.ident], W=[pb])
                  S.op('act', lambda e: e.activation(out=fm[:].rearrange("p j t -> p (j t)"), in_=pb[:], func=AF.Copy), R=[pb], W=[fm])
                  for p in range(2):
                      for hh in range(2):
                          pr = slice(hh * 64, (hh + 1) * 64)
                          S.op('act', lambda e, p=p, hh=hh, pr=pr: e.activation(out=artbd[p][pr, hh, 0:128], in_=fm[pr, 0 + p, :], func=AF.Copy), R=[fm, artbd[p]], W=[artbd[p]])
                          S.op('dve', lambda e, p=p, hh=hh, pr=pr: e.tensor_copy(out=artbd[p][pr, hh, 128:256], in_=fm[pr, 2 + p, :]), R=[fm, artbd[p]], W=[artbd[p]])
                          S.op('act', lambda e, p=p, hh=hh, pr=pr: e.activation(out=btbd[p][pr, hh, :], in_=fm[pr, 4 + p, :], func=AF.Copy), R=[fm, btbd[p]], W=[btbd[p]])
                  self._chk(7)
                  for p in range(2):
                      P1, P2, P3 = PF[0], PF[1], PF[2]
                      S.op('pe', lambda e, p=p: e.matmul(P1[:, 0:512], lhsT=fm[:, 4 + p, :], rhs=artbd[p][:].rearrange("p h c -> p (h c)"), start=True, stop=True), R=[fm, artbd[p]], W=[P1])
                      S.op('pe', lambda e, p=p: e.matmul(P2[:, 0:512], lhsT=fm[:, 6 + p, :], rhs=artbd[p][:].rearrange("p h c -> p (h c)"), start=True, stop=True), R=[fm, artbd[p]], W=[P2])
                      S.op('pe', lambda e, p=p: e.matmul(P3[:, 0:256], lhsT=fm[:, 0 + p, :], rhs=btbd[p][:].rearrange("p h c -> p (h c)"), start=True, stop=True), R=[fm, btbd[p]], W=[P3])
                      hs = slice(2 * p, 2 * p + 2)
                      v4 = lambda pf_: pf_[:, 0:512].rearrange("p (h w t) -> p h w t", h=2, w=2)
                      bc = lambda m: m[:].unsqueeze(1).broadcast_to([128, 2, 128])
                      S.op('dve', lambda e, hs=hs: e.tensor_tensor(out=NU[0][:, hs, :], in0=v4(P1)[:, :, 0, :], in1=bc(m_lt), op=ALU.mult), R=[P1, m_lt], W=[NU[0]])
                      S.op('dve', lambda e, hs=hs: e.tensor_tensor(out=RBm[:, hs, :], in0=v4(P1)[:, :, 1, :], in1=bc(m_le), op=ALU.mult), R=[P1, m_le], W=[RBm])
                      S.op('dve', lambda e, hs=hs: e.tensor_tensor(out=MKm[:, hs, :], in0=v4(P2)[:, :, 0, :], in1=bc(m_lt), op=ALU.mult), R=[P2, m_lt], W=[MKm])
                      S.op('dve', lambda e, hs=hs: e.tensor_tensor(out=RKm[:, hs, :], in0=v4(P2)[:, :, 1, :], in1=bc(m_le), op=ALU.mult), R=[P2, m_le], W=[RKm])
                      S.op('dve', lambda e, hs=hs: e.tensor_tensor(out=LL[0][:, hs, :], in0=P3[:, 0:256].rearrange("p (h t) -> p h t", h=2), in1=bc(m_gt), op=ALU.mult), R=[P3, m_gt], W=[LL[0]])
                  self._chk(8)
                  S.op('dve', lambda e: e.tensor_tensor(out=XX[0][:], in0=NU[0][:], in1=self.identf[:].unsqueeze(1).broadcast_to([128, 4, 128]), op=ALU.add), R=[NU[0], self.identf], W=[XX[0]])
                  cur = 0
                  for it in range(6):
                      nxt = 1 - cur
                      PL, PN, PX = PF[3], PF[4], PF[5]
                      for h in range(4):
                          S.op('pe', lambda e, h=h, cur=cur: e.matmul(PL[:, h * 128:(h + 1) * 128], lhsT=NU[cur][:, h, :], rhs=LL[cur][:, h, :], start=True, stop=True), R=[NU[cur], LL[cur]], W=[PL])
                      if it < 5:
                          for h in range(4):
                              S.op('pe', lambda e, h=h, cur=cur: e.matmul(PN[:, h * 128:(h + 1) * 128], lhsT=LL[cur][:, h, :], rhs=NU[cur][:, h, :], start=True, stop=True), R=[NU[cur], LL[cur]], W=[PN])
                      S.op('act', lambda e, nxt=nxt: e.activation(out=LL[nxt][:].rearrange("p h t -> p (h t)"), in_=PL[:], func=AF.Copy), R=[PL], W=[LL[nxt]])
                      if it < 5:
                          S.op('dve', lambda e, nxt=nxt: e.tensor_copy(out=NU[nxt][:].rearrange("p h t -> p (h t)"), in_=PN[:]), R=[PN], W=[NU[nxt]])
                      for h in range(4):
                          S.op('pe', lambda e, h=h, cur=cur, nxt=nxt: e.matmul(PX[:, h * 128:(h + 1) * 128], lhsT=LL[nxt][:, h, :], rhs=XX[cur][:, h, :], start=True, stop=True), R=[LL[nxt], XX[cur]], W=[PX])
                      S.op('dve', lambda e, cur=cur, nxt=nxt: e.tensor_tensor(out=XX[nxt][:].rearrange("p h t -> p (h t)"), in0=PX[:], in1=XX[cur][:].rearrange("p h t -> p (h t)"), op=ALU.add),
                           R=[PX, XX[cur]], W=[XX[nxt]])
                      cur = nxt
                  X = XX[cur]
                  self._chk(9)
                  PG, PU, PY, PS_ = PF[0], PF[1], PF[2], PF[3]
                  for p in range(2):
                      S.op('pe', lambda e, p=p: e.matmul(PG[:, p * 128:(p + 1) * 128], lhsT=fm[:, 0 + p, :], rhs=STb[p][:], start=True, stop=False), R=[fm, STb[p]], W=[PG])
                      for hh in range(2):
                          h = 2 * p + hh
                          S.op('pe', lambda e, h=h, hh=hh: e.matmul(PG[:, h * 64:(h + 1) * 64], lhsT=MKm[:, h, :], rhs=v_bf[:, h * 64:(h + 1) * 64], start=False, stop=(hh == 1)), R=[MKm, v_bf], W=[PG])
                  S.op('act', lambda e: e.activation(out=G_f[:], in_=PG[:, 0:256], func=AF.Copy), R=[PG], W=[G_f])
                  for h in range(4):
                      S.op('pe', lambda e, h=h, X=X: e.matmul(PU[:, h * 64:(h + 1) * 64], lhsT=X[:, h, :], rhs=G_f[:, h * 64:(h + 1) * 64], start=True, stop=True), R=[X, G_f], W=[PU])
                  S.op('dve', lambda e: e.tensor_copy(out=U_bf[:], in_=PU[:, 0:256]), R=[PU], W=[U_bf])
                  for p in range(2):
                      S.op('pe', lambda e, p=p: e.matmul(PY[:, p * 128:(p + 1) * 128], lhsT=fm[:, 2 + p, :], rhs=STb[p][:], start=True, stop=False), R=[fm, STb[p]], W=[PY])
                      for hh in range(2):
                          h = 2 * p + hh
                          S.op('pe', lambda e, h=h: e.matmul(PY[:, h * 64:(h + 1) * 64], lhsT=RBm[:, h, :], rhs=U_bf[:, h * 64:(h + 1) * 64], start=False, stop=False), R=[RBm, U_bf], W=[PY])
                          S.op('pe', lambda e, h=h, hh=hh: e.matmul(PY[:, h * 64:(h + 1) * 64], lhsT=RKm[:, h, :], rhs=v_bf[:, h * 64:(h + 1) * 64], start=False, stop=(hh == 1)), R=[RKm, v_bf], W=[PY])
                  S.op('act', lambda e: e.activation(out=o_sb[:], in_=PY[:, 0:256], func=AF.Copy), R=[PY], W=[o_sb])
                  for p in range(2):
                      cs_ = slice(p * 128, (p + 1) * 128)
                      S.op('pe', lambda e, cs_=cs_: e.matmul(PS_[:, cs_], lhsT=bt_b[:, cs_], rhs=U_bf[:, cs_], start=True, stop=False), R=[bt_b, U_bf], W=[PS_])
                      S.op('pe', lambda e, cs_=cs_: e.matmul(PS_[:, cs_], lhsT=kt_b[:, cs_], rhs=v_bf[:, cs_], start=False, stop=True), R=[kt_b, v_bf], W=[PS_])
                      for hh in range(2):
                          pr = slice(hh * 64, (hh + 1) * 64); cc = slice(hh * 64, (hh + 1) * 64); pc_ = slice(p * 128 + hh * 64, p * 128 + (hh + 1) * 64)
                          S.op('dve', lambda e, p=p, pr=pr, cc=cc, pc_=pc_: e.scalar_tensor_tensor(out=ST[p][pr, cc], in0=ST[p][pr, cc], scalar=1.0, in1=PS_[pr, pc_], op0=ALU.mult, op1=ALU.add),
                               R=[ST[p], PS_], W=[ST[p]])
                          S.op('dve', lambda e, p=p, pr=pr, cc=cc: e.tensor_scalar(out=ST[p][pr, cc], in0=ST[p][pr, cc], scalar1=ecl[pr, p:p + 1], scalar2=None, op0=ALU.mult), R=[ST[p], ecl], W=[ST[p]])
                      S.op('act', lambda e, p=p: e.activation(out=STb[p][:], in_=ST[p][:], func=AF.Copy), R=[ST[p]], W=[STb[p]])
                  self._chk(10)
                  self.head_norm(o_sb, cen, sq, st4, 4, 64e-5)
                  S.op('dve', lambda e: e.tensor_tensor(out=cen[:], in0=cen[:], in1=lnb[:], op=ALU.mult), R=[cen, lnb], W=[cen])
                  S.op('dve', lambda e: e.tensor_tensor(out=t1[:], in0=r_sb[:], in1=k2_sb[:], op=ALU.mult), R=[r_sb, k2_sb], W=[t1])
                  S.op('dve', lambda e: e.tensor_tensor(out=t1[:], in0=t1[:], in1=rkb[:], op=ALU.mult), R=[t1, rkb], W=[t1])
                  S.op('dve', lambda e: e.tensor_reduce(out=st4[:], in_=V3(t1[:]), axis=AX.X, op=ALU.add), R=[t1], W=[st4])
                  S.op('dve', lambda e: e.tensor_tensor(out=V3(t1[:]), in0=V3(v_sb[:]), in1=st4[:].unsqueeze(2).broadcast_to([128, 4, 64]), op=ALU.mult), R=[v_sb, st4], W=[t1])
                  S.op('dve', lambda e: e.tensor_tensor(out=cen[:], in0=cen[:], in1=t1[:], op=ALU.add), R=[cen, t1], W=[cen])
                  yt_ = ytok[c % 2]
                  S.op('dve', lambda e, yt_=yt_: e.tensor_tensor(out=yt_[:], in0=cen[:], in1=gate_sb[:], op=ALU.mult), R=[cen, gate_sb], W=[yt_])
                  self.y_store(2, c, yt_, ystage, [])
              S.barrier(); S.emit()
          except _Stop:
            S.barrier(); S.emit()


    def rope_apply(self, A, B_, C, Sg):
        S = self.S
        S.op('dve', lambda e: e.tensor_tensor(out=A[:], in0=A[:], in1=C[:], op=ALU.mult), R=[A, C], W=[A])
        S.op('pool', lambda e: e.tensor_tensor(out=B_[:], in0=B_[:], in1=Sg[:], op=ALU.mult), R=[B_, Sg], W=[B_])
        S.op('dve', lambda e: e.tensor_tensor(out=A[:], in0=A[:], in1=B_[:], op=ALU.add), R=[A, B_], W=[A])

    def dsa_phase(self, l):
        S = self.S
        PF = self.PF
        NBIS = 13
        with ExitStack() as st:
            QT = [self.sb(st, f"dsQT{i}", [128, SEQ], BF16) for i in range(2)]
            kT = self.sb(st, "dskT", [128, SEQ], BF16)
            hm2 = self.load_const(st, 'hm2', F32); hm4 = self.load_const(st, 'hm4', F32)
            vext = self.sb(st, "ds_vext", [128, NT, 65], BF16)
            wsc = self.sb(st, "ds_wsc", [128, NT, 8], F32)
            with ExitStack() as s1:
                qiT = [self.sb(s1, f"dsqiT{i}", [128, SEQ], BF16) for i in range(2)]
                kiT = self.sb(s1, "dskiT", [128, SEQ], BF16)
                with ExitStack() as s2:
                    wm, wo = self.load_w(s2, "wm_ds", l, ['ds_cq', 'ds_k', 'ds_ks', 'ds_ki', 'ds_kis', 'ds_vw'])
                    wup = self.sb(s2, "ds_wup", [128, 4, 256], BF16)
                    for j, src_ in enumerate((self.dsa_wq_up, self.dsa_wqs_up, self.dsa_wqi_up, self.dsa_wqis_up)):
                        self.dma('pool', wup[:, j, :], src_[l], W=[wup])
                    qn = self.sb(s2, "ds_qn", [128, 1], F32)
                    self.dma('sp', qn[:], self.dsa_qnorm[l, :].rearrange("(p o) -> p o", o=1), W=[qn], allow_slow_non_contiguous=True)
                    onesf = self.sb(s2, "ds_ones", [128, 128], F32)
                    S.op('dve', lambda e: e.memset(onesf[:], 1.0), W=[onesf])
                    cqn = self.sb(s2, "ds_cqn", [128, SEQ], BF16)
                    cqf = self.sb(s2, "ds_cqf", [128, 512], F32); cq2 = self.sb(s2, "ds_cq2", [128, 512], F32); rs = self.sb(s2, "ds_rs", [128, 512], F32)
                    for tg in range(8):
                        pf = PF[tg % 2]; pf2 = PF[2 + tg % 2]
                        self.proj_T(wm, wo['ds_cq'], pf, tg)
                        S.op('act', lambda e, pf=pf: e.activation(out=cqf[:], in_=pf[:], func=AF.Copy), R=[pf], W=[cqf])
                        S.op('dve', lambda e: e.tensor_tensor(out=cq2[:], in0=cqf[:], in1=cqf[:], op=ALU.mult), R=[cqf], W=[cq2])
                        S.op('pe', lambda e, pf2=pf2: e.matmul(pf2[:], lhsT=onesf[:], rhs=cq2[:], start=True, stop=True), R=[onesf, cq2], W=[pf2])
                        S.op('dve', lambda e, pf2=pf2: e.tensor_scalar(out=rs[:], in0=pf2[:], scalar1=1.0 / 128, scalar2=1e-5, op0=ALU.mult, op1=ALU.add), R=[pf2], W=[rs])
                        S.op('act', lambda e: e.activation(out=rs[:], in_=rs[:], func=AF.Sqrt), R=[rs], W=[rs])
                        S.op('dve', lambda e: e.reciprocal(out=rs[:], in_=rs[:]), R=[rs], W=[rs])
                        S.op('dve', lambda e, tg=tg: e.scalar_tensor_tensor(out=cqn[:, tg * 512:(tg + 1) * 512], in0=cqf[:], scalar=qn[:, 0:1], in1=rs[:], op0=ALU.mult, op1=ALU.mult),
                             R=[cqf, qn, rs], W=[cqn])
                    S.op('pool', lambda e: e.memset(vext[:, :, 64:65], 1.0), W=[vext])
                    for i in range(NT):
                        pf = PF[i % 2]
                        self.proj_tok(wm, wo['ds_vw'], 72, pf[:, 0:72], pf, i)
                        S.op('act', lambda e, pf=pf, i=i: e.activation(out=vext[:, i, 0:64], in_=pf[:, 0:64], func=AF.Copy), R=[pf], W=[vext])
                        S.op('dve', lambda e, pf=pf, i=i: e.tensor_scalar(out=wsc[:, i, :], in0=pf[:, 64:72], scalar1=1.0 / 16, scalar2=None, op0=ALU.mult), R=[pf], W=[wsc])
                    tmpA = self.sb(s2, "ds_tmpA", [128, SEQ], BF16)

                    def up_proj(j, cols, dst):
                        for tg in range(8):
                            pf = PF[tg % 2]
                            S.op('pe', lambda e, pf=pf, tg=tg: e.matmul(pf[:], lhsT=wup[:, j, cols], rhs=cqn[:, tg * 512:(tg + 1) * 512], start=True, stop=True), R=[wup, cqn], W=[pf])
                            S.op('act', lambda e, pf=pf, tg=tg: e.activation(out=dst[:, tg * 512:(tg + 1) * 512], in_=pf[:], func=AF.Copy), R=[pf], W=[dst])

                    def in_proj(name, dst):
                        for tg in range(8):
                            pf = PF[2 + tg % 2]
                            self.proj_T(wm, wo[name], pf, tg)
                            S.op('dve', lambda e, pf=pf, tg=tg: e.tensor_copy(out=dst[:, tg * 512:(tg + 1) * 512], in_=pf[:]), R=[pf], W=[dst])
                    with ExitStack() as s3:
                        C, Sg = self.rope_tables(s3, 'rope_dq', 'rdq')
                        for pr_ in range(2):
                            cols = slice(pr_ * 128, (pr_ + 1) * 128)
                            up_proj(0, cols, QT[pr_]); up_proj(1, cols, tmpA)
                            self.rope_apply(QT[pr_], tmpA, C, Sg)
                        in_proj('ds_k', kT); in_proj('ds_ks', tmpA)
                        self.rope_apply(kT, tmpA, C, Sg)
                        S.barrier(); S.emit()
                    with ExitStack() as s3:
                        C, Sg = self.rope_tables(s3, 'rope_di', 'rdi')
                        for t2 in range(2):
                            cols = slice(t2 * 128, (t2 + 1) * 128)
                            up_proj(2, cols, qiT[t2]); up_proj(3, cols, tmpA)
                            self.rope_apply(qiT[t2], tmpA, C, Sg)
                        in_proj('ds_ki', kiT); in_proj('ds_kis', tmpA)
                        self.rope_apply(kiT, tmpA, C, Sg)
                        S.barrier(); S.emit()
                with ExitStack() as s2:
                    score = self.sb(s2, "ds_score", [128, SEQ], F32)
                    mask = self.sb(s2, "ds_mask", [128, SEQ], BF16)
                    junk = self.sb(s2, "ds_junk", [128, SEQ], BF16)
                    relb = [self.sb(s2, f"ds_rel{i}", [128, 512], F32) for i in range(2)]
                    negm = self.load_const(s2, 'negmask', F32)
                    mT = [self.sb(s2, f"ds_mT{i}", [128, NT, 128], BF16) for i in range(2)]
                    sc = {n: self.sb(s2, "ds_" + n, [128, 1], F32) for n in ('lo', 'hi', 'mid', 'cnt', 'ge', 'd')}
                    cntr = 0
                    qm = [self.sb(s2, f"ds_qm{i}", [128, 8, 128], BF16) for i in range(2)]
                    for tb in range(NT):
                        Sc = (tb + 1) * 128
                        tsl = slice(tb * 128, (tb + 1) * 128)
                        qm_ = qm[tb % 2]
                        for ih in range(8):
                            S.op('pool', lambda e, ih=ih, qm_=qm_, tsl=tsl: e.tensor_scalar(out=qm_[:, ih, :], in0=qiT[ih // 4][:, tsl], scalar1=hm4[:, ih % 4:ih % 4 + 1], scalar2=None, op0=ALU.mult),
                                 R=[qiT[ih // 4], hm4], W=[qm_])
                        for sg in range((Sc + 511) // 512):
                            w = min(512, Sc - sg * 512)
                            ssl = slice(sg * 512, sg * 512 + w)
                            for ih in range(8):
                                t2, j = ih // 4, ih % 4
                                pf = PF[cntr % 4]; rl = relb[cntr % 2]; cntr += 1
                                S.op('pe', lambda e, pf=pf, ih=ih, qm_=qm_, ssl=ssl, w=w: e.matmul(pf[:, 0:w], lhsT=qm_[:, ih, :], rhs=kiT[:, ssl], start=True, stop=True),
                                     R=[qm_, kiT], W=[pf])
                                S.op('act', lambda e, pf=pf, rl=rl, w=w: e.activation(out=rl[:, 0:w], in_=pf[:, 0:w], func=AF.Relu), R=[pf], W=[rl])
                                if ih == 0:
                                    S.op('dve', lambda e, rl=rl, w=w, ssl=ssl, tb=tb, ih=ih: e.tensor_scalar(out=score[:, ssl], in0=rl[:, 0:w], scalar1=wsc[:, tb, ih:ih + 1], scalar2=None, op0=ALU.mult),
                                         R=[rl, wsc], W=[score])
                                else:
                                    S.op('dve', lambda e, rl=rl, w=w, ssl=ssl, tb=tb, ih=ih: e.scalar_tensor_tensor(out=score[:, ssl], in0=rl[:, 0:w], scalar=wsc[:, tb, ih:ih + 1], in1=score[:, ssl],
                                                                                                              op0=ALU.mult, op1=ALU.add), R=[rl, wsc, score], W=[score])
                        S.op('dve', lambda e, tsl=tsl: e.tensor_tensor(out=score[:, tsl], in0=score[:, tsl], in1=negm[:], op=ALU.add), R=[score, negm], W=[score])
                        if tb >= 2:
                            S.op('dve', lambda e, Sc=Sc: e.tensor_reduce(out=sc['hi'][:], in_=score[:, 0:Sc], axis=AX.X, op=ALU.max), R=[score], W=[sc['hi']])
                            S.op('dve', lambda e: e.tensor_reduce(out=sc['lo'][:], in_=score[:, 0:256], axis=AX.X, op=ALU.min), R=[score], W=[sc['lo']])
                            S.op('dve', lambda e: e.tensor_tensor(out=sc['mid'][:], in0=sc['lo'][:], in1=sc['hi'][:], op=ALU.add), R=[sc['lo'], sc['hi']], W=[sc['mid']])
                            S.op('dve', lambda e: e.tensor_scalar(out=sc['mid'][:], in0=sc['mid'][:], scalar1=0.5, scalar2=None, op0=ALU.mult), R=[sc['mid']], W=[sc['mid']])
                            S.op('dve', lambda e: e.tensor_tensor(out=sc['d'][:], in0=sc['hi'][:], in1=sc['lo'][:], op=ALU.subtract), R=[sc['lo'], sc['hi']], W=[sc['d']])
                            S.op('dve', lambda e: e.tensor_scalar(out=sc['d'][:], in0=sc['d'][:], scalar1=0.25, scalar2=None, op0=ALU.mult), R=[sc['d']], W=[sc['d']])
                            for it in range(NBIS):
                                S.op('dve', lambda e, Sc=Sc: e.tensor_scalar(out=junk[:, 0:Sc], in0=score[:, 0:Sc], scalar1=sc['mid'][:, 0:1], scalar2=None, op0=ALU.is_ge, op1=ALU.add,
                                                                            accum_out=sc['cnt'][:]), R=[score, sc['mid']], W=[junk, sc['cnt']])
                                S.op('dve', lambda e: e.tensor_scalar(out=sc['ge'][:], in0=sc['cnt'][:], scalar1=255.5, scalar2=2.0, op0=ALU.is_ge, op1=ALU.mult), R=[sc['cnt']], W=[sc['ge']])
                                S.op('dve', lambda e: e.scalar_tensor_tensor(out=sc['ge'][:], in0=sc['ge'][:], scalar=-1.0, in1=sc['d'][:], op0=ALU.add, op1=ALU.mult), R=[sc['ge'], sc['d']], W=[sc['ge']])
                                S.op('dve', lambda e: e.tensor_tensor(out=sc['mid'][:], in0=sc['mid'][:], in1=sc['ge'][:], op=ALU.add), R=[sc['mid'], sc['ge']], W=[sc['mid']])
                                S.op('dve', lambda e: e.tensor_scalar(out=sc['d'][:], in0=sc['d'][:], scalar1=0.5, scalar2=None, op0=ALU.mult), R=[sc['d']], W=[sc['d']])
                            S.op('dve', lambda e: e.scalar_tensor_tensor(out=sc['lo'][:], in0=sc['d'][:], scalar=-2.0, in1=sc['mid'][:], op0=ALU.mult, op1=ALU.add), R=[sc['d'], sc['mid']], W=[sc['lo']])
                        else:
                            S.op('dve', lambda e: e.memset(sc['lo'][:], -1e29), W=[sc['lo']])
                        S.op('dve', lambda e, Sc=Sc: e.tensor_scalar(out=mask[:, 0:Sc], in0=score[:, 0:Sc], scalar1=sc['lo'][:, 0:1], scalar2=None, op0=ALU.is_ge), R=[score, sc['lo']], W=[mask])
                        mt = mT[tb % 2]
                        for s0 in range(0, tb + 1, 8):
                            nb_ = min(8, tb + 1 - s0)
                            pb = self.PB[(s0 // 8) % 2]
                            for q in range(nb_):
                                S.op('pe', lambda e, pb=pb, q=q, s0=s0: e.transpose(out=pb[:, q * 128:(q + 1) * 128], in_=mask[:, (s0 + q) * 128:(s0 + q + 1) * 128], identity=self.ident[:]),
                                     R=[mask, self.ident], W=[pb])
                            S.op('act', lambda e, pb=pb, mt=mt, s0=s0, nb_=nb_: e.activation(out=mt[:, s0:s0 + nb_, :].rearrange("p b t -> p (b t)"), in_=pb[:, 0:nb_ * 128], func=AF.Copy), R=[pb], W=[mt])
                        self.dma('sp', self.maskT_d[tb, :, 0:tb + 1, :], mt[:, 0:tb + 1, :], R=[mt], W=[TB()])
                    S.barrier(); S.emit()
            with ExitStack() as s2:
                onb = self.bcast_row(s2, "ds_onb", self.dsa_onorm[l, :], 256)
                o_all = self.sb(s2, "ds_oall", [128, NT, 256], BF16)
                mT = [self.sb(s2, f"ds_mT2{i}", [128, NT, 128], BF16) for i in range(2)]
                ebuf = [self.sb(s2, f"ds_e{i}", [128, 4, 128], BF16) for i in range(2)]
                pbuf = [self.sb(s2, f"ds_p{i}", [128, 4, 128], BF16) for i in range(2)]
                zer = self.sb(s2, "ds_zero", [128, 260], BF16)
                S.op('pool', lambda e: e.memset(zer[:], 0.0), W=[zer])
                osb = self.sb(s2, "ds_osb", [128, 4, 65], F32); rden = self.sb(s2, "ds_rden", [128, 4], F32)
                cnt = 0
                QM = [self.sb(s2, f"ds_QM{i}", [128, 4, 128], BF16) for i in range(2)]
                for tb in range(NT):
                    tsl = slice(tb * 128, (tb + 1) * 128)
                    mt = mT[tb % 2]
                    QM_ = QM[tb % 2]
                    for h in range(4):
                        S.op('pool', lambda e, h=h, QM_=QM_, tsl=tsl: e.tensor_scalar(out=QM_[:, h, :], in0=QT[h // 2][:, tsl], scalar1=hm2[:, h % 2:h % 2 + 1], scalar2=None, op0=ALU.mult),
                             R=[QT[h // 2], hm2], W=[QM_])
                    self.dma('act', mt[:, 0:tb + 1, :], self.maskT_d[tb, :, 0:tb + 1, :], W=[mt])
                    po = PF[4 + tb % 2]
                    S.op('pe', lambda e, po=po: e.matmul(po[:, 0:260], lhsT=zer[:, 0:128], rhs=zer[:, 0:260], start=True, stop=False), R=[zer], W=[po])
                    for sb_ in range(tb + 1):
                        b2 = cnt % 2; cnt += 1
                        ssl = slice(sb_ * 128, (sb_ + 1) * 128)
                        pl = PF[b2 * 2]
                        S.op('pe', lambda e, pl=pl, ssl=ssl, QM_=QM_: e.matmul(pl[:], lhsT=kT[:, ssl], rhs=QM_[:].rearrange("p h t -> p (h t)"), start=True, stop=True),
                             R=[kT, QM_], W=[pl])
                        S.op('act', lambda e, pl=pl, b2=b2: e.activation(out=ebuf[b2][:].rearrange("p h t -> p (h t)"), in_=pl[:], func=AF.Exp, scale=0.125), R=[pl], W=[ebuf[b2]])
                        S.op('dve', lambda e, b2=b2, mt=mt, sb_=sb_: e.tensor_tensor(out=pbuf[b2][:], in0=ebuf[b2][:], in1=mt[:, sb_, :].unsqueeze(1).broadcast_to([128, 4, 128]), op=ALU.mult),
                             R=[ebuf[b2], mt], W=[pbuf[b2]])
                        for h in range(4):
                            S.op('pe', lambda e, h=h, po=po, b2=b2, sb_=sb_, tb=tb: e.matmul(po[:, h * 65:(h + 1) * 65], lhsT=pbuf[b2][:, h, :], rhs=vext[:, sb_, :], start=False, stop=(sb_ == tb and h == 3)),
                                 R=[pbuf[b2], vext], W=[po])
                    S.op('act', lambda e, po=po: e.activation(out=osb[:].rearrange("p h d -> p (h d)"), in_=po[:, 0:260], func=AF.Copy), R=[po], W=[osb])
                    S.op('dve', lambda e: e.reciprocal(out=rden[:], in_=osb[:, :, 64]), R=[osb], W=[rden])
                    S.op('dve', lambda e, tb=tb: e.tensor_tensor(out=o_all[:, tb, :].rearrange("p (h d) -> p h d", d=64), in0=osb[:, :, 0:64], in1=rden[:].unsqueeze(2).broadcast_to([128, 4, 64]), op=ALU.mult),
                         R=[osb, rden], W=[o_all])
                S.barrier(); S.emit()
                self.rms_finalize(s2, o_all, onb, 4, "dsf")
                S.barrier(); S.emit()

    def head_norm(self, o_sb, cen, sq, st4, nh, eps):
        S = self.S
        v3 = lambda b: b[:, 0:nh * 64].rearrange("p (h d) -> p h d", d=64)
        S.op('dve', lambda e: e.tensor_reduce(out=st4[:, 0:nh], in_=v3(o_sb), axis=AX.X, op=ALU.add), R=[o_sb], W=[st4])
        S.op('dve', lambda e: e.tensor_scalar(out=st4[:, 0:nh], in0=st4[:, 0:nh], scalar1=1.0 / 64, scalar2=None, op0=ALU.mult), R=[st4], W=[st4])
        S.op('dve', lambda e: e.tensor_tensor(out=v3(cen), in0=v3(o_sb), in1=st4[:, 0:nh].unsqueeze(2).broadcast_to([128, nh, 64]), op=ALU.subtract), R=[o_sb, st4], W=[cen])
        S.op('dve', lambda e: e.tensor_tensor(out=v3(sq), in0=v3(cen), in1=v3(cen), op=ALU.mult), R=[cen], W=[sq])
        S.op('dve', lambda e: e.tensor_reduce(out=st4[:, 0:nh], in_=v3(sq), axis=AX.X, op=ALU.add), R=[sq], W=[st4])
        S.op('dve', lambda e: e.tensor_scalar(out=st4[:, 0:nh], in0=st4[:, 0:nh], scalar1=1.0 / 64, scalar2=eps, op0=ALU.mult, op1=ALU.add), R=[st4], W=[st4])
        S.op('act', lambda e: e.activation(out=st4[:, 0:nh], in_=st4[:, 0:nh], func=AF.Sqrt), R=[st4], W=[st4])
        S.op('dve', lambda e: e.reciprocal(out=st4[:, 0:nh], in_=st4[:, 0:nh]), R=[st4], W=[st4])
        S.op('dve', lambda e: e.tensor_tensor(out=v3(cen), in0=v3(cen), in1=st4[:, 0:nh].unsqueeze(2).broadcast_to([128, nh, 64]), op=ALU.mult), R=[cen, st4], W=[cen])

    def wout_phase(self, l):
        S = self.S
        src = self.x if l == 0 else self.xres
        with ExitStack() as st:
            YT = self.sb(st, "YT", [128, 8, SEQ], BF16)
            for j in range(8):
                self.dma('sp' if j % 2 == 0 else 'act', YT[:, j, :], self.yT_d[j], W=[YT])
            wo_sb = self.sb(st, "wo_sb", [128, 8, D], BF16)
            for f in range(8):
                self.dma('pool', wo_sb[:, f, :], self.w_out[l, f * 128:(f + 1) * 128, :], W=[wo_sb])
            xt = [self.sb(st, f"wx{i}", [128, D], F32) for i in range(2)]
            tmp = self.sb(st, "wtmp", [128, D], F32)
            for i in range(NT):
                x_ = xt[i % 2]
                self.dma('sp' if i % 2 == 0 else 'act', x_[:], src[i * 128:(i + 1) * 128, :], W=[x_])
                for hf in range(2):
                    pf = self.PF[(i % 2) * 2 + hf]
                    for f in range(8):
                        S.op('pe', lambda e, f=f, pf=pf, i=i, hf=hf: e.matmul(pf[:], lhsT=YT[:, f, i * 128:(i + 1) * 128], rhs=wo_sb[:, f, hf * 512:(hf + 1) * 512], start=(f == 0), stop=(f == 7)),
                             R=[YT, wo_sb], W=[pf])
                    S.op('dve', lambda e, pf=pf, hf=hf: e.tensor_tensor(out=tmp[:, hf * 512:(hf + 1) * 512], in0=pf[:], in1=self.gb[0][:, hf * 512:(hf + 1) * 512], op=ALU.mult),
                         R=[pf, self.gb[0]], W=[tmp])
                S.op('pool', lambda e, x_=x_: e.tensor_tensor(out=x_[:], in0=x_[:], in1=tmp[:], op=ALU.add), R=[x_, tmp], W=[x_])
                self.dma('sp', self.xres[i * 128:(i + 1) * 128, :], x_[:], R=[x_], W=[TB()])
            S.barrier(); S.emit()

    def moe_phase(self, l):
        S = self.S; PF = self.PF; nc = self.nc
        with ExitStack() as st:
            off_all = self.sb(st, "mo_off", [128, NT, 4], I32)
            gsel_all = self.sb(st, "mo_gsel", [128, NT, 4], F32)
            widx = self.sb(st, "mo_widx", [128, NBLK, 8], I32)
            OH = self.sb(st, "mo_OH", [32, NBLK], F32)
            ones_bf = self.sb(st, "mo_ones", [128, 512], BF16)
            S.op('dve', lambda e: e.memset(ones_bf[:], 1.0), W=[ones_bf])
            with ExitStack() as s1:
                self.hT = self.sb(s1, "hT2", [128, 8, SEQ + 1], BF16)
                self.norm_phase(l, 1)
                rw = self.sb(s1, "mo_rw", [128, 8, 32], BF16)
                self.dma('pool', rw[:], self.router_w[l].rearrange("(k p) e -> p k e", p=128), W=[rw])
                rbb = self.bcast_row(s1, "mo_rbb", self.router_b[l, :], 32)
                M_bf = self.sb(s1, "mo_Mbf", [128, NT, 32], BF16); M32 = self.sb(s1, "mo_M32", [128, NT, 32], F32)
                G_all = self.sb(s1, "mo_G", [128, NT, 32], F32)
                lg = self.sb(s1, "mo_lg", [128, 32], F32); ex = self.sb(s1, "mo_ex", [128, 32], F32); junk32 = self.sb(s1, "mo_junk", [128, 32], F32)
                top8 = self.sb(s1, "mo_top8", [128, 8], F32); sc1 = self.sb(s1, "mo_sc1", [128, 2], F32)
                for i in range(NT):
                    pf = PF[i % 2]
                    for k in range(8):
                        S.op('pe', lambda e, k=k, pf=pf, i=i: e.matmul(pf[:, 0:32], lhsT=self.hT[:, k, 1 + i * 128: 1 + (i + 1) * 128], rhs=rw[:, k, :], start=(k == 0), stop=(k == 7)),
                             R=[self.hT, rw], W=[pf])
                    S.op('dve', lambda e, pf=pf: e.tensor_tensor(out=lg[:], in0=pf[:, 0:32], in1=rbb[:], op=ALU.add), R=[pf, rbb], W=[lg])
                    S.op('dve', lambda e: e.max(out=top8[:], in_=lg[:]), R=[lg], W=[top8])
                    S.op('dve', lambda e, i=i: e.tensor_scalar(out=M32[:, i, :], in0=lg[:], scalar1=top8[:, 3:4], scalar2=None, op0=ALU.is_ge), R=[lg, top8], W=[M32])
                    S.op('pool', lambda e, i=i: e.tensor_copy(out=M_bf[:, i, :], in_=M32[:, i, :]), R=[M32], W=[M_bf])
                    S.op('dve', lambda e: e.tensor_scalar(out=sc1[:, 0:1], in0=top8[:, 0:1], scalar1=-1.0, scalar2=None, op0=ALU.mult), R=[top8], W=[sc1])
                    S.op('act', lambda e: e.activation(out=ex[:], in_=lg[:], func=AF.Exp, bias=sc1[:, 0:1]), R=[lg, sc1], W=[ex])
                    S.op('dve', lambda e, i=i: e.scalar_tensor_tensor(out=ex[:], in0=ex[:], scalar=1.0, in1=M32[:, i, :], op0=ALU.mult, op1=ALU.mult, accum_out=sc1[:, 1:2]),
                         R=[ex, M32], W=[ex, sc1])
                    S.op('dve', lambda e: e.reciprocal(out=sc1[:, 1:2], in_=sc1[:, 1:2]), R=[sc1], W=[sc1])
                    S.op('dve', lambda e, i=i: e.tensor_scalar(out=G_all[:, i, :], in0=ex[:], scalar1=sc1[:, 1:2], scalar2=None, op0=ALU.mult), R=[ex, sc1], W=[G_all])
                pc = PF[2]
                for i in range(NT):
                    S.op('pe', lambda e, i=i: e.matmul(pc[0:1, 0:32], lhsT=ones_bf[:, 0:1], rhs=M_bf[:, i, :], start=(i == 0), stop=(i == NT - 1)), R=[ones_bf, M_bf], W=[pc])
                row = lambda n, w=32, d=F32: self.sb(s1, "mo_" + n, [1, w], d)
                cnt = row('cnt'); nbf = row('nbf'); nbi = row('nbi', 32, I32); endr = row('end'); baser = row('base'); onesr = row('onesr')
                iota = self.load_const(s1, 'iota_blk', F32); kp = self.load_const(s1, 'kp', F32)
                cmp3 = self.sb(s1, "mo_cmp3", [1, NBLK, 32], F32); ebf = row('ebf', NBLK); chgf = row('chgf', NBLK)
                S.op('dve', lambda e: e.memset(onesr[:], 1.0), W=[onesr])
                S.op('dve', lambda e: e.tensor_scalar(out=cnt[:], in0=pc[0:1, 0:32], scalar1=float(MB - 1), scalar2=1.0 / MB, op0=ALU.add, op1=ALU.mult), R=[pc], W=[cnt])
                S.op('dve', lambda e: e.tensor_scalar(out=cnt[:], in0=cnt[:], scalar1=-0.5 + 0.5 / MB, scalar2=None, op0=ALU.add), R=[cnt], W=[cnt])
                S.op('dve', lambda e: e.tensor_copy(out=nbi[:], in_=cnt[:]), R=[cnt], W=[nbi])
                S.op('dve', lambda e: e.tensor_copy(out=nbf[:], in_=nbi[:]), R=[nbi], W=[nbf])
                S.op('dve', lambda e: e.tensor_tensor_scan(out=endr[:], data0=onesr[:], data1=nbf[:], initial=0.0, op0=ALU.mult, op1=ALU.add), R=[onesr, nbf], W=[endr])
                S.op('dve', lambda e: e.tensor_tensor(out=baser[:], in0=endr[:], in1=nbf[:], op=ALU.subtract), R=[endr, nbf], W=[baser])
                S.op('dve', lambda e: e.tensor_scalar(out=baser[:], in0=baser[:], scalar1=float(MB), scalar2=None, op0=ALU.mult), R=[baser], W=[baser])
                basebc = self.sb(s1, "mo_basebc", [128, 32], F32)
                onescol = self.sb(s1, "mo_ones1", [1, 128], F32)
                S.op('dve', lambda e: e.memset(onescol[:], 1.0), W=[onescol])
                S.op('pe', lambda e: e.matmul(PF[3][:, 0:32], lhsT=onescol[0:1, :], rhs=baser[0:1, :], start=True, stop=True), R=[onescol, baser], W=[PF[3]])
                S.op('act', lambda e: e.activation(out=basebc[:], in_=PF[3][:, 0:32], func=AF.Copy), R=[PF[3]], W=[basebc])
                S.op('dve', lambda e: e.tensor_tensor(out=cmp3[:], in0=endr[:].unsqueeze(1).broadcast_to([1, NBLK, 32]), in1=iota[0:1, :].unsqueeze(2).broadcast_to([1, NBLK, 32]), op=ALU.is_le),
                     R=[endr, iota], W=[cmp3])
                S.op('dve', lambda e: e.tensor_reduce(out=ebf[:], in_=cmp3[:], axis=AX.X, op=ALU.add), R=[cmp3], W=[ebf])
                S.op('dve', lambda e: e.tensor_scalar(out=ebf[:], in0=ebf[:], scalar1=31.0, scalar2=None, op0=ALU.min), R=[ebf], W=[ebf])
                needf = row('needf', NBLK); ebrow = row('ebrow', NBLK)
                S.op('dve', lambda e: e.memset(needf[:], 1.0), W=[needf])
                S.op('dve', lambda e: e.tensor_tensor(out=needf[0:1, 2:NBLK], in0=ebf[0:1, 2:NBLK], in1=ebf[0:1, 0:NBLK - 2], op=ALU.not_equal), R=[ebf, needf], W=[needf])
                S.op('dve', lambda e: e.tensor_scalar(out=needf[:], in0=needf[:], scalar1=-1.0e6, scalar2=1.0e6, op0=ALU.mult, op1=ALU.add), R=[needf], W=[needf])
                S.op('dve', lambda e: e.tensor_scalar(out=ebrow[:], in0=ebf[:], scalar1=float(l * 32), scalar2=1024.0, op0=ALU.add, op1=ALU.mult), R=[ebf], W=[ebrow])
                S.op('dve', lambda e: e.tensor_tensor(out=ebrow[:], in0=ebrow[:], in1=needf[:], op=ALU.add), R=[ebrow, needf], W=[ebrow])
                ebbc = self.sb(s1, "mo_ebbc", [128, NBLK], F32); wf = self.sb(s1, "mo_wf", [128, NBLK, 8], F32)
                S.op('pe', lambda e: e.matmul(PF[3][:, 0:NBLK], lhsT=onescol[0:1, :], rhs=ebrow[0:1, :], start=True, stop=True), R=[onescol, ebrow], W=[PF[3]])
                S.op('act', lambda e: e.activation(out=ebbc[:], in_=PF[3][:, 0:NBLK], func=AF.Copy), R=[PF[3]], W=[ebbc])
                S.op('dve', lambda e: e.tensor_tensor(out=wf[:], in0=ebbc[:].unsqueeze(2).broadcast_to([128, NBLK, 8]), in1=kp[:].unsqueeze(1).broadcast_to([128, NBLK, 8]), op=ALU.add),
                     R=[ebbc, kp], W=[wf])
                S.op('dve', lambda e: e.tensor_copy(out=widx[:], in_=wf[:]), R=[wf], W=[widx])
                pcol = self.load_const(s1, 'pcol', F32)
                S.op('pe', lambda e: e.matmul(PF[3][0:32, 0:NBLK], lhsT=onescol[0:1, 0:32], rhs=ebf[0:1, :], start=True, stop=True), R=[onescol, ebf], W=[PF[3]])
                S.op('dve', lambda e: e.tensor_scalar(out=OH[:], in0=PF[3][0:32, 0:NBLK], scalar1=pcol[0:32, 0:1], scalar2=None, op0=ALU.is_equal), R=[PF[3], pcol], W=[OH])
                zt = self.sb(s1, "mo_zt", [128, NSLOT * 2 // 128], I32)
                S.op('dve', lambda e: e.memset(zt[:], 0), W=[zt])
                tokz = TB()
                self.dma('sp', self.tokidx_d.rearrange("(p b) o -> p (b o)", p=128), zt[:], R=[zt], W=[tokz])
                tidx = self.sb(s1, "mo_tidx", [128, NT, 2], I32)
                S.op('pool', lambda e: e.iota(tidx[:], pattern=[[128, NT], [0, 2]], base=0, channel_multiplier=1), W=[tidx])
                tri = self.load_const(s1, 'tri_lt', BF16)
                a1 = self.sb(s1, "mo_a1", [128, 32], F32); A8 = self.sb(s1, "mo_A8", [128, 8], F32); offf = self.sb(s1, "mo_offf", [128, 4], F32)
                for i in range(NT):
                    pp = PF[i % 2]
                    S.op('pe', lambda e, i=i, pp=pp: e.matmul(pp[:, 0:32], lhsT=tri[:], rhs=M_bf[:, i, :], start=True, stop=(i == 0)), R=[tri, M_bf], W=[pp])
                    for j in range(i):
                        S.op('pe', lambda e, i=i, j=j, pp=pp: e.matmul(pp[:, 0:32], lhsT=ones_bf[:, 0:128], rhs=M_bf[:, j, :], start=False, stop=(j == i - 1)), R=[ones_bf, M_bf], W=[pp])
                    S.op('dve', lambda e, pp=pp: e.tensor_tensor(out=a1[:], in0=pp[:, 0:32], in1=basebc[:], op=ALU.add), R=[pp, basebc], W=[a1])
                    S.op('dve', lambda e, i=i: e.scalar_tensor_tensor(out=a1[:], in0=a1[:], scalar=1.0, in1=M32[:, i, :], op0=ALU.add, op1=ALU.mult), R=[a1, M32], W=[a1])
                    S.op('dve', lambda e: e.max(out=A8[:], in_=a1[:]), R=[a1], W=[A8])
                    S.op('dve', lambda e: e.tensor_scalar(out=offf[:], in0=A8[:, 0:4], scalar1=-1.0, scalar2=None, op0=ALU.add), R=[A8], W=[offf])
                    S.op('dve', lambda e, i=i: e.tensor_copy(out=off_all[:, i, :], in_=offf[:]), R=[offf], W=[off_all])
                    for j in range(4):
                        S.op('dve', lambda e, i=i, j=j: e.scalar_tensor_tensor(out=junk32[:], in0=a1[:], scalar=A8[:, j:j + 1], in1=G_all[:, i, :], op0=ALU.is_equal, op1=ALU.mult,
                                                                              accum_out=gsel_all[:, i, j:j + 1]), R=[a1, A8, G_all], W=[junk32, gsel_all])
                    for j in range(4):
                        S.op('pool', lambda e, i=i, j=j: e.indirect_dma_start(out=self.tokidx_d[:, :], out_offset=bass.IndirectOffsetOnAxis(ap=off_all[:, i, j:j + 1], axis=0),
                                                                               in_=tidx[:, i, :], in_offset=None),
                             R=[off_all, tidx, tokz], W=[TB()], dma=True)
                S.barrier(); S.emit()
            if self.flags.get('moe_stop') == 2:
                return
            w1v = self.moe_w1.rearrange("l e d f -> (l e d) f"); w2v = self.moe_w2.rearrange("l e f d -> (l e f) d")
            b1v = self.moe_b1.rearrange("l e f -> (l e) f"); b2v = self.moe_b2.rearrange("l e d -> (l e) d")
            IO = bass.IndirectOffsetOnAxis
            with ExitStack() as s1:
                W1 = [self.sb(s1, f"mo_W1{i}", [128, 8, 2 * D], BF16) for i in range(2)]; W2 = [self.sb(s1, f"mo_W2{i}", [128, 8, D], BF16) for i in range(2)]
                b1all = self.sb(s1, "mo_b1all", [32, 2 * D], BF16); b2all = self.sb(s1, "mo_b2all", [32, D], BF16)
                self.dma('pool', b1all[:], self.moe_b1[l], W=[b1all]); self.dma('pool', b2all[:], self.moe_b2[l], W=[b2all])
                sel = [self.sb(s1, f"mo_sel{i}", [32, MB], BF16) for i in range(2)]
                bcdone = self.sb(s1, "mo_bcd", [1, 1], F32)

                def setbc(e):
                    e.reg_mov(self.reg_bc, 64 * 1024 - 1)
                    return e.memset(bcdone[:], 0.0)
                S.op('pool', setbc, W=[bcdone])
                wts = [TB(), TB()]
                idx = [self.sb(s1, f"mo_idx{i}", [128, 4, 2], I32) for i in range(2)]
                X = [self.sb(s1, f"mo_X{i}", [128, D], BF16) for i in range(2)]
                XT = [self.sb(s1, f"mo_XT{i}", [128, 8, MB], BF16) for i in range(2)]
                AT = self.sb(s1, "mo_AT", [128, 8, MB], BF16)
                g_ = self.sb(s1, "mo_g", [128, 512], F32); sg_ = self.sb(s1, "mo_sg", [128, 512], F32); ln_ = self.sb(s1, "mo_ln", [128, 512], F32)
                yrow = [self.sb(s1, f"mo_y{i}", [128, D], F32) for i in range(2)]
                xc = 0

                def issue_weights(b):
                    b2_ = b % 2
                    W1_, W2_, wt_ = W1[b2_], W2[b2_], wts[b2_]
                    for k in range(8):
                        S.op('pool', lambda e, k=k, b=b, W1_=W1_: e.indirect_dma_start(out=W1_[:, k, :], out_offset=None, in_=w1v[:, :], in_offset=IO(ap=widx[:, b, k:k + 1], axis=0),
                                                                                  bounds_check=self.reg_bc, oob_is_err=False), R=[widx, bcdone], W=[wt_], dma=True)
                        S.op('pool', lambda e, k=k, b=b, W2_=W2_: e.indirect_dma_start(out=W2_[:, k, :], out_offset=None, in_=w2v[:, :], in_offset=IO(ap=widx[:, b, k:k + 1], axis=0),
                                                                                  bounds_check=self.reg_bc, oob_is_err=False), R=[widx, bcdone], W=[wt_], dma=True)
                X4 = [self.sb(s1, f"mo_X4{i}", [128, D], BF16) for i in range(4)]

                def issue_gathers(b):
                    b2_ = b % 2
                    self.dma('sp', idx[b2_][:], self.tokidx_d[b * MB:(b + 1) * MB, :].rearrange("(q p) o -> p q o", p=128), W=[idx[b2_]])
                    for q in range(MB // 128):
                        x_ = X4[q]
                        S.op('pool', lambda e, b2_=b2_, q=q, x_=x_: e.indirect_dma_start(out=x_[:], out_offset=None, in_=self.hrow_d[:, :], in_offset=IO(ap=idx[b2_][:, q, 0:1], axis=0)),
                             R=[idx[b2_]], W=[x_], dma=True)

                def issue_transposes(b):
                    xt_ = XT[b % 2]
                    for q in range(MB // 128):
                        x_ = X4[q]; pb = self.PB[q % 2]
                        for k in range(8):
                            S.op('pe', lambda e, k=k, pb=pb, x_=x_: e.transpose(out=pb[:, k * 128:(k + 1) * 128], in_=x_[:, k * 128:(k + 1) * 128], identity=self.ident[:]), R=[x_, self.ident], W=[pb])
                        S.op('act', lambda e, pb=pb, xt_=xt_, q=q: e.activation(out=xt_[:, :, q * 128:(q + 1) * 128], in_=pb[:].rearrange("p (k t) -> p k t", k=8), func=AF.Copy), R=[pb], W=[xt_])
                issue_weights(0)
                issue_gathers(0)
                issue_transposes(0)
                for b in range(NBLK):
                    b2_ = b % 2
                    W1_, W2_, wt_ = W1[b2_], W2[b2_], wts[b2_]
                    sel_ = sel[b2_]
                    xt_ = XT[b2_]
                    S.op('dve', lambda e, b=b, sel_=sel_: e.tensor_copy(out=sel_[:], in_=OH[:, b:b + 1].broadcast_to([32, MB])), R=[OH], W=[sel_])
                    if b + 1 < NBLK:
                        issue_weights(b + 1)
                        issue_gathers(b + 1)
                    for c in range(8):
                        pg, pl = PF[(c % 2) * 2], PF[(c % 2) * 2 + 1]
                        for (pf, cbase) in ((pg, 0), (pl, D)):
                            for k in range(8):
                                S.op('pe', lambda e, k=k, pf=pf, c=c, cbase=cbase, W1_=W1_, xt_=xt_: e.matmul(pf[:], lhsT=W1_[:, k, cbase + c * 128: cbase + (c + 1) * 128], rhs=xt_[:, k, :], start=(k == 0), stop=False),
                                     R=[wt_, xt_], W=[pf])
                            S.op('pe', lambda e, pf=pf, c=c, cbase=cbase, sel_=sel_: e.matmul(pf[:], lhsT=b1all[0:32, cbase + c * 128: cbase + (c + 1) * 128], rhs=sel_[0:32, :], start=False, stop=True), R=[b1all, sel_], W=[pf])
                        S.op('dve', lambda e, pg=pg: e.tensor_scalar(out=g_[:], in0=pg[:], scalar1=7.0, scalar2=None, op0=ALU.min), R=[pg], W=[g_])
                        S.op('act', lambda e: e.activation(out=sg_[:], in_=g_[:], func=AF.Sigmoid, scale=1.702), R=[g_], W=[sg_])
                        S.op('dve', lambda e, pl=pl: e.tensor_scalar(out=ln_[:], in0=pl[:], scalar1=7.0, scalar2=-7.0, op0=ALU.min, op1=ALU.max), R=[pl], W=[ln_])
                        S.op('dve', lambda e: e.tensor_tensor(out=sg_[:], in0=sg_[:], in1=g_[:], op=ALU.mult), R=[sg_, g_], W=[sg_])
                        S.op('dve', lambda e, c=c: e.scalar_tensor_tensor(out=AT[:, c, :], in0=ln_[:], scalar=1.0, in1=sg_[:], op0=ALU.add, op1=ALU.mult), R=[ln_, sg_], W=[AT])
                    if b + 1 < NBLK:
                        issue_transposes(b + 1)
                    for q in range(MB // 128):
                        y_ = yrow[q % 2]
                        for hf in range(2):
                            py = PF[4 + hf]
                            for c in range(8):
                                S.op('pe', lambda e, c=c, py=py, hf=hf, q=q, W2_=W2_: e.matmul(py[:], lhsT=AT[:, c, q * 128:(q + 1) * 128], rhs=W2_[:, c, hf * 512:(hf + 1) * 512], start=(c == 0), stop=False), R=[AT, wt_], W=[py])
                            S.op('pe', lambda e, py=py, hf=hf, sel_=sel_: e.matmul(py[:], lhsT=sel_[0:32, 0:128], rhs=b2all[0:32, hf * 512:(hf + 1) * 512], start=False, stop=True), R=[sel_, b2all], W=[py])
                            if hf == 0:
                                S.op('act', lambda e, py=py, y_=y_: e.activation(out=y_[:, 0:512], in_=py[:], func=AF.Copy), R=[py], W=[y_])
                            else:
                                S.op('dve', lambda e, py=py, y_=y_: e.tensor_copy(out=y_[:, 512:1024], in_=py[:]), R=[py], W=[y_])
                        self.dma('sp', self.yslot_d[b * MB + q * 128: b * MB + (q + 1) * 128, :], y_[:], R=[y_], W=[TB()])
                S.barrier(); S.emit()
            if self.flags.get('moe_stop') == 3:
                return
            with ExitStack() as s1:
                YY = [[self.sb(s1, f"mo_Y{i}{j}", [128, D], F32) for j in range(4)] for i in range(2)]
                xt = [self.sb(s1, f"mo_x{i}", [128, D], F32) for i in range(2)]
                acc = self.sb(s1, "mo_acc", [128, D], F32)
                for i in range(NT):
                    x_ = xt[i % 2]
                    Y = YY[i % 2]
                    self.dma('sp', x_[:], self.xres[i * 128:(i + 1) * 128, :], W=[x_])
                    for j in range(4):
                        S.op('pool', lambda e, i=i, j=j, Y=Y: e.indirect_dma_start(out=Y[j][:], out_offset=None, in_=self.yslot_d[:, :], in_offset=bass.IndirectOffsetOnAxis(ap=off_all[:, i, j:j + 1], axis=0)),
                             R=[off_all], W=[Y[j]], dma=True)
                    S.op('dve', lambda e, i=i, Y=Y: e.tensor_scalar(out=acc[:], in0=Y[0][:], scalar1=gsel_all[:, i, 0:1], scalar2=None, op0=ALU.mult), R=[Y[0], gsel_all], W=[acc])
                    for j in range(1, 4):
                        S.op('dve', lambda e, i=i, j=j, Y=Y: e.scalar_tensor_tensor(out=acc[:], in0=Y[j][:], scalar=gsel_all[:, i, j:j + 1], in1=acc[:], op0=ALU.mult, op1=ALU.add), R=[Y[j], gsel_all, acc], W=[acc])
                    S.op('pool', lambda e: e.tensor_tensor(out=acc[:], in0=acc[:], in1=self.gb[1][:], op=ALU.mult), R=[acc, self.gb[1]], W=[acc])
                    S.op('dve', lambda e, x_=x_: e.tensor_tensor(out=x_[:], in0=x_[:], in1=acc[:], op=ALU.add), R=[x_, acc], W=[x_])
                    self.dma('sp', self.xres[i * 128:(i + 1) * 128, :], x_[:], R=[x_], W=[TB()])
                S.barrier(); S.emit()

    def final_norm(self):
        S = self.S
        with ExitStack() as st:
            gfb = self.bcast_row(st, "gfb", self.norm_final[0, :], D)
            xt = [self.sb(st, f"fx{i}", [128, D], F32) for i in range(2)]
            junk = self.sb(st, "fjunk", [128, D], F32)
            ss = [self.sb(st, f"fss{i}", [128, 1], F32) for i in range(2)]
            for i in range(NT):
                x_, s_ = xt[i % 2], ss[i % 2]
                self.dma('sp' if i % 2 == 0 else 'act', x_[:], self.xres[i * 128:(i + 1) * 128, :], W=[x_])
                S.op('act', lambda e, x_=x_, s_=s_: e.activation(out=junk[:], in_=x_[:], func=AF.Square, accum_out=s_[:]), R=[x_], W=[junk, s_])
                S.op('dve', lambda e, s_=s_: e.tensor_scalar(out=s_[:], in0=s_[:], scalar1=1.0 / D, scalar2=1e-5, op0=ALU.mult, op1=ALU.add), R=[s_], W=[s_])
                S.op('act', lambda e, s_=s_: e.activation(out=s_[:], in_=s_[:], func=AF.Sqrt), R=[s_], W=[s_])
                S.op('dve', lambda e, s_=s_: e.reciprocal(out=s_[:], in_=s_[:]), R=[s_], W=[s_])
                S.op('dve', lambda e, x_=x_, s_=s_: e.scalar_tensor_tensor(out=x_[:], in0=x_[:], scalar=s_[:, 0:1], in1=gfb[:], op0=ALU.mult, op1=ALU.mult), R=[x_, s_, gfb], W=[x_])
                t = TB()
                self.dma('sp', self.out[i * 128:(i + 1) * 128, :], x_[:], R=[x_], W=[t])
                self.final_tbs.append(t)
            S.barrier(); S.emit()


class _Stop(Exception):
    pass


class _View:
    def __init__(self, buf, i):
        self.buf = buf; self.i = i; self.tb = buf.tb

    def __getitem__(self, k):
        return self.buf.t[:, self.i, :][k]


def make_in_maps(inputs):
    f = lambda a: np.ascontiguousarray(np.asarray(a, dtype=np.float32))
    w_ext = np.ascontiguousarray(np.asarray(inputs['w_in'], np.float32)[:, :, WCOLS])
    lw = np.ascontiguousarray(np.concatenate([inputs['rwkv_w2'], inputs['rwkv_a2'], inputs['rwkv_g2']], axis=1).astype(np.float32))
    qs = np.array(_swap_cols(0, 4, 64, 8)); qis = np.array(_swap_cols(0, 8, 32, 4))
    shared = dict(
        cst=CST, ada_w=f(inputs['ada_w']), ada_b=f(inputs['ada_b']), norm_mix=f(inputs['norm_mix']), norm_ffn=f(inputs['norm_ffn']),
        w_ext=w_ext, ret_gn=f(inputs['ret_gn']), rwkv_mu=f(inputs['rwkv_mu']), rwkv_w0=f(inputs['rwkv_w0']), rwkv_lw=lw,
        rwkv_a0=f(inputs['rwkv_a0']), rwkv_kk=f(inputs['rwkv_kk']), rwkv_ka=f(inputs['rwkv_ka']),
        rwkv_rk=f(np.asarray(inputs['rwkv_rk']).reshape(L, 256)), rwkv_ln=f(inputs['rwkv_ln']),
        dsa_qnorm=f(inputs['dsa_qnorm']), dsa_wq_up=f(inputs['dsa_wq_up']), dsa_wqs_up=f(np.asarray(inputs['dsa_wq_up'])[:, :, qs]),
        dsa_wqi_up=f(inputs['dsa_wqi_up']), dsa_wqis_up=f(np.asarray(inputs['dsa_wqi_up'])[:, :, qis]),
        dsa_onorm=f(inputs['dsa_onorm']), sb_onorm=f(inputs['sb_onorm']), w_out=f(inputs['w_out']),
        router_w=f(inputs['router_w']), router_b=f(inputs['router_b']), moe_w1=f(inputs['moe_w1']), moe_b1=f(inputs['moe_b1']),
        moe_w2=f(inputs['moe_w2']), moe_b2=f(inputs['moe_b2']), norm_final=f(np.asarray(inputs['norm_final']).reshape(1, D)),
    )
    maps = []
    x = np.asarray(inputs['x'], np.float32); c = np.asarray(inputs['c'], np.float32); pos = np.asarray(inputs['positions'], np.int32)
    for b in range(x.shape[0]):
        m = dict(shared)
        m['x'] = np.ascontiguousarray(x[b]); m['c'] = np.ascontiguousarray(c[b:b + 1]); m['pos'] = np.ascontiguousarray(pos[b:b + 1])
        maps.append(m)
    return maps


def kernel(**inputs):
    maps = make_in_maps(inputs)
    nc = Prog().build()
    res = run_bass_kernel_spmd(nc, maps, core_ids=list(range(NB)))
    return np.stack([np.asarray(r['out'], dtype=np.float32) for r in res.results], axis=0)
```
